# Optimizing a Trainium2 kernel written in Bass

```python
import math
import jax, jax.numpy as jnp
from jax import lax
import numpy as np

D_MODEL = 2048
BATCH = 32
SEQ = 256
DEPTH = 2
DEC_BATCH = 8
DEC_SEQ = 1024
PAST_LEN = 512

GRID_W = 64
S5_WIDTH = D_MODEL // 2
S5_GROUP = 16
S5_GROUPS = S5_WIDTH // S5_GROUP
S5_STATE = 64
DT_MIN = 0.001
DT_MAX = 0.1
SC_WIDTH = D_MODEL // 4
SC_K = 3
CF_WIDTH = D_MODEL // 4
CF_K = 31
N_BRANCH = 3
SPLITS = (S5_WIDTH, S5_WIDTH + SC_WIDTH, S5_WIDTH + 2 * SC_WIDTH, S5_WIDTH + 3 * SC_WIDTH,
          S5_WIDTH + 3 * SC_WIDTH + 2 * CF_WIDTH)
IN_COLS = SPLITS[-1] + N_BRANCH * D_MODEL
N_KEYS = 128
N_EXPERTS = N_KEYS * N_KEYS
PEER_HEADS = 8
PEER_TOPK = 16
PEER_QDIM = 256
PEER_CHUNK = 128
ALPHA = (2 * DEPTH) ** 0.25
BETA = (8 * DEPTH) ** -0.25
LN_EPS = 1e-6

kernel_name = "hybrid_s5_conv_peer_diffusion_step"


def _ln(x):
    xf = x.astype(jnp.float32)
    mu = jnp.mean(xf, -1, keepdims=True)
    var = jnp.mean(jnp.square(xf - mu), -1, keepdims=True)
    return ((xf - mu) * lax.rsqrt(var + LN_EPS)).astype(x.dtype)


def _dwconv(x, w, b):
    k, ch = w.shape
    pad = (k - 1) // 2
    y = lax.conv_general_dilated(x, w[:, None, :].astype(x.dtype), window_strides=(1,),
                                 padding=[(pad, pad)], dimension_numbers=("NWC", "WIO", "NWC"),
                                 feature_group_count=ch)
    return y + b


def _grid_pos(seq, dtype):
    rows = seq // GRID_W
    t = jnp.arange(rows * GRID_W)
    r = (t // GRID_W).astype(jnp.float32)
    col = (t % GRID_W).astype(jnp.float32)
    nf = D_MODEL // 4
    freq = 1.0 / (10000.0 ** (jnp.arange(nf, dtype=jnp.float32) / nf))
    ar = r[:, None] * freq
    ac = col[:, None] * freq
    pe = jnp.concatenate([jnp.sin(ar), jnp.cos(ar), jnp.sin(ac), jnp.cos(ac)], -1)
    return pe.astype(dtype)


def _complex_affine_combine(e1, e2):
    a1r, a1i, b1r, b1i = e1
    a2r, a2i, b2r, b2i = e2
    return (a1r * a2r - a1i * a2i, a1r * a2i + a1i * a2r,
            a2r * b1r - a2i * b1i + b2r, a2r * b1i + a2i * b1r + b2i)


def _s5_scan(u, h0_re, h0_im, p, d, reverse):
    f32 = jnp.float32
    lam_re = p["s5_lam_re"][d].astype(f32)
    lam_im = p["s5_lam_im"][d].astype(f32)
    dt = jnp.exp(p["s5_log_dt"][d].astype(f32))[:, None]
    mag = jnp.exp(lam_re * dt)
    ang = lam_im * dt
    a_re = mag * jnp.cos(ang)
    a_im = mag * jnp.sin(ang)
    den = lam_re * lam_re + lam_im * lam_im
    z_re = ((a_re - 1.0) * lam_re + a_im * lam_im) / den
    z_im = (a_im * lam_re - (a_re - 1.0) * lam_im) / den
    b_re = p["s5_b_re"][d].astype(f32)
    b_im = p["s5_b_im"][d].astype(f32)
    bb_re = z_re[..., None] * b_re - z_im[..., None] * b_im
    bb_im = z_re[..., None] * b_im + z_im[..., None] * b_re
    bu_re = jnp.einsum("blgs,gps->blgp", u, bb_re)
    bu_im = jnp.einsum("blgs,gps->blgp", u, bb_im)
    edge = -1 if reverse else 0
    bu_re = bu_re.at[:, edge].add(a_re * h0_re - a_im * h0_im)
    bu_im = bu_im.at[:, edge].add(a_re * h0_im + a_im * h0_re)
    seq = u.shape[1]
    a_re_t = jnp.broadcast_to(a_re, (1, seq) + a_re.shape)
    a_im_t = jnp.broadcast_to(a_im, (1, seq) + a_im.shape)
    _, _, h_re, h_im = lax.associative_scan(_complex_affine_combine, (a_re_t, a_im_t, bu_re, bu_im),
                                            reverse=reverse, axis=1)
    c_re = p["s5_c_re"][d].astype(f32)
    c_im = p["s5_c_im"][d].astype(f32)
    y = jnp.einsum("blgp,gsp->blgs", h_re, c_re) - jnp.einsum("blgp,gsp->blgs", h_im, c_im)
    return y, h_re, h_im


def _token_mixer(h, h0_re, h0_im, p, with_state):
    bsz, seq, _ = h.shape
    proj = h @ p["w_in"]
    u_a, sc_b, sc_c, sc_x, cf_in, gates = jnp.split(proj, SPLITS, axis=-1)
    u = u_a.astype(jnp.float32).reshape(bsz, seq, S5_GROUPS, S5_GROUP)
    y_f, hr_f, hi_f = _s5_scan(u, h0_re[:, 0], h0_im[:, 0], p, 0, False)
    y_b, hr_b, hi_b = _s5_scan(u, h0_re[:, 1], h0_im[:, 1], p, 1, True)
    y_a = (y_f + y_b).reshape(bsz, seq, S5_WIDTH).astype(h.dtype) + p["s5_d"] * u_a
    y_a = jax.nn.gelu(y_a)
    y_a = y_a * jax.nn.sigmoid(y_a @ p["s5_w_glu"])
    y_s = sc_b * _dwconv(sc_c * sc_x, p["sc_conv_w"], p["sc_conv_b"])
    ga, gb = jnp.split(cf_in, 2, axis=-1)
    z = _dwconv(ga * jax.nn.sigmoid(gb), p["cf_conv_w"], p["cf_conv_b"])
    z = jax.nn.silu(_ln(z) * p["cf_ln_g"] + p["cf_ln_b"])
    g_a, g_s, g_c = jnp.split(jax.nn.sigmoid(gates), 3, axis=-1)
    merged = g_a * (y_a @ p["w_pa"]) + g_s * (y_s @ p["w_pb"]) + g_c * (z @ p["w_pc"])
    out = merged @ p["w_o"]
    if not with_state:
        return out, None
    state = jnp.stack([jnp.stack([hr_f[:, -1], hi_f[:, -1]], -1),
                       jnp.stack([hr_b[:, 0], hi_b[:, 0]], -1)], axis=1)
    return out, state


def _peer(h, p):
    bsz, seq, dm = h.shape
    tok = bsz * seq
    xt = h.reshape(tok, dm)
    q = (xt @ p["peer_w_q"]).reshape(tok, PEER_HEADS, 2, PEER_QDIM // 2)
    s1 = jnp.einsum("thd,nd->thn", q[:, :, 0], p["peer_k1"]).astype(jnp.float32)
    s2 = jnp.einsum("thd,nd->thn", q[:, :, 1], p["peer_k2"]).astype(jnp.float32)
    v1, i1 = lax.top_k(s1, PEER_TOPK)
    v2, i2 = lax.top_k(s2, PEER_TOPK)
    cand = (v1[..., :, None] + v2[..., None, :]).reshape(tok, PEER_HEADS, PEER_TOPK * PEER_TOPK)
    cidx = (i1[..., :, None] * N_KEYS + i2[..., None, :]).reshape(tok, PEER_HEADS, PEER_TOPK * PEER_TOPK)
    top, sel = lax.top_k(cand, PEER_TOPK)
    idx = jnp.take_along_axis(cidx, sel, axis=-1)
    g = jax.nn.softmax(top, axis=-1).astype(h.dtype)
    nblk = tok // PEER_CHUNK
    xb = xt.reshape(nblk, PEER_CHUNK, dm)
    ib = idx.reshape(nblk, PEER_CHUNK, PEER_HEADS * PEER_TOPK)
    gb = g.reshape(nblk, PEER_CHUNK, PEER_HEADS * PEER_TOPK)
    u_tab, v_tab = p["peer_u"], p["peer_v"]

    def block(args):
        xc, ic, gc = args
        act = jnp.einsum("cd,ckd->ck", xc, u_tab[ic])
        w = gc * jax.nn.gelu(act)
        return jnp.einsum("ck,ckd->cd", w, v_tab[ic])

    out = lax.map(block, (xb, ib, gb))
    return out.reshape(bsz, seq, dm)


def _layer(x, cond, h0_re, h0_im, p, with_state):
    mod = (jax.nn.silu(cond) @ p["w_mod"] + p["b_mod"])[:, None, :]
    sh1, sc1, gt1, sh2, sc2, gt2 = jnp.split(mod, 6, axis=-1)
    h = _ln(x) * (1.0 + sc1) + sh1
    y, state = _token_mixer(h, h0_re, h0_im, p, with_state)
    x = _ln(ALPHA * x + gt1 * y) * p["ln1_g"] + p["ln1_b"]
    h = _ln(x) * (1.0 + sc2) + sh2
    x = _ln(ALPHA * x + gt2 * _peer(h, p)) * p["ln2_g"] + p["ln2_b"]
    return x, state


def setup_inputs(seed: int = 0) -> dict:
    key = jax.random.key(seed)
    ks = iter(jax.random.split(key, 48))
    f32 = jnp.float32

    def nrm(shape, scale):
        return jax.random.normal(next(ks), shape, f32) * scale

    G, P, S = S5_GROUPS, S5_STATE, S5_GROUP
    lam_im0 = jnp.pi * jnp.arange(P, dtype=f32)
    return {
        "x_prompt": nrm((BATCH, SEQ, D_MODEL), 1.0),
        "x_sample": nrm((DEC_BATCH, DEC_SEQ, D_MODEL), 1.0),
        "state_ssm": nrm((DEC_BATCH, DEPTH, 2, G, P, 2), 0.5),
        "c": nrm((DEC_BATCH, D_MODEL), 1.0),
        "c_ctx": nrm((D_MODEL,), 1.0),
        "w_mod": nrm((DEPTH, D_MODEL, 6 * D_MODEL), 0.5 * D_MODEL ** -0.5),
        "b_mod": nrm((DEPTH, 6 * D_MODEL), 0.01),
        "w_in": nrm((DEPTH, D_MODEL, IN_COLS), D_MODEL ** -0.5),
        "s5_lam_re": -0.5 + nrm((DEPTH, 2, G, P), 0.01),
        "s5_lam_im": lam_im0 + nrm((DEPTH, 2, G, P), 0.01),
        "s5_log_dt": jax.random.uniform(next(ks), (DEPTH, 2, G), f32, math.log(DT_MIN), math.log(DT_MAX)),
        "s5_b_re": nrm((DEPTH, 2, G, P, S), (2 * S) ** -0.5),
        "s5_b_im": nrm((DEPTH, 2, G, P, S), (2 * S) ** -0.5),
        "s5_c_re": nrm((DEPTH, 2, G, S, P), (2 * P) ** -0.5),
        "s5_c_im": nrm((DEPTH, 2, G, S, P), (2 * P) ** -0.5),
        "s5_d": nrm((DEPTH, S5_WIDTH), 1.0),
        "s5_w_glu": nrm((DEPTH, S5_WIDTH, S5_WIDTH), S5_WIDTH ** -0.5),
        "sc_conv_w": nrm((DEPTH, SC_K, SC_WIDTH), SC_K ** -0.5),
        "sc_conv_b": nrm((DEPTH, SC_WIDTH), 0.01),
        "cf_conv_w": nrm((DEPTH, CF_K, CF_WIDTH), CF_K ** -0.5),
        "cf_conv_b": nrm((DEPTH, CF_WIDTH), 0.01),
        "cf_ln_g": 1.0 + nrm((DEPTH, CF_WIDTH), 0.01),
        "cf_ln_b": nrm((DEPTH, CF_WIDTH), 0.01),
        "w_pa": nrm((DEPTH, S5_WIDTH, D_MODEL), S5_WIDTH ** -0.5),
        "w_pb": nrm((DEPTH, SC_WIDTH, D_MODEL), SC_WIDTH ** -0.5),
        "w_pc": nrm((DEPTH, CF_WIDTH, D_MODEL), CF_WIDTH ** -0.5),
        "w_o": nrm((DEPTH, D_MODEL, D_MODEL), BETA * D_MODEL ** -0.5),
        "ln1_g": 1.0 + nrm((DEPTH, D_MODEL), 0.01),
        "ln1_b": nrm((DEPTH, D_MODEL), 0.01),
        "peer_w_q": nrm((DEPTH, D_MODEL, PEER_HEADS * PEER_QDIM), D_MODEL ** -0.5),
        "peer_k1": nrm((DEPTH, N_KEYS, PEER_QDIM // 2), (PEER_QDIM // 2) ** -0.5),
        "peer_k2": nrm((DEPTH, N_KEYS, PEER_QDIM // 2), (PEER_QDIM // 2) ** -0.5),
        "peer_u": nrm((DEPTH, N_EXPERTS, D_MODEL), D_MODEL ** -0.5),
        "peer_v": nrm((DEPTH, N_EXPERTS, D_MODEL), BETA),
        "ln2_g": 1.0 + nrm((DEPTH, D_MODEL), 0.01),
        "ln2_b": nrm((DEPTH, D_MODEL), 0.01),
    }


def reference(x_prompt, x_sample, state_ssm, c, c_ctx, w_mod, b_mod, w_in,
              s5_lam_re, s5_lam_im, s5_log_dt, s5_b_re, s5_b_im, s5_c_re, s5_c_im, s5_d, s5_w_glu,
              sc_conv_w, sc_conv_b, cf_conv_w, cf_conv_b, cf_ln_g, cf_ln_b,
              w_pa, w_pb, w_pc, w_o, ln1_g, ln1_b,
              peer_w_q, peer_k1, peer_k2, peer_u, peer_v, ln2_g, ln2_b):
    xp = x_prompt
    xs = x_sample + _grid_pos(x_sample.shape[1], x_sample.dtype)[None]
    ctx_cond = c_ctx[None, :]
    zeros = jnp.zeros((x_prompt.shape[0], 2, S5_GROUPS, S5_STATE), jnp.float32)
    states = []
    for l in range(DEPTH):
        p = {
            "w_mod": w_mod[l], "b_mod": b_mod[l], "w_in": w_in[l],
            "s5_lam_re": s5_lam_re[l], "s5_lam_im": s5_lam_im[l], "s5_log_dt": s5_log_dt[l],
            "s5_b_re": s5_b_re[l], "s5_b_im": s5_b_im[l], "s5_c_re": s5_c_re[l], "s5_c_im": s5_c_im[l],
            "s5_d": s5_d[l], "s5_w_glu": s5_w_glu[l],
            "sc_conv_w": sc_conv_w[l], "sc_conv_b": sc_conv_b[l],
            "cf_conv_w": cf_conv_w[l], "cf_conv_b": cf_conv_b[l], "cf_ln_g": cf_ln_g[l], "cf_ln_b": cf_ln_b[l],
            "w_pa": w_pa[l], "w_pb": w_pb[l], "w_pc": w_pc[l], "w_o": w_o[l],
            "ln1_g": ln1_g[l], "ln1_b": ln1_b[l],
            "peer_w_q": peer_w_q[l], "peer_k1": peer_k1[l], "peer_k2": peer_k2[l],
            "peer_u": peer_u[l], "peer_v": peer_v[l], "ln2_g": ln2_g[l], "ln2_b": ln2_b[l],
        }
        xp, st = _layer(xp, ctx_cond, zeros, zeros, p, True)
        states.append(st)
        h0_re = state_ssm[:, l, :, :, :, 0].astype(jnp.float32)
        h0_im = state_ssm[:, l, :, :, :, 1].astype(jnp.float32)
        xs, _ = _layer(xs, c, h0_re, h0_im, p, False)
    new_state_ssm = jnp.stack(states, axis=1).astype(x_prompt.dtype)
    return (xp, xs, new_state_ssm)
```

```python
import math
from contextlib import ExitStack

import numpy as np
import concourse.bass as bass
import concourse.mybir as mybir
from concourse.bass_utils import run_bass_kernel_spmd

F32 = mybir.dt.float32
BF16 = mybir.dt.bfloat16
I32 = mybir.dt.int32
U32 = mybir.dt.uint32
ALU = mybir.AluOpType
AF = mybir.ActivationFunctionType
AX = mybir.AxisListType

D = 2048
NCORE = 8
T = 1024
NTT = 8
KT = 16
NL = 2
ALPHA = 4.0 ** 0.25
LN_EPS = 1e-6
IN_COLS = 9728
GATE0 = 3584
NEG = -1.0e30


class Buf:
    __slots__ = ("name", "lw", "rd", "space", "dsem")

    def __init__(self, name, space="sb"):
        self.name = name
        self.lw = {}
        self.rd = {}
        self.space = space
        self.dsem = None


class Grp:
    __slots__ = ("sem", "cnt")

    def __init__(self, sem):
        self.sem = sem
        self.cnt = 0


class TT:
    __slots__ = ("ap", "buf")

    def __init__(self, ap, buf):
        self.ap = ap
        self.buf = buf

    def __getitem__(self, k):
        return TT(self.ap[k], self.buf)

    def r(self, pat, **kw):
        return TT(self.ap.rearrange(pat, **kw), self.buf)

    def bc(self, shape):
        return TT(self.ap.to_broadcast(list(shape)), self.buf)

    def un(self, ax):
        return TT(self.ap.unsqueeze(ax), self.buf)

    def bitcast(self, dt):
        return TT(self.ap.bitcast(dt), self.buf)


def _a(x):
    return x.ap if isinstance(x, TT) else x


class Prog:
    def __init__(self, nc, es):
        self.nc = nc
        self.es = es
        self.E = dict(pe=nc.tensor, dve=nc.vector, act=nc.scalar, pool=nc.gpsimd, sp=nc.sync)
        self.rec = {k: [] for k in self.E}
        self.esem = {k: es.enter_context(nc.semaphore("es_" + k)) for k in self.E}
        self.ecnt = {k: 0 for k in self.E}
        self.known = {k: {} for k in self.E}
        self.grps = []
        self.free = []
        self.active = []
        self.max_dsem = 84

    def grp(self, name):
        g = Grp(self.es.enter_context(self.nc.semaphore(name)))
        self.grps.append(g)
        return g

    def dsem_for(self, buf):
        if buf.dsem is None:
            if self.free:
                buf.dsem = self.free.pop()
            else:
                assert len(self.grps) < self.max_dsem, "out of DMA semaphores"
                buf.dsem = self.grp("dq%d" % len(self.grps))
            self.active.append(buf)
        return buf.dsem

    def op(self, e, fn, r=(), w=(), grp=None):
        need = {}
        own = self.esem[e]

        def add(tok):
            sem, val = tok
            if e == "pe" and sem is own:
                return
            k = id(sem)
            if k not in need or need[k][1] < val:
                need[k] = (sem, val)

        for b in list(r) + list(w):
            for tok in b.lw.values():
                add(tok)
        for b in w:
            for tok in b.rd.values():
                add(tok)
        waits = []
        kn = self.known[e]
        for k, (sem, val) in need.items():
            if kn.get(k, 0) >= val:
                continue
            kn[k] = val
            waits.append((sem, val))
        if grp is not None:
            grp.cnt += 16
            tok = (grp.sem, grp.cnt)
            inc = (grp.sem, 16)
        else:
            self.ecnt[e] += 1
            tok = (own, self.ecnt[e])
            inc = (own, 1)
        for b in w:
            if b.space == "dram":
                b.lw[id(tok[0])] = tok
            else:
                b.lw = {id(tok[0]): tok}
            b.rd = {}
        for b in r:
            if b in w:
                continue
            b.rd[id(tok[0])] = tok
        self.rec[e].append((waits, fn, inc))

    def barrier(self):
        toks = [(self.esem[k], self.ecnt[k]) for k in self.E] + [(g.sem, g.cnt) for g in self.grps]
        for e in self.E:
            waits = []
            kn = self.known[e]
            for sem, val in toks:
                if val == 0 or sem is self.esem[e]:
                    continue
                if kn.get(id(sem), 0) >= val:
                    continue
                kn[id(sem)] = val
                waits.append((sem, val))
            if waits:
                self.rec[e].append((waits, None, None))
        for b in self.active:
            self.free.append(b.dsem)
            b.dsem = None
        self.active = []

    def final_wait(self):
        e = "sp"
        waits = [(self.esem[k], self.ecnt[k]) for k in self.E if k != e and self.ecnt[k]]
        waits += [(g.sem, g.cnt) for g in self.grps if g.cnt]
        self.rec[e].append((waits, None, None))

    def emit(self):
        with self.nc.Block() as blk:
            def mk(e):
                def body(Eng):
                    for waits, fn, inc in self.rec[e]:
                        for sem, val in waits:
                            Eng.wait_ge(sem, val)
                        if fn is not None:
                            fn(Eng).then_inc(inc[0], inc[1])
                return body
            blk.tensor(mk("pe"))
            blk.vector(mk("dve"))
            blk.scalar(mk("act"))
            blk.gpsimd(mk("pool"))
            blk.sync(mk("sp"))

    def dma(self, q, out, in_, grp=None, **kw):
        if out.buf.space == "sb":
            g = self.dsem_for(out.buf)
        elif in_.buf.space == "sb":
            g = self.dsem_for(in_.buf)
        else:
            g = self.dsem_for(out.buf)
        self.op(q, lambda E: E.dma_start(out=out.ap, in_=in_.ap, **kw), r=[in_.buf], w=[out.buf], grp=g)

    def mm(self, out, lhsT, rhs, start=True, stop=True):
        self.op("pe", lambda E: E.matmul(out=out.ap, lhsT=lhsT.ap, rhs=rhs.ap, start=start, stop=stop),
                r=[lhsT.buf, rhs.buf], w=[out.buf])

    def tr(self, out, in_, ident):
        self.op("pe", lambda E: E.transpose(out=out.ap, in_=in_.ap, identity=ident.ap),
                r=[in_.buf, ident.buf], w=[out.buf])

    def act(self, out, in_, func, bias=None, scale=None, e="act"):
        rb = [in_.buf]
        kw = {}
        if bias is not None:
            kw["bias"] = _a(bias)
            if isinstance(bias, TT):
                rb.append(bias.buf)
        if scale is not None:
            kw["scale"] = _a(scale)
            if isinstance(scale, TT):
                rb.append(scale.buf)
        self.op("act", lambda E: E.activation(out=out.ap, in_=in_.ap, func=func, **kw), r=rb, w=[out.buf])

    def cp(self, e, out, in_):
        if e == "act":
            self.op("act", lambda E: E.copy(out=out.ap, in_=in_.ap), r=[in_.buf], w=[out.buf])
        else:
            self.op(e, lambda E: E.tensor_copy(out=out.ap, in_=in_.ap), r=[in_.buf], w=[out.buf])

    def tt(self, e, out, a, b, op):
        self.op(e, lambda E: E.tensor_tensor(out=out.ap, in0=a.ap, in1=b.ap, op=op), r=[a.buf, b.buf], w=[out.buf])

    def ts(self, e, out, a, s1, op0, s2=None, op1=None):
        rb = [a.buf] + [x.buf for x in (s1, s2) if isinstance(x, TT)]
        if op1 is None:
            self.op(e, lambda E: E.tensor_scalar(out=out.ap, in0=a.ap, scalar1=_a(s1), scalar2=None, op0=op0),
                    r=rb, w=[out.buf])
        else:
            self.op(e, lambda E: E.tensor_scalar(out=out.ap, in0=a.ap, scalar1=_a(s1), scalar2=_a(s2), op0=op0, op1=op1),
                    r=rb, w=[out.buf])

    def stt(self, out, a, s, b, op0, op1, e="dve"):
        rb = [a.buf, b.buf] + ([s.buf] if isinstance(s, TT) else [])
        self.op(e, lambda E: E.scalar_tensor_tensor(out=out.ap, in0=a.ap, scalar=_a(s), in1=b.ap, op0=op0, op1=op1),
                r=rb, w=[out.buf])

    def memset(self, e, out, val):
        self.op(e, lambda E: E.memset(out.ap, val), w=[out.buf])

    def recip(self, out, in_):
        self.op("dve", lambda E: E.reciprocal(out=out.ap, in_=in_.ap), r=[in_.buf], w=[out.buf])

    def scan(self, out, d0, d1, init):
        rb = [d0.buf, d1.buf] + ([init.buf] if isinstance(init, TT) else [])
        self.op("dve", lambda E: E.tensor_tensor_scan(out=out.ap, data0=d0.ap, data1=d1.ap, initial=_a(init),
                                                       op0=ALU.mult, op1=ALU.add), r=rb, w=[out.buf])

    def reduce(self, out, in_, op, axis=AX.X):
        self.op("dve", lambda E: E.tensor_reduce(out=out.ap, in_=in_.ap, axis=axis, op=op), r=[in_.buf], w=[out.buf])


class Stack:
    cnt = [0]

    def __init__(self, nc, base, limit):
        self.nc = nc
        self.base = base
        self.cur = base
        self.limit = limit

    def alloc(self, free_shape, dtype, name):
        nb = int(np.prod(free_shape)) * (2 if dtype == BF16 else 4)
        off = (self.cur + 31) // 32 * 32
        assert off + nb <= self.limit, (name, off, nb, self.limit)
        self.cur = off + nb
        Stack.cnt[0] += 1
        h = self.nc.alloc_sbuf_tensor_at("%s_%d" % (name, Stack.cnt[0]), [128] + list(free_shape), dtype, offset=off)
        return TT(h.ap(), Buf(name))

    def sub(self, nbytes):
        off = (self.cur + 31) // 32 * 32
        assert off + nbytes <= self.limit, (off, nbytes, self.limit)
        self.cur = off + nbytes
        return Stack(self.nc, off, off + nbytes)

    def reset(self):
        self.cur = self.base


def build(dbg=None, stop=None, nlayers=NL, units=(0, 1), b3_tiles=NTT):
    dbg = dbg or {}
    Stack.cnt[0] = 0
    nc = bass.Bass("TRN2", target_bir_lowering=False)
    es = ExitStack()
    P = Prog(nc, es)

    declared = []
    build.declared = declared

    def din(name, shape, dt=F32):
        declared.append(name)
        return TT(nc.dram_tensor(name, list(shape), dt, kind="ExternalInput").ap(), Buf(name, "dram"))

    def dout(name, shape, dt=F32):
        return TT(nc.dram_tensor(name, list(shape), dt, kind="ExternalOutput").ap(), Buf(name, "dram"))

    def dscr(name, shape, dt=F32):
        return TT(nc.dram_tensor(name, list(shape), dt, kind="Internal").ap(), Buf(name, "dram"))

    xin = [din("xp", [T, D]), din("xs", [T, D])]
    pe_d = din("pe", [T, D])
    st0 = din("st0", [NL, 2, 64, 64, 2])
    cond = din("cond", [2, D])
    ident_d = din("ident", [128, 128])
    iota_d = din("iota16", [128, 16])
    WSH = dict([("w_mod", [NL, D, 6 * D]), ("b_mod", [NL, 6 * D]), ("w_in", [NL, D, IN_COLS]),
                ("s5_lam_re", [NL, 2, 64, 64]), ("s5_lam_im", [NL, 2, 64, 64]), ("s5_log_dt", [NL, 2, 64]),
                ("s5_b_re", [NL, 2, 64, 64, 16]), ("s5_b_im", [NL, 2, 64, 64, 16]),
                ("s5_c_re", [NL, 2, 64, 16, 64]), ("s5_c_im", [NL, 2, 64, 16, 64]),
                ("s5_d", [NL, 1024]), ("s5_w_glu", [NL, 1024, 1024]),
                ("sc_conv_w", [NL, 3, 512]), ("sc_conv_b", [NL, 512]),
                ("cf_conv_w", [NL, 31, 512]), ("cf_conv_b", [NL, 512]),
                ("cf_ln_g", [NL, 512]), ("cf_ln_b", [NL, 512]),
                ("w_pa", [NL, 1024, D]), ("w_pb", [NL, 512, D]), ("w_pc", [NL, 512, D]), ("w_o", [NL, D, D]),
                ("ln1_g", [NL, D]), ("ln1_b", [NL, D]),
                ("peer_w_q", [NL, D, D]), ("peer_k1", [NL, 128, 128]), ("peer_k2", [NL, 128, 128]),
                ("peer_u", [NL, 16384, D]), ("peer_v", [NL, 16384, D]),
                ("ln2_g", [NL, D]), ("ln2_b", [NL, D])])

    class LazyW(dict):
        def __missing__(self, nm):
            self[nm] = din(nm, WSH[nm])
            return self[nm]
    W = LazyW()
    yout = [dout("yp", [T, D]), dout("ys", [T, D])]
    nst_o = dout("nst", [4, NL, 2, 64, 64, 2])
    dbg_out = {k: dout("dbg_" + k, v[0], v[1]) for k, v in dbg.items()}

    xres = [dscr("xres0", [T, D]), dscr("xres1", [T, D])]
    ymix = dscr("ymix", [T, D])
    x1res = dscr("x1res", [T, D])
    h2res = dscr("h2res", [T, D])
    mod_d = dscr("mod_d", [NL, 2, 6 * D])
    tabs = dscr("tabs", [64, 128, 2, 260])
    wbd = dscr("wbd", [8, 128, 16, 128], BF16)
    cwd = dscr("cwd", [8, 128, 16, 64], BF16)

    psh = nc.alloc_psum_tensor("psum_all", [128, 4096], F32)
    PS = [TT(psh.ap()[:, i * 1024:(i + 1) * 1024], Buf("ps%d" % i, "ps")) for i in range(4)]
    ps_rr = [0]

    def ps_next():
        ps_rr[0] = (ps_rr[0] + 1) % 4
        return PS[ps_rr[0]]

    SB_BASE = (int(nc.sbuf_base) + 63) // 64 * 64
    SB_TOP = int(nc.sbuf_top)
    root = Stack(nc, SB_BASE, SB_TOP)
    G = root.sub(5 * 1024)
    LP = root.sub(7 * 1024)
    UL = Stack(nc, root.cur, SB_TOP)

    g_st = g_c = None
    g_x = g_w = g_t = [None, None]

    ident_f = G.alloc([128], F32, "ident_f")
    ident_b = G.alloc([128], BF16, "ident_b")
    iota16 = G.alloc([16], F32, "iota16")
    halfpi = G.alloc([1], F32, "halfpi")
    X4 = G.alloc([4, 128], BF16, "X4")
    CWst = G.alloc([4, 64], BF16, "CWst")
    ones_f = G.alloc([128], F32, "ones_f")
    P.dma("sp", ident_f, ident_d, g_c)
    P.dma("sp", iota16, iota_d, g_c)
    P.cp("dve", ident_b, ident_f)
    P.memset("dve", halfpi, math.pi / 2)
    P.memset("dve", X4, 0.0)
    P.memset("dve", CWst, 0.0)
    P.memset("dve", ones_f, 1.0 / 512.0)

    Rt = LP.alloc([64], F32, "Rt")
    DSK = LP.alloc([8], F32, "DSK")
    H0 = LP.alloc([64, 2], F32, "H0")
    SCW = LP.alloc([4, 3], F32, "SCW")
    SCB = LP.alloc([4], F32, "SCB")
    CFW = LP.alloc([4, 31], F32, "CFW")
    CFB = LP.alloc([4], F32, "CFB")
    CFG = LP.alloc([4], F32, "CFG")
    CFBB = LP.alloc([4], F32, "CFBB")
    KTb = LP.alloc([2, 128], BF16, "KTb")
    NST = LP.alloc([64, 4, 2], F32, "NST")

    WSTG_B = 16 * 256 * 4
    WBF_B = 16 * 256 * 2

    def dbg_store(key, src, dst_slice=None):
        if key in dbg_out:
            dst = dbg_out[key] if dst_slice is None else dbg_out[key][dst_slice]
            P.dma("sp", dst, src, g_st)

    def phase_mod():
        UL.reset()
        condT = UL.alloc([16, 2], F32, "condT")
        bmod = UL.alloc([6 * D], F32, "bmod")
        stg = [UL.alloc([16, 256], F32, "mstg%d" % i) for i in range(2)]
        mo = [UL.alloc([256], F32, "mo%d" % i) for i in range(2)]
        for r_ in range(2):
            P.dma("sp", condT[:, :, r_], cond[r_].r("(kt p) -> p kt", p=128), g_c, allow_slow_non_contiguous=True)
        P.act(condT, condT, AF.Silu)
        for l in range(nlayers):
            P.dma("sp", bmod[0:2, :], TT(W["b_mod"].ap[l:l + 1, :].partition_broadcast(2), W["b_mod"].buf), g_c)
            for c in range(48):
                s_ = stg[c % 2]
                P.dma("sp", s_, W["w_mod"][l][:, c * 256:(c + 1) * 256].r("(kt p) c -> p kt c", p=128), g_w[c % 2])
                ps = ps_next()
                for kt in range(16):
                    P.mm(ps[0:2, 0:256], condT[:, kt, :], s_[:, kt, :], start=(kt == 0), stop=(kt == 15))
                m_ = mo[c % 2]
                P.tt("dve", m_[0:2, :], ps[0:2, 0:256], bmod[0:2, c * 256:(c + 1) * 256], ALU.add)
                if (8 <= c < 16) or (32 <= c < 40):
                    P.ts("dve", m_[0:2, :], m_[0:2, :], 1.0, ALU.add)
                P.dma("sp", mod_d[l][:, c * 256:(c + 1) * 256], m_[0:2, :], g_st)
        P.barrier()

    def phase_setup(l):
        UL.reset()
        A = UL
        nat = [A.alloc([128], F32, "nat%d" % i) for i in range(3)]
        dt2 = A.alloc([2], F32, "dt2")
        LR = A.alloc([64], F32, "LR")
        LI = A.alloc([64], F32, "LI")
        DT = A.alloc([64], F32, "DT")
        P.dma("sp", nat[0][0:64, :], W["s5_lam_re"][l].r("d (gp g2) p -> (d gp) (g2 p)", g2=2), g_c)
        P.dma("sp", nat[1][0:64, :], W["s5_lam_im"][l].r("d (gp g2) p -> (d gp) (g2 p)", g2=2), g_c)
        P.dma("sp", dt2[0:64, :], W["s5_log_dt"][l].r("d (gp g2) -> (d gp) g2", g2=2), g_c)
        P.cp("dve", nat[2][0:64, :].r("q (a b) -> q a b", a=2), dt2[0:64, :].un(2).bc([64, 2, 64]))
        for i, dst in enumerate((LR, LI, DT)):
            ps = ps_next()
            P.tr(ps[:, 0:64], nat[i][0:64, :], ident_f[0:64, 0:64])
            P.cp("act", dst, ps[:, 0:64])
        v = {k: A.alloc([64], F32, k) for k in ("dt", "ang", "c", "s", "t1", "t2", "are", "aim", "den", "am1", "zre", "zim")}
        P.act(v["dt"], DT, AF.Exp)
        P.tt("dve", v["t1"], LR, v["dt"], ALU.mult)
        P.act(Rt, v["t1"], AF.Exp)
        P.tt("dve", v["ang"], LI, v["dt"], ALU.mult)
        P.act(v["s"], v["ang"], AF.Sin, scale=1.0 / 32.0)
        P.act(v["c"], v["ang"], AF.Sin, bias=halfpi[:, 0:1], scale=1.0 / 32.0)
        for _ in range(5):
            P.tt("dve", v["t1"], v["c"], v["c"], ALU.mult)
            P.tt("dve", v["t2"], v["s"], v["s"], ALU.mult)
            P.stt(v["s"], v["c"], 2.0, v["s"], ALU.mult, ALU.mult)
            P.tt("dve", v["c"], v["t1"], v["t2"], ALU.subtract)
        C1, S1 = v["c"], v["s"]
        P.tt("dve", v["are"], Rt, C1, ALU.mult)
        P.tt("dve", v["aim"], Rt, S1, ALU.mult)
        P.tt("dve", v["t1"], LR, LR, ALU.mult)
        P.tt("dve", v["t2"], LI, LI, ALU.mult)
        P.tt("dve", v["den"], v["t1"], v["t2"], ALU.add)
        P.recip(v["den"], v["den"])
        P.ts("dve", v["am1"], v["are"], -1.0, ALU.add)
        P.tt("dve", v["t1"], v["am1"], LR, ALU.mult)
        P.tt("dve", v["t2"], v["aim"], LI, ALU.mult)
        P.tt("dve", v["t1"], v["t1"], v["t2"], ALU.add)
        P.tt("dve", v["zre"], v["t1"], v["den"], ALU.mult)
        P.tt("dve", v["t1"], v["aim"], LR, ALU.mult)
        P.tt("dve", v["t2"], v["am1"], LI, ALU.mult)
        P.tt("dve", v["t1"], v["t1"], v["t2"], ALU.subtract)
        P.tt("dve", v["zim"], v["t1"], v["den"], ALU.mult)
        BRE = A.alloc([64, 16], F32, "BRE")
        BIM = A.alloc([64, 16], F32, "BIM")
        tA = A.alloc([64, 16], F32, "tA")
        tB = A.alloc([64, 16], F32, "tB")
        BB = [A.alloc([64, 16], BF16, "BBre"), A.alloc([64, 16], BF16, "BBim")]
        P.dma("sp", BRE, W["s5_b_re"][l].r("d (gp g2) p s -> (g2 p) (d gp) s", g2=2), g_c)
        P.dma("sp", BIM, W["s5_b_im"][l].r("d (gp g2) p s -> (g2 p) (d gp) s", g2=2), g_c)
        zr = v["zre"].un(2).bc([128, 64, 16])
        zi = v["zim"].un(2).bc([128, 64, 16])
        P.tt("dve", tA, BRE, zr, ALU.mult)
        P.tt("dve", tB, BIM, zi, ALU.mult)
        P.tt("dve", BB[0], tA, tB, ALU.subtract)
        P.tt("dve", tA, BIM, zr, ALU.mult)
        P.tt("dve", tB, BRE, zi, ALU.mult)
        P.tt("dve", BB[1], tA, tB, ALU.add)
        wst = [A.alloc([4, 128], BF16, "wst%d" % i) for i in range(2)]
        it = 0
        for d in range(2):
            for ct in range(8):
                rt0 = d * 32 + ct * 4
                for reim in range(2):
                    for j in range(4):
                        P.cp("dve", X4[0:64, j, 32 * j:32 * j + 16], BB[reim][0:64, rt0 + j, :])
                        P.cp("dve", X4[64:128, j, 32 * j + 16:32 * j + 32], BB[reim][64:128, rt0 + j, :])
                    ps = ps_next()
                    psb = ps[:, 0:256].bitcast(BF16).r("p (j c) -> p j c", j=4)
                    for j in range(4):
                        P.tr(psb[:, j, :], X4[:, j, :], ident_b)
                    ws_ = wst[it % 2]
                    it += 1
                    P.cp("act", ws_, psb)
                    P.dma("sp", wbd[ct][:, d * 8 + reim:d * 8 + 8:2, :], ws_, g_st)
        CN = [A.alloc([64], F32, "CN%d" % i) for i in range(2)]
        CN2 = A.alloc([128], BF16, "CN2")
        cst = [A.alloc([4, 64], BF16, "cst%d" % i) for i in range(2)]
        it = 0
        for d in range(2):
            for ct in range(8):
                for reim in range(2):
                    src = W["s5_c_im" if reim else "s5_c_re"][l, d, 8 * ct:8 * ct + 8].r("g s p -> (g s) p")
                    cn = CN[it % 2]
                    P.dma("sp", cn, src, g_t[it % 2])
                    sgn = -1.0 if reim else 1.0
                    P.ts("dve", CN2[:, 0:64], cn, sgn, ALU.mult)
                    P.ts("dve", CN2[:, 64:128], cn, sgn, ALU.mult)
                    ps = ps_next()
                    psb = ps[:, 0:64].bitcast(BF16)
                    P.tr(psb, CN2, ident_b)
                    for j in range(4):
                        jl = j % 2
                        P.cp("dve", CWst[0:64, j, 32 * jl:32 * jl + 16], psb[0:64, 32 * j:32 * j + 16])
                        P.cp("dve", CWst[64:128, j, 32 * jl + 16:32 * jl + 32], psb[64:128, 32 * j + 16:32 * j + 32])
                    cs_ = cst[it % 2]
                    it += 1
                    P.cp("pool", cs_, CWst)
                    P.dma("sp", cwd[ct][:, d * 8 + reim:d * 8 + 8:2, :], cs_, g_st)
        ER = A.alloc([16, 260], F32, "ER")
        EI = A.alloc([16, 260], F32, "EI")
        q = {k: A.alloc([16, 128], F32, k) for k in ("q1", "q2", "q3", "q4")}
        MR = A.alloc([16], F32, "MR")
        MI = A.alloc([16], F32, "MI")
        m1 = A.alloc([16], F32, "m1")
        m2 = A.alloc([16], F32, "m2")
        for gq in range(4):
            sl = slice(gq * 16, gq * 16 + 16)
            P.memset("dve", ER[:, :, 0:1], 1.0)
            P.memset("dve", EI[:, :, 0:1], 0.0)
            P.cp("dve", MR, C1[:, sl])
            P.cp("dve", MI, S1[:, sl])
            k = 1
            while k < 260:
                cnt = min(k, 260 - k)
                o = 0
                while o < cnt:
                    n = min(128, cnt - o)
                    mrb = MR.un(2).bc([128, 16, n])
                    mib = MI.un(2).bc([128, 16, n])
                    sr = ER[:, :, o:o + n]
                    si = EI[:, :, o:o + n]
                    P.tt("dve", q["q1"][:, :, 0:n], sr, mrb, ALU.mult)
                    P.tt("dve", q["q2"][:, :, 0:n], si, mib, ALU.mult)
                    P.tt("pool", q["q3"][:, :, 0:n], sr, mib, ALU.mult)
                    P.tt("pool", q["q4"][:, :, 0:n], si, mrb, ALU.mult)
                    P.tt("dve", ER[:, :, k + o:k + o + n], q["q1"][:, :, 0:n], q["q2"][:, :, 0:n], ALU.subtract)
                    P.tt("pool", EI[:, :, k + o:k + o + n], q["q3"][:, :, 0:n], q["q4"][:, :, 0:n], ALU.add)
                    o += n
                if 2 * k < 260:
                    P.tt("dve", m1, MR, MR, ALU.mult)
                    P.tt("dve", m2, MI, MI, ALU.mult)
                    P.stt(MI, MR, 2.0, MI, ALU.mult, ALU.mult)
                    P.tt("dve", MR, m1, m2, ALU.subtract)
                k *= 2
            P.dma("sp", tabs[gq * 16:gq * 16 + 16, :, 0, :].r("rt p t -> p rt t"), ER, g_st)
            P.dma("sp", tabs[gq * 16:gq * 16 + 16, :, 1, :].r("rt p t -> p rt t"), EI, g_st)
        P.dma("sp", DSK, W["s5_d"][l].r("(ct p) -> p ct", p=128), g_c, allow_slow_non_contiguous=True)
        P.dma("sp", H0, st0[l].r("d (gp g2) p c -> (g2 p) (d gp) c", g2=2), g_c)
        cwn = A.alloc([512], F32, "cwn")
        for nm_, dst_, nk_ in (("sc_conv_w", SCW, 3), ("cf_conv_w", CFW, 31)):
            P.dma("sp", cwn[0:nk_, :], W[nm_][l], g_c)
            for ct_ in range(4):
                ps = ps_next()
                P.tr(ps[:, 0:nk_], cwn[0:nk_, ct_ * 128:(ct_ + 1) * 128], ident_f[0:nk_, 0:nk_])
                P.cp("act", dst_[:, ct_, :], ps[:, 0:nk_])
        P.dma("sp", SCB, W["sc_conv_b"][l].r("(ct p) -> p ct", p=128), g_c, allow_slow_non_contiguous=True)
        P.dma("sp", CFB, W["cf_conv_b"][l].r("(ct p) -> p ct", p=128), g_c, allow_slow_non_contiguous=True)
        P.dma("sp", CFG, W["cf_ln_g"][l].r("(ct p) -> p ct", p=128), g_c, allow_slow_non_contiguous=True)
        P.dma("sp", CFBB, W["cf_ln_b"][l].r("(ct p) -> p ct", p=128), g_c, allow_slow_non_contiguous=True)
        dbg_store("CFW", CFW)
        dbg_store("SCW", SCW)
        kn = [A.alloc([128], F32, "kn%d" % i) for i in range(2)]
        for hf, nm in enumerate(("peer_k1", "peer_k2")):
            P.dma("sp", kn[hf], W[nm][l], g_c)
            ps = ps_next()
            P.tr(ps[:, 0:128], kn[hf], ident_f)
            P.cp("act", KTb[:, hf, :], ps[:, 0:128])
        P.barrier()

    wslots = {}
    w_it = [0]

    def load_w(W2d, r0, nk, c0, ncol):
        i = w_it[0] % 2
        w_it[0] += 1
        stg, wbf = wslots["stg"][i], wslots["wbf"][i]
        P.dma("sp", stg[:, 0:nk, 0:ncol], W2d[r0:r0 + 128 * nk, c0:c0 + ncol].r("(kt p) c -> p kt c", p=128), g_w[i])
        if i == 0:
            P.cp("pool", wbf[:, 0:nk, 0:ncol], stg[:, 0:nk, 0:ncol])
        else:
            P.cp("act", wbf[:, 0:nk, 0:ncol], stg[:, 0:nk, 0:ncol])
        return wbf

    def linear_fm(in_tiles, W2d, r0, c0, ncols, consume, tile0=0):
        nk = len(in_tiles)
        for cc in range(0, ncols, 256):
            wb = load_w(W2d, r0, nk, c0 + cc, 256)
            for ti in range(2):
                ps = ps_next()
                for half in range(2):
                    hs = slice(half * 512, half * 512 + 512)
                    for kt in range(nk):
                        P.mm(ps[:, hs], wb[:, kt, ti * 128:(ti + 1) * 128], in_tiles[kt][:, hs],
                             start=(kt == 0), stop=(kt == nk - 1))
                consume(tile0 + cc // 128 + ti, ps)

    def ln_tm(x, out, st, mv, rs, e="dve"):
        for c in range(4):
            P.op("dve", lambda E, c=c: E.bn_stats(out=st.ap[:, c, :], in_=x.ap[:, c * 512:(c + 1) * 512]),
                 r=[x.buf], w=[st.buf])
        P.op("dve", lambda E: E.bn_aggr(out=mv.ap, in_=st.ap.rearrange("p a b -> p (a b)")), r=[st.buf], w=[mv.buf])
        P.ts("dve", rs, mv[:, 1:2], LN_EPS, ALU.add)
        P.act(rs, rs, AF.Sqrt)
        P.recip(rs, rs)
        P.ts("dve", out, x, mv[:, 0:1], ALU.subtract, rs[:, 0:1], ALU.mult)

    def phase_A(l, u):
        nseq, SL = (4, 256) if u == 0 else (1, 1024)
        UL.reset()
        hT = UL.alloc([16, T], BF16, "hT")
        bufP = UL.alloc([8, T], BF16, "bufP")
        bufQ = UL.alloc([8, T], BF16, "bufQ")
        ysT = UL.alloc([4, T], BF16, "ysT")
        zT = UL.alloc([4, T], BF16, "zT")
        wslots["wbf"] = [UL.alloc([16, 256], BF16, "wbf%d" % i) for i in range(2)]
        wslots["stg"] = [UL.alloc([16, 256], F32, "wstg%d" % i) for i in range(2)]
        X = Stack(nc, UL.cur, SB_TOP)

        X.reset()
        xt = [X.alloc([D], F32, "xt%d" % i) for i in range(2)]
        tmp = X.alloc([D], F32, "tmpA0")
        hb = X.alloc([D], BF16, "hb")
        sc1b = X.alloc([D], F32, "sc1b")
        sh1b = X.alloc([D], F32, "sh1b")
        pet = X.alloc([D], F32, "pet")
        st = X.alloc([4, 6], F32, "st")
        mv = X.alloc([2], F32, "mv")
        rs = X.alloc([1], F32, "rs")
        P.dma("sp", sh1b, TT(mod_d.ap[l, u:u + 1, 0:D].partition_broadcast(128), mod_d.buf), g_c)
        P.dma("sp", sc1b, TT(mod_d.ap[l, u:u + 1, D:2 * D].partition_broadcast(128), mod_d.buf), g_c)
        for tt_ in range(NTT):
            rows = slice(tt_ * 128, tt_ * 128 + 128)
            x_ = xt[tt_ % 2]
            if l == 0:
                P.dma("sp", x_, xin[u][rows, :], g_x[tt_ % 2])
                if u == 1:
                    P.dma("sp", pet, pe_d[rows, :], g_x[tt_ % 2])
                    P.tt("pool", x_, x_, pet, ALU.add)
                P.dma("sp", xres[u][rows, :], x_, g_st)
            else:
                P.dma("sp", x_, xres[u][rows, :], g_x[tt_ % 2])
            ln_tm(x_, tmp, st, mv, rs)
            P.tt("dve", tmp, tmp, sc1b, ALU.mult)
            P.tt("dve", hb, tmp, sh1b, ALU.add)
            ps = ps_next()
            psb = ps.bitcast(BF16)
            for kt in range(KT):
                P.tr(psb[:, kt * 128:(kt + 1) * 128], hb[:, kt * 128:(kt + 1) * 128], ident_b)
            P.cp("act", hT[:, :, rows], psb.r("p (k t) -> p k t", k=KT))
        dbg_store("hT", hT)
        if stop == "A0":
            return None
        hT_tiles = [hT[:, kt, :] for kt in range(KT)]
        P.barrier()

        uT = bufP
        ya = bufQ

        def cons_u(ti, ps):
            P.cp("act", uT[:, ti, :], ps)
        linear_fm(hT_tiles, W["w_in"][l], 0, 0, 1024, cons_u)
        dbg_store("uT", uT)
        X.reset()
        f = {k: X.alloc([T], F32, k) for k in ("bure", "buim", "t1", "t2", "t3", "t4", "br", "bi", "gre", "gim")}
        hre = X.alloc([T], BF16, "hre")
        him = X.alloc([T], BF16, "him")
        tbs = [X.alloc([2, 260], F32, "tb%d" % i) for i in range(2)]
        wbt = [X.alloc([16, 128], BF16, "wbt%d" % i) for i in range(2)]
        cwt = [X.alloc([16, 64], BF16, "cwt%d" % i) for i in range(2)]
        sm = {k: X.alloc([4], F32, k) for k in ("i_re", "i_im", "s1", "s2")}
        psA, psB, psC, psD = PS

        def v4(t_):
            return t_.r("p (c t) -> p c t", c=4)

        rts = []
        for ct in range(8):
            for d in range(2):
                for j in range(4):
                    rts.append((ct, d, j))

        state = {}

        def issue_bu(i):
            ct, d, j = rts[i]
            if d == 0 and j == 0:
                k = ct % 2
                P.dma("sp", wbt[k], wbd[ct], g_t[k])
                P.dma("sp", cwt[k], cwd[ct], g_t[k])
            rt = d * 32 + ct * 4 + j
            tb = tbs[i % 2]
            P.dma("sp", tb, tabs[rt], g_x[i % 2])
            wb_ = wbt[ct % 2]
            for reim, slot in ((0, psA), (1, psB)):
                for half in range(2):
                    hs = slice(half * 512, half * 512 + 512)
                    P.mm(slot[:, hs], wb_[:, (d * 4 + j) * 2 + reim, :], uT[:, ct, hs])

        issue_bu(0)
        for i, (ct, d, j) in enumerate(rts):
            rt = d * 32 + ct * 4 + j
            tb = tbs[i % 2]
            cw_ = cwt[ct % 2]
            Cb = tb[:, 0, 0:256].un(1).bc([128, 4, 256])
            Sb = tb[:, 1, 0:256].un(1).bc([128, 4, 256])
            for src, dst in ((psA, f["bure"]), (psB, f["buim"])):
                s3 = src.r("p (b t) -> p b t", b=nseq)
                if d == 1:
                    s3 = s3[:, :, ::-1]
                P.cp("act", dst.r("p (b t) -> p b t", b=nseq), s3)
            if i + 1 < len(rts):
                issue_bu(i + 1)
            P.tt("dve", v4(f["t1"]), v4(f["bure"]), Cb, ALU.mult)
            P.tt("dve", v4(f["t2"]), v4(f["buim"]), Sb, ALU.mult)
            P.tt("dve", f["br"], f["t1"], f["t2"], ALU.add)
            P.tt("pool", v4(f["t3"]), v4(f["buim"]), Cb, ALU.mult)
            P.tt("pool", v4(f["t4"]), v4(f["bure"]), Sb, ALU.mult)
            P.tt("pool", f["bi"], f["t3"], f["t4"], ALU.subtract)
            rcol = Rt[:, rt:rt + 1]
            for c in range(4):
                cs = slice(c * 256, c * 256 + 256)
                if u == 0:
                    ire, iim = 0.0, 0.0
                else:
                    if c == 0:
                        pre, pim = H0[:, rt, 0:1], H0[:, rt, 1:2]
                        er, ei = tb[:, 0, 1:2], tb[:, 1, 1:2]
                    else:
                        pre, pim = f["gre"][:, c * 256 - 1:c * 256], f["gim"][:, c * 256 - 1:c * 256]
                        er, ei = tb[:, 0, 256:257], tb[:, 1, 256:257]
                    P.ts("dve", sm["s1"][:, 0:1], pim, ei, ALU.mult)
                    P.stt(sm["i_re"][:, c:c + 1], pre, er, sm["s1"][:, 0:1], ALU.mult, ALU.subtract)
                    P.ts("dve", sm["s2"][:, 0:1], pim, er, ALU.mult)
                    P.stt(sm["i_im"][:, c:c + 1], pre, ei, sm["s2"][:, 0:1], ALU.mult, ALU.add)
                    ire, iim = sm["i_re"][:, c:c + 1], sm["i_im"][:, c:c + 1]
                P.scan(f["gre"][:, cs], rcol.bc([128, 256]), f["br"][:, cs], ire)
                P.scan(f["gim"][:, cs], rcol.bc([128, 256]), f["bi"][:, cs], iim)
            P.tt("dve", v4(f["t1"]), v4(f["gre"]), Cb, ALU.mult)
            P.tt("dve", v4(f["t2"]), v4(f["gim"]), Sb, ALU.mult)
            P.tt("dve", hre, f["t1"], f["t2"], ALU.subtract)
            P.tt("pool", v4(f["t3"]), v4(f["gre"]), Sb, ALU.mult)
            P.tt("pool", v4(f["t4"]), v4(f["gim"]), Cb, ALU.mult)
            P.tt("pool", him, f["t3"], f["t4"], ALU.add)
            if u == 0:
                glr = v4(f["gre"])[:, :, 255]
                gli = v4(f["gim"])[:, :, 255]
                c255, s255 = tb[:, 0, 255:256], tb[:, 1, 255:256]
                P.ts("dve", sm["s1"], gli, s255, ALU.mult)
                P.stt(NST[:, rt, :, 0], glr, c255, sm["s1"], ALU.mult, ALU.subtract)
                P.ts("dve", sm["s2"], gli, c255, ALU.mult)
                P.stt(NST[:, rt, :, 1], glr, s255, sm["s2"], ALU.mult, ALU.add)
            ysl = psC if d == 0 else psD
            jj = j // 2
            for reim, hs_ in ((0, hre), (1, him)):
                for half in range(2):
                    hs = slice(half * 512, half * 512 + 512)
                    P.mm(ysl[64 * jj:64 * jj + 64, hs], cw_[:, (d * 4 + j) * 2 + reim, :], hs_[:, hs],
                         start=(j % 2 == 0 and reim == 0), stop=(j % 2 == 1 and reim == 1))
            if d == 1 and j == 3:
                s3 = psD.r("p (b t) -> p b t", b=nseq)[:, :, ::-1]
                P.cp("act", f["t3"].r("p (b t) -> p b t", b=nseq), s3)
                P.tt("dve", f["t1"], psC, f["t3"], ALU.add)
                P.stt(f["t2"], uT[:, ct, :], DSK[:, ct:ct + 1], f["t1"], ALU.mult, ALU.add)
                P.act(ya[:, ct, :], f["t2"], AF.Gelu_apprx_tanh)
        if u == 0:
            for b_ in range(4):
                P.dma("sp", nst_o[b_, l].r("d (gp g2) p c -> (g2 p) (d gp) c", g2=2), NST[:, :, b_, :], g_st)
        dbg_store("ya", ya)
        dbg_store("nst", nst_o) if False else None
        if stop == "A1s":
            return None
        P.barrier()
        X.reset()
        sg = [X.alloc([T], F32, "sg%d" % i) for i in range(2)]
        yag = bufP
        ya_tiles = [ya[:, k, :] for k in range(8)]

        def cons_glu(ti, ps):
            s_ = sg[ti % 2]
            P.act(s_, ps, AF.Sigmoid)
            P.tt("dve", yag[:, ti, :], ya[:, ti, :], s_, ALU.mult)
        linear_fm(ya_tiles, W["s5_w_glu"][l], 0, 0, 1024, cons_glu)
        dbg_store("yag", yag)
        if stop == "A1":
            return None

        X.reset()
        fb = {k: X.alloc([T], F32, k) for k in ("cb", "cc", "prod", "acc")}
        keep = {}

        def seqv(t_):
            return t_.r("p (b t) -> p b t", b=nseq)

        def cons_sc(ti, ps):
            kind, i = divmod(ti, 4)
            if kind == 0:
                if i not in keep:
                    keep[i] = X.alloc([T], F32, "scb%d" % i)
                P.cp("act", keep[i], ps)
            elif kind == 1:
                if ("c", i) not in keep:
                    keep[("c", i)] = X.alloc([T], F32, "scc%d" % i)
                P.cp("act", keep[("c", i)], ps)
            else:
                P.tt("dve", fb["prod"], keep[("c", i)], ps, ALU.mult)
                P.ts("dve", fb["acc"], fb["prod"], SCW[:, i, 1:2], ALU.mult, SCB[:, i:i + 1], ALU.add)
                a3, p3 = seqv(fb["acc"]), seqv(fb["prod"])
                P.stt(a3[:, :, 1:SL], p3[:, :, 0:SL - 1], SCW[:, i, 0:1], a3[:, :, 1:SL], ALU.mult, ALU.add)
                P.stt(a3[:, :, 0:SL - 1], p3[:, :, 1:SL], SCW[:, i, 2:3], a3[:, :, 0:SL - 1], ALU.mult, ALU.add)
                P.tt("dve", ysT[:, i, :], keep[i], fb["acc"], ALU.mult)
        linear_fm(hT_tiles, W["w_in"][l], 0, 1024, 1536, cons_sc)
        dbg_store("ys", ysT)
        if stop == "A2":
            return None
        P.barrier()

        X.reset()
        ga = [X.alloc([T], F32, "ga%d" % i) for i in range(4)]
        cv = [X.alloc([T], F32, "cv%d" % i) for i in range(4)]
        fc = {k: X.alloc([T], F32, k) for k in ("sq", "rstd", "mr")}

        def cons_cf(ti, ps):
            kind, i = divmod(ti, 4)
            if kind == 0:
                P.cp("act", ga[i], ps)
            else:
                P.act(fc["sq"], ps, AF.Sigmoid)
                P.tt("dve", ga[i], ga[i], fc["sq"], ALU.mult)
                P.ts("dve", cv[i], ga[i], CFW[:, i, 15:16], ALU.mult, CFB[:, i:i + 1], ALU.add)
                a3, g3 = seqv(cv[i]), seqv(ga[i])
                for k in range(31):
                    sh = k - 15
                    if sh == 0:
                        continue
                    if sh > 0:
                        P.stt(a3[:, :, 0:SL - sh], g3[:, :, sh:SL], CFW[:, i, k:k + 1], a3[:, :, 0:SL - sh], ALU.mult, ALU.add)
                    else:
                        P.stt(a3[:, :, -sh:SL], g3[:, :, 0:SL + sh], CFW[:, i, k:k + 1], a3[:, :, -sh:SL], ALU.mult, ALU.add)
        linear_fm(hT_tiles, W["w_in"][l], 0, 2560, 1024, cons_cf)
        dbg_store("glu0", ga[0])
        dbg_store("cv0", cv[0])
        psM, psQ = PS[0], PS[1]
        for i in range(4):
            for half in range(2):
                hs = slice(half * 512, half * 512 + 512)
                P.mm(psM[:, hs], ones_f, cv[i][:, hs], start=(i == 0), stop=(i == 3))
        for i in range(4):
            P.act(ga[i], cv[i], AF.Square)
        for i in range(4):
            for half in range(2):
                hs = slice(half * 512, half * 512 + 512)
                P.mm(psQ[:, hs], ones_f, ga[i][:, hs], start=(i == 0), stop=(i == 3))
        P.cp("act", fc["mr"], psM)
        P.tt("dve", fc["sq"], fc["mr"], fc["mr"], ALU.mult)
        P.tt("dve", fc["rstd"], psQ, fc["sq"], ALU.subtract)
        P.ts("dve", fc["rstd"], fc["rstd"], LN_EPS, ALU.add)
        P.act(fc["rstd"], fc["rstd"], AF.Sqrt)
        P.recip(fc["rstd"], fc["rstd"])
        dbg_store("rstd", fc["rstd"])
        dbg_store("mean", psM) if False else None
        P.tt("dve", fc["mr"], fc["mr"], fc["rstd"], ALU.mult)
        for i in range(4):
            P.tt("dve", ga[i], cv[i], fc["rstd"], ALU.mult)
            P.tt("dve", ga[i], ga[i], fc["mr"], ALU.subtract)
            P.ts("dve", ga[i], ga[i], CFG[:, i:i + 1], ALU.mult, CFBB[:, i:i + 1], ALU.add)
            P.act(zT[:, i, :], ga[i], AF.Silu)
        dbg_store("z", zT)
        if stop == "A3":
            return None
        P.barrier()

        X.reset()
        mT = X.alloc([16, T], BF16, "mT")
        sgm = [X.alloc([T], F32, "sgm%d" % i) for i in range(2)]
        macc = [X.alloc([T], F32, "macc%d" % i) for i in range(2)]
        tmpm = X.alloc([T], F32, "tmpm")
        yag_t = [bufP[:, k, :] for k in range(8)]
        ys_t = [ysT[:, k, :] for k in range(4)]
        z_t = [zT[:, k, :] for k in range(4)]
        branches = [(yag_t, W["w_pa"][l], 0), (ys_t, W["w_pb"][l], 1), (z_t, W["w_pc"][l], 2)]
        for pair in range(8):
            cbase = pair * 256
            for bi, (tiles, Wp, gidx) in enumerate(branches):
                nk = len(tiles)
                wb = load_w(Wp, 0, nk, cbase, 256)
                pp = []
                for ti in range(2):
                    ps = ps_next()
                    for half in range(2):
                        hs = slice(half * 512, half * 512 + 512)
                        for kt in range(nk):
                            P.mm(ps[:, hs], wb[:, kt, ti * 128:(ti + 1) * 128], tiles[kt][:, hs], start=(kt == 0), stop=(kt == nk - 1))
                    pp.append(ps)
                wg = load_w(W["w_in"][l], 0, 16, GATE0 + gidx * D + cbase, 256)
                for ti in range(2):
                    ps = ps_next()
                    for half in range(2):
                        hs = slice(half * 512, half * 512 + 512)
                        for kt in range(16):
                            P.mm(ps[:, hs], wg[:, kt, ti * 128:(ti + 1) * 128], hT_tiles[kt][:, hs], start=(kt == 0), stop=(kt == 15))
                    P.act(sgm[ti], ps, AF.Sigmoid)
                    if bi == 0:
                        P.tt("dve", macc[ti], sgm[ti], pp[ti], ALU.mult)
                    elif bi == 1:
                        P.tt("dve", tmpm, sgm[ti], pp[ti], ALU.mult)
                        P.tt("pool", macc[ti], macc[ti], tmpm, ALU.add)
                    else:
                        P.tt("dve", tmpm, sgm[ti], pp[ti], ALU.mult)
                        P.tt("pool", mT[:, pair * 2 + ti, :], macc[ti], tmpm, ALU.add)
        dbg_store("mT", mT)
        if stop == "A4":
            return None
        ost = [X.alloc([256], F32, "ost%d" % i) for i in range(2)]
        it = 0
        for cc in range(8):
            wb = load_w(W["w_o"][l], 0, 16, cc * 256, 256)
            for tt_ in range(NTT):
                ps = ps_next()
                for kt in range(16):
                    P.mm(ps[:, 0:256], mT[:, kt, tt_ * 128:(tt_ + 1) * 128], wb[:, kt, :], start=(kt == 0), stop=(kt == 15))
                o_ = ost[it % 2]
                it += 1
                P.cp("act", o_, ps[:, 0:256])
                P.dma("sp", ymix[tt_ * 128:(tt_ + 1) * 128, cc * 256:(cc + 1) * 256], o_, g_st)
        P.barrier()
        return None

    def phase_B(l, u, last):
        UL.reset()
        h2T = UL.alloc([16, T], BF16, "h2T")
        qT = UL.alloc([16, T], BF16, "qT")
        wslots["wbf"] = [UL.alloc([16, 256], BF16, "wbfB%d" % i) for i in range(2)]
        Y = Stack(nc, UL.cur, SB_TOP)
        Y.reset()
        wslots["stg"] = None
        xt = [Y.alloc([D], F32, "xtB%d" % i) for i in range(2)]
        yt = [Y.alloc([D], F32, "ytB%d" % i) for i in range(2)]
        t1 = Y.alloc([D], F32, "t1B")
        hb = Y.alloc([D], BF16, "hbB")
        bct = {k: Y.alloc([D], F32, k) for k in ("gt1", "g1", "b1", "sc2", "sh2")}
        st = Y.alloc([4, 6], F32, "stB")
        mv = Y.alloc([2], F32, "mvB")
        rs = Y.alloc([1], F32, "rsB")

        def bload(dst, src2d, row, c0):
            P.dma("sp", dst, TT(src2d.ap[row:row + 1, c0:c0 + D].partition_broadcast(128), src2d.buf), g_c)
        bload(bct["gt1"], mod_d[l], u, 2 * D)
        bload(bct["sc2"], mod_d[l], u, 4 * D)
        bload(bct["sh2"], mod_d[l], u, 3 * D)
        bload(bct["g1"], W["ln1_g"], l, 0)
        bload(bct["b1"], W["ln1_b"], l, 0)
        for tt_ in range(NTT):
            rows = slice(tt_ * 128, tt_ * 128 + 128)
            x_, y_ = xt[tt_ % 2], yt[tt_ % 2]
            P.dma("sp", x_, xres[u][rows, :], g_x[tt_ % 2])
            P.dma("sp", y_, ymix[rows, :], g_x[tt_ % 2])
            P.tt("pool", y_, y_, bct["gt1"], ALU.mult)
            P.stt(y_, x_, ALPHA, y_, ALU.mult, ALU.add)
            ln_tm(y_, t1, st, mv, rs)
            P.tt("pool", t1, t1, bct["g1"], ALU.mult)
            P.tt("dve", x_, t1, bct["b1"], ALU.add)
            P.dma("sp", x1res[rows, :], x_, g_st)
            if tt_ == 0:
                dbg_store("x1", x_)
            ln_tm(x_, t1, st, mv, rs)
            P.tt("pool", t1, t1, bct["sc2"], ALU.mult)
            P.tt("dve", y_, t1, bct["sh2"], ALU.add)
            P.dma("sp", h2res[rows, :], y_, g_st)
            if tt_ == 0:
                dbg_store("h2", y_)
            P.cp("pool", hb, y_)
            ps = ps_next()
            psb = ps.bitcast(BF16)
            for kt in range(KT):
                P.tr(psb[:, kt * 128:(kt + 1) * 128], hb[:, kt * 128:(kt + 1) * 128], ident_b)
            P.cp("act", h2T[:, :, rows], psb.r("p (k t) -> p k t", k=KT))
        if stop == "B1":
            return
        P.barrier()
        Y.reset()
        wslots["stg"] = [Y.alloc([16, 256], F32, "wstgB%d" % i) for i in range(2)]
        h2T_t = [h2T[:, k, :] for k in range(KT)]

        def cons_q(ti, ps):
            P.cp("act", qT[:, ti, :], ps)
        linear_fm(h2T_t, W["peer_w_q"][l], 0, 0, D, cons_q)
        P.barrier()
        Y.reset()
        x1 = Y.alloc([D], F32, "x1")
        h2 = Y.alloc([D], F32, "h2")
        acc = Y.alloc([D], F32, "acc")
        tq = Y.alloc([D], BF16, "tq")
        bc3 = {k: Y.alloc([D], F32, k) for k in ("gt2", "g2", "b2")}
        gb = [Y.alloc([D], F32, "gb%d" % i) for i in range(3)]
        S = Y.alloc([16, 128], F32, "S")
        S2 = Y.alloc([256], F32, "S2")
        V = Y.alloc([16, 16], F32, "V")
        Iu = Y.alloc([16, 16], U32, "Iu")
        If = Y.alloc([16, 16], F32, "If")
        cand = Y.alloc([8, 256], F32, "cand")
        top = Y.alloc([8, 16], F32, "top")
        pos = Y.alloc([8, 16], U32, "pos")
        pa_ = Y.alloc([8, 16], U32, "pa")
        pb_ = Y.alloc([8, 16], U32, "pb")
        af = Y.alloc([8, 16], F32, "af")
        bf = Y.alloc([8, 16], F32, "bf")
        eq = Y.alloc([8, 16, 16], F32, "eq")
        i1s = Y.alloc([8, 16], F32, "i1s")
        i2s = Y.alloc([8, 16], F32, "i2s")
        idxf = Y.alloc([128], F32, "idxf")
        idxi = Y.alloc([128], I32, "idxi")
        gw = Y.alloc([8, 16], F32, "gw")
        gsum = Y.alloc([8], F32, "gsum")
        actv = Y.alloc([128], F32, "actv")
        wv = Y.alloc([128], F32, "wv")
        st = Y.alloc([4, 6], F32, "stC")
        mv = Y.alloc([2], F32, "mvC")
        rs = Y.alloc([1], F32, "rsC")
        bload(bc3["gt2"], mod_d[l], u, 5 * D)
        bload(bc3["g2"], W["ln2_g"], l, 0)
        bload(bc3["b2"], W["ln2_b"], l, 0)
        utab = W["peer_u"].r("l e d -> (l e) d")
        vtab = W["peer_v"].r("l e d -> (l e) d")
        git = [0]

        def gather(tab, c):
            k = git[0] % 3
            git[0] += 1
            dst = gb[k]
            P.op("pool", lambda E: E.indirect_dma_start(out=dst.ap, out_offset=None, in_=tab.ap,
                                                         in_offset=bass.IndirectOffsetOnAxis(ap=idxi.ap[:, c:c + 1], axis=0)),
                 r=[tab.buf, idxi.buf], w=[dst.buf], grp=P.dsem_for(dst.buf))
            return dst

        for tt_ in range(b3_tiles):
            rows = slice(tt_ * 128, tt_ * 128 + 128)
            P.dma("sp", x1, x1res[rows, :], g_x[0])
            P.dma("sp", h2, h2res[rows, :], g_x[1])
            for q4 in range(4):
                ps = ps_next()
                for m in range(4):
                    hh = q4 * 4 + m
                    P.mm(ps[:, m * 128:(m + 1) * 128], qT[:, hh, rows], KTb[:, hh % 2, :])
                P.cp("act", S[:, q4 * 4:q4 * 4 + 4, :], ps[:, 0:512].r("p (a n) -> p a n", a=4))
            for hh in range(16):
                P.op("dve", lambda E, hh=hh: E.max(out=V.ap[:, hh, 0:8], in_=S.ap[:, hh, :]), r=[S.buf], w=[V.buf])
                P.op("dve", lambda E, hh=hh: E.max_index(out=Iu.ap[:, hh, 0:8], in_max=V.ap[:, hh, 0:8], in_values=S.ap[:, hh, :]),
                     r=[S.buf, V.buf], w=[Iu.buf])
                P.op("dve", lambda E, hh=hh: E.match_replace(out=S2.ap[:, 0:128], in_to_replace=V.ap[:, hh, 0:8],
                                                             in_values=S.ap[:, hh, :], imm_value=NEG), r=[S.buf, V.buf], w=[S2.buf])
                P.op("dve", lambda E, hh=hh: E.max(out=V.ap[:, hh, 8:16], in_=S2.ap[:, 0:128]), r=[S2.buf], w=[V.buf])
                P.op("dve", lambda E, hh=hh: E.max_index(out=Iu.ap[:, hh, 8:16], in_max=V.ap[:, hh, 8:16], in_values=S2.ap[:, 0:128]),
                     r=[S2.buf, V.buf], w=[Iu.buf])
            P.cp("dve", If, Iu)
            V4 = V.r("p (h f) a -> p h f a", f=2)
            I4 = If.r("p (h f) a -> p h f a", f=2)
            c4 = cand.r("p h (a b) -> p h a b", a=16)
            P.tt("dve", c4, V4[:, :, 0, :].un(3).bc([128, 8, 16, 16]), V4[:, :, 1, :].un(2).bc([128, 8, 16, 16]), ALU.add)
            for h in range(8):
                P.op("dve", lambda E, h=h: E.max(out=top.ap[:, h, 0:8], in_=cand.ap[:, h, :]), r=[cand.buf], w=[top.buf])
                P.op("dve", lambda E, h=h: E.max_index(out=pos.ap[:, h, 0:8], in_max=top.ap[:, h, 0:8], in_values=cand.ap[:, h, :]),
                     r=[cand.buf, top.buf], w=[pos.buf])
                P.op("dve", lambda E, h=h: E.match_replace(out=S2.ap, in_to_replace=top.ap[:, h, 0:8], in_values=cand.ap[:, h, :],
                                                           imm_value=NEG), r=[cand.buf, top.buf], w=[S2.buf])
                P.op("dve", lambda E, h=h: E.max(out=top.ap[:, h, 8:16], in_=S2.ap), r=[S2.buf], w=[top.buf])
                P.op("dve", lambda E, h=h: E.max_index(out=pos.ap[:, h, 8:16], in_max=top.ap[:, h, 8:16], in_values=S2.ap),
                     r=[S2.buf, top.buf], w=[pos.buf])
            P.op("dve", lambda E: E.tensor_single_scalar(out=pa_.ap, in_=pos.ap, scalar=4, op=ALU.logical_shift_right),
                 r=[pos.buf], w=[pa_.buf])
            P.op("dve", lambda E: E.tensor_single_scalar(out=pb_.ap, in_=pos.ap, scalar=15, op=ALU.bitwise_and),
                 r=[pos.buf], w=[pb_.buf])
            P.cp("dve", af, pa_)
            P.cp("dve", bf, pb_)
            io4 = iota16.un(1).un(1).bc([128, 8, 16, 16])
            P.tt("dve", eq, af.un(3).bc([128, 8, 16, 16]), io4, ALU.is_equal)
            P.tt("dve", eq, eq, I4[:, :, 0, :].un(2).bc([128, 8, 16, 16]), ALU.mult)
            P.reduce(i1s, eq, ALU.add)
            P.tt("dve", eq, bf.un(3).bc([128, 8, 16, 16]), io4, ALU.is_equal)
            P.tt("dve", eq, eq, I4[:, :, 1, :].un(2).bc([128, 8, 16, 16]), ALU.mult)
            P.reduce(i2s, eq, ALU.add)
            P.stt(idxf.r("p (h k) -> p h k", h=8), i1s, 128.0, i2s, ALU.mult, ALU.add)
            if l > 0:
                P.ts("dve", idxf, idxf, float(16384 * l), ALU.add)
            P.cp("dve", idxi, idxf)
            P.tt("dve", gw, top, top[:, :, 0:1].bc([128, 8, 16]), ALU.subtract)
            P.act(gw, gw, AF.Exp)
            P.reduce(gsum, gw, ALU.add)
            P.recip(gsum, gsum)
            P.tt("dve", gw, gw, gsum.un(2).bc([128, 8, 16]), ALU.mult)
            if tt_ == 0:
                dbg_store("S", S)
                dbg_store("idx", idxi)
                dbg_store("gw", gw)
            for c in range(128):
                g_ = gather(utab, c)
                P.op("dve", lambda E, g_=g_, c=c: E.scalar_tensor_tensor(out=tq.ap, in0=h2.ap, scalar=1.0, in1=g_.ap,
                                                                         op0=ALU.mult, op1=ALU.mult, accum_out=actv.ap[:, c:c + 1]),
                     r=[h2.buf, g_.buf], w=[tq.buf, actv.buf])
            P.act(wv, actv, AF.Gelu_apprx_tanh)
            P.tt("dve", wv, wv, gw.r("p h k -> p (h k)"), ALU.mult)
            for c in range(128):
                g_ = gather(vtab, c)
                if c == 0:
                    P.ts("dve", acc, g_, wv[:, 0:1], ALU.mult)
                else:
                    P.stt(acc, g_, wv[:, c:c + 1], acc, ALU.mult, ALU.add)
            if tt_ == 0:
                dbg_store("actv", actv)
                dbg_store("peer", acc)
            P.tt("pool", acc, acc, bc3["gt2"], ALU.mult)
            P.stt(acc, x1, ALPHA, acc, ALU.mult, ALU.add)
            ln_tm(acc, gb[0], st, mv, rs)
            P.tt("pool", gb[0], gb[0], bc3["g2"], ALU.mult)
            P.tt("dve", x1, gb[0], bc3["b2"], ALU.add)
            if last:
                P.dma("sp", yout[u][rows, :], x1, g_st)
            else:
                P.dma("sp", xres[u][rows, :], x1, g_st)
        P.barrier()

    phase_mod()
    ret = None
    for l in range(nlayers):
        phase_setup(l)
        if stop == "setup":
            break
        for u in units:
            ret = phase_A(l, u)
            if stop is not None and stop.startswith("A"):
                break
            phase_B(l, u, last=(l == nlayers - 1))
            if stop is not None:
                break
        if stop is not None:
            break
    if ret is not None and "ret" in dbg_out:
        src = ret
        P.dma("sp", dbg_out["ret"], src, g_st)
    P.final_wait()
    P.emit()
    return nc


def _grid_pos():
    rows = T // 64
    t = np.arange(rows * 64)
    r = (t // 64).astype(np.float32)
    col = (t % 64).astype(np.float32)
    nf = D // 4
    freq = (1.0 / (np.float32(10000.0) ** (np.arange(nf, dtype=np.float32) / np.float32(nf)))).astype(np.float32)
    ar = r[:, None] * freq
    ac = col[:, None] * freq
    return np.concatenate([np.sin(ar), np.cos(ar), np.sin(ac), np.cos(ac)], -1).astype(np.float32)


WEIGHT_NAMES = ["w_mod", "b_mod", "w_in", "s5_lam_re", "s5_lam_im", "s5_log_dt", "s5_b_re", "s5_b_im", "s5_c_re",
                "s5_c_im", "s5_d", "s5_w_glu", "sc_conv_w", "sc_conv_b", "cf_conv_w", "cf_conv_b", "cf_ln_g", "cf_ln_b",
                "w_pa", "w_pb", "w_pc", "w_o", "ln1_g", "ln1_b", "peer_w_q", "peer_k1", "peer_k2", "peer_u", "peer_v",
                "ln2_g", "ln2_b"]


def make_in_maps(inputs, cores=range(NCORE)):
    f = lambda a: np.ascontiguousarray(np.asarray(a, dtype=np.float32))
    xp = f(inputs["x_prompt"])
    xs = f(inputs["x_sample"])
    stt = f(inputs["state_ssm"])
    c = f(inputs["c"])
    cc = f(inputs["c_ctx"])
    pe = _grid_pos()
    ident = np.eye(128, dtype=np.float32)
    iota = np.tile(np.arange(16, dtype=np.float32)[None, :], (128, 1))
    wts = {k: f(inputs[k]) for k in WEIGHT_NAMES}
    maps = []
    for i in cores:
        m = dict(wts)
        m["xp"] = np.ascontiguousarray(xp[4 * i:4 * i + 4].reshape(T, D))
        m["xs"] = np.ascontiguousarray(xs[i].reshape(T, D))
        m["pe"] = pe
        m["st0"] = np.ascontiguousarray(stt[i])
        m["cond"] = np.ascontiguousarray(np.stack([cc, c[i]], 0))
        m["ident"] = ident
        m["iota16"] = iota
        maps.append(m)
    return maps


def kernel(**inputs):
    nc = build()
    maps = make_in_maps(inputs)
    maps = [{k: m[k] for k in build.declared} for m in maps]
    res = run_bass_kernel_spmd(nc, maps, core_ids=list(range(NCORE)))
    yp = np.concatenate([r["yp"].reshape(4, 256, D) for r in res.results], 0).astype(np.float32)
    ys = np.stack([r["ys"].reshape(T, D) for r in res.results], 0).astype(np.float32)
    nst = np.concatenate([r["nst"] for r in res.results], 0).astype(np.float32)
    return (yp, ys, nst)
```

```python
import math
from contextlib import ExitStack

import numpy as np
import concourse.bass as bass
import concourse.mybir as mybir
from concourse.bass_utils import run_bass_kernel_spmd

F32 = mybir.dt.float32
BF16 = mybir.dt.bfloat16
I32 = mybir.dt.int32
U32 = mybir.dt.uint32
ALU = mybir.AluOpType
AF = mybir.ActivationFunctionType
AX = mybir.AxisListType

D = 2048
NCORE = 8
T = 1024
NTT = 8
KT = 16
NL = 2
ALPHA = 4.0 ** 0.25
LN_EPS = 1e-6
IN_COLS = 9728
GATE0 = 3584
NEG = -1.0e30


class Buf:
    __slots__ = ("name", "lw", "rd", "space", "dsem")

    def __init__(self, name, space="sb"):
        self.name = name
        self.lw = {}
        self.rd = {}
        self.space = space
        self.dsem = None


class Grp:
    __slots__ = ("sem", "cnt")

    def __init__(self, sem):
        self.sem = sem
        self.cnt = 0


class TT:
    __slots__ = ("ap", "buf")

    def __init__(self, ap, buf):
        self.ap = ap
        self.buf = buf

    def __getitem__(self, k):
        return TT(self.ap[k], self.buf)

    def r(self, pat, **kw):
        return TT(self.ap.rearrange(pat, **kw), self.buf)

    def bc(self, shape):
        return TT(self.ap.to_broadcast(list(shape)), self.buf)

    def un(self, ax):
        return TT(self.ap.unsqueeze(ax), self.buf)

    def bitcast(self, dt):
        return TT(self.ap.bitcast(dt), self.buf)


def _a(x):
    return x.ap if isinstance(x, TT) else x


class Prog:
    def __init__(self, nc, es):
        self.nc = nc
        self.es = es
        self.E = dict(pe=nc.tensor, dve=nc.vector, act=nc.scalar, pool=nc.gpsimd, sp=nc.sync)
        self.rec = {k: [] for k in self.E}
        self.esem = {k: es.enter_context(nc.semaphore("es_" + k)) for k in self.E}
        self.ecnt = {k: 0 for k in self.E}
        self.known = {k: {} for k in self.E}
        self.grps = []
        self.free = []
        self.active = []
        self.max_dsem = 84

    def grp(self, name):
        g = Grp(self.es.enter_context(self.nc.semaphore(name)))
        self.grps.append(g)
        return g

    def dsem_for(self, buf):
        if buf.dsem is None:
            if self.free:
                buf.dsem = self.free.pop()
            else:
                assert len(self.grps) < self.max_dsem, "out of DMA semaphores"
                buf.dsem = self.grp("dq%d" % len(self.grps))
            self.active.append(buf)
        return buf.dsem

    def op(self, e, fn, r=(), w=(), grp=None):
        need = {}
        own = self.esem[e]

        def add(tok):
            sem, val = tok
            if e == "pe" and sem is own:
                return
            k = id(sem)
            if k not in need or need[k][1] < val:
                need[k] = (sem, val)

        for b in list(r) + list(w):
            for tok in b.lw.values():
                add(tok)
        for b in w:
            for tok in b.rd.values():
                add(tok)
        waits = []
        kn = self.known[e]
        for k, (sem, val) in need.items():
            if kn.get(k, 0) >= val:
                continue
            kn[k] = val
            waits.append((sem, val))
        if grp is not None:
            grp.cnt += 16
            tok = (grp.sem, grp.cnt)
            inc = (grp.sem, 16)
        else:
            self.ecnt[e] += 1
            tok = (own, self.ecnt[e])
            inc = (own, 1)
        for b in w:
            if b.space == "dram":
                b.lw[id(tok[0])] = tok
            else:
                b.lw = {id(tok[0]): tok}
            b.rd = {}
        for b in r:
            if b in w:
                continue
            b.rd[id(tok[0])] = tok
        self.rec[e].append((waits, fn, inc))

    def barrier(self):
        toks = [(self.esem[k], self.ecnt[k]) for k in self.E] + [(g.sem, g.cnt) for g in self.grps]
        for e in self.E:
            waits = []
            kn = self.known[e]
            for sem, val in toks:
                if val == 0 or sem is self.esem[e]:
                    continue
                if kn.get(id(sem), 0) >= val:
                    continue
                kn[id(sem)] = val
                waits.append((sem, val))
            if waits:
                self.rec[e].append((waits, None, None))
        for b in self.active:
            self.free.append(b.dsem)
            b.dsem = None
        self.active = []

    def final_wait(self):
        e = "sp"
        waits = [(self.esem[k], self.ecnt[k]) for k in self.E if k != e and self.ecnt[k]]
        waits += [(g.sem, g.cnt) for g in self.grps if g.cnt]
        self.rec[e].append((waits, None, None))

    def emit(self):
        with self.nc.Block() as blk:
            def mk(e):
                def body(Eng):
                    for waits, fn, inc in self.rec[e]:
                        for sem, val in waits:
                            Eng.wait_ge(sem, val)
                        if fn is not None:
                            fn(Eng).then_inc(inc[0], inc[1])
                return body
            blk.tensor(mk("pe"))
            blk.vector(mk("dve"))
            blk.scalar(mk("act"))
            blk.gpsimd(mk("pool"))
            blk.sync(mk("sp"))

    def dma(self, q, out, in_, grp=None, **kw):
        if out.buf.space == "sb":
            g = self.dsem_for(out.buf)
        elif in_.buf.space == "sb":
            g = self.dsem_for(in_.buf)
        else:
            g = self.dsem_for(out.buf)
        self.op(q, lambda E: E.dma_start(out=out.ap, in_=in_.ap, **kw), r=[in_.buf], w=[out.buf], grp=g)

    def mm(self, out, lhsT, rhs, start=True, stop=True):
        self.op("pe", lambda E: E.matmul(out=out.ap, lhsT=lhsT.ap, rhs=rhs.ap, start=start, stop=stop),
                r=[lhsT.buf, rhs.buf], w=[out.buf])

    def tr(self, out, in_, ident):
        self.op("pe", lambda E: E.transpose(out=out.ap, in_=in_.ap, identity=ident.ap),
                r=[in_.buf, ident.buf], w=[out.buf])

    def act(self, out, in_, func, bias=None, scale=None, e="act"):
        rb = [in_.buf]
        kw = {}
        if bias is not None:
            kw["bias"] = _a(bias)
            if isinstance(bias, TT):
                rb.append(bias.buf)
        if scale is not None:
            kw["scale"] = _a(scale)
            if isinstance(scale, TT):
                rb.append(scale.buf)
        self.op("act", lambda E: E.activation(out=out.ap, in_=in_.ap, func=func, **kw), r=rb, w=[out.buf])

    def cp(self, e, out, in_):
        if e == "act":
            self.op("act", lambda E: E.copy(out=out.ap, in_=in_.ap), r=[in_.buf], w=[out.buf])
        else:
            self.op(e, lambda E: E.tensor_copy(out=out.ap, in_=in_.ap), r=[in_.buf], w=[out.buf])

    def tt(self, e, out, a, b, op):
        self.op(e, lambda E: E.tensor_tensor(out=out.ap, in0=a.ap, in1=b.ap, op=op), r=[a.buf, b.buf], w=[out.buf])

    def ts(self, e, out, a, s1, op0, s2=None, op1=None):
        rb = [a.buf] + [x.buf for x in (s1, s2) if isinstance(x, TT)]
        if op1 is None:
            self.op(e, lambda E: E.tensor_scalar(out=out.ap, in0=a.ap, scalar1=_a(s1), scalar2=None, op0=op0),
                    r=rb, w=[out.buf])
        else:
            self.op(e, lambda E: E.tensor_scalar(out=out.ap, in0=a.ap, scalar1=_a(s1), scalar2=_a(s2), op0=op0, op1=op1),
                    r=rb, w=[out.buf])

    def stt(self, out, a, s, b, op0, op1, e="dve"):
        rb = [a.buf, b.buf] + ([s.buf] if isinstance(s, TT) else [])
        self.op(e, lambda E: E.scalar_tensor_tensor(out=out.ap, in0=a.ap, scalar=_a(s), in1=b.ap, op0=op0, op1=op1),
                r=rb, w=[out.buf])

    def memset(self, e, out, val):
        self.op(e, lambda E: E.memset(out.ap, val), w=[out.buf])

    def recip(self, out, in_):
        self.op("dve", lambda E: E.reciprocal(out=out.ap, in_=in_.ap), r=[in_.buf], w=[out.buf])

    def scan(self, out, d0, d1, init):
        rb = [d0.buf, d1.buf] + ([init.buf] if isinstance(init, TT) else [])
        self.op("dve", lambda E: E.tensor_tensor_scan(out=out.ap, data0=d0.ap, data1=d1.ap, initial=_a(init),
                                                       op0=ALU.mult, op1=ALU.add), r=rb, w=[out.buf])

    def reduce(self, out, in_, op, axis=AX.X):
        self.op("dve", lambda E: E.tensor_reduce(out=out.ap, in_=in_.ap, axis=axis, op=op), r=[in_.buf], w=[out.buf])


class Stack:
    cnt = [0]

    def __init__(self, nc, base, limit):
        self.nc = nc
        self.base = base
        self.cur = base
        self.limit = limit

    def alloc(self, free_shape, dtype, name):
        nb = int(np.prod(free_shape)) * (2 if dtype == BF16 else 4)
        off = (self.cur + 31) // 32 * 32
        assert off + nb <= self.limit, (name, off, nb, self.limit)
        self.cur = off + nb
        Stack.cnt[0] += 1
        h = self.nc.alloc_sbuf_tensor_at("%s_%d" % (name, Stack.cnt[0]), [128] + list(free_shape), dtype, offset=off)
        return TT(h.ap(), Buf(name))

    def sub(self, nbytes):
        off = (self.cur + 31) // 32 * 32
        assert off + nbytes <= self.limit, (off, nbytes, self.limit)
        self.cur = off + nbytes
        return Stack(self.nc, off, off + nbytes)

    def reset(self):
        self.cur = self.base


def build(dbg=None, stop=None, nlayers=NL, units=(0, 1), b3_tiles=NTT):
    dbg = dbg or {}
    Stack.cnt[0] = 0
    nc = bass.Bass("TRN2", target_bir_lowering=False)
    es = ExitStack()
    P = Prog(nc, es)

    declared = []
    build.declared = declared

    def din(name, shape, dt=F32):
        declared.append(name)
        return TT(nc.dram_tensor(name, list(shape), dt, kind="ExternalInput").ap(), Buf(name, "dram"))

    def dout(name, shape, dt=F32):
        return TT(nc.dram_tensor(name, list(shape), dt, kind="ExternalOutput").ap(), Buf(name, "dram"))

    def dscr(name, shape, dt=F32):
        return TT(nc.dram_tensor(name, list(shape), dt, kind="Internal").ap(), Buf(name, "dram"))

    xin = [din("xp", [T, D]), din("xs", [T, D])]
    pe_d = din("pe", [T, D])
    st0 = din("st0", [NL, 2, 64, 64, 2])
    cond = din("cond", [2, D])
    ident_d = din("ident", [128, 128])
    iota_d = din("iota128", [128, 128])
    WSH = dict([("w_mod", [NL, D, 6 * D]), ("b_mod", [NL, 6 * D]), ("w_in", [NL, D, IN_COLS]),
                ("s5_lam_re", [NL, 2, 64, 64]), ("s5_lam_im", [NL, 2, 64, 64]), ("s5_log_dt", [NL, 2, 64]),
                ("s5_b_re", [NL, 2, 64, 64, 16]), ("s5_b_im", [NL, 2, 64, 64, 16]),
                ("s5_c_re", [NL, 2, 64, 16, 64]), ("s5_c_im", [NL, 2, 64, 16, 64]),
                ("s5_d", [NL, 1024]), ("s5_w_glu", [NL, 1024, 1024]),
                ("sc_conv_w", [NL, 3, 512]), ("sc_conv_b", [NL, 512]),
                ("cf_conv_w", [NL, 31, 512]), ("cf_conv_b", [NL, 512]),
                ("cf_ln_g", [NL, 512]), ("cf_ln_b", [NL, 512]),
                ("w_pa", [NL, 1024, D]), ("w_pb", [NL, 512, D]), ("w_pc", [NL, 512, D]), ("w_o", [NL, D, D]),
                ("ln1_g", [NL, D]), ("ln1_b", [NL, D]),
                ("peer_w_q", [NL, D, D]), ("peer_k1", [NL, 128, 128]), ("peer_k2", [NL, 128, 128]),
                ("peer_u", [NL, 16384, D]), ("peer_v", [NL, 16384, D]),
                ("ln2_g", [NL, D]), ("ln2_b", [NL, D])])

    class LazyW(dict):
        def __missing__(self, nm):
            self[nm] = din(nm, WSH[nm])
            return self[nm]
    W = LazyW()
    yout = [dout("yp", [T, D]), dout("ys", [T, D])]
    nst_o = dout("nst", [4, NL, 2, 64, 64, 2])
    dbg_out = {k: dout("dbg_" + k, v[0], v[1]) for k, v in dbg.items()}

    xres = [dscr("xres0", [T, D]), dscr("xres1", [T, D])]
    ymix = dscr("ymix", [T, D])
    x1res = dscr("x1res", [T, D])
    h2res = dscr("h2res", [T, D])
    mod_d = dscr("mod_d", [NL, 2, 6 * D])
    tabs = dscr("tabs", [64, 128, 2, 260])
    wbd = dscr("wbd", [8, 128, 16, 128], BF16)
    cwd = dscr("cwd", [8, 128, 16, 64], BF16)
    Gd = dscr("Gd", [128, 128, T], BF16)

    psh = nc.alloc_psum_tensor("psum_all", [128, 4096], F32)
    PS = [TT(psh.ap()[:, i * 1024:(i + 1) * 1024], Buf("ps%d" % i, "ps")) for i in range(4)]
    ps_rr = [0]

    def ps_next():
        ps_rr[0] = (ps_rr[0] + 1) % 4
        return PS[ps_rr[0]]

    SB_BASE = (int(nc.sbuf_base) + 63) // 64 * 64
    SB_TOP = int(nc.sbuf_top)
    root = Stack(nc, SB_BASE, SB_TOP)
    G = root.sub(6 * 1024)
    LP = root.sub(6 * 1024)
    UL = Stack(nc, root.cur, SB_TOP)

    g_st = g_c = None
    g_x = g_w = g_t = [None, None]

    ident_f = G.alloc([128], F32, "ident_f")
    ident_b = G.alloc([128], BF16, "ident_b")
    iota128 = G.alloc([128], F32, "iota128")
    iota16 = iota128[:, 0:16]
    halfpi = G.alloc([1], F32, "halfpi")
    X4 = G.alloc([4, 128], BF16, "X4")
    CWst = G.alloc([4, 64], BF16, "CWst")
    ones_f = G.alloc([128], F32, "ones_f")
    P.dma("sp", ident_f, ident_d, g_c)
    P.dma("sp", iota128, iota_d, g_c)
    P.cp("dve", ident_b, ident_f)
    P.memset("dve", halfpi, math.pi / 2)
    P.memset("dve", X4, 0.0)
    P.memset("dve", CWst, 0.0)
    P.memset("dve", ones_f, 1.0 / 512.0)

    Rt = LP.alloc([64], F32, "Rt")
    DSK = LP.alloc([8], F32, "DSK")
    H0 = LP.alloc([64, 2], F32, "H0")
    SCW = LP.alloc([4, 3], F32, "SCW")
    SCB = LP.alloc([4], F32, "SCB")
    CFW = LP.alloc([4, 31], F32, "CFW")
    CFB = LP.alloc([4], F32, "CFB")
    CFG = LP.alloc([4], F32, "CFG")
    CFBB = LP.alloc([4], F32, "CFBB")
    KTb = LP.alloc([2, 128], BF16, "KTb")
    NST = LP.alloc([64, 4, 2], F32, "NST")

    WSTG_B = 16 * 256 * 4
    WBF_B = 16 * 256 * 2

    def dbg_store(key, src, dst_slice=None):
        if key in dbg_out:
            dst = dbg_out[key] if dst_slice is None else dbg_out[key][dst_slice]
            P.dma("sp", dst, src, g_st)

    def phase_mod():
        UL.reset()
        condT = UL.alloc([16, 2], F32, "condT")
        bmod = UL.alloc([6 * D], F32, "bmod")
        stg = [UL.alloc([16, 256], F32, "mstg%d" % i) for i in range(2)]
        mo = [UL.alloc([256], F32, "mo%d" % i) for i in range(2)]
        for r_ in range(2):
            P.dma("sp", condT[:, :, r_], cond[r_].r("(kt p) -> p kt", p=128), g_c, allow_slow_non_contiguous=True)
        P.act(condT, condT, AF.Silu)
        for l in range(nlayers):
            P.dma("sp", bmod[0:2, :], TT(W["b_mod"].ap[l:l + 1, :].partition_broadcast(2), W["b_mod"].buf), g_c)
            for c in range(48):
                s_ = stg[c % 2]
                P.dma("sp", s_, W["w_mod"][l][:, c * 256:(c + 1) * 256].r("(kt p) c -> p kt c", p=128), g_w[c % 2])
                ps = ps_next()
                for kt in range(16):
                    P.mm(ps[0:2, 0:256], condT[:, kt, :], s_[:, kt, :], start=(kt == 0), stop=(kt == 15))
                m_ = mo[c % 2]
                P.tt("dve", m_[0:2, :], ps[0:2, 0:256], bmod[0:2, c * 256:(c + 1) * 256], ALU.add)
                if (8 <= c < 16) or (32 <= c < 40):
                    P.ts("dve", m_[0:2, :], m_[0:2, :], 1.0, ALU.add)
                P.dma("sp", mod_d[l][:, c * 256:(c + 1) * 256], m_[0:2, :], g_st)
        P.barrier()

    def phase_setup(l):
        UL.reset()
        A = UL
        nat = [A.alloc([128], F32, "nat%d" % i) for i in range(3)]
        dt2 = A.alloc([2], F32, "dt2")
        LR = A.alloc([64], F32, "LR")
        LI = A.alloc([64], F32, "LI")
        DT = A.alloc([64], F32, "DT")
        P.dma("sp", nat[0][0:64, :], W["s5_lam_re"][l].r("d (gp g2) p -> (d gp) (g2 p)", g2=2), g_c)
        P.dma("sp", nat[1][0:64, :], W["s5_lam_im"][l].r("d (gp g2) p -> (d gp) (g2 p)", g2=2), g_c)
        P.dma("sp", dt2[0:64, :], W["s5_log_dt"][l].r("d (gp g2) -> (d gp) g2", g2=2), g_c)
        P.cp("dve", nat[2][0:64, :].r("q (a b) -> q a b", a=2), dt2[0:64, :].un(2).bc([64, 2, 64]))
        for i, dst in enumerate((LR, LI, DT)):
            ps = ps_next()
            P.tr(ps[:, 0:64], nat[i][0:64, :], ident_f[0:64, 0:64])
            P.cp("act", dst, ps[:, 0:64])
        v = {k: A.alloc([64], F32, k) for k in ("dt", "ang", "c", "s", "t1", "t2", "are", "aim", "den", "am1", "zre", "zim")}
        P.act(v["dt"], DT, AF.Exp)
        P.tt("dve", v["t1"], LR, v["dt"], ALU.mult)
        P.act(Rt, v["t1"], AF.Exp)
        P.tt("dve", v["ang"], LI, v["dt"], ALU.mult)
        P.act(v["s"], v["ang"], AF.Sin, scale=1.0 / 32.0)
        P.act(v["c"], v["ang"], AF.Sin, bias=halfpi[:, 0:1], scale=1.0 / 32.0)
        for _ in range(5):
            P.tt("dve", v["t1"], v["c"], v["c"], ALU.mult)
            P.tt("dve", v["t2"], v["s"], v["s"], ALU.mult)
            P.stt(v["s"], v["c"], 2.0, v["s"], ALU.mult, ALU.mult)
            P.tt("dve", v["c"], v["t1"], v["t2"], ALU.subtract)
        C1, S1 = v["c"], v["s"]
        P.tt("dve", v["are"], Rt, C1, ALU.mult)
        P.tt("dve", v["aim"], Rt, S1, ALU.mult)
        P.tt("dve", v["t1"], LR, LR, ALU.mult)
        P.tt("dve", v["t2"], LI, LI, ALU.mult)
        P.tt("dve", v["den"], v["t1"], v["t2"], ALU.add)
        P.recip(v["den"], v["den"])
        P.ts("dve", v["am1"], v["are"], -1.0, ALU.add)
        P.tt("dve", v["t1"], v["am1"], LR, ALU.mult)
        P.tt("dve", v["t2"], v["aim"], LI, ALU.mult)
        P.tt("dve", v["t1"], v["t1"], v["t2"], ALU.add)
        P.tt("dve", v["zre"], v["t1"], v["den"], ALU.mult)
        P.tt("dve", v["t1"], v["aim"], LR, ALU.mult)
        P.tt("dve", v["t2"], v["am1"], LI, ALU.mult)
        P.tt("dve", v["t1"], v["t1"], v["t2"], ALU.subtract)
        P.tt("dve", v["zim"], v["t1"], v["den"], ALU.mult)
        BRE = A.alloc([64, 16], F32, "BRE")
        BIM = A.alloc([64, 16], F32, "BIM")
        tA = A.alloc([64, 16], F32, "tA")
        tB = A.alloc([64, 16], F32, "tB")
        BB = [A.alloc([64, 16], BF16, "BBre"), A.alloc([64, 16], BF16, "BBim")]
        P.dma("sp", BRE, W["s5_b_re"][l].r("d (gp g2) p s -> (g2 p) (d gp) s", g2=2), g_c)
        P.dma("sp", BIM, W["s5_b_im"][l].r("d (gp g2) p s -> (g2 p) (d gp) s", g2=2), g_c)
        zr = v["zre"].un(2).bc([128, 64, 16])
        zi = v["zim"].un(2).bc([128, 64, 16])
        P.tt("dve", tA, BRE, zr, ALU.mult)
        P.tt("dve", tB, BIM, zi, ALU.mult)
        P.tt("dve", BB[0], tA, tB, ALU.subtract)
        P.tt("dve", tA, BIM, zr, ALU.mult)
        P.tt("dve", tB, BRE, zi, ALU.mult)
        P.tt("dve", BB[1], tA, tB, ALU.add)
        wst = [A.alloc([4, 128], BF16, "wst%d" % i) for i in range(2)]
        it = 0
        for d in range(2):
            for ct in range(8):
                rt0 = d * 32 + ct * 4
                for reim in range(2):
                    for j in range(4):
                        P.cp("dve", X4[0:64, j, 32 * j:32 * j + 16], BB[reim][0:64, rt0 + j, :])
                        P.cp("dve", X4[64:128, j, 32 * j + 16:32 * j + 32], BB[reim][64:128, rt0 + j, :])
                    ps = ps_next()
                    psb = ps[:, 0:256].bitcast(BF16).r("p (j c) -> p j c", j=4)
                    for j in range(4):
                        P.tr(psb[:, j, :], X4[:, j, :], ident_b)
                    ws_ = wst[it % 2]
                    it += 1
                    P.cp("act", ws_, psb)
                    P.dma("sp", wbd[ct][:, d * 8 + reim:d * 8 + 8:2, :], ws_, g_st)
        CN = [A.alloc([64], F32, "CN%d" % i) for i in range(2)]
        CN2 = A.alloc([128], BF16, "CN2")
        cst = [A.alloc([4, 64], BF16, "cst%d" % i) for i in range(2)]
        it = 0
        for d in range(2):
            for ct in range(8):
                for reim in range(2):
                    src = W["s5_c_im" if reim else "s5_c_re"][l, d, 8 * ct:8 * ct + 8].r("g s p -> (g s) p")
                    cn = CN[it % 2]
                    P.dma("sp", cn, src, g_t[it % 2])
                    sgn = -1.0 if reim else 1.0
                    P.ts("dve", CN2[:, 0:64], cn, sgn, ALU.mult)
                    P.ts("dve", CN2[:, 64:128], cn, sgn, ALU.mult)
                    ps = ps_next()
                    psb = ps[:, 0:64].bitcast(BF16)
                    P.tr(psb, CN2, ident_b)
                    for j in range(4):
                        jl = j % 2
                        P.cp("dve", CWst[0:64, j, 32 * jl:32 * jl + 16], psb[0:64, 32 * j:32 * j + 16])
                        P.cp("dve", CWst[64:128, j, 32 * jl + 16:32 * jl + 32], psb[64:128, 32 * j + 16:32 * j + 32])
                    cs_ = cst[it % 2]
                    it += 1
                    P.cp("pool", cs_, CWst)
                    P.dma("sp", cwd[ct][:, d * 8 + reim:d * 8 + 8:2, :], cs_, g_st)
        ER = A.alloc([16, 260], F32, "ER")
        EI = A.alloc([16, 260], F32, "EI")
        q = {k: A.alloc([16, 128], F32, k) for k in ("q1", "q2", "q3", "q4")}
        MR = A.alloc([16], F32, "MR")
        MI = A.alloc([16], F32, "MI")
        m1 = A.alloc([16], F32, "m1")
        m2 = A.alloc([16], F32, "m2")
        for gq in range(4):
            sl = slice(gq * 16, gq * 16 + 16)
            P.memset("dve", ER[:, :, 0:1], 1.0)
            P.memset("dve", EI[:, :, 0:1], 0.0)
            P.cp("dve", MR, C1[:, sl])
            P.cp("dve", MI, S1[:, sl])
            k = 1
            while k < 260:
                cnt = min(k, 260 - k)
                o = 0
                while o < cnt:
                    n = min(128, cnt - o)
                    mrb = MR.un(2).bc([128, 16, n])
                    mib = MI.un(2).bc([128, 16, n])
                    sr = ER[:, :, o:o + n]
                    si = EI[:, :, o:o + n]
                    P.tt("dve", q["q1"][:, :, 0:n], sr, mrb, ALU.mult)
                    P.tt("dve", q["q2"][:, :, 0:n], si, mib, ALU.mult)
                    P.tt("pool", q["q3"][:, :, 0:n], sr, mib, ALU.mult)
                    P.tt("pool", q["q4"][:, :, 0:n], si, mrb, ALU.mult)
                    P.tt("dve", ER[:, :, k + o:k + o + n], q["q1"][:, :, 0:n], q["q2"][:, :, 0:n], ALU.subtract)
                    P.tt("pool", EI[:, :, k + o:k + o + n], q["q3"][:, :, 0:n], q["q4"][:, :, 0:n], ALU.add)
                    o += n
                if 2 * k < 260:
                    P.tt("dve", m1, MR, MR, ALU.mult)
                    P.tt("dve", m2, MI, MI, ALU.mult)
                    P.stt(MI, MR, 2.0, MI, ALU.mult, ALU.mult)
                    P.tt("dve", MR, m1, m2, ALU.subtract)
                k *= 2
            P.dma("sp", tabs[gq * 16:gq * 16 + 16, :, 0, :].r("rt p t -> p rt t"), ER, g_st)
            P.dma("sp", tabs[gq * 16:gq * 16 + 16, :, 1, :].r("rt p t -> p rt t"), EI, g_st)
        P.dma("sp", DSK, W["s5_d"][l].r("(ct p) -> p ct", p=128), g_c, allow_slow_non_contiguous=True)
        P.dma("sp", H0, st0[l].r("d (gp g2) p c -> (g2 p) (d gp) c", g2=2), g_c)
        cwn = A.alloc([512], F32, "cwn")
        for nm_, dst_, nk_ in (("sc_conv_w", SCW, 3), ("cf_conv_w", CFW, 31)):
            P.dma("sp", cwn[0:nk_, :], W[nm_][l], g_c)
            for ct_ in range(4):
                ps = ps_next()
                P.tr(ps[:, 0:nk_], cwn[0:nk_, ct_ * 128:(ct_ + 1) * 128], ident_f[0:nk_, 0:nk_])
                P.cp("act", dst_[:, ct_, :], ps[:, 0:nk_])
        P.dma("sp", SCB, W["sc_conv_b"][l].r("(ct p) -> p ct", p=128), g_c, allow_slow_non_contiguous=True)
        P.dma("sp", CFB, W["cf_conv_b"][l].r("(ct p) -> p ct", p=128), g_c, allow_slow_non_contiguous=True)
        P.dma("sp", CFG, W["cf_ln_g"][l].r("(ct p) -> p ct", p=128), g_c, allow_slow_non_contiguous=True)
        P.dma("sp", CFBB, W["cf_ln_b"][l].r("(ct p) -> p ct", p=128), g_c, allow_slow_non_contiguous=True)
        dbg_store("CFW", CFW)
        dbg_store("SCW", SCW)
        kn = [A.alloc([128], F32, "kn%d" % i) for i in range(2)]
        for hf, nm in enumerate(("peer_k1", "peer_k2")):
            P.dma("sp", kn[hf], W[nm][l], g_c)
            ps = ps_next()
            P.tr(ps[:, 0:128], kn[hf], ident_f)
            P.cp("act", KTb[:, hf, :], ps[:, 0:128])
        P.barrier()

    wslots = {}
    w_it = [0]

    def load_w(W2d, r0, nk, c0, ncol):
        i = w_it[0] % 2
        w_it[0] += 1
        stg, wbf = wslots["stg"][i], wslots["wbf"][i]
        P.dma("sp", stg[:, 0:nk, 0:ncol], W2d[r0:r0 + 128 * nk, c0:c0 + ncol].r("(kt p) c -> p kt c", p=128), g_w[i])
        if i == 0:
            P.cp("pool", wbf[:, 0:nk, 0:ncol], stg[:, 0:nk, 0:ncol])
        else:
            P.cp("act", wbf[:, 0:nk, 0:ncol], stg[:, 0:nk, 0:ncol])
        return wbf

    def linear_fm(in_tiles, W2d, r0, c0, ncols, consume, tile0=0):
        nk = len(in_tiles)
        for cc in range(0, ncols, 256):
            wb = load_w(W2d, r0, nk, c0 + cc, 256)
            for ti in range(2):
                ps = ps_next()
                for half in range(2):
                    hs = slice(half * 512, half * 512 + 512)
                    for kt in range(nk):
                        P.mm(ps[:, hs], wb[:, kt, ti * 128:(ti + 1) * 128], in_tiles[kt][:, hs],
                             start=(kt == 0), stop=(kt == nk - 1))
                consume(tile0 + cc // 128 + ti, ps)

    def ln_tm(x, out, st, mv, rs, e="dve"):
        for c in range(4):
            P.op("dve", lambda E, c=c: E.bn_stats(out=st.ap[:, c, :], in_=x.ap[:, c * 512:(c + 1) * 512]),
                 r=[x.buf], w=[st.buf])
        P.op("dve", lambda E: E.bn_aggr(out=mv.ap, in_=st.ap.rearrange("p a b -> p (a b)")), r=[st.buf], w=[mv.buf])
        P.ts("dve", rs, mv[:, 1:2], LN_EPS, ALU.add)
        P.act(rs, rs, AF.Sqrt)
        P.recip(rs, rs)
        P.ts("dve", out, x, mv[:, 0:1], ALU.subtract, rs[:, 0:1], ALU.mult)

    def phase_A(l, u):
        nseq, SL = (4, 256) if u == 0 else (1, 1024)
        UL.reset()
        hT = UL.alloc([16, T], BF16, "hT")
        bufP = UL.alloc([8, T], BF16, "bufP")
        bufQ = UL.alloc([8, T], BF16, "bufQ")
        ysT = UL.alloc([4, T], BF16, "ysT")
        zT = UL.alloc([4, T], BF16, "zT")
        wslots["wbf"] = [UL.alloc([16, 256], BF16, "wbf%d" % i) for i in range(2)]
        wslots["stg"] = [UL.alloc([16, 256], F32, "wstg%d" % i) for i in range(2)]
        X = Stack(nc, UL.cur, SB_TOP)

        X.reset()
        xt = [X.alloc([D], F32, "xt%d" % i) for i in range(2)]
        tmp = X.alloc([D], F32, "tmpA0")
        hb = X.alloc([D], BF16, "hb")
        sc1b = X.alloc([D], F32, "sc1b")
        sh1b = X.alloc([D], F32, "sh1b")
        pet = X.alloc([D], F32, "pet")
        st = X.alloc([4, 6], F32, "st")
        mv = X.alloc([2], F32, "mv")
        rs = X.alloc([1], F32, "rs")
        P.dma("sp", sh1b, TT(mod_d.ap[l, u:u + 1, 0:D].partition_broadcast(128), mod_d.buf), g_c)
        P.dma("sp", sc1b, TT(mod_d.ap[l, u:u + 1, D:2 * D].partition_broadcast(128), mod_d.buf), g_c)
        for tt_ in range(NTT):
            rows = slice(tt_ * 128, tt_ * 128 + 128)
            x_ = xt[tt_ % 2]
            if l == 0:
                P.dma("sp", x_, xin[u][rows, :], g_x[tt_ % 2])
                if u == 1:
                    P.dma("sp", pet, pe_d[rows, :], g_x[tt_ % 2])
                    P.tt("pool", x_, x_, pet, ALU.add)
                P.dma("sp", xres[u][rows, :], x_, g_st)
            else:
                P.dma("sp", x_, xres[u][rows, :], g_x[tt_ % 2])
            ln_tm(x_, tmp, st, mv, rs)
            P.tt("dve", tmp, tmp, sc1b, ALU.mult)
            P.tt("dve", hb, tmp, sh1b, ALU.add)
            ps = ps_next()
            psb = ps.bitcast(BF16)
            for kt in range(KT):
                P.tr(psb[:, kt * 128:(kt + 1) * 128], hb[:, kt * 128:(kt + 1) * 128], ident_b)
            P.cp("act", hT[:, :, rows], psb.r("p (k t) -> p k t", k=KT))
        dbg_store("hT", hT)
        if stop == "A0":
            return None
        hT_tiles = [hT[:, kt, :] for kt in range(KT)]
        P.barrier()

        uT = bufP
        ya = bufQ

        def cons_u(ti, ps):
            P.cp("act", uT[:, ti, :], ps)
        linear_fm(hT_tiles, W["w_in"][l], 0, 0, 1024, cons_u)
        dbg_store("uT", uT)
        X.reset()
        f = {k: X.alloc([T], F32, k) for k in ("bure", "buim", "t1", "t2", "t3", "t4", "br", "bi", "gre", "gim")}
        hre = X.alloc([T], BF16, "hre")
        him = X.alloc([T], BF16, "him")
        tbs = [X.alloc([2, 260], F32, "tb%d" % i) for i in range(2)]
        wbt = [X.alloc([16, 128], BF16, "wbt%d" % i) for i in range(2)]
        cwt = [X.alloc([16, 64], BF16, "cwt%d" % i) for i in range(2)]
        sm = {k: X.alloc([4], F32, k) for k in ("i_re", "i_im", "s1", "s2")}
        psA, psB, psC, psD = PS

        def v4(t_):
            return t_.r("p (c t) -> p c t", c=4)

        rts = []
        for ct in range(8):
            for d in range(2):
                for j in range(4):
                    rts.append((ct, d, j))

        state = {}

        def issue_bu(i):
            ct, d, j = rts[i]
            if d == 0 and j == 0:
                k = ct % 2
                P.dma("sp", wbt[k], wbd[ct], g_t[k])
                P.dma("sp", cwt[k], cwd[ct], g_t[k])
            rt = d * 32 + ct * 4 + j
            tb = tbs[i % 2]
            P.dma("sp", tb, tabs[rt], g_x[i % 2])
            wb_ = wbt[ct % 2]
            for reim, slot in ((0, psA), (1, psB)):
                for half in range(2):
                    hs = slice(half * 512, half * 512 + 512)
                    P.mm(slot[:, hs], wb_[:, (d * 4 + j) * 2 + reim, :], uT[:, ct, hs])

        issue_bu(0)
        for i, (ct, d, j) in enumerate(rts):
            rt = d * 32 + ct * 4 + j
            tb = tbs[i % 2]
            cw_ = cwt[ct % 2]
            Cb = tb[:, 0, 0:256].un(1).bc([128, 4, 256])
            Sb = tb[:, 1, 0:256].un(1).bc([128, 4, 256])
            for src, dst in ((psA, f["bure"]), (psB, f["buim"])):
                s3 = src.r("p (b t) -> p b t", b=nseq)
                if d == 1:
                    s3 = s3[:, :, ::-1]
                P.cp("act", dst.r("p (b t) -> p b t", b=nseq), s3)
            if i + 1 < len(rts):
                issue_bu(i + 1)
            P.tt("dve", v4(f["t1"]), v4(f["bure"]), Cb, ALU.mult)
            P.tt("dve", v4(f["t2"]), v4(f["buim"]), Sb, ALU.mult)
            P.tt("dve", f["br"], f["t1"], f["t2"], ALU.add)
            P.tt("pool", v4(f["t3"]), v4(f["buim"]), Cb, ALU.mult)
            P.tt("pool", v4(f["t4"]), v4(f["bure"]), Sb, ALU.mult)
            P.tt("pool", f["bi"], f["t3"], f["t4"], ALU.subtract)
            rcol = Rt[:, rt:rt + 1]
            for c in range(4):
                cs = slice(c * 256, c * 256 + 256)
                if u == 0:
                    ire, iim = 0.0, 0.0
                else:
                    if c == 0:
                        pre, pim = H0[:, rt, 0:1], H0[:, rt, 1:2]
                        er, ei = tb[:, 0, 1:2], tb[:, 1, 1:2]
                    else:
                        pre, pim = f["gre"][:, c * 256 - 1:c * 256], f["gim"][:, c * 256 - 1:c * 256]
                        er, ei = tb[:, 0, 256:257], tb[:, 1, 256:257]
                    P.ts("dve", sm["s1"][:, 0:1], pim, ei, ALU.mult)
                    P.stt(sm["i_re"][:, c:c + 1], pre, er, sm["s1"][:, 0:1], ALU.mult, ALU.subtract)
                    P.ts("dve", sm["s2"][:, 0:1], pim, er, ALU.mult)
                    P.stt(sm["i_im"][:, c:c + 1], pre, ei, sm["s2"][:, 0:1], ALU.mult, ALU.add)
                    ire, iim = sm["i_re"][:, c:c + 1], sm["i_im"][:, c:c + 1]
                P.scan(f["gre"][:, cs], rcol.bc([128, 256]), f["br"][:, cs], ire)
                P.scan(f["gim"][:, cs], rcol.bc([128, 256]), f["bi"][:, cs], iim)
            P.tt("dve", v4(f["t1"]), v4(f["gre"]), Cb, ALU.mult)
            P.tt("dve", v4(f["t2"]), v4(f["gim"]), Sb, ALU.mult)
            P.tt("dve", hre, f["t1"], f["t2"], ALU.subtract)
            P.tt("pool", v4(f["t3"]), v4(f["gre"]), Sb, ALU.mult)
            P.tt("pool", v4(f["t4"]), v4(f["gim"]), Cb, ALU.mult)
            P.tt("pool", him, f["t3"], f["t4"], ALU.add)
            if u == 0:
                glr = v4(f["gre"])[:, :, 255]
                gli = v4(f["gim"])[:, :, 255]
                c255, s255 = tb[:, 0, 255:256], tb[:, 1, 255:256]
                P.ts("dve", sm["s1"], gli, s255, ALU.mult)
                P.stt(NST[:, rt, :, 0], glr, c255, sm["s1"], ALU.mult, ALU.subtract)
                P.ts("dve", sm["s2"], gli, c255, ALU.mult)
                P.stt(NST[:, rt, :, 1], glr, s255, sm["s2"], ALU.mult, ALU.add)
            ysl = psC if d == 0 else psD
            jj = j // 2
            for reim, hs_ in ((0, hre), (1, him)):
                for half in range(2):
                    hs = slice(half * 512, half * 512 + 512)
                    P.mm(ysl[64 * jj:64 * jj + 64, hs], cw_[:, (d * 4 + j) * 2 + reim, :], hs_[:, hs],
                         start=(j % 2 == 0 and reim == 0), stop=(j % 2 == 1 and reim == 1))
            if d == 1 and j == 3:
                s3 = psD.r("p (b t) -> p b t", b=nseq)[:, :, ::-1]
                P.cp("act", f["t3"].r("p (b t) -> p b t", b=nseq), s3)
                P.tt("dve", f["t1"], psC, f["t3"], ALU.add)
                P.stt(f["t2"], uT[:, ct, :], DSK[:, ct:ct + 1], f["t1"], ALU.mult, ALU.add)
                P.act(ya[:, ct, :], f["t2"], AF.Gelu_apprx_tanh)
        if u == 0:
            for b_ in range(4):
                P.dma("sp", nst_o[b_, l].r("d (gp g2) p c -> (g2 p) (d gp) c", g2=2), NST[:, :, b_, :], g_st)
        dbg_store("ya", ya)
        dbg_store("nst", nst_o) if False else None
        if stop == "A1s":
            return None
        P.barrier()
        X.reset()
        sg = [X.alloc([T], F32, "sg%d" % i) for i in range(2)]
        yag = bufP
        ya_tiles = [ya[:, k, :] for k in range(8)]

        def cons_glu(ti, ps):
            s_ = sg[ti % 2]
            P.act(s_, ps, AF.Sigmoid)
            P.tt("dve", yag[:, ti, :], ya[:, ti, :], s_, ALU.mult)
        linear_fm(ya_tiles, W["s5_w_glu"][l], 0, 0, 1024, cons_glu)
        dbg_store("yag", yag)
        if stop == "A1":
            return None

        X.reset()
        fb = {k: X.alloc([T], F32, k) for k in ("cb", "cc", "prod", "acc")}
        keep = {}

        def seqv(t_):
            return t_.r("p (b t) -> p b t", b=nseq)

        def cons_sc(ti, ps):
            kind, i = divmod(ti, 4)
            if kind == 0:
                if i not in keep:
                    keep[i] = X.alloc([T], F32, "scb%d" % i)
                P.cp("act", keep[i], ps)
            elif kind == 1:
                if ("c", i) not in keep:
                    keep[("c", i)] = X.alloc([T], F32, "scc%d" % i)
                P.cp("act", keep[("c", i)], ps)
            else:
                P.tt("dve", fb["prod"], keep[("c", i)], ps, ALU.mult)
                P.ts("dve", fb["acc"], fb["prod"], SCW[:, i, 1:2], ALU.mult, SCB[:, i:i + 1], ALU.add)
                a3, p3 = seqv(fb["acc"]), seqv(fb["prod"])
                P.stt(a3[:, :, 1:SL], p3[:, :, 0:SL - 1], SCW[:, i, 0:1], a3[:, :, 1:SL], ALU.mult, ALU.add)
                P.stt(a3[:, :, 0:SL - 1], p3[:, :, 1:SL], SCW[:, i, 2:3], a3[:, :, 0:SL - 1], ALU.mult, ALU.add)
                P.tt("dve", ysT[:, i, :], keep[i], fb["acc"], ALU.mult)
        linear_fm(hT_tiles, W["w_in"][l], 0, 1024, 1536, cons_sc)
        dbg_store("ys", ysT)
        if stop == "A2":
            return None
        P.barrier()

        X.reset()
        ga = [X.alloc([T], F32, "ga%d" % i) for i in range(4)]
        cv = [X.alloc([T], F32, "cv%d" % i) for i in range(4)]
        fc = {k: X.alloc([T], F32, k) for k in ("sq", "rstd", "mr")}

        def cons_cf(ti, ps):
            kind, i = divmod(ti, 4)
            if kind == 0:
                P.cp("act", ga[i], ps)
            else:
                P.act(fc["sq"], ps, AF.Sigmoid)
                P.tt("dve", ga[i], ga[i], fc["sq"], ALU.mult)
                P.ts("dve", cv[i], ga[i], CFW[:, i, 15:16], ALU.mult, CFB[:, i:i + 1], ALU.add)
                a3, g3 = seqv(cv[i]), seqv(ga[i])
                for k in range(31):
                    sh = k - 15
                    if sh == 0:
                        continue
                    if sh > 0:
                        P.stt(a3[:, :, 0:SL - sh], g3[:, :, sh:SL], CFW[:, i, k:k + 1], a3[:, :, 0:SL - sh], ALU.mult, ALU.add)
                    else:
                        P.stt(a3[:, :, -sh:SL], g3[:, :, 0:SL + sh], CFW[:, i, k:k + 1], a3[:, :, -sh:SL], ALU.mult, ALU.add)
        linear_fm(hT_tiles, W["w_in"][l], 0, 2560, 1024, cons_cf)
        dbg_store("glu0", ga[0])
        dbg_store("cv0", cv[0])
        psM, psQ = PS[0], PS[1]
        for i in range(4):
            for half in range(2):
                hs = slice(half * 512, half * 512 + 512)
                P.mm(psM[:, hs], ones_f, cv[i][:, hs], start=(i == 0), stop=(i == 3))
        for i in range(4):
            P.act(ga[i], cv[i], AF.Square)
        for i in range(4):
            for half in range(2):
                hs = slice(half * 512, half * 512 + 512)
                P.mm(psQ[:, hs], ones_f, ga[i][:, hs], start=(i == 0), stop=(i == 3))
        P.cp("act", fc["mr"], psM)
        P.tt("dve", fc["sq"], fc["mr"], fc["mr"], ALU.mult)
        P.tt("dve", fc["rstd"], psQ, fc["sq"], ALU.subtract)
        P.ts("dve", fc["rstd"], fc["rstd"], LN_EPS, ALU.add)
        P.act(fc["rstd"], fc["rstd"], AF.Sqrt)
        P.recip(fc["rstd"], fc["rstd"])
        dbg_store("rstd", fc["rstd"])
        dbg_store("mean", psM) if False else None
        P.tt("dve", fc["mr"], fc["mr"], fc["rstd"], ALU.mult)
        for i in range(4):
            P.tt("dve", ga[i], cv[i], fc["rstd"], ALU.mult)
            P.tt("dve", ga[i], ga[i], fc["mr"], ALU.subtract)
            P.ts("dve", ga[i], ga[i], CFG[:, i:i + 1], ALU.mult, CFBB[:, i:i + 1], ALU.add)
            P.act(zT[:, i, :], ga[i], AF.Silu)
        dbg_store("z", zT)
        if stop == "A3":
            return None
        P.barrier()

        X.reset()
        mT = X.alloc([16, T], BF16, "mT")
        sgm = [X.alloc([T], F32, "sgm%d" % i) for i in range(2)]
        macc = [X.alloc([T], F32, "macc%d" % i) for i in range(2)]
        tmpm = X.alloc([T], F32, "tmpm")
        yag_t = [bufP[:, k, :] for k in range(8)]
        ys_t = [ysT[:, k, :] for k in range(4)]
        z_t = [zT[:, k, :] for k in range(4)]
        branches = [(yag_t, W["w_pa"][l], 0), (ys_t, W["w_pb"][l], 1), (z_t, W["w_pc"][l], 2)]
        for pair in range(8):
            cbase = pair * 256
            for bi, (tiles, Wp, gidx) in enumerate(branches):
                nk = len(tiles)
                wb = load_w(Wp, 0, nk, cbase, 256)
                pp = []
                for ti in range(2):
                    ps = ps_next()
                    for half in range(2):
                        hs = slice(half * 512, half * 512 + 512)
                        for kt in range(nk):
                            P.mm(ps[:, hs], wb[:, kt, ti * 128:(ti + 1) * 128], tiles[kt][:, hs], start=(kt == 0), stop=(kt == nk - 1))
                    pp.append(ps)
                wg = load_w(W["w_in"][l], 0, 16, GATE0 + gidx * D + cbase, 256)
                for ti in range(2):
                    ps = ps_next()
                    for half in range(2):
                        hs = slice(half * 512, half * 512 + 512)
                        for kt in range(16):
                            P.mm(ps[:, hs], wg[:, kt, ti * 128:(ti + 1) * 128], hT_tiles[kt][:, hs], start=(kt == 0), stop=(kt == 15))
                    P.act(sgm[ti], ps, AF.Sigmoid)
                    if bi == 0:
                        P.tt("dve", macc[ti], sgm[ti], pp[ti], ALU.mult)
                    elif bi == 1:
                        P.tt("dve", tmpm, sgm[ti], pp[ti], ALU.mult)
                        P.tt("pool", macc[ti], macc[ti], tmpm, ALU.add)
                    else:
                        P.tt("dve", tmpm, sgm[ti], pp[ti], ALU.mult)
                        P.tt("pool", mT[:, pair * 2 + ti, :], macc[ti], tmpm, ALU.add)
        dbg_store("mT", mT)
        if stop == "A4":
            return None
        ost = [X.alloc([256], F32, "ost%d" % i) for i in range(2)]
        it = 0
        for cc in range(8):
            wb = load_w(W["w_o"][l], 0, 16, cc * 256, 256)
            for tt_ in range(NTT):
                ps = ps_next()
                for kt in range(16):
                    P.mm(ps[:, 0:256], mT[:, kt, tt_ * 128:(tt_ + 1) * 128], wb[:, kt, :], start=(kt == 0), stop=(kt == 15))
                o_ = ost[it % 2]
                it += 1
                P.cp("act", o_, ps[:, 0:256])
                P.dma("sp", ymix[tt_ * 128:(tt_ + 1) * 128, cc * 256:(cc + 1) * 256], o_, g_st)
        P.barrier()
        return None

    def phase_B(l, u, last):
        UL.reset()
        h2T = UL.alloc([16, T], BF16, "h2T")
        Zbase = UL.cur
        qT = UL.alloc([16, T], BF16, "qT")
        Y3base = UL.cur
        wslots["wbf"] = [UL.alloc([16, 256], BF16, "wbfB%d" % i) for i in range(2)]
        Y = Stack(nc, UL.cur, SB_TOP)
        Y.reset()
        wslots["stg"] = None
        xt = [Y.alloc([D], F32, "xtB%d" % i) for i in range(2)]
        yt = [Y.alloc([D], F32, "ytB%d" % i) for i in range(2)]
        t1 = Y.alloc([D], F32, "t1B")
        hb = Y.alloc([D], BF16, "hbB")
        bct = {k: Y.alloc([D], F32, k) for k in ("gt1", "g1", "b1", "sc2", "sh2")}
        st = Y.alloc([4, 6], F32, "stB")
        mv = Y.alloc([2], F32, "mvB")
        rs = Y.alloc([1], F32, "rsB")

        def bload(dst, src2d, row, c0):
            P.dma("sp", dst, TT(src2d.ap[row:row + 1, c0:c0 + D].partition_broadcast(128), src2d.buf), g_c)
        bload(bct["gt1"], mod_d[l], u, 2 * D)
        bload(bct["sc2"], mod_d[l], u, 4 * D)
        bload(bct["sh2"], mod_d[l], u, 3 * D)
        bload(bct["g1"], W["ln1_g"], l, 0)
        bload(bct["b1"], W["ln1_b"], l, 0)
        for tt_ in range(NTT):
            rows = slice(tt_ * 128, tt_ * 128 + 128)
            x_, y_ = xt[tt_ % 2], yt[tt_ % 2]
            P.dma("sp", x_, xres[u][rows, :], g_x[tt_ % 2])
            P.dma("sp", y_, ymix[rows, :], g_x[tt_ % 2])
            P.tt("pool", y_, y_, bct["gt1"], ALU.mult)
            P.stt(y_, x_, ALPHA, y_, ALU.mult, ALU.add)
            ln_tm(y_, t1, st, mv, rs)
            P.tt("pool", t1, t1, bct["g1"], ALU.mult)
            P.tt("dve", x_, t1, bct["b1"], ALU.add)
            P.dma("sp", x1res[rows, :], x_, g_st)
            if tt_ == 0:
                dbg_store("x1", x_)
            ln_tm(x_, t1, st, mv, rs)
            P.tt("pool", t1, t1, bct["sc2"], ALU.mult)
            P.tt("dve", y_, t1, bct["sh2"], ALU.add)
            if tt_ == 0:
                dbg_store("h2", y_)
            P.cp("pool", hb, y_)
            ps = ps_next()
            psb = ps.bitcast(BF16)
            for kt in range(KT):
                P.tr(psb[:, kt * 128:(kt + 1) * 128], hb[:, kt * 128:(kt + 1) * 128], ident_b)
            P.cp("act", h2T[:, :, rows], psb.r("p (k t) -> p k t", k=KT))
        if stop == "B1":
            return
        P.barrier()
        Y.reset()
        wslots["stg"] = [Y.alloc([16, 256], F32, "wstgB%d" % i) for i in range(2)]
        h2T_t = [h2T[:, k, :] for k in range(KT)]

        def cons_q(ti, ps):
            P.cp("act", qT[:, ti, :], ps)
        linear_fm(h2T_t, W["peer_w_q"][l], 0, 0, D, cons_q)
        P.barrier()
        Y = Stack(nc, Y3base, SB_TOP)
        S = Y.alloc([16, 128], F32, "S")
        S2 = Y.alloc([256], F32, "S2")
        V = Y.alloc([16, 16], F32, "V")
        Iu = Y.alloc([16, 16], U32, "Iu")
        If = Y.alloc([16, 16], F32, "If")
        cand = Y.alloc([8, 256], F32, "cand")
        top = Y.alloc([8, 16], F32, "top")
        pos = Y.alloc([8, 16], U32, "pos")
        pa_ = Y.alloc([8, 16], U32, "pa")
        pb_ = Y.alloc([8, 16], U32, "pb")
        af = Y.alloc([8, 16], F32, "af")
        bf = Y.alloc([8, 16], F32, "bf")
        eq = Y.alloc([8, 16, 16], F32, "eq")
        i1s = Y.alloc([8, 16], F32, "i1s")
        i2s = Y.alloc([8, 16], F32, "i2s")
        gw = Y.alloc([8, 16], F32, "gw")
        gsum = Y.alloc([8], F32, "gsum")
        i1T = Y.alloc([128], F32, "i1T")
        i2T = Y.alloc([128], F32, "i2T")
        gT = Y.alloc([128], F32, "gT")
        Pm = Y.alloc([128, 128], BF16, "Pm")
        Qm = Y.alloc([128, 128], BF16, "Qm")
        Gst = Y.alloc([128, 128], BF16, "Gst")
        for tt_ in range(b3_tiles):
            rows = slice(tt_ * 128, tt_ * 128 + 128)
            for q4 in range(4):
                ps = ps_next()
                for m in range(4):
                    hh = q4 * 4 + m
                    P.mm(ps[:, m * 128:(m + 1) * 128], qT[:, hh, rows], KTb[:, hh % 2, :])
                P.cp("act", S[:, q4 * 4:q4 * 4 + 4, :], ps[:, 0:512].r("p (a n) -> p a n", a=4))
            for hh in range(16):
                P.op("dve", lambda E, hh=hh: E.max(out=V.ap[:, hh, 0:8], in_=S.ap[:, hh, :]), r=[S.buf], w=[V.buf])
                P.op("dve", lambda E, hh=hh: E.max_index(out=Iu.ap[:, hh, 0:8], in_max=V.ap[:, hh, 0:8], in_values=S.ap[:, hh, :]),
                     r=[S.buf, V.buf], w=[Iu.buf])
                P.op("dve", lambda E, hh=hh: E.match_replace(out=S2.ap[:, 0:128], in_to_replace=V.ap[:, hh, 0:8],
                                                             in_values=S.ap[:, hh, :], imm_value=NEG), r=[S.buf, V.buf], w=[S2.buf])
                P.op("dve", lambda E, hh=hh: E.max(out=V.ap[:, hh, 8:16], in_=S2.ap[:, 0:128]), r=[S2.buf], w=[V.buf])
                P.op("dve", lambda E, hh=hh: E.max_index(out=Iu.ap[:, hh, 8:16], in_max=V.ap[:, hh, 8:16], in_values=S2.ap[:, 0:128]),
                     r=[S2.buf, V.buf], w=[Iu.buf])
            P.cp("dve", If, Iu)
            V4 = V.r("p (h f) a -> p h f a", f=2)
            I4 = If.r("p (h f) a -> p h f a", f=2)
            c4 = cand.r("p h (a b) -> p h a b", a=16)
            P.tt("dve", c4, V4[:, :, 0, :].un(3).bc([128, 8, 16, 16]), V4[:, :, 1, :].un(2).bc([128, 8, 16, 16]), ALU.add)
            for h in range(8):
                P.op("dve", lambda E, h=h: E.max(out=top.ap[:, h, 0:8], in_=cand.ap[:, h, :]), r=[cand.buf], w=[top.buf])
                P.op("dve", lambda E, h=h: E.max_index(out=pos.ap[:, h, 0:8], in_max=top.ap[:, h, 0:8], in_values=cand.ap[:, h, :]),
                     r=[cand.buf, top.buf], w=[pos.buf])
                P.op("dve", lambda E, h=h: E.match_replace(out=S2.ap, in_to_replace=top.ap[:, h, 0:8], in_values=cand.ap[:, h, :],
                                                           imm_value=NEG), r=[cand.buf, top.buf], w=[S2.buf])
                P.op("dve", lambda E, h=h: E.max(out=top.ap[:, h, 8:16], in_=S2.ap), r=[S2.buf], w=[top.buf])
                P.op("dve", lambda E, h=h: E.max_index(out=pos.ap[:, h, 8:16], in_max=top.ap[:, h, 8:16], in_values=S2.ap),
                     r=[S2.buf, top.buf], w=[pos.buf])
            P.op("dve", lambda E: E.tensor_single_scalar(out=pa_.ap, in_=pos.ap, scalar=4, op=ALU.logical_shift_right),
                 r=[pos.buf], w=[pa_.buf])
            P.op("dve", lambda E: E.tensor_single_scalar(out=pb_.ap, in_=pos.ap, scalar=15, op=ALU.bitwise_and),
                 r=[pos.buf], w=[pb_.buf])
            P.cp("dve", af, pa_)
            P.cp("dve", bf, pb_)
            io4 = iota16.un(1).un(1).bc([128, 8, 16, 16])
            P.tt("dve", eq, af.un(3).bc([128, 8, 16, 16]), io4, ALU.is_equal)
            P.tt("dve", eq, eq, I4[:, :, 0, :].un(2).bc([128, 8, 16, 16]), ALU.mult)
            P.reduce(i1s, eq, ALU.add)
            P.tt("dve", eq, bf.un(3).bc([128, 8, 16, 16]), io4, ALU.is_equal)
            P.tt("dve", eq, eq, I4[:, :, 1, :].un(2).bc([128, 8, 16, 16]), ALU.mult)
            P.reduce(i2s, eq, ALU.add)
            P.tt("dve", gw, top, top[:, :, 0:1].bc([128, 8, 16]), ALU.subtract)
            P.act(gw, gw, AF.Exp)
            P.reduce(gsum, gw, ALU.add)
            P.recip(gsum, gsum)
            P.tt("dve", gw, gw, gsum.un(2).bc([128, 8, 16]), ALU.mult)
            for src, dst in ((i1s, i1T), (i2s, i2T), (gw, gT)):
                ps = ps_next()
                P.tr(ps[:, 0:128], src.r("p h k -> p (h k)"), ident_f)
                P.cp("act", dst, ps[:, 0:128])
            io3 = iota128.un(1).bc([128, 128, 128])
            P.tt("dve", Pm, io3, i1T.un(2).bc([128, 128, 128]), ALU.is_equal)
            P.tt("dve", Qm, io3, i2T.un(2).bc([128, 128, 128]), ALU.is_equal)
            P.tt("pool", Qm, Qm, gT.un(2).bc([128, 128, 128]), ALU.mult)
            for t8 in range(16):
                ps = ps_next()
                for k in range(8):
                    t_ = t8 * 8 + k
                    P.mm(ps[:, k * 128:(k + 1) * 128], Pm[:, t_, :], Qm[:, t_, :])
                P.cp("act", Gst[:, :, t8 * 8:(t8 + 1) * 8].r("p i t -> p t i"), ps.r("p (t i) -> p t i", t=8))
            P.dma("sp", Gd[:, :, rows], Gst)
        P.barrier()
        Z = Stack(nc, Zbase, SB_TOP)
        ACC = Z.alloc([NTT, D], F32, "ACC")
        Z2 = Stack(nc, Z.cur, SB_TOP)
        stg = [Z2.alloc([D], F32, "pstg%d" % i) for i in range(3)]
        ubf = [Z2.alloc([D], BF16, "ubf%d" % i) for i in range(2)]
        uTt = [Z2.alloc([16, 128], BF16, "uTt%d" % i) for i in range(2)]
        vbf = [[Z2.alloc([D], BF16, "vbf%d_%d" % (a, b)) for b in range(4)] for a in range(2)]
        Wt = [[Z2.alloc([T], BF16, "Wt%d_%d" % (a, b)) for b in range(4)] for a in range(2)]
        Gt = [Z2.alloc([T], BF16, "Gt%d" % i) for i in range(2)]
        ge = [Z2.alloc([T], BF16, "ge%d" % i) for i in range(2)]
        psT = PS[0]
        psA = [PS[1], PS[2]]
        psO = [TT(PS[3].ap[:, 0:512], Buf("psO0", "ps")), TT(PS[3].ap[:, 512:1024], Buf("psO1", "ps"))]
        urows = W["peer_u"][l].r("(i j) d -> j i d", j=128)
        vrows = W["peer_v"][l].r("(i j) d -> j i d", j=128)
        si = [0]
        NJ = 128

        def stage_a(j):
            s_u = stg[si[0] % 3]
            si[0] += 1
            P.dma("sp", s_u, urows[j])
            ub = ubf[j % 2]
            P.cp("act", ub, s_u)
            psb = psT.bitcast(BF16).r("p (k i) -> p k i", k=16)
            for kt in range(16):
                P.tr(psb[:, kt, :], ub[:, kt * 128:(kt + 1) * 128], ident_b)
            P.cp("act", uTt[j % 2], psb)

        stage_a(0)
        for j in range(NJ):
            gi, jj = (j // 4) % 2, j % 4
            if j + 1 < NJ:
                stage_a(j + 1)
            pa2 = psA[j % 2]
            ut = uTt[j % 2]
            for half in range(2):
                hs = slice(half * 512, half * 512 + 512)
                for kt in range(16):
                    P.mm(pa2[:, hs], ut[:, kt, :], h2T[:, kt, hs], start=(kt == 0), stop=(kt == 15))
            g_ = Gt[j % 2]
            P.dma("sp", g_, Gd[:, j, :])
            e_ = ge[j % 2]
            P.act(e_, pa2, AF.Gelu_apprx_tanh)
            P.tt("dve", Wt[gi][jj], e_, g_, ALU.mult)
            s_v = stg[si[0] % 3]
            si[0] += 1
            P.dma("sp", s_v, vrows[j])
            P.cp("pool", vbf[gi][jj], s_v)
            if jj == 3:
                first = (j == 3)
                for tt_ in range(NTT):
                    for dc in range(4):
                        po = psO[(tt_ * 4 + dc) % 2]
                        for q_ in range(4):
                            P.mm(po, Wt[gi][q_][:, tt_ * 128:(tt_ + 1) * 128], vbf[gi][q_][:, dc * 512:(dc + 1) * 512],
                                 start=(q_ == 0), stop=(q_ == 3))
                        dst = ACC[:, tt_, dc * 512:(dc + 1) * 512]
                        if first:
                            P.cp("act", dst, po)
                        else:
                            P.tt("dve", dst, dst, po, ALU.add)
        P.barrier()
        Z2.reset()
        x1 = [Z2.alloc([D], F32, "x1_%d" % i) for i in range(2)]
        tq2 = Z2.alloc([D], F32, "tq2")
        bc3 = {k: Z2.alloc([D], F32, k) for k in ("gt2", "g2", "b2")}
        st = Z2.alloc([4, 6], F32, "stC")
        mv = Z2.alloc([2], F32, "mvC")
        rs = Z2.alloc([1], F32, "rsC")
        bload(bc3["gt2"], mod_d[l], u, 5 * D)
        bload(bc3["g2"], W["ln2_g"], l, 0)
        bload(bc3["b2"], W["ln2_b"], l, 0)
        for tt_ in range(NTT):
            rows = slice(tt_ * 128, tt_ * 128 + 128)
            x1_ = x1[tt_ % 2]
            acc = ACC[:, tt_, :]
            P.dma("sp", x1_, x1res[rows, :], g_x[0])
            if tt_ == 0:
                dbg_store("peer", acc)
            P.tt("pool", acc, acc, bc3["gt2"], ALU.mult)
            P.stt(acc, x1_, ALPHA, acc, ALU.mult, ALU.add)
            ln_tm(acc, tq2, st, mv, rs)
            P.tt("pool", tq2, tq2, bc3["g2"], ALU.mult)
            P.tt("dve", x1_, tq2, bc3["b2"], ALU.add)
            if last:
                P.dma("sp", yout[u][rows, :], x1_, g_st)
            else:
                P.dma("sp", xres[u][rows, :], x1_, g_st)
        P.barrier()

    phase_mod()
    ret = None
    for l in range(nlayers):
        phase_setup(l)
        if stop == "setup":
            break
        for u in units:
            ret = phase_A(l, u)
            if stop is not None and stop.startswith("A"):
                break
            phase_B(l, u, last=(l == nlayers - 1))
            if stop is not None:
                break
        if stop is not None:
            break
    if ret is not None and "ret" in dbg_out:
        src = ret
        P.dma("sp", dbg_out["ret"], src, g_st)
    P.final_wait()
    P.emit()
    return nc


def _grid_pos():
    rows = T // 64
    t = np.arange(rows * 64)
    r = (t // 64).astype(np.float32)
    col = (t % 64).astype(np.float32)
    nf = D // 4
    freq = (1.0 / (np.float32(10000.0) ** (np.arange(nf, dtype=np.float32) / np.float32(nf)))).astype(np.float32)
    ar = r[:, None] * freq
    ac = col[:, None] * freq
    return np.concatenate([np.sin(ar), np.cos(ar), np.sin(ac), np.cos(ac)], -1).astype(np.float32)


WEIGHT_NAMES = ["w_mod", "b_mod", "w_in", "s5_lam_re", "s5_lam_im", "s5_log_dt", "s5_b_re", "s5_b_im", "s5_c_re",
                "s5_c_im", "s5_d", "s5_w_glu", "sc_conv_w", "sc_conv_b", "cf_conv_w", "cf_conv_b", "cf_ln_g", "cf_ln_b",
                "w_pa", "w_pb", "w_pc", "w_o", "ln1_g", "ln1_b", "peer_w_q", "peer_k1", "peer_k2", "peer_u", "peer_v",
                "ln2_g", "ln2_b"]


def make_in_maps(inputs, cores=range(NCORE)):
    f = lambda a: np.ascontiguousarray(np.asarray(a, dtype=np.float32))
    xp = f(inputs["x_prompt"])
    xs = f(inputs["x_sample"])
    stt = f(inputs["state_ssm"])
    c = f(inputs["c"])
    cc = f(inputs["c_ctx"])
    pe = _grid_pos()
    ident = np.eye(128, dtype=np.float32)
    iota = np.tile(np.arange(128, dtype=np.float32)[None, :], (128, 1))
    wts = {k: f(inputs[k]) for k in WEIGHT_NAMES}
    maps = []
    for i in cores:
        m = dict(wts)
        m["xp"] = np.ascontiguousarray(xp[4 * i:4 * i + 4].reshape(T, D))
        m["xs"] = np.ascontiguousarray(xs[i].reshape(T, D))
        m["pe"] = pe
        m["st0"] = np.ascontiguousarray(stt[i])
        m["cond"] = np.ascontiguousarray(np.stack([cc, c[i]], 0))
        m["ident"] = ident
        m["iota128"] = iota
        maps.append(m)
    return maps


def kernel(**inputs):
    nc = build()
    maps = make_in_maps(inputs)
    maps = [{k: m[k] for k in build.declared} for m in maps]
    res = run_bass_kernel_spmd(nc, maps, core_ids=list(range(NCORE)))
    yp = np.concatenate([r["yp"].reshape(4, 256, D) for r in res.results], 0).astype(np.float32)
    ys = np.stack([r["ys"].reshape(T, D) for r in res.results], 0).astype(np.float32)
    nst = np.concatenate([r["nst"] for r in res.results], 0).astype(np.float32)
    return (yp, ys, nst)
```

```python
import math
from contextlib import ExitStack

import numpy as np
import concourse.bass as bass
import concourse.mybir as mybir
from concourse.bass_utils import run_bass_kernel_spmd

F32 = mybir.dt.float32
BF16 = mybir.dt.bfloat16
I32 = mybir.dt.int32
U32 = mybir.dt.uint32
ALU = mybir.AluOpType
AF = mybir.ActivationFunctionType
AX = mybir.AxisListType

D = 2048
NCORE = 8
T = 1024
NTT = 8
KT = 16
NL = 2
ALPHA = 4.0 ** 0.25
LN_EPS = 1e-6
IN_COLS = 9728
GATE0 = 3584
NEG = -1.0e30


class Buf:
    __slots__ = ("name", "lw", "rd", "space", "dsem")

    def __init__(self, name, space="sb"):
        self.name = name
        self.lw = {}
        self.rd = {}
        self.space = space
        self.dsem = None


class Grp:
    __slots__ = ("sem", "cnt")

    def __init__(self, sem):
        self.sem = sem
        self.cnt = 0


class TT:
    __slots__ = ("ap", "buf")

    def __init__(self, ap, buf):
        self.ap = ap
        self.buf = buf

    def __getitem__(self, k):
        return TT(self.ap[k], self.buf)

    def r(self, pat, **kw):
        return TT(self.ap.rearrange(pat, **kw), self.buf)

    def bc(self, shape):
        return TT(self.ap.to_broadcast(list(shape)), self.buf)

    def un(self, ax):
        return TT(self.ap.unsqueeze(ax), self.buf)

    def bitcast(self, dt):
        return TT(self.ap.bitcast(dt), self.buf)


def _a(x):
    return x.ap if isinstance(x, TT) else x


class Prog:
    def __init__(self, nc, es):
        self.nc = nc
        self.es = es
        self.E = dict(pe=nc.tensor, dve=nc.vector, act=nc.scalar, pool=nc.gpsimd, sp=nc.sync)
        self.rec = {k: [] for k in self.E}
        self.esem = {k: es.enter_context(nc.semaphore("es_" + k)) for k in self.E}
        self.ecnt = {k: 0 for k in self.E}
        self.known = {k: {} for k in self.E}
        self.grps = []
        self.free = []
        self.active = []
        self.max_dsem = 84

    def grp(self, name):
        g = Grp(self.es.enter_context(self.nc.semaphore(name)))
        self.grps.append(g)
        return g

    def dsem_for(self, buf):
        if buf.dsem is None:
            if self.free:
                buf.dsem = self.free.pop()
            else:
                assert len(self.grps) < self.max_dsem, "out of DMA semaphores"
                buf.dsem = self.grp("dq%d" % len(self.grps))
            self.active.append(buf)
        return buf.dsem

    def op(self, e, fn, r=(), w=(), grp=None):
        need = {}
        own = self.esem[e]

        def add(tok):
            sem, val = tok
            if e == "pe" and sem is own:
                return
            k = id(sem)
            if k not in need or need[k][1] < val:
                need[k] = (sem, val)

        for b in list(r) + list(w):
            for tok in b.lw.values():
                add(tok)
        for b in w:
            for tok in b.rd.values():
                add(tok)
        waits = []
        kn = self.known[e]
        for k, (sem, val) in need.items():
            if kn.get(k, 0) >= val:
                continue
            kn[k] = val
            waits.append((sem, val))
        if grp is not None:
            grp.cnt += 16
            tok = (grp.sem, grp.cnt)
            inc = (grp.sem, 16)
        else:
            self.ecnt[e] += 1
            tok = (own, self.ecnt[e])
            inc = (own, 1)
        for b in w:
            if b.space == "dram":
                b.lw[id(tok[0])] = tok
            else:
                b.lw = {id(tok[0]): tok}
            b.rd = {}
        for b in r:
            if b in w:
                continue
            b.rd[id(tok[0])] = tok
        self.rec[e].append((waits, fn, inc))

    def barrier(self):
        toks = [(self.esem[k], self.ecnt[k]) for k in self.E] + [(g.sem, g.cnt) for g in self.grps]
        for e in self.E:
            waits = []
            kn = self.known[e]
            for sem, val in toks:
                if val == 0 or sem is self.esem[e]:
                    continue
                if kn.get(id(sem), 0) >= val:
                    continue
                kn[id(sem)] = val
                waits.append((sem, val))
            if waits:
                self.rec[e].append((waits, None, None))
        for b in self.active:
            self.free.append(b.dsem)
            b.dsem = None
        self.active = []

    def final_wait(self):
        e = "sp"
        waits = [(self.esem[k], self.ecnt[k]) for k in self.E if k != e and self.ecnt[k]]
        waits += [(g.sem, g.cnt) for g in self.grps if g.cnt]
        self.rec[e].append((waits, None, None))

    def emit(self):
        with self.nc.Block() as blk:
            def mk(e):
                def body(Eng):
                    for waits, fn, inc in self.rec[e]:
                        for sem, val in waits:
                            Eng.wait_ge(sem, val)
                        if fn is not None:
                            fn(Eng).then_inc(inc[0], inc[1])
                return body
            blk.tensor(mk("pe"))
            blk.vector(mk("dve"))
            blk.scalar(mk("act"))
            blk.gpsimd(mk("pool"))
            blk.sync(mk("sp"))

    def dma(self, q, out, in_, grp=None, **kw):
        if out.buf.space == "sb":
            g = self.dsem_for(out.buf)
        elif in_.buf.space == "sb":
            g = self.dsem_for(in_.buf)
        else:
            g = self.dsem_for(out.buf)
        self.op(q, lambda E: E.dma_start(out=out.ap, in_=in_.ap, **kw), r=[in_.buf], w=[out.buf], grp=g)

    def mm(self, out, lhsT, rhs, start=True, stop=True):
        self.op("pe", lambda E: E.matmul(out=out.ap, lhsT=lhsT.ap, rhs=rhs.ap, start=start, stop=stop),
                r=[lhsT.buf, rhs.buf], w=[out.buf])

    def tr(self, out, in_, ident):
        self.op("pe", lambda E: E.transpose(out=out.ap, in_=in_.ap, identity=ident.ap),
                r=[in_.buf, ident.buf], w=[out.buf])

    def act(self, out, in_, func, bias=None, scale=None, e="act"):
        rb = [in_.buf]
        kw = {}
        if bias is not None:
            kw["bias"] = _a(bias)
            if isinstance(bias, TT):
                rb.append(bias.buf)
        if scale is not None:
            kw["scale"] = _a(scale)
            if isinstance(scale, TT):
                rb.append(scale.buf)
        self.op("act", lambda E: E.activation(out=out.ap, in_=in_.ap, func=func, **kw), r=rb, w=[out.buf])

    def cp(self, e, out, in_):
        if e == "act":
            self.op("act", lambda E: E.copy(out=out.ap, in_=in_.ap), r=[in_.buf], w=[out.buf])
        else:
            self.op(e, lambda E: E.tensor_copy(out=out.ap, in_=in_.ap), r=[in_.buf], w=[out.buf])

    def tt(self, e, out, a, b, op):
        self.op(e, lambda E: E.tensor_tensor(out=out.ap, in0=a.ap, in1=b.ap, op=op), r=[a.buf, b.buf], w=[out.buf])

    def ts(self, e, out, a, s1, op0, s2=None, op1=None):
        rb = [a.buf] + [x.buf for x in (s1, s2) if isinstance(x, TT)]
        if op1 is None:
            self.op(e, lambda E: E.tensor_scalar(out=out.ap, in0=a.ap, scalar1=_a(s1), scalar2=None, op0=op0),
                    r=rb, w=[out.buf])
        else:
            self.op(e, lambda E: E.tensor_scalar(out=out.ap, in0=a.ap, scalar1=_a(s1), scalar2=_a(s2), op0=op0, op1=op1),
                    r=rb, w=[out.buf])

    def stt(self, out, a, s, b, op0, op1, e="dve"):
        rb = [a.buf, b.buf] + ([s.buf] if isinstance(s, TT) else [])
        self.op(e, lambda E: E.scalar_tensor_tensor(out=out.ap, in0=a.ap, scalar=_a(s), in1=b.ap, op0=op0, op1=op1),
                r=rb, w=[out.buf])

    def memset(self, e, out, val):
        self.op(e, lambda E: E.memset(out.ap, val), w=[out.buf])

    def recip(self, out, in_):
        self.op("dve", lambda E: E.reciprocal(out=out.ap, in_=in_.ap), r=[in_.buf], w=[out.buf])

    def scan(self, out, d0, d1, init):
        rb = [d0.buf, d1.buf] + ([init.buf] if isinstance(init, TT) else [])
        self.op("dve", lambda E: E.tensor_tensor_scan(out=out.ap, data0=d0.ap, data1=d1.ap, initial=_a(init),
                                                       op0=ALU.mult, op1=ALU.add), r=rb, w=[out.buf])

    def reduce(self, out, in_, op, axis=AX.X):
        self.op("dve", lambda E: E.tensor_reduce(out=out.ap, in_=in_.ap, axis=axis, op=op), r=[in_.buf], w=[out.buf])


class Stack:
    cnt = [0]

    def __init__(self, nc, base, limit):
        self.nc = nc
        self.base = base
        self.cur = base
        self.limit = limit

    def alloc(self, free_shape, dtype, name):
        nb = int(np.prod(free_shape)) * (2 if dtype == BF16 else 4)
        off = (self.cur + 31) // 32 * 32
        assert off + nb <= self.limit, (name, off, nb, self.limit)
        self.cur = off + nb
        Stack.cnt[0] += 1
        h = self.nc.alloc_sbuf_tensor_at("%s_%d" % (name, Stack.cnt[0]), [128] + list(free_shape), dtype, offset=off)
        return TT(h.ap(), Buf(name))

    def sub(self, nbytes):
        off = (self.cur + 31) // 32 * 32
        assert off + nbytes <= self.limit, (off, nbytes, self.limit)
        self.cur = off + nbytes
        return Stack(self.nc, off, off + nbytes)

    def reset(self):
        self.cur = self.base


def build(dbg=None, stop=None, nlayers=NL, units=(0, 1), b3_tiles=NTT):
    dbg = dbg or {}
    Stack.cnt[0] = 0
    nc = bass.Bass("TRN2", target_bir_lowering=False)
    es = ExitStack()
    P = Prog(nc, es)

    declared = []
    build.declared = declared

    def din(name, shape, dt=F32):
        declared.append(name)
        return TT(nc.dram_tensor(name, list(shape), dt, kind="ExternalInput").ap(), Buf(name, "dram"))

    def dout(name, shape, dt=F32):
        return TT(nc.dram_tensor(name, list(shape), dt, kind="ExternalOutput").ap(), Buf(name, "dram"))

    def dscr(name, shape, dt=F32):
        return TT(nc.dram_tensor(name, list(shape), dt, kind="Internal").ap(), Buf(name, "dram"))

    xin = [din("xp", [T, D]), din("xs", [T, D])]
    pe_d = din("pe", [T, D])
    st0 = din("st0", [NL, 2, 64, 64, 2])
    cond = din("cond", [2, D])
    ident_d = din("ident", [128, 128])
    iota_d = din("iota128", [128, 128])
    WSH = dict([("w_mod", [NL, D, 6 * D]), ("b_mod", [NL, 6 * D]), ("w_in", [NL, D, IN_COLS]),
                ("s5_lam_re", [NL, 2, 64, 64]), ("s5_lam_im", [NL, 2, 64, 64]), ("s5_log_dt", [NL, 2, 64]),
                ("s5_b_re", [NL, 2, 64, 64, 16]), ("s5_b_im", [NL, 2, 64, 64, 16]),
                ("s5_c_re", [NL, 2, 64, 16, 64]), ("s5_c_im", [NL, 2, 64, 16, 64]),
                ("s5_d", [NL, 1024]), ("s5_w_glu", [NL, 1024, 1024]),
                ("sc_conv_w", [NL, 3, 512]), ("sc_conv_b", [NL, 512]),
                ("cf_conv_w", [NL, 31, 512]), ("cf_conv_b", [NL, 512]),
                ("cf_ln_g", [NL, 512]), ("cf_ln_b", [NL, 512]),
                ("w_pa", [NL, 1024, D]), ("w_pb", [NL, 512, D]), ("w_pc", [NL, 512, D]), ("w_o", [NL, D, D]),
                ("ln1_g", [NL, D]), ("ln1_b", [NL, D]),
                ("peer_w_q", [NL, D, D]), ("peer_k1", [NL, 128, 128]), ("peer_k2", [NL, 128, 128]),
                ("peer_u", [NL, 16384, D]), ("peer_v", [NL, 16384, D]),
                ("ln2_g", [NL, D]), ("ln2_b", [NL, D])])

    class LazyW(dict):
        def __missing__(self, nm):
            self[nm] = din(nm, WSH[nm])
            return self[nm]
    W = LazyW()
    yout = [dout("yp", [T, D]), dout("ys", [T, D])]
    nst_o = dout("nst", [4, NL, 2, 64, 64, 2])
    dbg_out = {k: dout("dbg_" + k, v[0], v[1]) for k, v in dbg.items()}

    xres = [dscr("xres0", [T, D]), dscr("xres1", [T, D])]
    ymix = dscr("ymix", [T, D])
    x1res = dscr("x1res", [T, D])
    h2res = dscr("h2res", [T, D])
    mod_d = dscr("mod_d", [NL, 2, 6 * D])
    tabs = dscr("tabs", [64, 128, 2, 260])
    wbd = dscr("wbd", [8, 128, 16, 128], BF16)
    cwd = dscr("cwd", [8, 128, 16, 64], BF16)
    Gd = dscr("Gd", [128, 128, T], BF16)

    psh = nc.alloc_psum_tensor("psum_all", [128, 4096], F32)
    PS = [TT(psh.ap()[:, i * 1024:(i + 1) * 1024], Buf("ps%d" % i, "ps")) for i in range(4)]
    ps_rr = [0]

    def ps_next():
        ps_rr[0] = (ps_rr[0] + 1) % 4
        return PS[ps_rr[0]]

    SB_BASE = (int(nc.sbuf_base) + 63) // 64 * 64
    SB_TOP = int(nc.sbuf_top)
    root = Stack(nc, SB_BASE, SB_TOP)
    G = root.sub(4 * 1024)
    LP = root.sub(4608)
    UL = Stack(nc, root.cur, SB_TOP)

    g_st = g_c = None
    g_x = g_w = g_t = [None, None]

    ident_f = G.alloc([128], F32, "ident_f")
    ident_b = G.alloc([128], BF16, "ident_b")
    iota128 = G.alloc([128], F32, "iota128")
    iota16 = iota128[:, 0:16]
    halfpi = G.alloc([1], F32, "halfpi")
    X4 = G.alloc([4, 128], BF16, "X4")
    CWst = G.alloc([4, 64], BF16, "CWst")
    ones_f = G.alloc([128], F32, "ones_f")
    P.dma("sp", ident_f, ident_d, g_c)
    P.dma("sp", iota128, iota_d, g_c)
    P.cp("dve", ident_b, ident_f)
    P.memset("dve", halfpi, math.pi / 2)
    P.memset("dve", X4, 0.0)
    P.memset("dve", CWst, 0.0)
    P.memset("dve", ones_f, 1.0 / 512.0)

    Rt = LP.alloc([64], F32, "Rt")
    DSK = LP.alloc([8], F32, "DSK")
    H0 = LP.alloc([64, 2], F32, "H0")
    SCW = LP.alloc([4, 3], F32, "SCW")
    SCB = LP.alloc([4], F32, "SCB")
    CFW = LP.alloc([4, 31], F32, "CFW")
    CFB = LP.alloc([4], F32, "CFB")
    CFG = LP.alloc([4], F32, "CFG")
    CFBB = LP.alloc([4], F32, "CFBB")
    KTb = LP.alloc([2, 128], BF16, "KTb")
    NST = LP.alloc([64, 4, 2], F32, "NST")

    WSTG_B = 16 * 256 * 4
    WBF_B = 16 * 256 * 2

    def dbg_store(key, src, dst_slice=None):
        if key in dbg_out:
            dst = dbg_out[key] if dst_slice is None else dbg_out[key][dst_slice]
            P.dma("sp", dst, src, g_st)

    def phase_mod():
        UL.reset()
        condT = UL.alloc([16, 2], F32, "condT")
        bmod = UL.alloc([6 * D], F32, "bmod")
        stg = [UL.alloc([16, 256], F32, "mstg%d" % i) for i in range(2)]
        mo = [UL.alloc([256], F32, "mo%d" % i) for i in range(2)]
        for r_ in range(2):
            P.dma("sp", condT[:, :, r_], cond[r_].r("(kt p) -> p kt", p=128), g_c, allow_slow_non_contiguous=True)
        P.act(condT, condT, AF.Silu)
        for l in range(nlayers):
            P.dma("sp", bmod[0:2, :], TT(W["b_mod"].ap[l:l + 1, :].partition_broadcast(2), W["b_mod"].buf), g_c)
            for c in range(48):
                s_ = stg[c % 2]
                P.dma("sp", s_, W["w_mod"][l][:, c * 256:(c + 1) * 256].r("(kt p) c -> p kt c", p=128), g_w[c % 2])
                ps = ps_next()
                for kt in range(16):
                    P.mm(ps[0:2, 0:256], condT[:, kt, :], s_[:, kt, :], start=(kt == 0), stop=(kt == 15))
                m_ = mo[c % 2]
                P.tt("dve", m_[0:2, :], ps[0:2, 0:256], bmod[0:2, c * 256:(c + 1) * 256], ALU.add)
                if (8 <= c < 16) or (32 <= c < 40):
                    P.ts("dve", m_[0:2, :], m_[0:2, :], 1.0, ALU.add)
                P.dma("sp", mod_d[l][:, c * 256:(c + 1) * 256], m_[0:2, :], g_st)
        P.barrier()

    def phase_setup(l):
        UL.reset()
        A = UL
        nat = [A.alloc([128], F32, "nat%d" % i) for i in range(3)]
        dt2 = A.alloc([2], F32, "dt2")
        LR = A.alloc([64], F32, "LR")
        LI = A.alloc([64], F32, "LI")
        DT = A.alloc([64], F32, "DT")
        P.dma("sp", nat[0][0:64, :], W["s5_lam_re"][l].r("d (gp g2) p -> (d gp) (g2 p)", g2=2), g_c)
        P.dma("sp", nat[1][0:64, :], W["s5_lam_im"][l].r("d (gp g2) p -> (d gp) (g2 p)", g2=2), g_c)
        P.dma("sp", dt2[0:64, :], W["s5_log_dt"][l].r("d (gp g2) -> (d gp) g2", g2=2), g_c)
        P.cp("dve", nat[2][0:64, :].r("q (a b) -> q a b", a=2), dt2[0:64, :].un(2).bc([64, 2, 64]))
        for i, dst in enumerate((LR, LI, DT)):
            ps = ps_next()
            P.tr(ps[:, 0:64], nat[i][0:64, :], ident_f[0:64, 0:64])
            P.cp("act", dst, ps[:, 0:64])
        v = {k: A.alloc([64], F32, k) for k in ("dt", "ang", "c", "s", "t1", "t2", "are", "aim", "den", "am1", "zre", "zim")}
        P.act(v["dt"], DT, AF.Exp)
        P.tt("dve", v["t1"], LR, v["dt"], ALU.mult)
        P.act(Rt, v["t1"], AF.Exp)
        P.tt("dve", v["ang"], LI, v["dt"], ALU.mult)
        P.act(v["s"], v["ang"], AF.Sin, scale=1.0 / 32.0)
        P.act(v["c"], v["ang"], AF.Sin, bias=halfpi[:, 0:1], scale=1.0 / 32.0)
        for _ in range(5):
            P.tt("dve", v["t1"], v["c"], v["c"], ALU.mult)
            P.tt("dve", v["t2"], v["s"], v["s"], ALU.mult)
            P.stt(v["s"], v["c"], 2.0, v["s"], ALU.mult, ALU.mult)
            P.tt("dve", v["c"], v["t1"], v["t2"], ALU.subtract)
        C1, S1 = v["c"], v["s"]
        P.tt("dve", v["are"], Rt, C1, ALU.mult)
        P.tt("dve", v["aim"], Rt, S1, ALU.mult)
        P.tt("dve", v["t1"], LR, LR, ALU.mult)
        P.tt("dve", v["t2"], LI, LI, ALU.mult)
        P.tt("dve", v["den"], v["t1"], v["t2"], ALU.add)
        P.recip(v["den"], v["den"])
        P.ts("dve", v["am1"], v["are"], -1.0, ALU.add)
        P.tt("dve", v["t1"], v["am1"], LR, ALU.mult)
        P.tt("dve", v["t2"], v["aim"], LI, ALU.mult)
        P.tt("dve", v["t1"], v["t1"], v["t2"], ALU.add)
        P.tt("dve", v["zre"], v["t1"], v["den"], ALU.mult)
        P.tt("dve", v["t1"], v["aim"], LR, ALU.mult)
        P.tt("dve", v["t2"], v["am1"], LI, ALU.mult)
        P.tt("dve", v["t1"], v["t1"], v["t2"], ALU.subtract)
        P.tt("dve", v["zim"], v["t1"], v["den"], ALU.mult)
        BRE = A.alloc([64, 16], F32, "BRE")
        BIM = A.alloc([64, 16], F32, "BIM")
        tA = A.alloc([64, 16], F32, "tA")
        tB = A.alloc([64, 16], F32, "tB")
        BB = [A.alloc([64, 16], BF16, "BBre"), A.alloc([64, 16], BF16, "BBim")]
        P.dma("sp", BRE, W["s5_b_re"][l].r("d (gp g2) p s -> (g2 p) (d gp) s", g2=2), g_c)
        P.dma("sp", BIM, W["s5_b_im"][l].r("d (gp g2) p s -> (g2 p) (d gp) s", g2=2), g_c)
        zr = v["zre"].un(2).bc([128, 64, 16])
        zi = v["zim"].un(2).bc([128, 64, 16])
        P.tt("dve", tA, BRE, zr, ALU.mult)
        P.tt("dve", tB, BIM, zi, ALU.mult)
        P.tt("dve", BB[0], tA, tB, ALU.subtract)
        P.tt("dve", tA, BIM, zr, ALU.mult)
        P.tt("dve", tB, BRE, zi, ALU.mult)
        P.tt("dve", BB[1], tA, tB, ALU.add)
        wst = [A.alloc([4, 128], BF16, "wst%d" % i) for i in range(2)]
        it = 0
        for d in range(2):
            for ct in range(8):
                rt0 = d * 32 + ct * 4
                for reim in range(2):
                    for j in range(4):
                        P.cp("dve", X4[0:64, j, 32 * j:32 * j + 16], BB[reim][0:64, rt0 + j, :])
                        P.cp("dve", X4[64:128, j, 32 * j + 16:32 * j + 32], BB[reim][64:128, rt0 + j, :])
                    ps = ps_next()
                    psb = ps[:, 0:256].bitcast(BF16).r("p (j c) -> p j c", j=4)
                    for j in range(4):
                        P.tr(psb[:, j, :], X4[:, j, :], ident_b)
                    ws_ = wst[it % 2]
                    it += 1
                    P.cp("act", ws_, psb)
                    P.dma("sp", wbd[ct][:, d * 8 + reim:d * 8 + 8:2, :], ws_, g_st)
        CN = [A.alloc([64], F32, "CN%d" % i) for i in range(2)]
        CN2 = A.alloc([128], BF16, "CN2")
        cst = [A.alloc([4, 64], BF16, "cst%d" % i) for i in range(2)]
        it = 0
        for d in range(2):
            for ct in range(8):
                for reim in range(2):
                    src = W["s5_c_im" if reim else "s5_c_re"][l, d, 8 * ct:8 * ct + 8].r("g s p -> (g s) p")
                    cn = CN[it % 2]
                    P.dma("sp", cn, src, g_t[it % 2])
                    sgn = -1.0 if reim else 1.0
                    P.ts("dve", CN2[:, 0:64], cn, sgn, ALU.mult)
                    P.ts("dve", CN2[:, 64:128], cn, sgn, ALU.mult)
                    ps = ps_next()
                    psb = ps[:, 0:64].bitcast(BF16)
                    P.tr(psb, CN2, ident_b)
                    for j in range(4):
                        jl = j % 2
                        P.cp("dve", CWst[0:64, j, 32 * jl:32 * jl + 16], psb[0:64, 32 * j:32 * j + 16])
                        P.cp("dve", CWst[64:128, j, 32 * jl + 16:32 * jl + 32], psb[64:128, 32 * j + 16:32 * j + 32])
                    cs_ = cst[it % 2]
                    it += 1
                    P.cp("pool", cs_, CWst)
                    P.dma("sp", cwd[ct][:, d * 8 + reim:d * 8 + 8:2, :], cs_, g_st)
        ER = A.alloc([16, 260], F32, "ER")
        EI = A.alloc([16, 260], F32, "EI")
        q = {k: A.alloc([16, 128], F32, k) for k in ("q1", "q2", "q3", "q4")}
        MR = A.alloc([16], F32, "MR")
        MI = A.alloc([16], F32, "MI")
        m1 = A.alloc([16], F32, "m1")
        m2 = A.alloc([16], F32, "m2")
        for gq in range(4):
            sl = slice(gq * 16, gq * 16 + 16)
            P.memset("dve", ER[:, :, 0:1], 1.0)
            P.memset("dve", EI[:, :, 0:1], 0.0)
            P.cp("dve", MR, C1[:, sl])
            P.cp("dve", MI, S1[:, sl])
            k = 1
            while k < 260:
                cnt = min(k, 260 - k)
                o = 0
                while o < cnt:
                    n = min(128, cnt - o)
                    mrb = MR.un(2).bc([128, 16, n])
                    mib = MI.un(2).bc([128, 16, n])
                    sr = ER[:, :, o:o + n]
                    si = EI[:, :, o:o + n]
                    P.tt("dve", q["q1"][:, :, 0:n], sr, mrb, ALU.mult)
                    P.tt("dve", q["q2"][:, :, 0:n], si, mib, ALU.mult)
                    P.tt("pool", q["q3"][:, :, 0:n], sr, mib, ALU.mult)
                    P.tt("pool", q["q4"][:, :, 0:n], si, mrb, ALU.mult)
                    P.tt("dve", ER[:, :, k + o:k + o + n], q["q1"][:, :, 0:n], q["q2"][:, :, 0:n], ALU.subtract)
                    P.tt("pool", EI[:, :, k + o:k + o + n], q["q3"][:, :, 0:n], q["q4"][:, :, 0:n], ALU.add)
                    o += n
                if 2 * k < 260:
                    P.tt("dve", m1, MR, MR, ALU.mult)
                    P.tt("dve", m2, MI, MI, ALU.mult)
                    P.stt(MI, MR, 2.0, MI, ALU.mult, ALU.mult)
                    P.tt("dve", MR, m1, m2, ALU.subtract)
                k *= 2
            P.dma("sp", tabs[gq * 16:gq * 16 + 16, :, 0, :].r("rt p t -> p rt t"), ER, g_st)
            P.dma("sp", tabs[gq * 16:gq * 16 + 16, :, 1, :].r("rt p t -> p rt t"), EI, g_st)
        P.dma("sp", DSK, W["s5_d"][l].r("(ct p) -> p ct", p=128), g_c, allow_slow_non_contiguous=True)
        P.dma("sp", H0, st0[l].r("d (gp g2) p c -> (g2 p) (d gp) c", g2=2), g_c)
        cwn = A.alloc([512], F32, "cwn")
        for nm_, dst_, nk_ in (("sc_conv_w", SCW, 3), ("cf_conv_w", CFW, 31)):
            P.dma("sp", cwn[0:nk_, :], W[nm_][l], g_c)
            for ct_ in range(4):
                ps = ps_next()
                P.tr(ps[:, 0:nk_], cwn[0:nk_, ct_ * 128:(ct_ + 1) * 128], ident_f[0:nk_, 0:nk_])
                P.cp("act", dst_[:, ct_, :], ps[:, 0:nk_])
        P.dma("sp", SCB, W["sc_conv_b"][l].r("(ct p) -> p ct", p=128), g_c, allow_slow_non_contiguous=True)
        P.dma("sp", CFB, W["cf_conv_b"][l].r("(ct p) -> p ct", p=128), g_c, allow_slow_non_contiguous=True)
        P.dma("sp", CFG, W["cf_ln_g"][l].r("(ct p) -> p ct", p=128), g_c, allow_slow_non_contiguous=True)
        P.dma("sp", CFBB, W["cf_ln_b"][l].r("(ct p) -> p ct", p=128), g_c, allow_slow_non_contiguous=True)
        dbg_store("CFW", CFW)
        dbg_store("SCW", SCW)
        kn = [A.alloc([128], F32, "kn%d" % i) for i in range(2)]
        for hf, nm in enumerate(("peer_k1", "peer_k2")):
            P.dma("sp", kn[hf], W[nm][l], g_c)
            ps = ps_next()
            P.tr(ps[:, 0:128], kn[hf], ident_f)
            P.cp("act", KTb[:, hf, :], ps[:, 0:128])
        P.barrier()

    wslots = {}
    w_it = [0]

    def load_w(W2d, r0, nk, c0, ncol):
        i = w_it[0] % 2
        w_it[0] += 1
        stg, wbf = wslots["stg"][i], wslots["wbf"][i]
        P.dma("sp", stg[:, 0:nk, 0:ncol], W2d[r0:r0 + 128 * nk, c0:c0 + ncol].r("(kt p) c -> p kt c", p=128), g_w[i])
        if i == 0:
            P.cp("pool", wbf[:, 0:nk, 0:ncol], stg[:, 0:nk, 0:ncol])
        else:
            P.cp("act", wbf[:, 0:nk, 0:ncol], stg[:, 0:nk, 0:ncol])
        return wbf

    def linear_fm(in_tiles, W2d, r0, c0, ncols, consume, tile0=0):
        nk = len(in_tiles)
        for cc in range(0, ncols, 256):
            wb = load_w(W2d, r0, nk, c0 + cc, 256)
            for ti in range(2):
                ps = ps_next()
                for half in range(2):
                    hs = slice(half * 512, half * 512 + 512)
                    for kt in range(nk):
                        P.mm(ps[:, hs], wb[:, kt, ti * 128:(ti + 1) * 128], in_tiles[kt][:, hs],
                             start=(kt == 0), stop=(kt == nk - 1))
                consume(tile0 + cc // 128 + ti, ps)

    def ln_tm(x, out, st, mv, rs, e="dve"):
        for c in range(4):
            P.op("dve", lambda E, c=c: E.bn_stats(out=st.ap[:, c, :], in_=x.ap[:, c * 512:(c + 1) * 512]),
                 r=[x.buf], w=[st.buf])
        P.op("dve", lambda E: E.bn_aggr(out=mv.ap, in_=st.ap.rearrange("p a b -> p (a b)")), r=[st.buf], w=[mv.buf])
        P.ts("dve", rs, mv[:, 1:2], LN_EPS, ALU.add)
        P.act(rs, rs, AF.Sqrt)
        P.recip(rs, rs)
        P.ts("dve", out, x, mv[:, 0:1], ALU.subtract, rs[:, 0:1], ALU.mult)

    def phase_A(l, u):
        nseq, SL = (4, 256) if u == 0 else (1, 1024)
        UL.reset()
        hT = UL.alloc([16, T], BF16, "hT")
        bufP = UL.alloc([8, T], BF16, "bufP")
        bufQ = UL.alloc([8, T], BF16, "bufQ")
        ysT = UL.alloc([4, T], BF16, "ysT")
        zT = UL.alloc([4, T], BF16, "zT")
        wslots["wbf"] = [UL.alloc([16, 256], BF16, "wbf%d" % i) for i in range(2)]
        wslots["stg"] = [UL.alloc([16, 256], F32, "wstg%d" % i) for i in range(2)]
        X = Stack(nc, UL.cur, SB_TOP)

        X.reset()
        xt = [X.alloc([D], F32, "xt%d" % i) for i in range(2)]
        tmp = X.alloc([D], F32, "tmpA0")
        hb = X.alloc([D], BF16, "hb")
        sc1b = X.alloc([D], F32, "sc1b")
        sh1b = X.alloc([D], F32, "sh1b")
        pet = X.alloc([D], F32, "pet")
        st = X.alloc([4, 6], F32, "st")
        mv = X.alloc([2], F32, "mv")
        rs = X.alloc([1], F32, "rs")
        P.dma("sp", sh1b, TT(mod_d.ap[l, u:u + 1, 0:D].partition_broadcast(128), mod_d.buf), g_c)
        P.dma("sp", sc1b, TT(mod_d.ap[l, u:u + 1, D:2 * D].partition_broadcast(128), mod_d.buf), g_c)
        for tt_ in range(NTT):
            rows = slice(tt_ * 128, tt_ * 128 + 128)
            x_ = xt[tt_ % 2]
            if l == 0:
                P.dma("sp", x_, xin[u][rows, :], g_x[tt_ % 2])
                if u == 1:
                    P.dma("sp", pet, pe_d[rows, :], g_x[tt_ % 2])
                    P.tt("pool", x_, x_, pet, ALU.add)
                P.dma("sp", xres[u][rows, :], x_, g_st)
            else:
                P.dma("sp", x_, xres[u][rows, :], g_x[tt_ % 2])
            ln_tm(x_, tmp, st, mv, rs)
            P.tt("dve", tmp, tmp, sc1b, ALU.mult)
            P.tt("dve", hb, tmp, sh1b, ALU.add)
            ps = ps_next()
            psb = ps.bitcast(BF16)
            for kt in range(KT):
                P.tr(psb[:, kt * 128:(kt + 1) * 128], hb[:, kt * 128:(kt + 1) * 128], ident_b)
            P.cp("act", hT[:, :, rows], psb.r("p (k t) -> p k t", k=KT))
        dbg_store("hT", hT)
        if stop == "A0":
            return None
        hT_tiles = [hT[:, kt, :] for kt in range(KT)]
        P.barrier()

        uT = bufP
        ya = bufQ

        def cons_u(ti, ps):
            P.cp("act", uT[:, ti, :], ps)
        linear_fm(hT_tiles, W["w_in"][l], 0, 0, 1024, cons_u)
        dbg_store("uT", uT)
        X.reset()
        f = {k: X.alloc([T], F32, k) for k in ("bure", "buim", "t1", "t2", "t3", "br", "bi", "gre", "gim")}
        fbh = {k: X.alloc([T], BF16, k) for k in ("greb", "gimb", "q1", "q2", "q3", "q4")}
        tbb = X.alloc([2, 256], BF16, "tbb")
        hre = X.alloc([T], BF16, "hre")
        him = X.alloc([T], BF16, "him")
        tbs = [X.alloc([2, 260], F32, "tb%d" % i) for i in range(2)]
        wbt = [X.alloc([16, 128], BF16, "wbt%d" % i) for i in range(2)]
        cwt = [X.alloc([16, 64], BF16, "cwt%d" % i) for i in range(2)]
        sm = {k: X.alloc([4], F32, k) for k in ("i_re", "i_im", "s1", "s2")}
        psA, psB, psC, psD = PS

        def v4(t_):
            return t_.r("p (c t) -> p c t", c=4)

        rts = []
        for ct in range(8):
            for d in range(2):
                for j in range(4):
                    rts.append((ct, d, j))

        state = {}

        def issue_bu(i):
            ct, d, j = rts[i]
            if d == 0 and j == 0:
                k = ct % 2
                P.dma("sp", wbt[k], wbd[ct], g_t[k])
                P.dma("sp", cwt[k], cwd[ct], g_t[k])
            rt = d * 32 + ct * 4 + j
            tb = tbs[i % 2]
            P.dma("sp", tb, tabs[rt], g_x[i % 2])
            wb_ = wbt[ct % 2]
            for reim, slot in ((0, psA), (1, psB)):
                for half in range(2):
                    hs = slice(half * 512, half * 512 + 512)
                    P.mm(slot[:, hs], wb_[:, (d * 4 + j) * 2 + reim, :], uT[:, ct, hs])

        issue_bu(0)
        for i, (ct, d, j) in enumerate(rts):
            rt = d * 32 + ct * 4 + j
            tb = tbs[i % 2]
            cw_ = cwt[ct % 2]
            Cb = tb[:, 0, 0:256].un(1).bc([128, 4, 256])
            Sb = tb[:, 1, 0:256].un(1).bc([128, 4, 256])
            for src, dst in ((psA, f["bure"]), (psB, f["buim"])):
                s3 = src.r("p (b t) -> p b t", b=nseq)
                if d == 1:
                    s3 = s3[:, :, ::-1]
                P.cp("act", dst.r("p (b t) -> p b t", b=nseq), s3)
            if i + 1 < len(rts):
                issue_bu(i + 1)
            P.tt("dve", v4(f["t1"]), v4(f["bure"]), Cb, ALU.mult)
            P.tt("dve", v4(f["t2"]), v4(f["buim"]), Sb, ALU.mult)
            P.tt("dve", f["br"], f["t1"], f["t2"], ALU.add)
            P.tt("dve", v4(f["t1"]), v4(f["buim"]), Cb, ALU.mult)
            P.tt("dve", v4(f["t2"]), v4(f["bure"]), Sb, ALU.mult)
            P.tt("dve", f["bi"], f["t1"], f["t2"], ALU.subtract)
            rcol = Rt[:, rt:rt + 1]
            for c in range(4):
                cs = slice(c * 256, c * 256 + 256)
                if u == 0:
                    ire, iim = 0.0, 0.0
                else:
                    if c == 0:
                        pre, pim = H0[:, rt, 0:1], H0[:, rt, 1:2]
                        er, ei = tb[:, 0, 1:2], tb[:, 1, 1:2]
                    else:
                        pre, pim = f["gre"][:, c * 256 - 1:c * 256], f["gim"][:, c * 256 - 1:c * 256]
                        er, ei = tb[:, 0, 256:257], tb[:, 1, 256:257]
                    P.ts("dve", sm["s1"][:, 0:1], pim, ei, ALU.mult)
                    P.stt(sm["i_re"][:, c:c + 1], pre, er, sm["s1"][:, 0:1], ALU.mult, ALU.subtract)
                    P.ts("dve", sm["s2"][:, 0:1], pim, er, ALU.mult)
                    P.stt(sm["i_im"][:, c:c + 1], pre, ei, sm["s2"][:, 0:1], ALU.mult, ALU.add)
                    ire, iim = sm["i_re"][:, c:c + 1], sm["i_im"][:, c:c + 1]
                P.scan(f["gre"][:, cs], rcol.bc([128, 256]), f["br"][:, cs], ire)
                P.scan(f["gim"][:, cs], rcol.bc([128, 256]), f["bi"][:, cs], iim)
            P.cp("act", tbb, tb[:, :, 0:256])
            P.cp("act", fbh["greb"], f["gre"])
            P.cp("act", fbh["gimb"], f["gim"])
            Cbb = tbb[:, 0, :].un(1).bc([128, 4, 256])
            Sbb = tbb[:, 1, :].un(1).bc([128, 4, 256])
            P.tt("dve", v4(fbh["q1"]), v4(fbh["greb"]), Cbb, ALU.mult)
            P.tt("dve", v4(fbh["q2"]), v4(fbh["gimb"]), Sbb, ALU.mult)
            P.tt("dve", hre, fbh["q1"], fbh["q2"], ALU.subtract)
            P.tt("dve", v4(fbh["q3"]), v4(fbh["greb"]), Sbb, ALU.mult)
            P.tt("dve", v4(fbh["q4"]), v4(fbh["gimb"]), Cbb, ALU.mult)
            P.tt("dve", him, fbh["q3"], fbh["q4"], ALU.add)
            if u == 0:
                glr = v4(f["gre"])[:, :, 255]
                gli = v4(f["gim"])[:, :, 255]
                c255, s255 = tb[:, 0, 255:256], tb[:, 1, 255:256]
                P.ts("dve", sm["s1"], gli, s255, ALU.mult)
                P.stt(NST[:, rt, :, 0], glr, c255, sm["s1"], ALU.mult, ALU.subtract)
                P.ts("dve", sm["s2"], gli, c255, ALU.mult)
                P.stt(NST[:, rt, :, 1], glr, s255, sm["s2"], ALU.mult, ALU.add)
            ysl = psC if d == 0 else psD
            jj = j // 2
            for reim, hs_ in ((0, hre), (1, him)):
                for half in range(2):
                    hs = slice(half * 512, half * 512 + 512)
                    P.mm(ysl[64 * jj:64 * jj + 64, hs], cw_[:, (d * 4 + j) * 2 + reim, :], hs_[:, hs],
                         start=(j % 2 == 0 and reim == 0), stop=(j % 2 == 1 and reim == 1))
            if d == 1 and j == 3:
                s3 = psD.r("p (b t) -> p b t", b=nseq)[:, :, ::-1]
                P.cp("act", f["t3"].r("p (b t) -> p b t", b=nseq), s3)
                P.tt("dve", f["t1"], psC, f["t3"], ALU.add)
                P.stt(f["t2"], uT[:, ct, :], DSK[:, ct:ct + 1], f["t1"], ALU.mult, ALU.add)
                P.act(ya[:, ct, :], f["t2"], AF.Gelu_apprx_tanh)
        if u == 0:
            for b_ in range(4):
                P.dma("sp", nst_o[b_, l].r("d (gp g2) p c -> (g2 p) (d gp) c", g2=2), NST[:, :, b_, :], g_st)
        dbg_store("ya", ya)
        dbg_store("nst", nst_o) if False else None
        if stop == "A1s":
            return None
        P.barrier()
        X.reset()
        sg = [X.alloc([T], F32, "sg%d" % i) for i in range(2)]
        yag = bufP
        ya_tiles = [ya[:, k, :] for k in range(8)]

        def cons_glu(ti, ps):
            s_ = sg[ti % 2]
            P.act(s_, ps, AF.Sigmoid)
            P.tt("dve", yag[:, ti, :], ya[:, ti, :], s_, ALU.mult)
        linear_fm(ya_tiles, W["s5_w_glu"][l], 0, 0, 1024, cons_glu)
        dbg_store("yag", yag)
        if stop == "A1":
            return None

        X.reset()
        fb = {k: X.alloc([T], F32, k) for k in ("cb", "cc", "prod", "acc")}
        keep = {}

        def seqv(t_):
            return t_.r("p (b t) -> p b t", b=nseq)

        def cons_sc(ti, ps):
            kind, i = divmod(ti, 4)
            if kind == 0:
                if i not in keep:
                    keep[i] = X.alloc([T], F32, "scb%d" % i)
                P.cp("act", keep[i], ps)
            elif kind == 1:
                if ("c", i) not in keep:
                    keep[("c", i)] = X.alloc([T], F32, "scc%d" % i)
                P.cp("act", keep[("c", i)], ps)
            else:
                P.tt("dve", fb["prod"], keep[("c", i)], ps, ALU.mult)
                P.ts("dve", fb["acc"], fb["prod"], SCW[:, i, 1:2], ALU.mult, SCB[:, i:i + 1], ALU.add)
                a3, p3 = seqv(fb["acc"]), seqv(fb["prod"])
                P.stt(a3[:, :, 1:SL], p3[:, :, 0:SL - 1], SCW[:, i, 0:1], a3[:, :, 1:SL], ALU.mult, ALU.add)
                P.stt(a3[:, :, 0:SL - 1], p3[:, :, 1:SL], SCW[:, i, 2:3], a3[:, :, 0:SL - 1], ALU.mult, ALU.add)
                P.tt("dve", ysT[:, i, :], keep[i], fb["acc"], ALU.mult)
        linear_fm(hT_tiles, W["w_in"][l], 0, 1024, 1536, cons_sc)
        dbg_store("ys", ysT)
        if stop == "A2":
            return None
        P.barrier()

        X.reset()
        ga = [X.alloc([T], F32, "ga%d" % i) for i in range(4)]
        cv = [X.alloc([T], F32, "cv%d" % i) for i in range(4)]
        fc = {k: X.alloc([T], F32, k) for k in ("sq", "rstd", "mr")}

        def cons_cf(ti, ps):
            kind, i = divmod(ti, 4)
            if kind == 0:
                P.cp("act", ga[i], ps)
            else:
                P.act(fc["sq"], ps, AF.Sigmoid)
                P.tt("dve", ga[i], ga[i], fc["sq"], ALU.mult)
                P.ts("dve", cv[i], ga[i], CFW[:, i, 15:16], ALU.mult, CFB[:, i:i + 1], ALU.add)
                a3, g3 = seqv(cv[i]), seqv(ga[i])
                for k in range(31):
                    sh = k - 15
                    if sh == 0:
                        continue
                    if sh > 0:
                        P.stt(a3[:, :, 0:SL - sh], g3[:, :, sh:SL], CFW[:, i, k:k + 1], a3[:, :, 0:SL - sh], ALU.mult, ALU.add)
                    else:
                        P.stt(a3[:, :, -sh:SL], g3[:, :, 0:SL + sh], CFW[:, i, k:k + 1], a3[:, :, -sh:SL], ALU.mult, ALU.add)
        linear_fm(hT_tiles, W["w_in"][l], 0, 2560, 1024, cons_cf)
        dbg_store("glu0", ga[0])
        dbg_store("cv0", cv[0])
        psM, psQ = PS[0], PS[1]
        for i in range(4):
            for half in range(2):
                hs = slice(half * 512, half * 512 + 512)
                P.mm(psM[:, hs], ones_f, cv[i][:, hs], start=(i == 0), stop=(i == 3))
        for i in range(4):
            P.act(ga[i], cv[i], AF.Square)
        for i in range(4):
            for half in range(2):
                hs = slice(half * 512, half * 512 + 512)
                P.mm(psQ[:, hs], ones_f, ga[i][:, hs], start=(i == 0), stop=(i == 3))
        P.cp("act", fc["mr"], psM)
        P.tt("dve", fc["sq"], fc["mr"], fc["mr"], ALU.mult)
        P.tt("dve", fc["rstd"], psQ, fc["sq"], ALU.subtract)
        P.ts("dve", fc["rstd"], fc["rstd"], LN_EPS, ALU.add)
        P.act(fc["rstd"], fc["rstd"], AF.Sqrt)
        P.recip(fc["rstd"], fc["rstd"])
        dbg_store("rstd", fc["rstd"])
        dbg_store("mean", psM) if False else None
        P.tt("dve", fc["mr"], fc["mr"], fc["rstd"], ALU.mult)
        for i in range(4):
            P.tt("dve", ga[i], cv[i], fc["rstd"], ALU.mult)
            P.tt("dve", ga[i], ga[i], fc["mr"], ALU.subtract)
            P.ts("dve", ga[i], ga[i], CFG[:, i:i + 1], ALU.mult, CFBB[:, i:i + 1], ALU.add)
            P.act(zT[:, i, :], ga[i], AF.Silu)
        dbg_store("z", zT)
        if stop == "A3":
            return None
        P.barrier()

        X.reset()
        mT = X.alloc([16, T], BF16, "mT")
        sgm = [X.alloc([T], F32, "sgm%d" % i) for i in range(2)]
        macc = [X.alloc([T], F32, "macc%d" % i) for i in range(2)]
        tmpm = X.alloc([T], F32, "tmpm")
        yag_t = [bufP[:, k, :] for k in range(8)]
        ys_t = [ysT[:, k, :] for k in range(4)]
        z_t = [zT[:, k, :] for k in range(4)]
        branches = [(yag_t, W["w_pa"][l], 0), (ys_t, W["w_pb"][l], 1), (z_t, W["w_pc"][l], 2)]
        for pair in range(8):
            cbase = pair * 256
            for bi, (tiles, Wp, gidx) in enumerate(branches):
                nk = len(tiles)
                wb = load_w(Wp, 0, nk, cbase, 256)
                pp = []
                for ti in range(2):
                    ps = ps_next()
                    for half in range(2):
                        hs = slice(half * 512, half * 512 + 512)
                        for kt in range(nk):
                            P.mm(ps[:, hs], wb[:, kt, ti * 128:(ti + 1) * 128], tiles[kt][:, hs], start=(kt == 0), stop=(kt == nk - 1))
                    pp.append(ps)
                wg = load_w(W["w_in"][l], 0, 16, GATE0 + gidx * D + cbase, 256)
                for ti in range(2):
                    ps = ps_next()
                    for half in range(2):
                        hs = slice(half * 512, half * 512 + 512)
                        for kt in range(16):
                            P.mm(ps[:, hs], wg[:, kt, ti * 128:(ti + 1) * 128], hT_tiles[kt][:, hs], start=(kt == 0), stop=(kt == 15))
                    P.act(sgm[ti], ps, AF.Sigmoid)
                    if bi == 0:
                        P.tt("dve", macc[ti], sgm[ti], pp[ti], ALU.mult)
                    elif bi == 1:
                        P.tt("dve", tmpm, sgm[ti], pp[ti], ALU.mult)
                        P.tt("pool", macc[ti], macc[ti], tmpm, ALU.add)
                    else:
                        P.tt("dve", tmpm, sgm[ti], pp[ti], ALU.mult)
                        P.tt("pool", mT[:, pair * 2 + ti, :], macc[ti], tmpm, ALU.add)
        dbg_store("mT", mT)
        if stop == "A4":
            return None
        ost = [X.alloc([256], F32, "ost%d" % i) for i in range(2)]
        it = 0
        for cc in range(8):
            wb = load_w(W["w_o"][l], 0, 16, cc * 256, 256)
            for tt_ in range(NTT):
                ps = ps_next()
                for kt in range(16):
                    P.mm(ps[:, 0:256], mT[:, kt, tt_ * 128:(tt_ + 1) * 128], wb[:, kt, :], start=(kt == 0), stop=(kt == 15))
                o_ = ost[it % 2]
                it += 1
                P.cp("act", o_, ps[:, 0:256])
                P.dma("sp", ymix[tt_ * 128:(tt_ + 1) * 128, cc * 256:(cc + 1) * 256], o_, g_st)
        P.barrier()
        return None

    def phase_B(l, u, last):
        UL.reset()
        h2T = UL.alloc([16, T], BF16, "h2T")
        Zbase = UL.cur
        qT = UL.alloc([16, T], BF16, "qT")
        Y3base = UL.cur
        wslots["wbf"] = [UL.alloc([16, 256], BF16, "wbfB%d" % i) for i in range(2)]
        Y = Stack(nc, UL.cur, SB_TOP)
        Y.reset()
        wslots["stg"] = None
        xt = [Y.alloc([D], F32, "xtB%d" % i) for i in range(2)]
        yt = [Y.alloc([D], F32, "ytB%d" % i) for i in range(2)]
        t1 = Y.alloc([D], F32, "t1B")
        hb = Y.alloc([D], BF16, "hbB")
        bct = {k: Y.alloc([D], F32, k) for k in ("gt1", "g1", "b1", "sc2", "sh2")}
        st = Y.alloc([4, 6], F32, "stB")
        mv = Y.alloc([2], F32, "mvB")
        rs = Y.alloc([1], F32, "rsB")

        def bload(dst, src2d, row, c0):
            P.dma("sp", dst, TT(src2d.ap[row:row + 1, c0:c0 + D].partition_broadcast(128), src2d.buf), g_c)
        bload(bct["gt1"], mod_d[l], u, 2 * D)
        bload(bct["sc2"], mod_d[l], u, 4 * D)
        bload(bct["sh2"], mod_d[l], u, 3 * D)
        bload(bct["g1"], W["ln1_g"], l, 0)
        bload(bct["b1"], W["ln1_b"], l, 0)
        for tt_ in range(NTT):
            rows = slice(tt_ * 128, tt_ * 128 + 128)
            x_, y_ = xt[tt_ % 2], yt[tt_ % 2]
            P.dma("sp", x_, xres[u][rows, :], g_x[tt_ % 2])
            P.dma("sp", y_, ymix[rows, :], g_x[tt_ % 2])
            P.tt("pool", y_, y_, bct["gt1"], ALU.mult)
            P.stt(y_, x_, ALPHA, y_, ALU.mult, ALU.add)
            ln_tm(y_, t1, st, mv, rs)
            P.tt("pool", t1, t1, bct["g1"], ALU.mult)
            P.tt("dve", x_, t1, bct["b1"], ALU.add)
            P.dma("sp", x1res[rows, :], x_, g_st)
            if tt_ == 0:
                dbg_store("x1", x_)
            ln_tm(x_, t1, st, mv, rs)
            P.tt("pool", t1, t1, bct["sc2"], ALU.mult)
            P.tt("dve", y_, t1, bct["sh2"], ALU.add)
            if tt_ == 0:
                dbg_store("h2", y_)
            P.cp("pool", hb, y_)
            ps = ps_next()
            psb = ps.bitcast(BF16)
            for kt in range(KT):
                P.tr(psb[:, kt * 128:(kt + 1) * 128], hb[:, kt * 128:(kt + 1) * 128], ident_b)
            P.cp("act", h2T[:, :, rows], psb.r("p (k t) -> p k t", k=KT))
        if stop == "B1":
            return
        P.barrier()
        Y.reset()
        wslots["stg"] = [Y.alloc([16, 256], F32, "wstgB%d" % i) for i in range(2)]
        h2T_t = [h2T[:, k, :] for k in range(KT)]

        def cons_q(ti, ps):
            P.cp("act", qT[:, ti, :], ps)
        linear_fm(h2T_t, W["peer_w_q"][l], 0, 0, D, cons_q)
        P.barrier()
        Y = Stack(nc, Y3base, SB_TOP)
        S = Y.alloc([16, 128], F32, "S")
        S2 = Y.alloc([256], F32, "S2")
        V = Y.alloc([16, 16], F32, "V")
        Iu = Y.alloc([16, 16], U32, "Iu")
        If = Y.alloc([16, 16], F32, "If")
        cand = Y.alloc([8, 256], F32, "cand")
        top = Y.alloc([8, 16], F32, "top")
        pos = Y.alloc([8, 16], U32, "pos")
        pa_ = Y.alloc([8, 16], U32, "pa")
        pb_ = Y.alloc([8, 16], U32, "pb")
        af = Y.alloc([8, 16], F32, "af")
        bf = Y.alloc([8, 16], F32, "bf")
        eq = Y.alloc([8, 16, 16], F32, "eq")
        i1s = Y.alloc([8, 16], F32, "i1s")
        i2s = Y.alloc([8, 16], F32, "i2s")
        gw = Y.alloc([8, 16], F32, "gw")
        gsum = Y.alloc([8], F32, "gsum")
        i1T = Y.alloc([128], F32, "i1T")
        i2T = Y.alloc([128], F32, "i2T")
        gT = Y.alloc([128], F32, "gT")
        Pm = Y.alloc([128, 128], BF16, "Pm")
        Qm = Y.alloc([128, 128], BF16, "Qm")
        Gst = Y.alloc([128, 128], BF16, "Gst")
        for tt_ in range(b3_tiles):
            rows = slice(tt_ * 128, tt_ * 128 + 128)
            for q4 in range(4):
                ps = ps_next()
                for m in range(4):
                    hh = q4 * 4 + m
                    P.mm(ps[:, m * 128:(m + 1) * 128], qT[:, hh, rows], KTb[:, hh % 2, :])
                P.cp("act", S[:, q4 * 4:q4 * 4 + 4, :], ps[:, 0:512].r("p (a n) -> p a n", a=4))
            for hh in range(16):
                P.op("dve", lambda E, hh=hh: E.max(out=V.ap[:, hh, 0:8], in_=S.ap[:, hh, :]), r=[S.buf], w=[V.buf])
                P.op("dve", lambda E, hh=hh: E.max_index(out=Iu.ap[:, hh, 0:8], in_max=V.ap[:, hh, 0:8], in_values=S.ap[:, hh, :]),
                     r=[S.buf, V.buf], w=[Iu.buf])
                P.op("dve", lambda E, hh=hh: E.match_replace(out=S2.ap[:, 0:128], in_to_replace=V.ap[:, hh, 0:8],
                                                             in_values=S.ap[:, hh, :], imm_value=NEG), r=[S.buf, V.buf], w=[S2.buf])
                P.op("dve", lambda E, hh=hh: E.max(out=V.ap[:, hh, 8:16], in_=S2.ap[:, 0:128]), r=[S2.buf], w=[V.buf])
                P.op("dve", lambda E, hh=hh: E.max_index(out=Iu.ap[:, hh, 8:16], in_max=V.ap[:, hh, 8:16], in_values=S2.ap[:, 0:128]),
                     r=[S2.buf, V.buf], w=[Iu.buf])
            P.cp("dve", If, Iu)
            V4 = V.r("p (h f) a -> p h f a", f=2)
            I4 = If.r("p (h f) a -> p h f a", f=2)
            c4 = cand.r("p h (a b) -> p h a b", a=16)
            P.tt("dve", c4, V4[:, :, 0, :].un(3).bc([128, 8, 16, 16]), V4[:, :, 1, :].un(2).bc([128, 8, 16, 16]), ALU.add)
            for h in range(8):
                P.op("dve", lambda E, h=h: E.max(out=top.ap[:, h, 0:8], in_=cand.ap[:, h, :]), r=[cand.buf], w=[top.buf])
                P.op("dve", lambda E, h=h: E.max_index(out=pos.ap[:, h, 0:8], in_max=top.ap[:, h, 0:8], in_values=cand.ap[:, h, :]),
                     r=[cand.buf, top.buf], w=[pos.buf])
                P.op("dve", lambda E, h=h: E.match_replace(out=S2.ap, in_to_replace=top.ap[:, h, 0:8], in_values=cand.ap[:, h, :],
                                                           imm_value=NEG), r=[cand.buf, top.buf], w=[S2.buf])
                P.op("dve", lambda E, h=h: E.max(out=top.ap[:, h, 8:16], in_=S2.ap), r=[S2.buf], w=[top.buf])
                P.op("dve", lambda E, h=h: E.max_index(out=pos.ap[:, h, 8:16], in_max=top.ap[:, h, 8:16], in_values=S2.ap),
                     r=[S2.buf, top.buf], w=[pos.buf])
            P.op("dve", lambda E: E.tensor_single_scalar(out=pa_.ap, in_=pos.ap, scalar=4, op=ALU.logical_shift_right),
                 r=[pos.buf], w=[pa_.buf])
            P.op("dve", lambda E: E.tensor_single_scalar(out=pb_.ap, in_=pos.ap, scalar=15, op=ALU.bitwise_and),
                 r=[pos.buf], w=[pb_.buf])
            P.cp("dve", af, pa_)
            P.cp("dve", bf, pb_)
            io4 = iota16.un(1).un(1).bc([128, 8, 16, 16])
            P.tt("dve", eq, af.un(3).bc([128, 8, 16, 16]), io4, ALU.is_equal)
            P.tt("dve", eq, eq, I4[:, :, 0, :].un(2).bc([128, 8, 16, 16]), ALU.mult)
            P.reduce(i1s, eq, ALU.add)
            P.tt("dve", eq, bf.un(3).bc([128, 8, 16, 16]), io4, ALU.is_equal)
            P.tt("dve", eq, eq, I4[:, :, 1, :].un(2).bc([128, 8, 16, 16]), ALU.mult)
            P.reduce(i2s, eq, ALU.add)
            P.tt("dve", gw, top, top[:, :, 0:1].bc([128, 8, 16]), ALU.subtract)
            P.act(gw, gw, AF.Exp)
            P.reduce(gsum, gw, ALU.add)
            P.recip(gsum, gsum)
            P.tt("dve", gw, gw, gsum.un(2).bc([128, 8, 16]), ALU.mult)
            for src, dst in ((i1s, i1T), (i2s, i2T), (gw, gT)):
                ps = ps_next()
                P.tr(ps[:, 0:128], src.r("p h k -> p (h k)"), ident_f)
                P.cp("act", dst, ps[:, 0:128])
            io3 = iota128.un(1).bc([128, 128, 128])
            P.tt("dve", Pm, io3, i1T.un(2).bc([128, 128, 128]), ALU.is_equal)
            P.tt("dve", Qm, io3, i2T.un(2).bc([128, 128, 128]), ALU.is_equal)
            P.tt("dve", Qm, Qm, gT.un(2).bc([128, 128, 128]), ALU.mult)
            for t8 in range(16):
                ps = ps_next()
                for k in range(8):
                    t_ = t8 * 8 + k
                    P.mm(ps[:, k * 128:(k + 1) * 128], Pm[:, t_, :], Qm[:, t_, :])
                P.cp("act", Gst[:, :, t8 * 8:(t8 + 1) * 8].r("p i t -> p t i"), ps.r("p (t i) -> p t i", t=8))
            P.dma("sp", Gd[:, :, rows], Gst)
        P.barrier()
        Z = Stack(nc, Zbase, SB_TOP)
        ACC = Z.alloc([NTT, D], F32, "ACC")
        Z2 = Stack(nc, Z.cur, SB_TOP)
        JG = 4
        ustg = [Z2.alloc([D], F32, "ustg%d" % i) for i in range(3)]
        vstg = [Z2.alloc([D], F32, "vstg%d" % i) for i in range(2)]
        ubf = [Z2.alloc([D], BF16, "ubf%d" % i) for i in range(2)]
        uTt = [Z2.alloc([16, 128], BF16, "uTt%d" % i) for i in range(3)]
        vbf = [Z2.alloc([D], BF16, "vbf%d" % b) for b in range(JG)]
        Wt = [Z2.alloc([T], BF16, "Wt%d" % b) for b in range(JG)]
        Gt = [Z2.alloc([T], BF16, "Gt%d" % i) for i in range(2)]
        ge = [Z2.alloc([T], BF16, "ge%d" % i) for i in range(2)]
        psT = PS[0]
        psA = [PS[1], PS[2]]
        psO = [TT(PS[3].ap[:, 0:512], Buf("psO0", "ps")), TT(PS[3].ap[:, 512:1024], Buf("psO1", "ps"))]
        urows = W["peer_u"][l].r("(i j) d -> j i d", j=128)
        vrows = W["peer_v"][l].r("(i j) d -> j i d", j=128)
        NJ = 128

        def load_cast(j):
            s_u = ustg[j % 3]
            P.dma("sp", s_u, urows[j])
            P.cp("dve", ubf[j % 2], s_u)

        def trans(j):
            psb = psT.bitcast(BF16).r("p (k i) -> p k i", k=16)
            ub = ubf[j % 2]
            for kt in range(16):
                P.tr(psb[:, kt, :], ub[:, kt * 128:(kt + 1) * 128], ident_b)

        def evac(j):
            psb = psT.bitcast(BF16).r("p (k i) -> p k i", k=16)
            P.cp("act", uTt[j % 3], psb)

        load_cast(0)
        trans(0)
        evac(0)
        load_cast(1)
        trans(1)
        evac(1)
        load_cast(2)
        P.dma("sp", Gt[0], Gd[:, 0, :])
        for j in range(NJ):
            jj = j % JG
            if j + 3 < NJ:
                load_cast(j + 3)
            s_v = vstg[j % 2]
            P.dma("sp", s_v, vrows[j])
            P.cp("act", vbf[jj], s_v)
            if j + 1 < NJ:
                P.dma("sp", Gt[(j + 1) % 2], Gd[:, j + 1, :])
            pa2 = psA[j % 2]
            ut = uTt[j % 3]
            for half in range(2):
                hs = slice(half * 512, half * 512 + 512)
                for kt in range(16):
                    P.mm(pa2[:, hs], ut[:, kt, :], h2T[:, kt, hs], start=(kt == 0), stop=(kt == 15))
            if j + 2 < NJ:
                trans(j + 2)
            e_ = ge[j % 2]
            P.act(e_, pa2, AF.Gelu_apprx_tanh)
            if j + 2 < NJ:
                evac(j + 2)
            P.tt("dve", Wt[jj], e_, Gt[j % 2], ALU.mult)
            if jj == JG - 1:
                first = (j == JG - 1)
                for tt_ in range(NTT):
                    for dc in range(4):
                        po = psO[(tt_ * 4 + dc) % 2]
                        for q_ in range(JG):
                            P.mm(po, Wt[q_][:, tt_ * 128:(tt_ + 1) * 128], vbf[q_][:, dc * 512:(dc + 1) * 512],
                                 start=(q_ == 0), stop=(q_ == JG - 1))
                        dst = ACC[:, tt_, dc * 512:(dc + 1) * 512]
                        if first:
                            P.cp("act", dst, po)
                        else:
                            P.tt("dve", dst, dst, po, ALU.add)
        P.barrier()
        Z2.reset()
        x1 = [Z2.alloc([D], F32, "x1_%d" % i) for i in range(2)]
        tq2 = Z2.alloc([D], F32, "tq2")
        bc3 = {k: Z2.alloc([D], F32, k) for k in ("gt2", "g2", "b2")}
        st = Z2.alloc([4, 6], F32, "stC")
        mv = Z2.alloc([2], F32, "mvC")
        rs = Z2.alloc([1], F32, "rsC")
        bload(bc3["gt2"], mod_d[l], u, 5 * D)
        bload(bc3["g2"], W["ln2_g"], l, 0)
        bload(bc3["b2"], W["ln2_b"], l, 0)
        for tt_ in range(NTT):
            rows = slice(tt_ * 128, tt_ * 128 + 128)
            x1_ = x1[tt_ % 2]
            acc = ACC[:, tt_, :]
            P.dma("sp", x1_, x1res[rows, :], g_x[0])
            if tt_ == 0:
                dbg_store("peer", acc)
            P.tt("pool", acc, acc, bc3["gt2"], ALU.mult)
            P.stt(acc, x1_, ALPHA, acc, ALU.mult, ALU.add)
            ln_tm(acc, tq2, st, mv, rs)
            P.tt("pool", tq2, tq2, bc3["g2"], ALU.mult)
            P.tt("dve", x1_, tq2, bc3["b2"], ALU.add)
            if last:
                P.dma("sp", yout[u][rows, :], x1_, g_st)
            else:
                P.dma("sp", xres[u][rows, :], x1_, g_st)
        P.barrier()

    phase_mod()
    ret = None
    for l in range(nlayers):
        phase_setup(l)
        if stop == "setup":
            break
        for u in units:
            ret = phase_A(l, u)
            if stop is not None and stop.startswith("A"):
                break
            phase_B(l, u, last=(l == nlayers - 1))
            if stop is not None:
                break
        if stop is not None:
            break
    if ret is not None and "ret" in dbg_out:
        src = ret
        P.dma("sp", dbg_out["ret"], src, g_st)
    P.final_wait()
    P.emit()
    return nc


def _grid_pos():
    rows = T // 64
    t = np.arange(rows * 64)
    r = (t // 64).astype(np.float32)
    col = (t % 64).astype(np.float32)
    nf = D // 4
    freq = (1.0 / (np.float32(10000.0) ** (np.arange(nf, dtype=np.float32) / np.float32(nf)))).astype(np.float32)
    ar = r[:, None] * freq
    ac = col[:, None] * freq
    return np.concatenate([np.sin(ar), np.cos(ar), np.sin(ac), np.cos(ac)], -1).astype(np.float32)


WEIGHT_NAMES = ["w_mod", "b_mod", "w_in", "s5_lam_re", "s5_lam_im", "s5_log_dt", "s5_b_re", "s5_b_im", "s5_c_re",
                "s5_c_im", "s5_d", "s5_w_glu", "sc_conv_w", "sc_conv_b", "cf_conv_w", "cf_conv_b", "cf_ln_g", "cf_ln_b",
                "w_pa", "w_pb", "w_pc", "w_o", "ln1_g", "ln1_b", "peer_w_q", "peer_k1", "peer_k2", "peer_u", "peer_v",
                "ln2_g", "ln2_b"]


def make_in_maps(inputs, cores=range(NCORE)):
    f = lambda a: np.ascontiguousarray(np.asarray(a, dtype=np.float32))
    xp = f(inputs["x_prompt"])
    xs = f(inputs["x_sample"])
    stt = f(inputs["state_ssm"])
    c = f(inputs["c"])
    cc = f(inputs["c_ctx"])
    pe = _grid_pos()
    ident = np.eye(128, dtype=np.float32)
    iota = np.tile(np.arange(128, dtype=np.float32)[None, :], (128, 1))
    wts = {k: f(inputs[k]) for k in WEIGHT_NAMES}
    maps = []
    for i in cores:
        m = dict(wts)
        m["xp"] = np.ascontiguousarray(xp[4 * i:4 * i + 4].reshape(T, D))
        m["xs"] = np.ascontiguousarray(xs[i].reshape(T, D))
        m["pe"] = pe
        m["st0"] = np.ascontiguousarray(stt[i])
        m["cond"] = np.ascontiguousarray(np.stack([cc, c[i]], 0))
        m["ident"] = ident
        m["iota128"] = iota
        maps.append(m)
    return maps


def kernel(**inputs):
    nc = build()
    maps = make_in_maps(inputs)
    maps = [{k: m[k] for k in build.declared} for m in maps]
    res = run_bass_kernel_spmd(nc, maps, core_ids=list(range(NCORE)))
    yp = np.concatenate([r["yp"].reshape(4, 256, D) for r in res.results], 0).astype(np.float32)
    ys = np.stack([r["ys"].reshape(T, D) for r in res.results], 0).astype(np.float32)
    nst = np.concatenate([r["nst"] for r in res.results], 0).astype(np.float32)
    return (yp, ys, nst)
```

```python
import math
from contextlib import ExitStack

import numpy as np
import concourse.bass as bass
import concourse.mybir as mybir
from concourse.bass_utils import run_bass_kernel_spmd

F32 = mybir.dt.float32
BF16 = mybir.dt.bfloat16
I32 = mybir.dt.int32
U32 = mybir.dt.uint32
ALU = mybir.AluOpType
AF = mybir.ActivationFunctionType
AX = mybir.AxisListType

D = 2048
NCORE = 8
T = 1024
NTT = 8
KT = 16
NL = 2
ALPHA = 4.0 ** 0.25
LN_EPS = 1e-6
IN_COLS = 9728
GATE0 = 3584
NEG = -1.0e30


class Buf:
    __slots__ = ("name", "lw", "rd", "space", "dsem")

    def __init__(self, name, space="sb"):
        self.name = name
        self.lw = {}
        self.rd = {}
        self.space = space
        self.dsem = None


class Grp:
    __slots__ = ("sem", "cnt")

    def __init__(self, sem):
        self.sem = sem
        self.cnt = 0


class TT:
    __slots__ = ("ap", "buf")

    def __init__(self, ap, buf):
        self.ap = ap
        self.buf = buf

    def __getitem__(self, k):
        return TT(self.ap[k], self.buf)

    def r(self, pat, **kw):
        return TT(self.ap.rearrange(pat, **kw), self.buf)

    def bc(self, shape):
        return TT(self.ap.to_broadcast(list(shape)), self.buf)

    def un(self, ax):
        return TT(self.ap.unsqueeze(ax), self.buf)

    def bitcast(self, dt):
        return TT(self.ap.bitcast(dt), self.buf)


def _a(x):
    return x.ap if isinstance(x, TT) else x


class Prog:
    def __init__(self, nc, es):
        self.nc = nc
        self.es = es
        self.E = dict(pe=nc.tensor, dve=nc.vector, act=nc.scalar, pool=nc.gpsimd, sp=nc.sync)
        self.rec = {k: [] for k in self.E}
        self.esem = {k: es.enter_context(nc.semaphore("es_" + k)) for k in self.E}
        self.ecnt = {k: 0 for k in self.E}
        self.known = {k: {} for k in self.E}
        self.grps = []
        self.free = []
        self.active = []
        self.max_dsem = 84

    def grp(self, name):
        g = Grp(self.es.enter_context(self.nc.semaphore(name)))
        self.grps.append(g)
        return g

    def dsem_for(self, buf):
        if buf.dsem is None:
            if self.free:
                buf.dsem = self.free.pop()
            else:
                assert len(self.grps) < self.max_dsem, "out of DMA semaphores"
                buf.dsem = self.grp("dq%d" % len(self.grps))
            self.active.append(buf)
        return buf.dsem

    def op(self, e, fn, r=(), w=(), grp=None):
        need = {}
        own = self.esem[e]

        def add(tok):
            sem, val = tok
            if e == "pe" and sem is own:
                return
            k = id(sem)
            if k not in need or need[k][1] < val:
                need[k] = (sem, val)

        for b in list(r) + list(w):
            for tok in b.lw.values():
                add(tok)
        for b in w:
            for tok in b.rd.values():
                add(tok)
        waits = []
        kn = self.known[e]
        for k, (sem, val) in need.items():
            if kn.get(k, 0) >= val:
                continue
            kn[k] = val
            waits.append((sem, val))
        if grp is not None:
            grp.cnt += 16
            tok = (grp.sem, grp.cnt)
            inc = (grp.sem, 16)
        else:
            self.ecnt[e] += 1
            tok = (own, self.ecnt[e])
            inc = (own, 1)
        for b in w:
            if b.space == "dram":
                b.lw[id(tok[0])] = tok
            else:
                b.lw = {id(tok[0]): tok}
            b.rd = {}
        for b in r:
            if b in w:
                continue
            b.rd[id(tok[0])] = tok
        self.rec[e].append((waits, fn, inc))

    def barrier(self):
        toks = [(self.esem[k], self.ecnt[k]) for k in self.E] + [(g.sem, g.cnt) for g in self.grps]
        for e in self.E:
            waits = []
            kn = self.known[e]
            for sem, val in toks:
                if val == 0 or sem is self.esem[e]:
                    continue
                if kn.get(id(sem), 0) >= val:
                    continue
                kn[id(sem)] = val
                waits.append((sem, val))
            if waits:
                self.rec[e].append((waits, None, None))
        for b in self.active:
            self.free.append(b.dsem)
            b.dsem = None
        self.active = []

    def final_wait(self):
        e = "sp"
        waits = [(self.esem[k], self.ecnt[k]) for k in self.E if k != e and self.ecnt[k]]
        waits += [(g.sem, g.cnt) for g in self.grps if g.cnt]
        self.rec[e].append((waits, None, None))

    def emit(self):
        with self.nc.Block() as blk:
            def mk(e):
                def body(Eng):
                    for waits, fn, inc in self.rec[e]:
                        for sem, val in waits:
                            Eng.wait_ge(sem, val)
                        if fn is not None:
                            fn(Eng).then_inc(inc[0], inc[1])
                return body
            blk.tensor(mk("pe"))
            blk.vector(mk("dve"))
            blk.scalar(mk("act"))
            blk.gpsimd(mk("pool"))
            blk.sync(mk("sp"))

    def dma(self, q, out, in_, grp=None, **kw):
        if out.buf.space == "sb":
            g = self.dsem_for(out.buf)
        elif in_.buf.space == "sb":
            g = self.dsem_for(in_.buf)
        else:
            g = self.dsem_for(out.buf)
        self.op(q, lambda E: E.dma_start(out=out.ap, in_=in_.ap, **kw), r=[in_.buf], w=[out.buf], grp=g)

    def mm(self, out, lhsT, rhs, start=True, stop=True):
        self.op("pe", lambda E: E.matmul(out=out.ap, lhsT=lhsT.ap, rhs=rhs.ap, start=start, stop=stop),
                r=[lhsT.buf, rhs.buf], w=[out.buf])

    def tr(self, out, in_, ident):
        self.op("pe", lambda E: E.transpose(out=out.ap, in_=in_.ap, identity=ident.ap),
                r=[in_.buf, ident.buf], w=[out.buf])

    def act(self, out, in_, func, bias=None, scale=None, e="act"):
        rb = [in_.buf]
        kw = {}
        if bias is not None:
            kw["bias"] = _a(bias)
            if isinstance(bias, TT):
                rb.append(bias.buf)
        if scale is not None:
            kw["scale"] = _a(scale)
            if isinstance(scale, TT):
                rb.append(scale.buf)
        self.op("act", lambda E: E.activation(out=out.ap, in_=in_.ap, func=func, **kw), r=rb, w=[out.buf])

    def cp(self, e, out, in_):
        if e == "act":
            self.op("act", lambda E: E.copy(out=out.ap, in_=in_.ap), r=[in_.buf], w=[out.buf])
        else:
            self.op(e, lambda E: E.tensor_copy(out=out.ap, in_=in_.ap), r=[in_.buf], w=[out.buf])

    def tt(self, e, out, a, b, op):
        self.op(e, lambda E: E.tensor_tensor(out=out.ap, in0=a.ap, in1=b.ap, op=op), r=[a.buf, b.buf], w=[out.buf])

    def ts(self, e, out, a, s1, op0, s2=None, op1=None):
        rb = [a.buf] + [x.buf for x in (s1, s2) if isinstance(x, TT)]
        if op1 is None:
            self.op(e, lambda E: E.tensor_scalar(out=out.ap, in0=a.ap, scalar1=_a(s1), scalar2=None, op0=op0),
                    r=rb, w=[out.buf])
        else:
            self.op(e, lambda E: E.tensor_scalar(out=out.ap, in0=a.ap, scalar1=_a(s1), scalar2=_a(s2), op0=op0, op1=op1),
                    r=rb, w=[out.buf])

    def stt(self, out, a, s, b, op0, op1, e="dve"):
        rb = [a.buf, b.buf] + ([s.buf] if isinstance(s, TT) else [])
        self.op(e, lambda E: E.scalar_tensor_tensor(out=out.ap, in0=a.ap, scalar=_a(s), in1=b.ap, op0=op0, op1=op1),
                r=rb, w=[out.buf])

    def memset(self, e, out, val):
        self.op(e, lambda E: E.memset(out.ap, val), w=[out.buf])

    def recip(self, out, in_):
        self.op("dve", lambda E: E.reciprocal(out=out.ap, in_=in_.ap), r=[in_.buf], w=[out.buf])

    def scan(self, out, d0, d1, init):
        rb = [d0.buf, d1.buf] + ([init.buf] if isinstance(init, TT) else [])
        self.op("dve", lambda E: E.tensor_tensor_scan(out=out.ap, data0=d0.ap, data1=d1.ap, initial=_a(init),
                                                       op0=ALU.mult, op1=ALU.add), r=rb, w=[out.buf])

    def reduce(self, out, in_, op, axis=AX.X):
        self.op("dve", lambda E: E.tensor_reduce(out=out.ap, in_=in_.ap, axis=axis, op=op), r=[in_.buf], w=[out.buf])


class Stack:
    cnt = [0]

    def __init__(self, nc, base, limit):
        self.nc = nc
        self.base = base
        self.cur = base
        self.limit = limit

    def alloc(self, free_shape, dtype, name):
        nb = int(np.prod(free_shape)) * (2 if dtype == BF16 else 4)
        off = (self.cur + 31) // 32 * 32
        assert off + nb <= self.limit, (name, off, nb, self.limit)
        self.cur = off + nb
        Stack.cnt[0] += 1
        h = self.nc.alloc_sbuf_tensor_at("%s_%d" % (name, Stack.cnt[0]), [128] + list(free_shape), dtype, offset=off)
        return TT(h.ap(), Buf(name))

    def sub(self, nbytes):
        off = (self.cur + 31) // 32 * 32
        assert off + nbytes <= self.limit, (off, nbytes, self.limit)
        self.cur = off + nbytes
        return Stack(self.nc, off, off + nbytes)

    def reset(self):
        self.cur = self.base


def build(dbg=None, stop=None, nlayers=NL, units=(0, 1), b3_tiles=NTT):
    dbg = dbg or {}
    Stack.cnt[0] = 0
    nc = bass.Bass("TRN2", target_bir_lowering=False)
    es = ExitStack()
    P = Prog(nc, es)

    declared = []
    build.declared = declared

    def din(name, shape, dt=F32):
        declared.append(name)
        return TT(nc.dram_tensor(name, list(shape), dt, kind="ExternalInput").ap(), Buf(name, "dram"))

    def dout(name, shape, dt=F32):
        return TT(nc.dram_tensor(name, list(shape), dt, kind="ExternalOutput").ap(), Buf(name, "dram"))

    def dscr(name, shape, dt=F32):
        return TT(nc.dram_tensor(name, list(shape), dt, kind="Internal").ap(), Buf(name, "dram"))

    xin = [din("xp", [T, D]), din("xs", [T, D])]
    pe_d = din("pe", [T, D])
    st0 = din("st0", [NL, 2, 64, 64, 2])
    cond = din("cond", [2, D])
    ident_d = din("ident", [128, 128])
    iota_d = din("iota128", [128, 128])
    WSH = dict([("w_mod", [NL, D, 6 * D]), ("b_mod", [NL, 6 * D]), ("w_in", [NL, D, IN_COLS]),
                ("s5_lam_re", [NL, 2, 64, 64]), ("s5_lam_im", [NL, 2, 64, 64]), ("s5_log_dt", [NL, 2, 64]),
                ("s5_b_re", [NL, 2, 64, 64, 16]), ("s5_b_im", [NL, 2, 64, 64, 16]),
                ("s5_c_re", [NL, 2, 64, 16, 64]), ("s5_c_im", [NL, 2, 64, 16, 64]),
                ("s5_d", [NL, 1024]), ("s5_w_glu", [NL, 1024, 1024]),
                ("sc_conv_w", [NL, 3, 512]), ("sc_conv_b", [NL, 512]),
                ("cf_conv_w", [NL, 31, 512]), ("cf_conv_b", [NL, 512]),
                ("cf_ln_g", [NL, 512]), ("cf_ln_b", [NL, 512]),
                ("w_pa", [NL, 1024, D]), ("w_pb", [NL, 512, D]), ("w_pc", [NL, 512, D]), ("w_o", [NL, D, D]),
                ("ln1_g", [NL, D]), ("ln1_b", [NL, D]),
                ("peer_w_q", [NL, D, D]), ("peer_k1", [NL, 128, 128]), ("peer_k2", [NL, 128, 128]),
                ("peer_u", [NL, 16384, D]), ("peer_v", [NL, 16384, D]),
                ("ln2_g", [NL, D]), ("ln2_b", [NL, D])])

    class LazyW(dict):
        def __missing__(self, nm):
            self[nm] = din(nm, WSH[nm])
            return self[nm]
    W = LazyW()
    yout = [dout("yp", [T, D]), dout("ys", [T, D])]
    nst_o = dout("nst", [4, NL, 2, 64, 64, 2])
    dbg_out = {k: dout("dbg_" + k, v[0], v[1]) for k, v in dbg.items()}

    xres = [dscr("xres0", [T, D]), dscr("xres1", [T, D])]
    ymix = dscr("ymix", [T, D])
    x1res = dscr("x1res", [T, D])
    h2res = dscr("h2res", [T, D])
    mod_d = dscr("mod_d", [NL, 2, 6 * D])
    tabs = dscr("tabs", [64, 128, 2, 260])
    wbd = dscr("wbd", [8, 128, 16, 128], BF16)
    cwd = dscr("cwd", [8, 128, 16, 64], BF16)
    Gd = dscr("Gd", [128, 128, T], BF16)

    psh = nc.alloc_psum_tensor("psum_all", [128, 4096], F32)
    PS = [TT(psh.ap()[:, i * 1024:(i + 1) * 1024], Buf("ps%d" % i, "ps")) for i in range(4)]
    ps_rr = [0]

    def ps_next():
        ps_rr[0] = (ps_rr[0] + 1) % 4
        return PS[ps_rr[0]]

    SB_BASE = (int(nc.sbuf_base) + 63) // 64 * 64
    SB_TOP = int(nc.sbuf_top)
    root = Stack(nc, SB_BASE, SB_TOP)
    G = root.sub(4 * 1024)
    LP = root.sub(4608)
    UL = Stack(nc, root.cur, SB_TOP)

    g_st = g_c = None
    g_x = g_w = g_t = [None, None]

    ident_f = G.alloc([128], F32, "ident_f")
    ident_b = G.alloc([128], BF16, "ident_b")
    iota128 = G.alloc([128], F32, "iota128")
    iota16 = iota128[:, 0:16]
    halfpi = G.alloc([1], F32, "halfpi")
    X4 = G.alloc([4, 128], BF16, "X4")
    CWst = G.alloc([4, 64], BF16, "CWst")
    ones_f = G.alloc([128], F32, "ones_f")
    P.dma("sp", ident_f, ident_d, g_c)
    P.dma("sp", iota128, iota_d, g_c)
    P.cp("dve", ident_b, ident_f)
    P.memset("dve", halfpi, math.pi / 2)
    P.memset("dve", X4, 0.0)
    P.memset("dve", CWst, 0.0)
    P.memset("dve", ones_f, 1.0 / 512.0)

    Rt = LP.alloc([64], F32, "Rt")
    DSK = LP.alloc([8], F32, "DSK")
    H0 = LP.alloc([64, 2], F32, "H0")
    SCW = LP.alloc([4, 3], F32, "SCW")
    SCB = LP.alloc([4], F32, "SCB")
    CFW = LP.alloc([4, 31], F32, "CFW")
    CFB = LP.alloc([4], F32, "CFB")
    CFG = LP.alloc([4], F32, "CFG")
    CFBB = LP.alloc([4], F32, "CFBB")
    KTb = LP.alloc([2, 128], BF16, "KTb")
    NST = LP.alloc([64, 4, 2], F32, "NST")

    WSTG_B = 16 * 256 * 4
    WBF_B = 16 * 256 * 2

    def dbg_store(key, src, dst_slice=None):
        if key in dbg_out:
            dst = dbg_out[key] if dst_slice is None else dbg_out[key][dst_slice]
            P.dma("sp", dst, src, g_st)

    def phase_mod():
        UL.reset()
        condT = UL.alloc([16, 2], F32, "condT")
        bmod = UL.alloc([6 * D], F32, "bmod")
        stg = [UL.alloc([16, 256], F32, "mstg%d" % i) for i in range(2)]
        mo = [UL.alloc([256], F32, "mo%d" % i) for i in range(2)]
        for r_ in range(2):
            P.dma("sp", condT[:, :, r_], cond[r_].r("(kt p) -> p kt", p=128), g_c, allow_slow_non_contiguous=True)
        P.act(condT, condT, AF.Silu)
        for l in range(nlayers):
            P.dma("sp", bmod[0:2, :], TT(W["b_mod"].ap[l:l + 1, :].partition_broadcast(2), W["b_mod"].buf), g_c)
            for c in range(48):
                s_ = stg[c % 2]
                P.dma("sp", s_, W["w_mod"][l][:, c * 256:(c + 1) * 256].r("(kt p) c -> p kt c", p=128), g_w[c % 2])
                ps = ps_next()
                for kt in range(16):
                    P.mm(ps[0:2, 0:256], condT[:, kt, :], s_[:, kt, :], start=(kt == 0), stop=(kt == 15))
                m_ = mo[c % 2]
                P.tt("dve", m_[0:2, :], ps[0:2, 0:256], bmod[0:2, c * 256:(c + 1) * 256], ALU.add)
                if (8 <= c < 16) or (32 <= c < 40):
                    P.ts("dve", m_[0:2, :], m_[0:2, :], 1.0, ALU.add)
                P.dma("sp", mod_d[l][:, c * 256:(c + 1) * 256], m_[0:2, :], g_st)
        P.barrier()

    def phase_setup(l):
        UL.reset()
        A = UL
        nat = [A.alloc([128], F32, "nat%d" % i) for i in range(3)]
        dt2 = A.alloc([2], F32, "dt2")
        LR = A.alloc([64], F32, "LR")
        LI = A.alloc([64], F32, "LI")
        DT = A.alloc([64], F32, "DT")
        P.dma("sp", nat[0][0:64, :], W["s5_lam_re"][l].r("d (gp g2) p -> (d gp) (g2 p)", g2=2), g_c)
        P.dma("sp", nat[1][0:64, :], W["s5_lam_im"][l].r("d (gp g2) p -> (d gp) (g2 p)", g2=2), g_c)
        P.dma("sp", dt2[0:64, :], W["s5_log_dt"][l].r("d (gp g2) -> (d gp) g2", g2=2), g_c)
        P.cp("dve", nat[2][0:64, :].r("q (a b) -> q a b", a=2), dt2[0:64, :].un(2).bc([64, 2, 64]))
        for i, dst in enumerate((LR, LI, DT)):
            ps = ps_next()
            P.tr(ps[:, 0:64], nat[i][0:64, :], ident_f[0:64, 0:64])
            P.cp("act", dst, ps[:, 0:64])
        v = {k: A.alloc([64], F32, k) for k in ("dt", "ang", "c", "s", "t1", "t2", "are", "aim", "den", "am1", "zre", "zim")}
        P.act(v["dt"], DT, AF.Exp)
        P.tt("dve", v["t1"], LR, v["dt"], ALU.mult)
        P.act(Rt, v["t1"], AF.Exp)
        P.tt("dve", v["ang"], LI, v["dt"], ALU.mult)
        P.act(v["s"], v["ang"], AF.Sin, scale=1.0 / 32.0)
        P.act(v["c"], v["ang"], AF.Sin, bias=halfpi[:, 0:1], scale=1.0 / 32.0)
        for _ in range(5):
            P.tt("dve", v["t1"], v["c"], v["c"], ALU.mult)
            P.tt("dve", v["t2"], v["s"], v["s"], ALU.mult)
            P.stt(v["s"], v["c"], 2.0, v["s"], ALU.mult, ALU.mult)
            P.tt("dve", v["c"], v["t1"], v["t2"], ALU.subtract)
        C1, S1 = v["c"], v["s"]
        P.tt("dve", v["are"], Rt, C1, ALU.mult)
        P.tt("dve", v["aim"], Rt, S1, ALU.mult)
        P.tt("dve", v["t1"], LR, LR, ALU.mult)
        P.tt("dve", v["t2"], LI, LI, ALU.mult)
        P.tt("dve", v["den"], v["t1"], v["t2"], ALU.add)
        P.recip(v["den"], v["den"])
        P.ts("dve", v["am1"], v["are"], -1.0, ALU.add)
        P.tt("dve", v["t1"], v["am1"], LR, ALU.mult)
        P.tt("dve", v["t2"], v["aim"], LI, ALU.mult)
        P.tt("dve", v["t1"], v["t1"], v["t2"], ALU.add)
        P.tt("dve", v["zre"], v["t1"], v["den"], ALU.mult)
        P.tt("dve", v["t1"], v["aim"], LR, ALU.mult)
        P.tt("dve", v["t2"], v["am1"], LI, ALU.mult)
        P.tt("dve", v["t1"], v["t1"], v["t2"], ALU.subtract)
        P.tt("dve", v["zim"], v["t1"], v["den"], ALU.mult)
        BRE = A.alloc([64, 16], F32, "BRE")
        BIM = A.alloc([64, 16], F32, "BIM")
        tA = A.alloc([64, 16], F32, "tA")
        tB = A.alloc([64, 16], F32, "tB")
        BB = [A.alloc([64, 16], BF16, "BBre"), A.alloc([64, 16], BF16, "BBim")]
        P.dma("sp", BRE, W["s5_b_re"][l].r("d (gp g2) p s -> (g2 p) (d gp) s", g2=2), g_c)
        P.dma("sp", BIM, W["s5_b_im"][l].r("d (gp g2) p s -> (g2 p) (d gp) s", g2=2), g_c)
        zr = v["zre"].un(2).bc([128, 64, 16])
        zi = v["zim"].un(2).bc([128, 64, 16])
        P.tt("dve", tA, BRE, zr, ALU.mult)
        P.tt("dve", tB, BIM, zi, ALU.mult)
        P.tt("dve", BB[0], tA, tB, ALU.subtract)
        P.tt("dve", tA, BIM, zr, ALU.mult)
        P.tt("dve", tB, BRE, zi, ALU.mult)
        P.tt("dve", BB[1], tA, tB, ALU.add)
        wst = [A.alloc([4, 128], BF16, "wst%d" % i) for i in range(2)]
        it = 0
        for d in range(2):
            for ct in range(8):
                rt0 = d * 32 + ct * 4
                for reim in range(2):
                    for j in range(4):
                        P.cp("dve", X4[0:64, j, 32 * j:32 * j + 16], BB[reim][0:64, rt0 + j, :])
                        P.cp("dve", X4[64:128, j, 32 * j + 16:32 * j + 32], BB[reim][64:128, rt0 + j, :])
                    ps = ps_next()
                    psb = ps[:, 0:256].bitcast(BF16).r("p (j c) -> p j c", j=4)
                    for j in range(4):
                        P.tr(psb[:, j, :], X4[:, j, :], ident_b)
                    ws_ = wst[it % 2]
                    it += 1
                    P.cp("act", ws_, psb)
                    P.dma("sp", wbd[ct][:, d * 8 + reim:d * 8 + 8:2, :], ws_, g_st)
        CN = [A.alloc([64], F32, "CN%d" % i) for i in range(2)]
        CN2 = A.alloc([128], BF16, "CN2")
        cst = [A.alloc([4, 64], BF16, "cst%d" % i) for i in range(2)]
        it = 0
        for d in range(2):
            for ct in range(8):
                for reim in range(2):
                    src = W["s5_c_im" if reim else "s5_c_re"][l, d, 8 * ct:8 * ct + 8].r("g s p -> (g s) p")
                    cn = CN[it % 2]
                    P.dma("sp", cn, src, g_t[it % 2])
                    sgn = -1.0 if reim else 1.0
                    P.ts("dve", CN2[:, 0:64], cn, sgn, ALU.mult)
                    P.ts("dve", CN2[:, 64:128], cn, sgn, ALU.mult)
                    ps = ps_next()
                    psb = ps[:, 0:64].bitcast(BF16)
                    P.tr(psb, CN2, ident_b)
                    for j in range(4):
                        jl = j % 2
                        P.cp("dve", CWst[0:64, j, 32 * jl:32 * jl + 16], psb[0:64, 32 * j:32 * j + 16])
                        P.cp("dve", CWst[64:128, j, 32 * jl + 16:32 * jl + 32], psb[64:128, 32 * j + 16:32 * j + 32])
                    cs_ = cst[it % 2]
                    it += 1
                    P.cp("dve", cs_, CWst)
                    P.dma("sp", cwd[ct][:, d * 8 + reim:d * 8 + 8:2, :], cs_, g_st)
        ER = A.alloc([16, 260], F32, "ER")
        EI = A.alloc([16, 260], F32, "EI")
        q = {k: A.alloc([16, 128], F32, k) for k in ("q1", "q2", "q3", "q4")}
        MR = A.alloc([16], F32, "MR")
        MI = A.alloc([16], F32, "MI")
        m1 = A.alloc([16], F32, "m1")
        m2 = A.alloc([16], F32, "m2")
        for gq in range(4):
            sl = slice(gq * 16, gq * 16 + 16)
            P.memset("dve", ER[:, :, 0:1], 1.0)
            P.memset("dve", EI[:, :, 0:1], 0.0)
            P.cp("dve", MR, C1[:, sl])
            P.cp("dve", MI, S1[:, sl])
            k = 1
            while k < 260:
                cnt = min(k, 260 - k)
                o = 0
                while o < cnt:
                    n = min(128, cnt - o)
                    mrb = MR.un(2).bc([128, 16, n])
                    mib = MI.un(2).bc([128, 16, n])
                    sr = ER[:, :, o:o + n]
                    si = EI[:, :, o:o + n]
                    P.tt("dve", q["q1"][:, :, 0:n], sr, mrb, ALU.mult)
                    P.tt("dve", q["q2"][:, :, 0:n], si, mib, ALU.mult)
                    P.tt("dve", q["q3"][:, :, 0:n], sr, mib, ALU.mult)
                    P.tt("dve", q["q4"][:, :, 0:n], si, mrb, ALU.mult)
                    P.tt("dve", ER[:, :, k + o:k + o + n], q["q1"][:, :, 0:n], q["q2"][:, :, 0:n], ALU.subtract)
                    P.tt("dve", EI[:, :, k + o:k + o + n], q["q3"][:, :, 0:n], q["q4"][:, :, 0:n], ALU.add)
                    o += n
                if 2 * k < 260:
                    P.tt("dve", m1, MR, MR, ALU.mult)
                    P.tt("dve", m2, MI, MI, ALU.mult)
                    P.stt(MI, MR, 2.0, MI, ALU.mult, ALU.mult)
                    P.tt("dve", MR, m1, m2, ALU.subtract)
                k *= 2
            P.dma("sp", tabs[gq * 16:gq * 16 + 16, :, 0, :].r("rt p t -> p rt t"), ER, g_st)
            P.dma("sp", tabs[gq * 16:gq * 16 + 16, :, 1, :].r("rt p t -> p rt t"), EI, g_st)
        P.dma("sp", DSK, W["s5_d"][l].r("(ct p) -> p ct", p=128), g_c, allow_slow_non_contiguous=True)
        P.dma("sp", H0, st0[l].r("d (gp g2) p c -> (g2 p) (d gp) c", g2=2), g_c)
        cwn = A.alloc([512], F32, "cwn")
        for nm_, dst_, nk_ in (("sc_conv_w", SCW, 3), ("cf_conv_w", CFW, 31)):
            P.dma("sp", cwn[0:nk_, :], W[nm_][l], g_c)
            for ct_ in range(4):
                ps = ps_next()
                P.tr(ps[:, 0:nk_], cwn[0:nk_, ct_ * 128:(ct_ + 1) * 128], ident_f[0:nk_, 0:nk_])
                P.cp("act", dst_[:, ct_, :], ps[:, 0:nk_])
        P.dma("sp", SCB, W["sc_conv_b"][l].r("(ct p) -> p ct", p=128), g_c, allow_slow_non_contiguous=True)
        P.dma("sp", CFB, W["cf_conv_b"][l].r("(ct p) -> p ct", p=128), g_c, allow_slow_non_contiguous=True)
        P.dma("sp", CFG, W["cf_ln_g"][l].r("(ct p) -> p ct", p=128), g_c, allow_slow_non_contiguous=True)
        P.dma("sp", CFBB, W["cf_ln_b"][l].r("(ct p) -> p ct", p=128), g_c, allow_slow_non_contiguous=True)
        dbg_store("CFW", CFW)
        dbg_store("SCW", SCW)
        kn = [A.alloc([128], F32, "kn%d" % i) for i in range(2)]
        for hf, nm in enumerate(("peer_k1", "peer_k2")):
            P.dma("sp", kn[hf], W[nm][l], g_c)
            ps = ps_next()
            P.tr(ps[:, 0:128], kn[hf], ident_f)
            P.cp("act", KTb[:, hf, :], ps[:, 0:128])
        P.barrier()

    wslots = {}
    w_it = [0]

    def load_w(W2d, r0, nk, c0, ncol):
        i = w_it[0] % 2
        w_it[0] += 1
        stg, wbf = wslots["stg"][i], wslots["wbf"][i]
        P.dma("sp", stg[:, 0:nk, 0:ncol], W2d[r0:r0 + 128 * nk, c0:c0 + ncol].r("(kt p) c -> p kt c", p=128), g_w[i])
        if i == 0:
            P.cp("dve", wbf[:, 0:nk, 0:ncol], stg[:, 0:nk, 0:ncol])
        else:
            P.cp("act", wbf[:, 0:nk, 0:ncol], stg[:, 0:nk, 0:ncol])
        return wbf

    def linear_fm(in_tiles, W2d, r0, c0, ncols, consume, tile0=0):
        nk = len(in_tiles)
        for cc in range(0, ncols, 256):
            wb = load_w(W2d, r0, nk, c0 + cc, 256)
            for ti in range(2):
                ps = ps_next()
                for half in range(2):
                    hs = slice(half * 512, half * 512 + 512)
                    for kt in range(nk):
                        P.mm(ps[:, hs], wb[:, kt, ti * 128:(ti + 1) * 128], in_tiles[kt][:, hs],
                             start=(kt == 0), stop=(kt == nk - 1))
                consume(tile0 + cc // 128 + ti, ps)

    def ln_tm(x, out, st, mv, rs, e="dve"):
        for c in range(4):
            P.op("dve", lambda E, c=c: E.bn_stats(out=st.ap[:, c, :], in_=x.ap[:, c * 512:(c + 1) * 512]),
                 r=[x.buf], w=[st.buf])
        P.op("dve", lambda E: E.bn_aggr(out=mv.ap, in_=st.ap.rearrange("p a b -> p (a b)")), r=[st.buf], w=[mv.buf])
        P.ts("dve", rs, mv[:, 1:2], LN_EPS, ALU.add)
        P.act(rs, rs, AF.Sqrt)
        P.recip(rs, rs)
        P.ts("dve", out, x, mv[:, 0:1], ALU.subtract, rs[:, 0:1], ALU.mult)

    def phase_A(l, u):
        nseq, SL = (4, 256) if u == 0 else (1, 1024)
        UL.reset()
        hT = UL.alloc([16, T], BF16, "hT")
        bufP = UL.alloc([8, T], BF16, "bufP")
        bufQ = UL.alloc([8, T], BF16, "bufQ")
        ysT = UL.alloc([4, T], BF16, "ysT")
        zT = UL.alloc([4, T], BF16, "zT")
        wslots["wbf"] = [UL.alloc([16, 256], BF16, "wbf%d" % i) for i in range(2)]
        wslots["stg"] = [UL.alloc([16, 256], F32, "wstg%d" % i) for i in range(2)]
        X = Stack(nc, UL.cur, SB_TOP)

        X.reset()
        xt = [X.alloc([D], F32, "xt%d" % i) for i in range(2)]
        tmp = X.alloc([D], F32, "tmpA0")
        hb = X.alloc([D], BF16, "hb")
        sc1b = X.alloc([D], F32, "sc1b")
        sh1b = X.alloc([D], F32, "sh1b")
        pet = X.alloc([D], F32, "pet")
        st = X.alloc([4, 6], F32, "st")
        mv = X.alloc([2], F32, "mv")
        rs = X.alloc([1], F32, "rs")
        P.dma("sp", sh1b, TT(mod_d.ap[l, u:u + 1, 0:D].partition_broadcast(128), mod_d.buf), g_c)
        P.dma("sp", sc1b, TT(mod_d.ap[l, u:u + 1, D:2 * D].partition_broadcast(128), mod_d.buf), g_c)
        for tt_ in range(NTT):
            rows = slice(tt_ * 128, tt_ * 128 + 128)
            x_ = xt[tt_ % 2]
            if l == 0:
                P.dma("sp", x_, xin[u][rows, :], g_x[tt_ % 2])
                if u == 1:
                    P.dma("sp", pet, pe_d[rows, :], g_x[tt_ % 2])
                    P.tt("dve", x_, x_, pet, ALU.add)
                P.dma("sp", xres[u][rows, :], x_, g_st)
            else:
                P.dma("sp", x_, xres[u][rows, :], g_x[tt_ % 2])
            ln_tm(x_, tmp, st, mv, rs)
            P.tt("dve", tmp, tmp, sc1b, ALU.mult)
            P.tt("dve", hb, tmp, sh1b, ALU.add)
            ps = ps_next()
            psb = ps.bitcast(BF16)
            for kt in range(KT):
                P.tr(psb[:, kt * 128:(kt + 1) * 128], hb[:, kt * 128:(kt + 1) * 128], ident_b)
            P.cp("act", hT[:, :, rows], psb.r("p (k t) -> p k t", k=KT))
        dbg_store("hT", hT)
        if stop == "A0":
            return None
        hT_tiles = [hT[:, kt, :] for kt in range(KT)]
        P.barrier()

        uT = bufP
        ya = bufQ

        def cons_u(ti, ps):
            P.cp("act", uT[:, ti, :], ps)
        linear_fm(hT_tiles, W["w_in"][l], 0, 0, 1024, cons_u)
        dbg_store("uT", uT)
        X.reset()
        f = {k: X.alloc([T], F32, k) for k in ("bure", "buim", "t1", "t2", "t3", "br", "bi", "gre", "gim")}
        fbh = {k: X.alloc([T], BF16, k) for k in ("greb", "gimb", "q1", "q2", "q3", "q4")}
        tbb = X.alloc([2, 256], BF16, "tbb")
        hre = X.alloc([T], BF16, "hre")
        him = X.alloc([T], BF16, "him")
        tbs = [X.alloc([2, 260], F32, "tb%d" % i) for i in range(2)]
        wbt = [X.alloc([16, 128], BF16, "wbt%d" % i) for i in range(2)]
        cwt = [X.alloc([16, 64], BF16, "cwt%d" % i) for i in range(2)]
        sm = {k: X.alloc([4], F32, k) for k in ("i_re", "i_im", "s1", "s2")}
        psA, psB, psC, psD = PS

        def v4(t_):
            return t_.r("p (c t) -> p c t", c=4)

        rts = []
        for ct in range(8):
            for d in range(2):
                for j in range(4):
                    rts.append((ct, d, j))

        state = {}

        def issue_bu(i):
            ct, d, j = rts[i]
            if d == 0 and j == 0:
                k = ct % 2
                P.dma("sp", wbt[k], wbd[ct], g_t[k])
                P.dma("sp", cwt[k], cwd[ct], g_t[k])
            rt = d * 32 + ct * 4 + j
            tb = tbs[i % 2]
            P.dma("sp", tb, tabs[rt], g_x[i % 2])
            wb_ = wbt[ct % 2]
            for reim, slot in ((0, psA), (1, psB)):
                for half in range(2):
                    hs = slice(half * 512, half * 512 + 512)
                    P.mm(slot[:, hs], wb_[:, (d * 4 + j) * 2 + reim, :], uT[:, ct, hs])

        issue_bu(0)
        for i, (ct, d, j) in enumerate(rts):
            rt = d * 32 + ct * 4 + j
            tb = tbs[i % 2]
            cw_ = cwt[ct % 2]
            Cb = tb[:, 0, 0:256].un(1).bc([128, 4, 256])
            Sb = tb[:, 1, 0:256].un(1).bc([128, 4, 256])
            for src, dst in ((psA, f["bure"]), (psB, f["buim"])):
                s3 = src.r("p (b t) -> p b t", b=nseq)
                if d == 1:
                    s3 = s3[:, :, ::-1]
                P.cp("act", dst.r("p (b t) -> p b t", b=nseq), s3)
            if i + 1 < len(rts):
                issue_bu(i + 1)
            P.tt("dve", v4(f["t1"]), v4(f["bure"]), Cb, ALU.mult)
            P.tt("dve", v4(f["t2"]), v4(f["buim"]), Sb, ALU.mult)
            P.tt("dve", f["br"], f["t1"], f["t2"], ALU.add)
            P.tt("dve", v4(f["t1"]), v4(f["buim"]), Cb, ALU.mult)
            P.tt("dve", v4(f["t2"]), v4(f["bure"]), Sb, ALU.mult)
            P.tt("dve", f["bi"], f["t1"], f["t2"], ALU.subtract)
            rcol = Rt[:, rt:rt + 1]
            for c in range(4):
                cs = slice(c * 256, c * 256 + 256)
                if u == 0:
                    ire, iim = 0.0, 0.0
                else:
                    if c == 0:
                        pre, pim = H0[:, rt, 0:1], H0[:, rt, 1:2]
                        er, ei = tb[:, 0, 1:2], tb[:, 1, 1:2]
                    else:
                        pre, pim = f["gre"][:, c * 256 - 1:c * 256], f["gim"][:, c * 256 - 1:c * 256]
                        er, ei = tb[:, 0, 256:257], tb[:, 1, 256:257]
                    P.ts("dve", sm["s1"][:, 0:1], pim, ei, ALU.mult)
                    P.stt(sm["i_re"][:, c:c + 1], pre, er, sm["s1"][:, 0:1], ALU.mult, ALU.subtract)
                    P.ts("dve", sm["s2"][:, 0:1], pim, er, ALU.mult)
                    P.stt(sm["i_im"][:, c:c + 1], pre, ei, sm["s2"][:, 0:1], ALU.mult, ALU.add)
                    ire, iim = sm["i_re"][:, c:c + 1], sm["i_im"][:, c:c + 1]
                P.scan(f["gre"][:, cs], rcol.bc([128, 256]), f["br"][:, cs], ire)
                P.scan(f["gim"][:, cs], rcol.bc([128, 256]), f["bi"][:, cs], iim)
            P.cp("act", tbb, tb[:, :, 0:256])
            P.cp("act", fbh["greb"], f["gre"])
            P.cp("act", fbh["gimb"], f["gim"])
            Cbb = tbb[:, 0, :].un(1).bc([128, 4, 256])
            Sbb = tbb[:, 1, :].un(1).bc([128, 4, 256])
            P.tt("dve", v4(fbh["q1"]), v4(fbh["greb"]), Cbb, ALU.mult)
            P.tt("dve", v4(fbh["q2"]), v4(fbh["gimb"]), Sbb, ALU.mult)
            P.tt("dve", hre, fbh["q1"], fbh["q2"], ALU.subtract)
            P.tt("dve", v4(fbh["q3"]), v4(fbh["greb"]), Sbb, ALU.mult)
            P.tt("dve", v4(fbh["q4"]), v4(fbh["gimb"]), Cbb, ALU.mult)
            P.tt("dve", him, fbh["q3"], fbh["q4"], ALU.add)
            if u == 0:
                glr = v4(f["gre"])[:, :, 255]
                gli = v4(f["gim"])[:, :, 255]
                c255, s255 = tb[:, 0, 255:256], tb[:, 1, 255:256]
                P.ts("dve", sm["s1"], gli, s255, ALU.mult)
                P.stt(NST[:, rt, :, 0], glr, c255, sm["s1"], ALU.mult, ALU.subtract)
                P.ts("dve", sm["s2"], gli, c255, ALU.mult)
                P.stt(NST[:, rt, :, 1], glr, s255, sm["s2"], ALU.mult, ALU.add)
            ysl = psC if d == 0 else psD
            jj = j // 2
            for reim, hs_ in ((0, hre), (1, him)):
                for half in range(2):
                    hs = slice(half * 512, half * 512 + 512)
                    P.mm(ysl[64 * jj:64 * jj + 64, hs], cw_[:, (d * 4 + j) * 2 + reim, :], hs_[:, hs],
                         start=(j % 2 == 0 and reim == 0), stop=(j % 2 == 1 and reim == 1))
            if d == 1 and j == 3:
                s3 = psD.r("p (b t) -> p b t", b=nseq)[:, :, ::-1]
                P.cp("act", f["t3"].r("p (b t) -> p b t", b=nseq), s3)
                P.tt("dve", f["t1"], psC, f["t3"], ALU.add)
                P.stt(f["t2"], uT[:, ct, :], DSK[:, ct:ct + 1], f["t1"], ALU.mult, ALU.add)
                P.act(ya[:, ct, :], f["t2"], AF.Gelu_apprx_tanh)
        if u == 0:
            for b_ in range(4):
                P.dma("sp", nst_o[b_, l].r("d (gp g2) p c -> (g2 p) (d gp) c", g2=2), NST[:, :, b_, :], g_st)
        dbg_store("ya", ya)
        dbg_store("nst", nst_o) if False else None
        if stop == "A1s":
            return None
        P.barrier()
        X.reset()
        sg = [X.alloc([T], F32, "sg%d" % i) for i in range(2)]
        yag = bufP
        ya_tiles = [ya[:, k, :] for k in range(8)]

        def cons_glu(ti, ps):
            s_ = sg[ti % 2]
            P.act(s_, ps, AF.Sigmoid)
            P.tt("dve", yag[:, ti, :], ya[:, ti, :], s_, ALU.mult)
        linear_fm(ya_tiles, W["s5_w_glu"][l], 0, 0, 1024, cons_glu)
        dbg_store("yag", yag)
        if stop == "A1":
            return None

        X.reset()
        fb = {k: X.alloc([T], F32, k) for k in ("cb", "cc", "prod", "acc")}
        keep = {}

        def seqv(t_):
            return t_.r("p (b t) -> p b t", b=nseq)

        def cons_sc(ti, ps):
            kind, i = divmod(ti, 4)
            if kind == 0:
                if i not in keep:
                    keep[i] = X.alloc([T], F32, "scb%d" % i)
                P.cp("act", keep[i], ps)
            elif kind == 1:
                if ("c", i) not in keep:
                    keep[("c", i)] = X.alloc([T], F32, "scc%d" % i)
                P.cp("act", keep[("c", i)], ps)
            else:
                P.tt("dve", fb["prod"], keep[("c", i)], ps, ALU.mult)
                P.ts("dve", fb["acc"], fb["prod"], SCW[:, i, 1:2], ALU.mult, SCB[:, i:i + 1], ALU.add)
                a3, p3 = seqv(fb["acc"]), seqv(fb["prod"])
                P.stt(a3[:, :, 1:SL], p3[:, :, 0:SL - 1], SCW[:, i, 0:1], a3[:, :, 1:SL], ALU.mult, ALU.add)
                P.stt(a3[:, :, 0:SL - 1], p3[:, :, 1:SL], SCW[:, i, 2:3], a3[:, :, 0:SL - 1], ALU.mult, ALU.add)
                P.tt("dve", ysT[:, i, :], keep[i], fb["acc"], ALU.mult)
        linear_fm(hT_tiles, W["w_in"][l], 0, 1024, 1536, cons_sc)
        dbg_store("ys", ysT)
        if stop == "A2":
            return None
        P.barrier()

        X.reset()
        ga = [X.alloc([T], F32, "ga%d" % i) for i in range(4)]
        cv = [X.alloc([T], F32, "cv%d" % i) for i in range(4)]
        fc = {k: X.alloc([T], F32, k) for k in ("sq", "rstd", "mr")}

        def cons_cf(ti, ps):
            kind, i = divmod(ti, 4)
            if kind == 0:
                P.cp("act", ga[i], ps)
            else:
                P.act(fc["sq"], ps, AF.Sigmoid)
                P.tt("dve", ga[i], ga[i], fc["sq"], ALU.mult)
                P.ts("dve", cv[i], ga[i], CFW[:, i, 15:16], ALU.mult, CFB[:, i:i + 1], ALU.add)
                a3, g3 = seqv(cv[i]), seqv(ga[i])
                for k in range(31):
                    sh = k - 15
                    if sh == 0:
                        continue
                    if sh > 0:
                        P.stt(a3[:, :, 0:SL - sh], g3[:, :, sh:SL], CFW[:, i, k:k + 1], a3[:, :, 0:SL - sh], ALU.mult, ALU.add)
                    else:
                        P.stt(a3[:, :, -sh:SL], g3[:, :, 0:SL + sh], CFW[:, i, k:k + 1], a3[:, :, -sh:SL], ALU.mult, ALU.add)
        linear_fm(hT_tiles, W["w_in"][l], 0, 2560, 1024, cons_cf)
        dbg_store("glu0", ga[0])
        dbg_store("cv0", cv[0])
        psM, psQ = PS[0], PS[1]
        for i in range(4):
            for half in range(2):
                hs = slice(half * 512, half * 512 + 512)
                P.mm(psM[:, hs], ones_f, cv[i][:, hs], start=(i == 0), stop=(i == 3))
        for i in range(4):
            P.act(ga[i], cv[i], AF.Square)
        for i in range(4):
            for half in range(2):
                hs = slice(half * 512, half * 512 + 512)
                P.mm(psQ[:, hs], ones_f, ga[i][:, hs], start=(i == 0), stop=(i == 3))
        P.cp("act", fc["mr"], psM)
        P.tt("dve", fc["sq"], fc["mr"], fc["mr"], ALU.mult)
        P.tt("dve", fc["rstd"], psQ, fc["sq"], ALU.subtract)
        P.ts("dve", fc["rstd"], fc["rstd"], LN_EPS, ALU.add)
        P.act(fc["rstd"], fc["rstd"], AF.Sqrt)
        P.recip(fc["rstd"], fc["rstd"])
        dbg_store("rstd", fc["rstd"])
        dbg_store("mean", psM) if False else None
        P.tt("dve", fc["mr"], fc["mr"], fc["rstd"], ALU.mult)
        for i in range(4):
            P.tt("dve", ga[i], cv[i], fc["rstd"], ALU.mult)
            P.tt("dve", ga[i], ga[i], fc["mr"], ALU.subtract)
            P.ts("dve", ga[i], ga[i], CFG[:, i:i + 1], ALU.mult, CFBB[:, i:i + 1], ALU.add)
            P.act(zT[:, i, :], ga[i], AF.Silu)
        dbg_store("z", zT)
        if stop == "A3":
            return None
        P.barrier()

        X.reset()
        mT = X.alloc([16, T], BF16, "mT")
        sgm = [X.alloc([T], F32, "sgm%d" % i) for i in range(2)]
        macc = [X.alloc([T], F32, "macc%d" % i) for i in range(2)]
        tmpm = X.alloc([T], F32, "tmpm")
        yag_t = [bufP[:, k, :] for k in range(8)]
        ys_t = [ysT[:, k, :] for k in range(4)]
        z_t = [zT[:, k, :] for k in range(4)]
        branches = [(yag_t, W["w_pa"][l], 0), (ys_t, W["w_pb"][l], 1), (z_t, W["w_pc"][l], 2)]
        for pair in range(8):
            cbase = pair * 256
            for bi, (tiles, Wp, gidx) in enumerate(branches):
                nk = len(tiles)
                wb = load_w(Wp, 0, nk, cbase, 256)
                pp = []
                for ti in range(2):
                    ps = ps_next()
                    for half in range(2):
                        hs = slice(half * 512, half * 512 + 512)
                        for kt in range(nk):
                            P.mm(ps[:, hs], wb[:, kt, ti * 128:(ti + 1) * 128], tiles[kt][:, hs], start=(kt == 0), stop=(kt == nk - 1))
                    pp.append(ps)
                wg = load_w(W["w_in"][l], 0, 16, GATE0 + gidx * D + cbase, 256)
                for ti in range(2):
                    ps = ps_next()
                    for half in range(2):
                        hs = slice(half * 512, half * 512 + 512)
                        for kt in range(16):
                            P.mm(ps[:, hs], wg[:, kt, ti * 128:(ti + 1) * 128], hT_tiles[kt][:, hs], start=(kt == 0), stop=(kt == 15))
                    P.act(sgm[ti], ps, AF.Sigmoid)
                    if bi == 0:
                        P.tt("dve", macc[ti], sgm[ti], pp[ti], ALU.mult)
                    elif bi == 1:
                        P.tt("dve", tmpm, sgm[ti], pp[ti], ALU.mult)
                        P.tt("dve", macc[ti], macc[ti], tmpm, ALU.add)
                    else:
                        P.tt("dve", tmpm, sgm[ti], pp[ti], ALU.mult)
                        P.tt("dve", mT[:, pair * 2 + ti, :], macc[ti], tmpm, ALU.add)
        dbg_store("mT", mT)
        if stop == "A4":
            return None
        ost = [X.alloc([256], F32, "ost%d" % i) for i in range(2)]
        it = 0
        for cc in range(8):
            wb = load_w(W["w_o"][l], 0, 16, cc * 256, 256)
            for tt_ in range(NTT):
                ps = ps_next()
                for kt in range(16):
                    P.mm(ps[:, 0:256], mT[:, kt, tt_ * 128:(tt_ + 1) * 128], wb[:, kt, :], start=(kt == 0), stop=(kt == 15))
                o_ = ost[it % 2]
                it += 1
                P.cp("act", o_, ps[:, 0:256])
                P.dma("sp", ymix[tt_ * 128:(tt_ + 1) * 128, cc * 256:(cc + 1) * 256], o_, g_st)
        P.barrier()
        return None

    def phase_B(l, u, last):
        UL.reset()
        h2T = UL.alloc([16, T], BF16, "h2T")
        Zbase = UL.cur
        qT = UL.alloc([16, T], BF16, "qT")
        Y3base = UL.cur
        wslots["wbf"] = [UL.alloc([16, 256], BF16, "wbfB%d" % i) for i in range(2)]
        Y = Stack(nc, UL.cur, SB_TOP)
        Y.reset()
        wslots["stg"] = None
        xt = [Y.alloc([D], F32, "xtB%d" % i) for i in range(2)]
        yt = [Y.alloc([D], F32, "ytB%d" % i) for i in range(2)]
        t1 = Y.alloc([D], F32, "t1B")
        hb = Y.alloc([D], BF16, "hbB")
        bct = {k: Y.alloc([D], F32, k) for k in ("gt1", "g1", "b1", "sc2", "sh2")}
        st = Y.alloc([4, 6], F32, "stB")
        mv = Y.alloc([2], F32, "mvB")
        rs = Y.alloc([1], F32, "rsB")

        def bload(dst, src2d, row, c0):
            P.dma("sp", dst, TT(src2d.ap[row:row + 1, c0:c0 + D].partition_broadcast(128), src2d.buf), g_c)
        bload(bct["gt1"], mod_d[l], u, 2 * D)
        bload(bct["sc2"], mod_d[l], u, 4 * D)
        bload(bct["sh2"], mod_d[l], u, 3 * D)
        bload(bct["g1"], W["ln1_g"], l, 0)
        bload(bct["b1"], W["ln1_b"], l, 0)
        for tt_ in range(NTT):
            rows = slice(tt_ * 128, tt_ * 128 + 128)
            x_, y_ = xt[tt_ % 2], yt[tt_ % 2]
            P.dma("sp", x_, xres[u][rows, :], g_x[tt_ % 2])
            P.dma("sp", y_, ymix[rows, :], g_x[tt_ % 2])
            P.tt("dve", y_, y_, bct["gt1"], ALU.mult)
            P.stt(y_, x_, ALPHA, y_, ALU.mult, ALU.add)
            ln_tm(y_, t1, st, mv, rs)
            P.tt("dve", t1, t1, bct["g1"], ALU.mult)
            P.tt("dve", x_, t1, bct["b1"], ALU.add)
            P.dma("sp", x1res[rows, :], x_, g_st)
            if tt_ == 0:
                dbg_store("x1", x_)
            ln_tm(x_, t1, st, mv, rs)
            P.tt("dve", t1, t1, bct["sc2"], ALU.mult)
            P.tt("dve", y_, t1, bct["sh2"], ALU.add)
            if tt_ == 0:
                dbg_store("h2", y_)
            P.cp("dve", hb, y_)
            ps = ps_next()
            psb = ps.bitcast(BF16)
            for kt in range(KT):
                P.tr(psb[:, kt * 128:(kt + 1) * 128], hb[:, kt * 128:(kt + 1) * 128], ident_b)
            P.cp("act", h2T[:, :, rows], psb.r("p (k t) -> p k t", k=KT))
        if stop == "B1":
            return
        P.barrier()
        Y.reset()
        wslots["stg"] = [Y.alloc([16, 256], F32, "wstgB%d" % i) for i in range(2)]
        h2T_t = [h2T[:, k, :] for k in range(KT)]

        def cons_q(ti, ps):
            P.cp("act", qT[:, ti, :], ps)
        linear_fm(h2T_t, W["peer_w_q"][l], 0, 0, D, cons_q)
        P.barrier()
        Y = Stack(nc, Y3base, SB_TOP)
        S = Y.alloc([16, 128], F32, "S")
        S2 = Y.alloc([256], F32, "S2")
        V = Y.alloc([16, 16], F32, "V")
        Iu = Y.alloc([16, 16], U32, "Iu")
        If = Y.alloc([16, 16], F32, "If")
        cand = Y.alloc([8, 256], F32, "cand")
        top = Y.alloc([8, 16], F32, "top")
        pos = Y.alloc([8, 16], U32, "pos")
        pa_ = Y.alloc([8, 16], U32, "pa")
        pb_ = Y.alloc([8, 16], U32, "pb")
        af = Y.alloc([8, 16], F32, "af")
        bf = Y.alloc([8, 16], F32, "bf")
        eq = Y.alloc([8, 16, 16], F32, "eq")
        i1s = Y.alloc([8, 16], F32, "i1s")
        i2s = Y.alloc([8, 16], F32, "i2s")
        gw = Y.alloc([8, 16], F32, "gw")
        gsum = Y.alloc([8], F32, "gsum")
        i1T = Y.alloc([128], F32, "i1T")
        i2T = Y.alloc([128], F32, "i2T")
        gT = Y.alloc([128], F32, "gT")
        Pm = Y.alloc([128, 128], BF16, "Pm")
        Qm = Y.alloc([128, 128], BF16, "Qm")
        Gst = Y.alloc([128, 128], BF16, "Gst")
        for tt_ in range(b3_tiles):
            rows = slice(tt_ * 128, tt_ * 128 + 128)
            for q4 in range(4):
                ps = ps_next()
                for m in range(4):
                    hh = q4 * 4 + m
                    P.mm(ps[:, m * 128:(m + 1) * 128], qT[:, hh, rows], KTb[:, hh % 2, :])
                P.cp("act", S[:, q4 * 4:q4 * 4 + 4, :], ps[:, 0:512].r("p (a n) -> p a n", a=4))
            for hh in range(16):
                P.op("dve", lambda E, hh=hh: E.max(out=V.ap[:, hh, 0:8], in_=S.ap[:, hh, :]), r=[S.buf], w=[V.buf])
                P.op("dve", lambda E, hh=hh: E.max_index(out=Iu.ap[:, hh, 0:8], in_max=V.ap[:, hh, 0:8], in_values=S.ap[:, hh, :]),
                     r=[S.buf, V.buf], w=[Iu.buf])
                P.op("dve", lambda E, hh=hh: E.match_replace(out=S2.ap[:, 0:128], in_to_replace=V.ap[:, hh, 0:8],
                                                             in_values=S.ap[:, hh, :], imm_value=NEG), r=[S.buf, V.buf], w=[S2.buf])
                P.op("dve", lambda E, hh=hh: E.max(out=V.ap[:, hh, 8:16], in_=S2.ap[:, 0:128]), r=[S2.buf], w=[V.buf])
                P.op("dve", lambda E, hh=hh: E.max_index(out=Iu.ap[:, hh, 8:16], in_max=V.ap[:, hh, 8:16], in_values=S2.ap[:, 0:128]),
                     r=[S2.buf, V.buf], w=[Iu.buf])
            P.cp("dve", If, Iu)
            V4 = V.r("p (h f) a -> p h f a", f=2)
            I4 = If.r("p (h f) a -> p h f a", f=2)
            c4 = cand.r("p h (a b) -> p h a b", a=16)
            P.tt("dve", c4, V4[:, :, 0, :].un(3).bc([128, 8, 16, 16]), V4[:, :, 1, :].un(2).bc([128, 8, 16, 16]), ALU.add)
            for h in range(8):
                P.op("dve", lambda E, h=h: E.max(out=top.ap[:, h, 0:8], in_=cand.ap[:, h, :]), r=[cand.buf], w=[top.buf])
                P.op("dve", lambda E, h=h: E.max_index(out=pos.ap[:, h, 0:8], in_max=top.ap[:, h, 0:8], in_values=cand.ap[:, h, :]),
                     r=[cand.buf, top.buf], w=[pos.buf])
                P.op("dve", lambda E, h=h: E.match_replace(out=S2.ap, in_to_replace=top.ap[:, h, 0:8], in_values=cand.ap[:, h, :],
                                                           imm_value=NEG), r=[cand.buf, top.buf], w=[S2.buf])
                P.op("dve", lambda E, h=h: E.max(out=top.ap[:, h, 8:16], in_=S2.ap), r=[S2.buf], w=[top.buf])
                P.op("dve", lambda E, h=h: E.max_index(out=pos.ap[:, h, 8:16], in_max=top.ap[:, h, 8:16], in_values=S2.ap),
                     r=[S2.buf, top.buf], w=[pos.buf])
            P.op("dve", lambda E: E.tensor_single_scalar(out=pa_.ap, in_=pos.ap, scalar=4, op=ALU.logical_shift_right),
                 r=[pos.buf], w=[pa_.buf])
            P.op("dve", lambda E: E.tensor_single_scalar(out=pb_.ap, in_=pos.ap, scalar=15, op=ALU.bitwise_and),
                 r=[pos.buf], w=[pb_.buf])
            P.cp("dve", af, pa_)
            P.cp("dve", bf, pb_)
            io4 = iota16.un(1).un(1).bc([128, 8, 16, 16])
            P.tt("dve", eq, af.un(3).bc([128, 8, 16, 16]), io4, ALU.is_equal)
            P.tt("dve", eq, eq, I4[:, :, 0, :].un(2).bc([128, 8, 16, 16]), ALU.mult)
            P.reduce(i1s, eq, ALU.add)
            P.tt("dve", eq, bf.un(3).bc([128, 8, 16, 16]), io4, ALU.is_equal)
            P.tt("dve", eq, eq, I4[:, :, 1, :].un(2).bc([128, 8, 16, 16]), ALU.mult)
            P.reduce(i2s, eq, ALU.add)
            P.tt("dve", gw, top, top[:, :, 0:1].bc([128, 8, 16]), ALU.subtract)
            P.act(gw, gw, AF.Exp)
            P.reduce(gsum, gw, ALU.add)
            P.recip(gsum, gsum)
            P.tt("dve", gw, gw, gsum.un(2).bc([128, 8, 16]), ALU.mult)
            for src, dst in ((i1s, i1T), (i2s, i2T), (gw, gT)):
                ps = ps_next()
                P.tr(ps[:, 0:128], src.r("p h k -> p (h k)"), ident_f)
                P.cp("act", dst, ps[:, 0:128])
            io3 = iota128.un(1).bc([128, 128, 128])
            P.tt("dve", Pm, io3, i1T.un(2).bc([128, 128, 128]), ALU.is_equal)
            P.tt("dve", Qm, io3, i2T.un(2).bc([128, 128, 128]), ALU.is_equal)
            P.tt("dve", Qm, Qm, gT.un(2).bc([128, 128, 128]), ALU.mult)
            for t8 in range(16):
                ps = ps_next()
                for k in range(8):
                    t_ = t8 * 8 + k
                    P.mm(ps[:, k * 128:(k + 1) * 128], Pm[:, t_, :], Qm[:, t_, :])
                P.cp("act", Gst[:, :, t8 * 8:(t8 + 1) * 8].r("p i t -> p t i"), ps.r("p (t i) -> p t i", t=8))
            P.dma("sp", Gd[:, :, rows], Gst)
        P.barrier()
        Z = Stack(nc, Zbase, SB_TOP)
        ACC = Z.alloc([NTT, D], F32, "ACC")
        Z2 = Stack(nc, Z.cur, SB_TOP)
        JG = 4
        ustg = [Z2.alloc([D], F32, "ustg%d" % i) for i in range(3)]
        vstg = [Z2.alloc([D], F32, "vstg%d" % i) for i in range(2)]
        ubf = [Z2.alloc([D], BF16, "ubf%d" % i) for i in range(2)]
        uTt = [Z2.alloc([16, 128], BF16, "uTt%d" % i) for i in range(3)]
        vbf = [Z2.alloc([D], BF16, "vbf%d" % b) for b in range(JG)]
        Wt = [Z2.alloc([T], BF16, "Wt%d" % b) for b in range(JG)]
        Gt = [Z2.alloc([T], BF16, "Gt%d" % i) for i in range(2)]
        ge = [Z2.alloc([T], BF16, "ge%d" % i) for i in range(2)]
        psT = PS[0]
        psA = [PS[1], PS[2]]
        psO = [TT(PS[3].ap[:, 0:512], Buf("psO0", "ps")), TT(PS[3].ap[:, 512:1024], Buf("psO1", "ps"))]
        urows = W["peer_u"][l].r("(i j) d -> j i d", j=128)
        vrows = W["peer_v"][l].r("(i j) d -> j i d", j=128)
        NJ = 128

        def load_cast(j):
            s_u = ustg[j % 3]
            P.dma("sp", s_u, urows[j])
            P.cp("dve", ubf[j % 2], s_u)

        def trans(j):
            psb = psT.bitcast(BF16).r("p (k i) -> p k i", k=16)
            ub = ubf[j % 2]
            for kt in range(16):
                P.tr(psb[:, kt, :], ub[:, kt * 128:(kt + 1) * 128], ident_b)

        def evac(j):
            psb = psT.bitcast(BF16).r("p (k i) -> p k i", k=16)
            P.cp("act", uTt[j % 3], psb)

        load_cast(0)
        trans(0)
        evac(0)
        load_cast(1)
        trans(1)
        evac(1)
        load_cast(2)
        P.dma("sp", Gt[0], Gd[:, 0, :])
        for j in range(NJ):
            jj = j % JG
            if j + 3 < NJ:
                load_cast(j + 3)
            s_v = vstg[j % 2]
            P.dma("sp", s_v, vrows[j])
            P.cp("act", vbf[jj], s_v)
            if j + 1 < NJ:
                P.dma("sp", Gt[(j + 1) % 2], Gd[:, j + 1, :])
            pa2 = psA[j % 2]
            ut = uTt[j % 3]
            for half in range(2):
                hs = slice(half * 512, half * 512 + 512)
                for kt in range(16):
                    P.mm(pa2[:, hs], ut[:, kt, :], h2T[:, kt, hs], start=(kt == 0), stop=(kt == 15))
            if j + 2 < NJ:
                trans(j + 2)
            e_ = ge[j % 2]
            P.act(e_, pa2, AF.Gelu_apprx_tanh)
            if j + 2 < NJ:
                evac(j + 2)
            P.tt("dve", Wt[jj], e_, Gt[j % 2], ALU.mult)
            if jj == JG - 1:
                first = (j == JG - 1)
                for tt_ in range(NTT):
                    for dc in range(4):
                        po = psO[(tt_ * 4 + dc) % 2]
                        for q_ in range(JG):
                            P.mm(po, Wt[q_][:, tt_ * 128:(tt_ + 1) * 128], vbf[q_][:, dc * 512:(dc + 1) * 512],
                                 start=(q_ == 0), stop=(q_ == JG - 1))
                        dst = ACC[:, tt_, dc * 512:(dc + 1) * 512]
                        if first:
                            P.cp("act", dst, po)
                        else:
                            P.tt("dve", dst, dst, po, ALU.add)
        P.barrier()
        Z2.reset()
        x1 = [Z2.alloc([D], F32, "x1_%d" % i) for i in range(2)]
        tq2 = Z2.alloc([D], F32, "tq2")
        bc3 = {k: Z2.alloc([D], F32, k) for k in ("gt2", "g2", "b2")}
        st = Z2.alloc([4, 6], F32, "stC")
        mv = Z2.alloc([2], F32, "mvC")
        rs = Z2.alloc([1], F32, "rsC")
        bload(bc3["gt2"], mod_d[l], u, 5 * D)
        bload(bc3["g2"], W["ln2_g"], l, 0)
        bload(bc3["b2"], W["ln2_b"], l, 0)
        for tt_ in range(NTT):
            rows = slice(tt_ * 128, tt_ * 128 + 128)
            x1_ = x1[tt_ % 2]
            acc = ACC[:, tt_, :]
            P.dma("sp", x1_, x1res[rows, :], g_x[0])
            if tt_ == 0:
                dbg_store("peer", acc)
            P.tt("dve", acc, acc, bc3["gt2"], ALU.mult)
            P.stt(acc, x1_, ALPHA, acc, ALU.mult, ALU.add)
            ln_tm(acc, tq2, st, mv, rs)
            P.tt("dve", tq2, tq2, bc3["g2"], ALU.mult)
            P.tt("dve", x1_, tq2, bc3["b2"], ALU.add)
            if last:
                P.dma("sp", yout[u][rows, :], x1_, g_st)
            else:
                P.dma("sp", xres[u][rows, :], x1_, g_st)
        P.barrier()

    phase_mod()
    ret = None
    for l in range(nlayers):
        phase_setup(l)
        if stop == "setup":
            break
        for u in units:
            ret = phase_A(l, u)
            if stop is not None and stop.startswith("A"):
                break
            phase_B(l, u, last=(l == nlayers - 1))
            if stop is not None:
                break
        if stop is not None:
            break
    if ret is not None and "ret" in dbg_out:
        src = ret
        P.dma("sp", dbg_out["ret"], src, g_st)
    P.final_wait()
    P.emit()
    return nc


def _grid_pos():
    rows = T // 64
    t = np.arange(rows * 64)
    r = (t // 64).astype(np.float32)
    col = (t % 64).astype(np.float32)
    nf = D // 4
    freq = (1.0 / (np.float32(10000.0) ** (np.arange(nf, dtype=np.float32) / np.float32(nf)))).astype(np.float32)
    ar = r[:, None] * freq
    ac = col[:, None] * freq
    return np.concatenate([np.sin(ar), np.cos(ar), np.sin(ac), np.cos(ac)], -1).astype(np.float32)


WEIGHT_NAMES = ["w_mod", "b_mod", "w_in", "s5_lam_re", "s5_lam_im", "s5_log_dt", "s5_b_re", "s5_b_im", "s5_c_re",
                "s5_c_im", "s5_d", "s5_w_glu", "sc_conv_w", "sc_conv_b", "cf_conv_w", "cf_conv_b", "cf_ln_g", "cf_ln_b",
                "w_pa", "w_pb", "w_pc", "w_o", "ln1_g", "ln1_b", "peer_w_q", "peer_k1", "peer_k2", "peer_u", "peer_v",
                "ln2_g", "ln2_b"]


def make_in_maps(inputs, cores=range(NCORE)):
    f = lambda a: np.ascontiguousarray(np.asarray(a, dtype=np.float32))
    xp = f(inputs["x_prompt"])
    xs = f(inputs["x_sample"])
    stt = f(inputs["state_ssm"])
    c = f(inputs["c"])
    cc = f(inputs["c_ctx"])
    pe = _grid_pos()
    ident = np.eye(128, dtype=np.float32)
    iota = np.tile(np.arange(128, dtype=np.float32)[None, :], (128, 1))
    wts = {k: f(inputs[k]) for k in WEIGHT_NAMES}
    maps = []
    for i in cores:
        m = dict(wts)
        m["xp"] = np.ascontiguousarray(xp[4 * i:4 * i + 4].reshape(T, D))
        m["xs"] = np.ascontiguousarray(xs[i].reshape(T, D))
        m["pe"] = pe
        m["st0"] = np.ascontiguousarray(stt[i])
        m["cond"] = np.ascontiguousarray(np.stack([cc, c[i]], 0))
        m["ident"] = ident
        m["iota128"] = iota
        maps.append(m)
    return maps


def kernel(**inputs):
    nc = build()
    maps = make_in_maps(inputs)
    maps = [{k: m[k] for k in build.declared} for m in maps]
    res = run_bass_kernel_spmd(nc, maps, core_ids=list(range(NCORE)))
    yp = np.concatenate([r["yp"].reshape(4, 256, D) for r in res.results], 0).astype(np.float32)
    ys = np.stack([r["ys"].reshape(T, D) for r in res.results], 0).astype(np.float32)
    nst = np.concatenate([r["nst"] for r in res.results], 0).astype(np.float32)
    return (yp, ys, nst)
```

```python
import math
from contextlib import ExitStack

import numpy as np
import concourse.bass as bass
import concourse.mybir as mybir
from concourse.bass_utils import run_bass_kernel_spmd

F32 = mybir.dt.float32
BF16 = mybir.dt.bfloat16
I32 = mybir.dt.int32
U32 = mybir.dt.uint32
ALU = mybir.AluOpType
AF = mybir.ActivationFunctionType
AX = mybir.AxisListType

D = 2048
NCORE = 8
T = 1024
NTT = 8
KT = 16
NL = 2
ALPHA = 4.0 ** 0.25
LN_EPS = 1e-6
IN_COLS = 9728
GATE0 = 3584
NEG = -1.0e30


class Buf:
    __slots__ = ("name", "lw", "rd", "space", "dsem")

    def __init__(self, name, space="sb"):
        self.name = name
        self.lw = {}
        self.rd = {}
        self.space = space
        self.dsem = None


class Grp:
    __slots__ = ("sem", "cnt")

    def __init__(self, sem):
        self.sem = sem
        self.cnt = 0


class TT:
    __slots__ = ("ap", "buf")

    def __init__(self, ap, buf):
        self.ap = ap
        self.buf = buf

    def __getitem__(self, k):
        return TT(self.ap[k], self.buf)

    def r(self, pat, **kw):
        return TT(self.ap.rearrange(pat, **kw), self.buf)

    def bc(self, shape):
        return TT(self.ap.to_broadcast(list(shape)), self.buf)

    def un(self, ax):
        return TT(self.ap.unsqueeze(ax), self.buf)

    def bitcast(self, dt):
        return TT(self.ap.bitcast(dt), self.buf)


def _a(x):
    return x.ap if isinstance(x, TT) else x


class Prog:
    def __init__(self, nc, es):
        self.nc = nc
        self.es = es
        self.E = dict(pe=nc.tensor, dve=nc.vector, act=nc.scalar, pool=nc.gpsimd, sp=nc.sync)
        self.rec = {k: [] for k in self.E}
        self.esem = {k: es.enter_context(nc.semaphore("es_" + k)) for k in self.E}
        self.ecnt = {k: 0 for k in self.E}
        self.known = {k: {} for k in self.E}
        self.grps = []
        self.free = []
        self.active = []
        self.max_dsem = 84

    def grp(self, name):
        g = Grp(self.es.enter_context(self.nc.semaphore(name)))
        self.grps.append(g)
        return g

    def dsem_for(self, buf):
        if buf.dsem is None:
            if self.free:
                buf.dsem = self.free.pop()
            else:
                assert len(self.grps) < self.max_dsem, "out of DMA semaphores"
                buf.dsem = self.grp("dq%d" % len(self.grps))
            self.active.append(buf)
        return buf.dsem

    def op(self, e, fn, r=(), w=(), grp=None):
        need = {}
        own = self.esem[e]

        def add(tok):
            sem, val = tok
            if e == "pe" and sem is own:
                return
            k = id(sem)
            if k not in need or need[k][1] < val:
                need[k] = (sem, val)

        for b in list(r) + list(w):
            for tok in b.lw.values():
                add(tok)
        for b in w:
            for tok in b.rd.values():
                add(tok)
        waits = []
        kn = self.known[e]
        for k, (sem, val) in need.items():
            if kn.get(k, 0) >= val:
                continue
            kn[k] = val
            waits.append((sem, val))
        if grp is not None:
            grp.cnt += 16
            tok = (grp.sem, grp.cnt)
            inc = (grp.sem, 16)
        else:
            self.ecnt[e] += 1
            tok = (own, self.ecnt[e])
            inc = (own, 1)
        for b in w:
            if b.space == "dram":
                b.lw[id(tok[0])] = tok
            else:
                b.lw = {id(tok[0]): tok}
            b.rd = {}
        for b in r:
            if b in w:
                continue
            b.rd[id(tok[0])] = tok
        self.rec[e].append((waits, fn, inc))

    def barrier(self):
        toks = [(self.esem[k], self.ecnt[k]) for k in self.E] + [(g.sem, g.cnt) for g in self.grps]
        for e in self.E:
            waits = []
            kn = self.known[e]
            for sem, val in toks:
                if val == 0 or sem is self.esem[e]:
                    continue
                if kn.get(id(sem), 0) >= val:
                    continue
                kn[id(sem)] = val
                waits.append((sem, val))
            if waits:
                self.rec[e].append((waits, None, None))
        for b in self.active:
            self.free.append(b.dsem)
            b.dsem = None
        self.active = []

    def final_wait(self):
        e = "sp"
        waits = [(self.esem[k], self.ecnt[k]) for k in self.E if k != e and self.ecnt[k]]
        waits += [(g.sem, g.cnt) for g in self.grps if g.cnt]
        self.rec[e].append((waits, None, None))

    def emit(self):
        with self.nc.Block() as blk:
            def mk(e):
                def body(Eng):
                    for waits, fn, inc in self.rec[e]:
                        for sem, val in waits:
                            Eng.wait_ge(sem, val)
                        if fn is not None:
                            fn(Eng).then_inc(inc[0], inc[1])
                return body
            blk.tensor(mk("pe"))
            blk.vector(mk("dve"))
            blk.scalar(mk("act"))
            blk.gpsimd(mk("pool"))
            blk.sync(mk("sp"))

    def dma(self, q, out, in_, grp=None, **kw):
        if out.buf.space == "sb":
            g = self.dsem_for(out.buf)
        elif in_.buf.space == "sb":
            g = self.dsem_for(in_.buf)
        else:
            g = self.dsem_for(out.buf)
        self.op(q, lambda E: E.dma_start(out=out.ap, in_=in_.ap, **kw), r=[in_.buf], w=[out.buf], grp=g)

    def mm(self, out, lhsT, rhs, start=True, stop=True):
        self.op("pe", lambda E: E.matmul(out=out.ap, lhsT=lhsT.ap, rhs=rhs.ap, start=start, stop=stop),
                r=[lhsT.buf, rhs.buf], w=[out.buf])

    def tr(self, out, in_, ident):
        self.op("pe", lambda E: E.transpose(out=out.ap, in_=in_.ap, identity=ident.ap),
                r=[in_.buf, ident.buf], w=[out.buf])

    def act(self, out, in_, func, bias=None, scale=None, e="act"):
        rb = [in_.buf]
        kw = {}
        if bias is not None:
            kw["bias"] = _a(bias)
            if isinstance(bias, TT):
                rb.append(bias.buf)
        if scale is not None:
            kw["scale"] = _a(scale)
            if isinstance(scale, TT):
                rb.append(scale.buf)
        self.op("act", lambda E: E.activation(out=out.ap, in_=in_.ap, func=func, **kw), r=rb, w=[out.buf])

    def cp(self, e, out, in_):
        if e == "act":
            self.op("act", lambda E: E.copy(out=out.ap, in_=in_.ap), r=[in_.buf], w=[out.buf])
        else:
            self.op(e, lambda E: E.tensor_copy(out=out.ap, in_=in_.ap), r=[in_.buf], w=[out.buf])

    def tt(self, e, out, a, b, op):
        self.op(e, lambda E: E.tensor_tensor(out=out.ap, in0=a.ap, in1=b.ap, op=op), r=[a.buf, b.buf], w=[out.buf])

    def ts(self, e, out, a, s1, op0, s2=None, op1=None):
        rb = [a.buf] + [x.buf for x in (s1, s2) if isinstance(x, TT)]
        if op1 is None:
            self.op(e, lambda E: E.tensor_scalar(out=out.ap, in0=a.ap, scalar1=_a(s1), scalar2=None, op0=op0),
                    r=rb, w=[out.buf])
        else:
            self.op(e, lambda E: E.tensor_scalar(out=out.ap, in0=a.ap, scalar1=_a(s1), scalar2=_a(s2), op0=op0, op1=op1),
                    r=rb, w=[out.buf])

    def stt(self, out, a, s, b, op0, op1, e="dve"):
        rb = [a.buf, b.buf] + ([s.buf] if isinstance(s, TT) else [])
        self.op(e, lambda E: E.scalar_tensor_tensor(out=out.ap, in0=a.ap, scalar=_a(s), in1=b.ap, op0=op0, op1=op1),
                r=rb, w=[out.buf])

    def memset(self, e, out, val):
        self.op(e, lambda E: E.memset(out.ap, val), w=[out.buf])

    def recip(self, out, in_):
        self.op("dve", lambda E: E.reciprocal(out=out.ap, in_=in_.ap), r=[in_.buf], w=[out.buf])

    def scan(self, out, d0, d1, init):
        rb = [d0.buf, d1.buf] + ([init.buf] if isinstance(init, TT) else [])
        self.op("dve", lambda E: E.tensor_tensor_scan(out=out.ap, data0=d0.ap, data1=d1.ap, initial=_a(init),
                                                       op0=ALU.mult, op1=ALU.add), r=rb, w=[out.buf])

    def reduce(self, out, in_, op, axis=AX.X):
        self.op("dve", lambda E: E.tensor_reduce(out=out.ap, in_=in_.ap, axis=axis, op=op), r=[in_.buf], w=[out.buf])


class Stack:
    cnt = [0]

    def __init__(self, nc, base, limit):
        self.nc = nc
        self.base = base
        self.cur = base
        self.limit = limit

    def alloc(self, free_shape, dtype, name):
        nb = int(np.prod(free_shape)) * (2 if dtype == BF16 else 4)
        off = (self.cur + 31) // 32 * 32
        assert off + nb <= self.limit, (name, off, nb, self.limit)
        self.cur = off + nb
        Stack.cnt[0] += 1
        h = self.nc.alloc_sbuf_tensor_at("%s_%d" % (name, Stack.cnt[0]), [128] + list(free_shape), dtype, offset=off)
        return TT(h.ap(), Buf(name))

    def sub(self, nbytes):
        off = (self.cur + 31) // 32 * 32
        assert off + nbytes <= self.limit, (off, nbytes, self.limit)
        self.cur = off + nbytes
        return Stack(self.nc, off, off + nbytes)

    def reset(self):
        self.cur = self.base


def build(dbg=None, stop=None, nlayers=NL, units=(0, 1), b3_tiles=NTT):
    dbg = dbg or {}
    Stack.cnt[0] = 0
    nc = bass.Bass("TRN2", target_bir_lowering=False)
    es = ExitStack()
    P = Prog(nc, es)

    declared = []
    build.declared = declared

    def din(name, shape, dt=F32):
        declared.append(name)
        return TT(nc.dram_tensor(name, list(shape), dt, kind="ExternalInput").ap(), Buf(name, "dram"))

    def dout(name, shape, dt=F32):
        return TT(nc.dram_tensor(name, list(shape), dt, kind="ExternalOutput").ap(), Buf(name, "dram"))

    def dscr(name, shape, dt=F32):
        return TT(nc.dram_tensor(name, list(shape), dt, kind="Internal").ap(), Buf(name, "dram"))

    xin = [din("xp", [T, D]), din("xs", [T, D])]
    pe_d = din("pe", [T, D])
    st0 = din("st0", [NL, 2, 64, 64, 2])
    cond = din("cond", [2, D])
    ident_d = din("ident", [128, 128])
    iota_d = din("iota128", [128, 128])
    WSH = dict([("w_mod", [NL, D, 6 * D]), ("b_mod", [NL, 6 * D]), ("w_in", [NL, D, IN_COLS]),
                ("s5_lam_re", [NL, 2, 64, 64]), ("s5_lam_im", [NL, 2, 64, 64]), ("s5_log_dt", [NL, 2, 64]),
                ("s5_b_re", [NL, 2, 64, 64, 16]), ("s5_b_im", [NL, 2, 64, 64, 16]),
                ("s5_c_re", [NL, 2, 64, 16, 64]), ("s5_c_im", [NL, 2, 64, 16, 64]),
                ("s5_d", [NL, 1024]), ("s5_w_glu", [NL, 1024, 1024]),
                ("sc_conv_w", [NL, 3, 512]), ("sc_conv_b", [NL, 512]),
                ("cf_conv_w", [NL, 31, 512]), ("cf_conv_b", [NL, 512]),
                ("cf_ln_g", [NL, 512]), ("cf_ln_b", [NL, 512]),
                ("w_pa", [NL, 1024, D]), ("w_pb", [NL, 512, D]), ("w_pc", [NL, 512, D]), ("w_o", [NL, D, D]),
                ("ln1_g", [NL, D]), ("ln1_b", [NL, D]),
                ("peer_w_q", [NL, D, D]), ("peer_k1", [NL, 128, 128]), ("peer_k2", [NL, 128, 128]),
                ("peer_u", [NL, 16384, D]), ("peer_v", [NL, 16384, D]),
                ("ln2_g", [NL, D]), ("ln2_b", [NL, D])])

    class LazyW(dict):
        def __missing__(self, nm):
            self[nm] = din(nm, WSH[nm])
            return self[nm]
    W = LazyW()
    yout = [dout("yp", [T, D]), dout("ys", [T, D])]
    nst_o = dout("nst", [4, NL, 2, 64, 64, 2])
    dbg_out = {k: dout("dbg_" + k, v[0], v[1]) for k, v in dbg.items()}

    xres = [dscr("xres0", [T, D]), dscr("xres1", [T, D])]
    ymix = dscr("ymix", [T, D])
    x1res = dscr("x1res", [T, D])
    h2res = dscr("h2res", [T, D])
    mod_d = dscr("mod_d", [NL, 2, 6 * D])
    tabs = dscr("tabs", [64, 128, 2, 260])
    wbd = dscr("wbd", [8, 128, 16, 128], BF16)
    cwd = dscr("cwd", [8, 128, 16, 64], BF16)
    Gd = dscr("Gd", [128, 128, T], BF16)

    psh = nc.alloc_psum_tensor("psum_all", [128, 4096], F32)
    PS = [TT(psh.ap()[:, i * 1024:(i + 1) * 1024], Buf("ps%d" % i, "ps")) for i in range(4)]
    ps_rr = [0]

    def ps_next():
        ps_rr[0] = (ps_rr[0] + 1) % 4
        return PS[ps_rr[0]]

    SB_BASE = (int(nc.sbuf_base) + 63) // 64 * 64
    SB_TOP = int(nc.sbuf_top)
    root = Stack(nc, SB_BASE, SB_TOP)
    G = root.sub(4 * 1024)
    LP = root.sub(4608)
    UL = Stack(nc, root.cur, SB_TOP)

    g_st = g_c = None
    g_x = g_w = g_t = [None, None]

    ident_f = G.alloc([128], F32, "ident_f")
    ident_b = G.alloc([128], BF16, "ident_b")
    iota128 = G.alloc([128], F32, "iota128")
    iota16 = iota128[:, 0:16]
    halfpi = G.alloc([1], F32, "halfpi")
    X4 = G.alloc([4, 128], BF16, "X4")
    CWst = G.alloc([4, 64], BF16, "CWst")
    ones_f = G.alloc([128], F32, "ones_f")
    P.dma("sp", ident_f, ident_d, g_c)
    P.dma("sp", iota128, iota_d, g_c)
    P.cp("dve", ident_b, ident_f)
    P.memset("dve", halfpi, math.pi / 2)
    P.memset("dve", X4, 0.0)
    P.memset("dve", CWst, 0.0)
    P.memset("dve", ones_f, 1.0 / 512.0)

    Rt = LP.alloc([64], F32, "Rt")
    DSK = LP.alloc([8], F32, "DSK")
    H0 = LP.alloc([64, 2], F32, "H0")
    SCW = LP.alloc([4, 3], F32, "SCW")
    SCB = LP.alloc([4], F32, "SCB")
    CFW = LP.alloc([4, 31], F32, "CFW")
    CFB = LP.alloc([4], F32, "CFB")
    CFG = LP.alloc([4], F32, "CFG")
    CFBB = LP.alloc([4], F32, "CFBB")
    KTb = LP.alloc([2, 128], BF16, "KTb")
    NST = LP.alloc([64, 4, 2], F32, "NST")

    WSTG_B = 16 * 256 * 4
    WBF_B = 16 * 256 * 2

    def dbg_store(key, src, dst_slice=None):
        if key in dbg_out:
            dst = dbg_out[key] if dst_slice is None else dbg_out[key][dst_slice]
            P.dma("sp", dst, src, g_st)

    def phase_mod():
        UL.reset()
        condT = UL.alloc([16, 2], F32, "condT")
        bmod = UL.alloc([6 * D], F32, "bmod")
        stg = [UL.alloc([16, 256], F32, "mstg%d" % i) for i in range(2)]
        mo = [UL.alloc([256], F32, "mo%d" % i) for i in range(2)]
        for r_ in range(2):
            P.dma("sp", condT[:, :, r_], cond[r_].r("(kt p) -> p kt", p=128), g_c, allow_slow_non_contiguous=True)
        P.act(condT, condT, AF.Silu)
        for l in range(nlayers):
            P.dma("sp", bmod[0:2, :], TT(W["b_mod"].ap[l:l + 1, :].partition_broadcast(2), W["b_mod"].buf), g_c)
            for c in range(48):
                s_ = stg[c % 2]
                P.dma("sp", s_, W["w_mod"][l][:, c * 256:(c + 1) * 256].r("(kt p) c -> p kt c", p=128), g_w[c % 2])
                ps = ps_next()
                for kt in range(16):
                    P.mm(ps[0:2, 0:256], condT[:, kt, :], s_[:, kt, :], start=(kt == 0), stop=(kt == 15))
                m_ = mo[c % 2]
                P.tt("dve", m_[0:2, :], ps[0:2, 0:256], bmod[0:2, c * 256:(c + 1) * 256], ALU.add)
                if (8 <= c < 16) or (32 <= c < 40):
                    P.ts("dve", m_[0:2, :], m_[0:2, :], 1.0, ALU.add)
                P.dma("sp", mod_d[l][:, c * 256:(c + 1) * 256], m_[0:2, :], g_st)
        P.barrier()

    def phase_setup(l):
        UL.reset()
        A = UL
        nat = [A.alloc([128], F32, "nat%d" % i) for i in range(3)]
        dt2 = A.alloc([2], F32, "dt2")
        LR = A.alloc([64], F32, "LR")
        LI = A.alloc([64], F32, "LI")
        DT = A.alloc([64], F32, "DT")
        P.dma("sp", nat[0][0:64, :], W["s5_lam_re"][l].r("d (gp g2) p -> (d gp) (g2 p)", g2=2), g_c)
        P.dma("sp", nat[1][0:64, :], W["s5_lam_im"][l].r("d (gp g2) p -> (d gp) (g2 p)", g2=2), g_c)
        P.dma("sp", dt2[0:64, :], W["s5_log_dt"][l].r("d (gp g2) -> (d gp) g2", g2=2), g_c)
        P.cp("dve", nat[2][0:64, :].r("q (a b) -> q a b", a=2), dt2[0:64, :].un(2).bc([64, 2, 64]))
        for i, dst in enumerate((LR, LI, DT)):
            ps = ps_next()
            P.tr(ps[:, 0:64], nat[i][0:64, :], ident_f[0:64, 0:64])
            P.cp("act", dst, ps[:, 0:64])
        v = {k: A.alloc([64], F32, k) for k in ("dt", "ang", "c", "s", "t1", "t2", "are", "aim", "den", "am1", "zre", "zim")}
        P.act(v["dt"], DT, AF.Exp)
        P.tt("dve", v["t1"], LR, v["dt"], ALU.mult)
        P.act(Rt, v["t1"], AF.Exp)
        P.tt("dve", v["ang"], LI, v["dt"], ALU.mult)
        P.act(v["s"], v["ang"], AF.Sin, scale=1.0 / 32.0)
        P.act(v["c"], v["ang"], AF.Sin, bias=halfpi[:, 0:1], scale=1.0 / 32.0)
        for _ in range(5):
            P.tt("dve", v["t1"], v["c"], v["c"], ALU.mult)
            P.tt("dve", v["t2"], v["s"], v["s"], ALU.mult)
            P.stt(v["s"], v["c"], 2.0, v["s"], ALU.mult, ALU.mult)
            P.tt("dve", v["c"], v["t1"], v["t2"], ALU.subtract)
        C1, S1 = v["c"], v["s"]
        P.tt("dve", v["are"], Rt, C1, ALU.mult)
        P.tt("dve", v["aim"], Rt, S1, ALU.mult)
        P.tt("dve", v["t1"], LR, LR, ALU.mult)
        P.tt("dve", v["t2"], LI, LI, ALU.mult)
        P.tt("dve", v["den"], v["t1"], v["t2"], ALU.add)
        P.recip(v["den"], v["den"])
        P.ts("dve", v["am1"], v["are"], -1.0, ALU.add)
        P.tt("dve", v["t1"], v["am1"], LR, ALU.mult)
        P.tt("dve", v["t2"], v["aim"], LI, ALU.mult)
        P.tt("dve", v["t1"], v["t1"], v["t2"], ALU.add)
        P.tt("dve", v["zre"], v["t1"], v["den"], ALU.mult)
        P.tt("dve", v["t1"], v["aim"], LR, ALU.mult)
        P.tt("dve", v["t2"], v["am1"], LI, ALU.mult)
        P.tt("dve", v["t1"], v["t1"], v["t2"], ALU.subtract)
        P.tt("dve", v["zim"], v["t1"], v["den"], ALU.mult)
        BRE = A.alloc([64, 16], F32, "BRE")
        BIM = A.alloc([64, 16], F32, "BIM")
        tA = A.alloc([64, 16], F32, "tA")
        tB = A.alloc([64, 16], F32, "tB")
        BB = [A.alloc([64, 16], BF16, "BBre"), A.alloc([64, 16], BF16, "BBim")]
        P.dma("sp", BRE, W["s5_b_re"][l].r("d (gp g2) p s -> (g2 p) (d gp) s", g2=2), g_c)
        P.dma("sp", BIM, W["s5_b_im"][l].r("d (gp g2) p s -> (g2 p) (d gp) s", g2=2), g_c)
        zr = v["zre"].un(2).bc([128, 64, 16])
        zi = v["zim"].un(2).bc([128, 64, 16])
        P.tt("dve", tA, BRE, zr, ALU.mult)
        P.tt("dve", tB, BIM, zi, ALU.mult)
        P.tt("dve", BB[0], tA, tB, ALU.subtract)
        P.tt("dve", tA, BIM, zr, ALU.mult)
        P.tt("dve", tB, BRE, zi, ALU.mult)
        P.tt("dve", BB[1], tA, tB, ALU.add)
        wst = [A.alloc([4, 128], BF16, "wst%d" % i) for i in range(2)]
        it = 0
        for d in range(2):
            for ct in range(8):
                rt0 = d * 32 + ct * 4
                for reim in range(2):
                    for j in range(4):
                        P.cp("dve", X4[0:64, j, 32 * j:32 * j + 16], BB[reim][0:64, rt0 + j, :])
                        P.cp("dve", X4[64:128, j, 32 * j + 16:32 * j + 32], BB[reim][64:128, rt0 + j, :])
                    ps = ps_next()
                    psb = ps[:, 0:256].bitcast(BF16).r("p (j c) -> p j c", j=4)
                    for j in range(4):
                        P.tr(psb[:, j, :], X4[:, j, :], ident_b)
                    ws_ = wst[it % 2]
                    it += 1
                    P.cp("act", ws_, psb)
                    P.dma("sp", wbd[ct][:, d * 8 + reim:d * 8 + 8:2, :], ws_, g_st)
        CN = [A.alloc([64], F32, "CN%d" % i) for i in range(2)]
        CN2 = A.alloc([128], BF16, "CN2")
        cst = [A.alloc([4, 64], BF16, "cst%d" % i) for i in range(2)]
        it = 0
        for d in range(2):
            for ct in range(8):
                for reim in range(2):
                    src = W["s5_c_im" if reim else "s5_c_re"][l, d, 8 * ct:8 * ct + 8].r("g s p -> (g s) p")
                    cn = CN[it % 2]
                    P.dma("sp", cn, src, g_t[it % 2])
                    sgn = -1.0 if reim else 1.0
                    P.ts("dve", CN2[:, 0:64], cn, sgn, ALU.mult)
                    P.ts("dve", CN2[:, 64:128], cn, sgn, ALU.mult)
                    ps = ps_next()
                    psb = ps[:, 0:64].bitcast(BF16)
                    P.tr(psb, CN2, ident_b)
                    for j in range(4):
                        jl = j % 2
                        P.cp("dve", CWst[0:64, j, 32 * jl:32 * jl + 16], psb[0:64, 32 * j:32 * j + 16])
                        P.cp("dve", CWst[64:128, j, 32 * jl + 16:32 * jl + 32], psb[64:128, 32 * j + 16:32 * j + 32])
                    cs_ = cst[it % 2]
                    it += 1
                    P.cp("dve", cs_, CWst)
                    P.dma("sp", cwd[ct][:, d * 8 + reim:d * 8 + 8:2, :], cs_, g_st)
        ER = A.alloc([16, 260], F32, "ER")
        EI = A.alloc([16, 260], F32, "EI")
        q = {k: A.alloc([16, 128], F32, k) for k in ("q1", "q2", "q3", "q4")}
        MR = A.alloc([16], F32, "MR")
        MI = A.alloc([16], F32, "MI")
        m1 = A.alloc([16], F32, "m1")
        m2 = A.alloc([16], F32, "m2")
        for gq in range(4):
            sl = slice(gq * 16, gq * 16 + 16)
            P.memset("dve", ER[:, :, 0:1], 1.0)
            P.memset("dve", EI[:, :, 0:1], 0.0)
            P.cp("dve", MR, C1[:, sl])
            P.cp("dve", MI, S1[:, sl])
            k = 1
            while k < 260:
                cnt = min(k, 260 - k)
                o = 0
                while o < cnt:
                    n = min(128, cnt - o)
                    mrb = MR.un(2).bc([128, 16, n])
                    mib = MI.un(2).bc([128, 16, n])
                    sr = ER[:, :, o:o + n]
                    si = EI[:, :, o:o + n]
                    P.tt("dve", q["q1"][:, :, 0:n], sr, mrb, ALU.mult)
                    P.tt("dve", q["q2"][:, :, 0:n], si, mib, ALU.mult)
                    P.tt("dve", q["q3"][:, :, 0:n], sr, mib, ALU.mult)
                    P.tt("dve", q["q4"][:, :, 0:n], si, mrb, ALU.mult)
                    P.tt("dve", ER[:, :, k + o:k + o + n], q["q1"][:, :, 0:n], q["q2"][:, :, 0:n], ALU.subtract)
                    P.tt("dve", EI[:, :, k + o:k + o + n], q["q3"][:, :, 0:n], q["q4"][:, :, 0:n], ALU.add)
                    o += n
                if 2 * k < 260:
                    P.tt("dve", m1, MR, MR, ALU.mult)
                    P.tt("dve", m2, MI, MI, ALU.mult)
                    P.stt(MI, MR, 2.0, MI, ALU.mult, ALU.mult)
                    P.tt("dve", MR, m1, m2, ALU.subtract)
                k *= 2
            P.dma("sp", tabs[gq * 16:gq * 16 + 16, :, 0, :].r("rt p t -> p rt t"), ER, g_st)
            P.dma("sp", tabs[gq * 16:gq * 16 + 16, :, 1, :].r("rt p t -> p rt t"), EI, g_st)
        P.dma("sp", DSK, W["s5_d"][l].r("(ct p) -> p ct", p=128), g_c, allow_slow_non_contiguous=True)
        P.dma("sp", H0, st0[l].r("d (gp g2) p c -> (g2 p) (d gp) c", g2=2), g_c)
        cwn = A.alloc([512], F32, "cwn")
        for nm_, dst_, nk_ in (("sc_conv_w", SCW, 3), ("cf_conv_w", CFW, 31)):
            P.dma("sp", cwn[0:nk_, :], W[nm_][l], g_c)
            for ct_ in range(4):
                ps = ps_next()
                P.tr(ps[:, 0:nk_], cwn[0:nk_, ct_ * 128:(ct_ + 1) * 128], ident_f[0:nk_, 0:nk_])
                P.cp("act", dst_[:, ct_, :], ps[:, 0:nk_])
        P.dma("sp", SCB, W["sc_conv_b"][l].r("(ct p) -> p ct", p=128), g_c, allow_slow_non_contiguous=True)
        P.dma("sp", CFB, W["cf_conv_b"][l].r("(ct p) -> p ct", p=128), g_c, allow_slow_non_contiguous=True)
        P.dma("sp", CFG, W["cf_ln_g"][l].r("(ct p) -> p ct", p=128), g_c, allow_slow_non_contiguous=True)
        P.dma("sp", CFBB, W["cf_ln_b"][l].r("(ct p) -> p ct", p=128), g_c, allow_slow_non_contiguous=True)
        dbg_store("CFW", CFW)
        dbg_store("SCW", SCW)
        kn = [A.alloc([128], F32, "kn%d" % i) for i in range(2)]
        for hf, nm in enumerate(("peer_k1", "peer_k2")):
            P.dma("sp", kn[hf], W[nm][l], g_c)
            ps = ps_next()
            P.tr(ps[:, 0:128], kn[hf], ident_f)
            P.cp("act", KTb[:, hf, :], ps[:, 0:128])
        P.barrier()

    wslots = {}
    w_it = [0]

    def load_w(W2d, r0, nk, c0, ncol):
        i = w_it[0] % 2
        w_it[0] += 1
        stg, wbf = wslots["stg"][i], wslots["wbf"][i]
        P.dma("sp", stg[:, 0:nk, 0:ncol], W2d[r0:r0 + 128 * nk, c0:c0 + ncol].r("(kt p) c -> p kt c", p=128), g_w[i])
        if i == 0:
            P.cp("dve", wbf[:, 0:nk, 0:ncol], stg[:, 0:nk, 0:ncol])
        else:
            P.cp("act", wbf[:, 0:nk, 0:ncol], stg[:, 0:nk, 0:ncol])
        return wbf

    def linear_fm(in_tiles, W2d, r0, c0, ncols, consume, tile0=0):
        nk = len(in_tiles)
        for cc in range(0, ncols, 256):
            wb = load_w(W2d, r0, nk, c0 + cc, 256)
            for ti in range(2):
                ps = ps_next()
                for half in range(2):
                    hs = slice(half * 512, half * 512 + 512)
                    for kt in range(nk):
                        P.mm(ps[:, hs], wb[:, kt, ti * 128:(ti + 1) * 128], in_tiles[kt][:, hs],
                             start=(kt == 0), stop=(kt == nk - 1))
                consume(tile0 + cc // 128 + ti, ps)

    def ln_tm(x, out, st, mv, rs, e="dve"):
        for c in range(4):
            P.op("dve", lambda E, c=c: E.bn_stats(out=st.ap[:, c, :], in_=x.ap[:, c * 512:(c + 1) * 512]),
                 r=[x.buf], w=[st.buf])
        P.op("dve", lambda E: E.bn_aggr(out=mv.ap, in_=st.ap.rearrange("p a b -> p (a b)")), r=[st.buf], w=[mv.buf])
        P.ts("dve", rs, mv[:, 1:2], LN_EPS, ALU.add)
        P.act(rs, rs, AF.Sqrt)
        P.recip(rs, rs)
        P.ts("dve", out, x, mv[:, 0:1], ALU.subtract, rs[:, 0:1], ALU.mult)

    def phase_A(l, u):
        nseq, SL = (4, 256) if u == 0 else (1, 1024)
        UL.reset()
        hT = UL.alloc([16, T], BF16, "hT")
        bufP = UL.alloc([8, T], BF16, "bufP")
        bufQ = UL.alloc([8, T], BF16, "bufQ")
        ysT = UL.alloc([4, T], BF16, "ysT")
        zT = UL.alloc([4, T], BF16, "zT")
        wslots["wbf"] = [UL.alloc([16, 256], BF16, "wbf%d" % i) for i in range(2)]
        wslots["stg"] = [UL.alloc([16, 256], F32, "wstg%d" % i) for i in range(2)]
        X = Stack(nc, UL.cur, SB_TOP)

        X.reset()
        xt = [X.alloc([D], F32, "xt%d" % i) for i in range(2)]
        tmp = X.alloc([D], F32, "tmpA0")
        hb = X.alloc([D], BF16, "hb")
        sc1b = X.alloc([D], F32, "sc1b")
        sh1b = X.alloc([D], F32, "sh1b")
        pet = X.alloc([D], F32, "pet")
        st = X.alloc([4, 6], F32, "st")
        mv = X.alloc([2], F32, "mv")
        rs = X.alloc([1], F32, "rs")
        P.dma("sp", sh1b, TT(mod_d.ap[l, u:u + 1, 0:D].partition_broadcast(128), mod_d.buf), g_c)
        P.dma("sp", sc1b, TT(mod_d.ap[l, u:u + 1, D:2 * D].partition_broadcast(128), mod_d.buf), g_c)
        for tt_ in range(NTT):
            rows = slice(tt_ * 128, tt_ * 128 + 128)
            x_ = xt[tt_ % 2]
            if l == 0:
                P.dma("sp", x_, xin[u][rows, :], g_x[tt_ % 2])
                if u == 1:
                    P.dma("sp", pet, pe_d[rows, :], g_x[tt_ % 2])
                    P.tt("dve", x_, x_, pet, ALU.add)
                P.dma("sp", xres[u][rows, :], x_, g_st)
            else:
                P.dma("sp", x_, xres[u][rows, :], g_x[tt_ % 2])
            ln_tm(x_, tmp, st, mv, rs)
            P.tt("dve", tmp, tmp, sc1b, ALU.mult)
            P.tt("dve", hb, tmp, sh1b, ALU.add)
            ps = ps_next()
            psb = ps.bitcast(BF16)
            for kt in range(KT):
                P.tr(psb[:, kt * 128:(kt + 1) * 128], hb[:, kt * 128:(kt + 1) * 128], ident_b)
            P.cp("act", hT[:, :, rows], psb.r("p (k t) -> p k t", k=KT))
        dbg_store("hT", hT)
        if stop == "A0":
            return None
        hT_tiles = [hT[:, kt, :] for kt in range(KT)]
        P.barrier()

        uT = bufP
        ya = bufQ

        def cons_u(ti, ps):
            P.cp("act", uT[:, ti, :], ps)
        linear_fm(hT_tiles, W["w_in"][l], 0, 0, 1024, cons_u)
        dbg_store("uT", uT)
        X.reset()
        f = {k: X.alloc([T], F32, k) for k in ("bure", "buim", "t1", "t2", "t3", "br", "bi", "gre", "gim")}
        fbh = {k: X.alloc([T], BF16, k) for k in ("greb", "gimb", "q1", "q2", "q3", "q4")}
        tbb = X.alloc([2, 256], BF16, "tbb")
        hre = X.alloc([T], BF16, "hre")
        him = X.alloc([T], BF16, "him")
        tbs = [X.alloc([2, 260], F32, "tb%d" % i) for i in range(2)]
        wbt = [X.alloc([16, 128], BF16, "wbt%d" % i) for i in range(2)]
        cwt = [X.alloc([16, 64], BF16, "cwt%d" % i) for i in range(2)]
        sm = {k: X.alloc([4], F32, k) for k in ("i_re", "i_im", "s1", "s2")}
        psA, psB, psC, psD = PS

        def v4(t_):
            return t_.r("p (c t) -> p c t", c=4)

        rts = []
        for ct in range(8):
            for d in range(2):
                for j in range(4):
                    rts.append((ct, d, j))

        state = {}

        def issue_bu(i):
            ct, d, j = rts[i]
            if d == 0 and j == 0:
                k = ct % 2
                P.dma("sp", wbt[k], wbd[ct], g_t[k])
                P.dma("sp", cwt[k], cwd[ct], g_t[k])
            rt = d * 32 + ct * 4 + j
            tb = tbs[i % 2]
            P.dma("sp", tb, tabs[rt], g_x[i % 2])
            wb_ = wbt[ct % 2]
            for reim, slot in ((0, psA), (1, psB)):
                for half in range(2):
                    hs = slice(half * 512, half * 512 + 512)
                    P.mm(slot[:, hs], wb_[:, (d * 4 + j) * 2 + reim, :], uT[:, ct, hs])

        issue_bu(0)
        for i, (ct, d, j) in enumerate(rts):
            rt = d * 32 + ct * 4 + j
            tb = tbs[i % 2]
            cw_ = cwt[ct % 2]
            Cb = tb[:, 0, 0:256].un(1).bc([128, 4, 256])
            Sb = tb[:, 1, 0:256].un(1).bc([128, 4, 256])
            for src, dst in ((psA, f["bure"]), (psB, f["buim"])):
                s3 = src.r("p (b t) -> p b t", b=nseq)
                if d == 1:
                    s3 = s3[:, :, ::-1]
                P.cp("act", dst.r("p (b t) -> p b t", b=nseq), s3)
            if i + 1 < len(rts):
                issue_bu(i + 1)
            P.tt("dve", v4(f["t1"]), v4(f["bure"]), Cb, ALU.mult)
            P.tt("dve", v4(f["t2"]), v4(f["buim"]), Sb, ALU.mult)
            P.tt("dve", f["br"], f["t1"], f["t2"], ALU.add)
            P.tt("dve", v4(f["t1"]), v4(f["buim"]), Cb, ALU.mult)
            P.tt("dve", v4(f["t2"]), v4(f["bure"]), Sb, ALU.mult)
            P.tt("dve", f["bi"], f["t1"], f["t2"], ALU.subtract)
            rcol = Rt[:, rt:rt + 1]
            for c in range(4):
                cs = slice(c * 256, c * 256 + 256)
                if u == 0:
                    ire, iim = 0.0, 0.0
                else:
                    if c == 0:
                        pre, pim = H0[:, rt, 0:1], H0[:, rt, 1:2]
                        er, ei = tb[:, 0, 1:2], tb[:, 1, 1:2]
                    else:
                        pre, pim = f["gre"][:, c * 256 - 1:c * 256], f["gim"][:, c * 256 - 1:c * 256]
                        er, ei = tb[:, 0, 256:257], tb[:, 1, 256:257]
                    P.ts("dve", sm["s1"][:, 0:1], pim, ei, ALU.mult)
                    P.stt(sm["i_re"][:, c:c + 1], pre, er, sm["s1"][:, 0:1], ALU.mult, ALU.subtract)
                    P.ts("dve", sm["s2"][:, 0:1], pim, er, ALU.mult)
                    P.stt(sm["i_im"][:, c:c + 1], pre, ei, sm["s2"][:, 0:1], ALU.mult, ALU.add)
                    ire, iim = sm["i_re"][:, c:c + 1], sm["i_im"][:, c:c + 1]
                P.scan(f["gre"][:, cs], rcol.bc([128, 256]), f["br"][:, cs], ire)
                P.scan(f["gim"][:, cs], rcol.bc([128, 256]), f["bi"][:, cs], iim)
            P.cp("act", tbb, tb[:, :, 0:256])
            P.cp("act", fbh["greb"], f["gre"])
            P.cp("act", fbh["gimb"], f["gim"])
            Cbb = tbb[:, 0, :].un(1).bc([128, 4, 256])
            Sbb = tbb[:, 1, :].un(1).bc([128, 4, 256])
            P.tt("dve", v4(fbh["q1"]), v4(fbh["greb"]), Cbb, ALU.mult)
            P.tt("dve", v4(fbh["q2"]), v4(fbh["gimb"]), Sbb, ALU.mult)
            P.tt("dve", hre, fbh["q1"], fbh["q2"], ALU.subtract)
            P.tt("dve", v4(fbh["q3"]), v4(fbh["greb"]), Sbb, ALU.mult)
            P.tt("dve", v4(fbh["q4"]), v4(fbh["gimb"]), Cbb, ALU.mult)
            P.tt("dve", him, fbh["q3"], fbh["q4"], ALU.add)
            if u == 0:
                glr = v4(f["gre"])[:, :, 255]
                gli = v4(f["gim"])[:, :, 255]
                c255, s255 = tb[:, 0, 255:256], tb[:, 1, 255:256]
                P.ts("dve", sm["s1"], gli, s255, ALU.mult)
                P.stt(NST[:, rt, :, 0], glr, c255, sm["s1"], ALU.mult, ALU.subtract)
                P.ts("dve", sm["s2"], gli, c255, ALU.mult)
                P.stt(NST[:, rt, :, 1], glr, s255, sm["s2"], ALU.mult, ALU.add)
            ysl = psC if d == 0 else psD
            jj = j // 2
            for reim, hs_ in ((0, hre), (1, him)):
                for half in range(2):
                    hs = slice(half * 512, half * 512 + 512)
                    P.mm(ysl[64 * jj:64 * jj + 64, hs], cw_[:, (d * 4 + j) * 2 + reim, :], hs_[:, hs],
                         start=(j % 2 == 0 and reim == 0), stop=(j % 2 == 1 and reim == 1))
            if d == 1 and j == 3:
                s3 = psD.r("p (b t) -> p b t", b=nseq)[:, :, ::-1]
                P.cp("act", f["t3"].r("p (b t) -> p b t", b=nseq), s3)
                P.tt("dve", f["t1"], psC, f["t3"], ALU.add)
                P.stt(f["t2"], uT[:, ct, :], DSK[:, ct:ct + 1], f["t1"], ALU.mult, ALU.add)
                P.act(ya[:, ct, :], f["t2"], AF.Gelu_apprx_tanh)
        if u == 0:
            for b_ in range(4):
                P.dma("sp", nst_o[b_, l].r("d (gp g2) p c -> (g2 p) (d gp) c", g2=2), NST[:, :, b_, :], g_st)
        dbg_store("ya", ya)
        dbg_store("nst", nst_o) if False else None
        if stop == "A1s":
            return None
        P.barrier()
        X.reset()
        sg = [X.alloc([T], F32, "sg%d" % i) for i in range(2)]
        yag = bufP
        ya_tiles = [ya[:, k, :] for k in range(8)]

        def cons_glu(ti, ps):
            s_ = sg[ti % 2]
            P.act(s_, ps, AF.Sigmoid)
            P.tt("dve", yag[:, ti, :], ya[:, ti, :], s_, ALU.mult)
        linear_fm(ya_tiles, W["s5_w_glu"][l], 0, 0, 1024, cons_glu)
        dbg_store("yag", yag)
        if stop == "A1":
            return None

        X.reset()
        fb = {k: X.alloc([T], F32, k) for k in ("cb", "cc", "prod", "acc")}
        keep = {}

        def seqv(t_):
            return t_.r("p (b t) -> p b t", b=nseq)

        def cons_sc(ti, ps):
            kind, i = divmod(ti, 4)
            if kind == 0:
                if i not in keep:
                    keep[i] = X.alloc([T], F32, "scb%d" % i)
                P.cp("act", keep[i], ps)
            elif kind == 1:
                if ("c", i) not in keep:
                    keep[("c", i)] = X.alloc([T], F32, "scc%d" % i)
                P.cp("act", keep[("c", i)], ps)
            else:
                P.tt("dve", fb["prod"], keep[("c", i)], ps, ALU.mult)
                P.ts("dve", fb["acc"], fb["prod"], SCW[:, i, 1:2], ALU.mult, SCB[:, i:i + 1], ALU.add)
                a3, p3 = seqv(fb["acc"]), seqv(fb["prod"])
                P.stt(a3[:, :, 1:SL], p3[:, :, 0:SL - 1], SCW[:, i, 0:1], a3[:, :, 1:SL], ALU.mult, ALU.add)
                P.stt(a3[:, :, 0:SL - 1], p3[:, :, 1:SL], SCW[:, i, 2:3], a3[:, :, 0:SL - 1], ALU.mult, ALU.add)
                P.tt("dve", ysT[:, i, :], keep[i], fb["acc"], ALU.mult)
        linear_fm(hT_tiles, W["w_in"][l], 0, 1024, 1536, cons_sc)
        dbg_store("ys", ysT)
        if stop == "A2":
            return None
        P.barrier()

        X.reset()
        ga = [X.alloc([T], F32, "ga%d" % i) for i in range(4)]
        cv = [X.alloc([T], F32, "cv%d" % i) for i in range(4)]
        fc = {k: X.alloc([T], F32, k) for k in ("sq", "rstd", "mr")}

        def cons_cf(ti, ps):
            kind, i = divmod(ti, 4)
            if kind == 0:
                P.cp("act", ga[i], ps)
            else:
                P.act(fc["sq"], ps, AF.Sigmoid)
                P.tt("dve", ga[i], ga[i], fc["sq"], ALU.mult)
                P.ts("dve", cv[i], ga[i], CFW[:, i, 15:16], ALU.mult, CFB[:, i:i + 1], ALU.add)
                a3, g3 = seqv(cv[i]), seqv(ga[i])
                for k in range(31):
                    sh = k - 15
                    if sh == 0:
                        continue
                    if sh > 0:
                        P.stt(a3[:, :, 0:SL - sh], g3[:, :, sh:SL], CFW[:, i, k:k + 1], a3[:, :, 0:SL - sh], ALU.mult, ALU.add)
                    else:
                        P.stt(a3[:, :, -sh:SL], g3[:, :, 0:SL + sh], CFW[:, i, k:k + 1], a3[:, :, -sh:SL], ALU.mult, ALU.add)
        linear_fm(hT_tiles, W["w_in"][l], 0, 2560, 1024, cons_cf)
        dbg_store("glu0", ga[0])
        dbg_store("cv0", cv[0])
        psM, psQ = PS[0], PS[1]
        for i in range(4):
            for half in range(2):
                hs = slice(half * 512, half * 512 + 512)
                P.mm(psM[:, hs], ones_f, cv[i][:, hs], start=(i == 0), stop=(i == 3))
        for i in range(4):
            P.act(ga[i], cv[i], AF.Square)
        for i in range(4):
            for half in range(2):
                hs = slice(half * 512, half * 512 + 512)
                P.mm(psQ[:, hs], ones_f, ga[i][:, hs], start=(i == 0), stop=(i == 3))
        P.cp("act", fc["mr"], psM)
        P.tt("dve", fc["sq"], fc["mr"], fc["mr"], ALU.mult)
        P.tt("dve", fc["rstd"], psQ, fc["sq"], ALU.subtract)
        P.ts("dve", fc["rstd"], fc["rstd"], LN_EPS, ALU.add)
        P.act(fc["rstd"], fc["rstd"], AF.Sqrt)
        P.recip(fc["rstd"], fc["rstd"])
        dbg_store("rstd", fc["rstd"])
        dbg_store("mean", psM) if False else None
        P.tt("dve", fc["mr"], fc["mr"], fc["rstd"], ALU.mult)
        for i in range(4):
            P.tt("dve", ga[i], cv[i], fc["rstd"], ALU.mult)
            P.tt("dve", ga[i], ga[i], fc["mr"], ALU.subtract)
            P.ts("dve", ga[i], ga[i], CFG[:, i:i + 1], ALU.mult, CFBB[:, i:i + 1], ALU.add)
            P.act(zT[:, i, :], ga[i], AF.Silu)
        dbg_store("z", zT)
        if stop == "A3":
            return None
        P.barrier()

        X.reset()
        mT = X.alloc([16, T], BF16, "mT")
        sgm = [X.alloc([T], F32, "sgm%d" % i) for i in range(2)]
        macc = [X.alloc([T], F32, "macc%d" % i) for i in range(2)]
        tmpm = X.alloc([T], F32, "tmpm")
        yag_t = [bufP[:, k, :] for k in range(8)]
        ys_t = [ysT[:, k, :] for k in range(4)]
        z_t = [zT[:, k, :] for k in range(4)]
        branches = [(yag_t, W["w_pa"][l], 0), (ys_t, W["w_pb"][l], 1), (z_t, W["w_pc"][l], 2)]
        for pair in range(8):
            cbase = pair * 256
            for bi, (tiles, Wp, gidx) in enumerate(branches):
                nk = len(tiles)
                wb = load_w(Wp, 0, nk, cbase, 256)
                pp = []
                for ti in range(2):
                    ps = ps_next()
                    for half in range(2):
                        hs = slice(half * 512, half * 512 + 512)
                        for kt in range(nk):
                            P.mm(ps[:, hs], wb[:, kt, ti * 128:(ti + 1) * 128], tiles[kt][:, hs], start=(kt == 0), stop=(kt == nk - 1))
                    pp.append(ps)
                wg = load_w(W["w_in"][l], 0, 16, GATE0 + gidx * D + cbase, 256)
                for ti in range(2):
                    ps = ps_next()
                    for half in range(2):
                        hs = slice(half * 512, half * 512 + 512)
                        for kt in range(16):
                            P.mm(ps[:, hs], wg[:, kt, ti * 128:(ti + 1) * 128], hT_tiles[kt][:, hs], start=(kt == 0), stop=(kt == 15))
                    P.act(sgm[ti], ps, AF.Sigmoid)
                    if bi == 0:
                        P.tt("dve", macc[ti], sgm[ti], pp[ti], ALU.mult)
                    elif bi == 1:
                        P.tt("dve", tmpm, sgm[ti], pp[ti], ALU.mult)
                        P.tt("dve", macc[ti], macc[ti], tmpm, ALU.add)
                    else:
                        P.tt("dve", tmpm, sgm[ti], pp[ti], ALU.mult)
                        P.tt("dve", mT[:, pair * 2 + ti, :], macc[ti], tmpm, ALU.add)
        dbg_store("mT", mT)
        if stop == "A4":
            return None
        ost = [X.alloc([256], F32, "ost%d" % i) for i in range(2)]
        it = 0
        for cc in range(8):
            wb = load_w(W["w_o"][l], 0, 16, cc * 256, 256)
            for tt_ in range(NTT):
                ps = ps_next()
                for kt in range(16):
                    P.mm(ps[:, 0:256], mT[:, kt, tt_ * 128:(tt_ + 1) * 128], wb[:, kt, :], start=(kt == 0), stop=(kt == 15))
                o_ = ost[it % 2]
                it += 1
                P.cp("act", o_, ps[:, 0:256])
                P.dma("sp", ymix[tt_ * 128:(tt_ + 1) * 128, cc * 256:(cc + 1) * 256], o_, g_st)
        P.barrier()
        return None

    def phase_B(l, u, last):
        UL.reset()
        h2T = UL.alloc([16, T], BF16, "h2T")
        Zbase = UL.cur
        qT = UL.alloc([16, T], BF16, "qT")
        Y3base = UL.cur
        wslots["wbf"] = [UL.alloc([16, 256], BF16, "wbfB%d" % i) for i in range(2)]
        Y = Stack(nc, UL.cur, SB_TOP)
        Y.reset()
        wslots["stg"] = None
        xt = [Y.alloc([D], F32, "xtB%d" % i) for i in range(2)]
        yt = [Y.alloc([D], F32, "ytB%d" % i) for i in range(2)]
        t1 = Y.alloc([D], F32, "t1B")
        hb = Y.alloc([D], BF16, "hbB")
        bct = {k: Y.alloc([D], F32, k) for k in ("gt1", "g1", "b1", "sc2", "sh2")}
        st = Y.alloc([4, 6], F32, "stB")
        mv = Y.alloc([2], F32, "mvB")
        rs = Y.alloc([1], F32, "rsB")

        def bload(dst, src2d, row, c0):
            P.dma("sp", dst, TT(src2d.ap[row:row + 1, c0:c0 + D].partition_broadcast(128), src2d.buf), g_c)
        bload(bct["gt1"], mod_d[l], u, 2 * D)
        bload(bct["sc2"], mod_d[l], u, 4 * D)
        bload(bct["sh2"], mod_d[l], u, 3 * D)
        bload(bct["g1"], W["ln1_g"], l, 0)
        bload(bct["b1"], W["ln1_b"], l, 0)
        for tt_ in range(NTT):
            rows = slice(tt_ * 128, tt_ * 128 + 128)
            x_, y_ = xt[tt_ % 2], yt[tt_ % 2]
            P.dma("sp", x_, xres[u][rows, :], g_x[tt_ % 2])
            P.dma("sp", y_, ymix[rows, :], g_x[tt_ % 2])
            P.tt("dve", y_, y_, bct["gt1"], ALU.mult)
            P.stt(y_, x_, ALPHA, y_, ALU.mult, ALU.add)
            ln_tm(y_, t1, st, mv, rs)
            P.tt("dve", t1, t1, bct["g1"], ALU.mult)
            P.tt("dve", x_, t1, bct["b1"], ALU.add)
            P.dma("sp", x1res[rows, :], x_, g_st)
            if tt_ == 0:
                dbg_store("x1", x_)
            ln_tm(x_, t1, st, mv, rs)
            P.tt("dve", t1, t1, bct["sc2"], ALU.mult)
            P.tt("dve", y_, t1, bct["sh2"], ALU.add)
            if tt_ == 0:
                dbg_store("h2", y_)
            P.cp("dve", hb, y_)
            ps = ps_next()
            psb = ps.bitcast(BF16)
            for kt in range(KT):
                P.tr(psb[:, kt * 128:(kt + 1) * 128], hb[:, kt * 128:(kt + 1) * 128], ident_b)
            P.cp("act", h2T[:, :, rows], psb.r("p (k t) -> p k t", k=KT))
        if stop == "B1":
            return
        P.barrier()
        Y.reset()
        wslots["stg"] = [Y.alloc([16, 256], F32, "wstgB%d" % i) for i in range(2)]
        h2T_t = [h2T[:, k, :] for k in range(KT)]

        def cons_q(ti, ps):
            P.cp("act", qT[:, ti, :], ps)
        linear_fm(h2T_t, W["peer_w_q"][l], 0, 0, D, cons_q)
        P.barrier()
        if stop == "B2":
            return
        Y = Stack(nc, Y3base, SB_TOP)
        S = Y.alloc([16, 128], F32, "S")
        S2a = [Y.alloc([128], F32, "S2a%d" % i) for i in range(16)]
        S2b = [Y.alloc([256], F32, "S2b%d" % i) for i in range(8)]
        V = Y.alloc([16, 16], F32, "V")
        Iu = Y.alloc([16, 16], U32, "Iu")
        If = Y.alloc([16, 16], F32, "If")
        cand = Y.alloc([8, 256], F32, "cand")
        top = Y.alloc([8, 16], F32, "top")
        pos = Y.alloc([8, 16], U32, "pos")
        pa_ = Y.alloc([8, 16], U32, "pa")
        pb_ = Y.alloc([8, 16], U32, "pb")
        af = Y.alloc([8, 16], F32, "af")
        bf = Y.alloc([8, 16], F32, "bf")
        eq = Y.alloc([8, 16, 16], F32, "eq")
        i1s = Y.alloc([8, 16], F32, "i1s")
        i2s = Y.alloc([8, 16], F32, "i2s")
        gw = Y.alloc([8, 16], F32, "gw")
        gsum = Y.alloc([8], F32, "gsum")
        i1T = Y.alloc([128], F32, "i1T")
        i2T = Y.alloc([128], F32, "i2T")
        gT = Y.alloc([128], F32, "gT")
        Pm = Y.alloc([64, 128], BF16, "Pm")
        Qm = Y.alloc([64, 128], BF16, "Qm")
        Vh = [TT(V.ap[:, i, :], Buf("Vh%d" % i)) for i in range(16)]
        Ih = [TT(Iu.ap[:, i, :], Buf("Ih%d" % i)) for i in range(16)]
        toph = [TT(top.ap[:, i, :], Buf("toph%d" % i)) for i in range(8)]
        posh = [TT(pos.ap[:, i, :], Buf("posh%d" % i)) for i in range(8)]
        Vb = [x.buf for x in Vh]
        Ib = [x.buf for x in Ih]
        tb_ = [x.buf for x in toph]
        pb2 = [x.buf for x in posh]
        Gst = Y.alloc([128, 128], BF16, "Gst")
        for tt_ in range(b3_tiles):
            rows = slice(tt_ * 128, tt_ * 128 + 128)
            for q4 in range(4):
                ps = ps_next()
                for m in range(4):
                    hh = q4 * 4 + m
                    P.mm(ps[:, m * 128:(m + 1) * 128], qT[:, hh, rows], KTb[:, hh % 2, :])
                P.cp("act", S[:, q4 * 4:q4 * 4 + 4, :], ps[:, 0:512].r("p (a n) -> p a n", a=4))
            for hh in range(16):
                P.op("dve", lambda E, hh=hh: E.max(out=Vh[hh].ap[:, 0:8], in_=S.ap[:, hh, :]), r=[S.buf], w=[Vb[hh]])
            for hh in range(16):
                P.op("dve", lambda E, hh=hh: E.max_index(out=Ih[hh].ap[:, 0:8], in_max=Vh[hh].ap[:, 0:8], in_values=S.ap[:, hh, :]),
                     r=[S.buf, Vb[hh]], w=[Ib[hh]])
            for hh in range(16):
                P.op("dve", lambda E, hh=hh: E.match_replace(out=S2a[hh].ap, in_to_replace=Vh[hh].ap[:, 0:8],
                                                             in_values=S.ap[:, hh, :], imm_value=NEG), r=[S.buf, Vb[hh]], w=[S2a[hh].buf])
            for hh in range(16):
                P.op("dve", lambda E, hh=hh: E.max(out=Vh[hh].ap[:, 8:16], in_=S2a[hh].ap), r=[S2a[hh].buf], w=[Vb[hh]])
            for hh in range(16):
                P.op("dve", lambda E, hh=hh: E.max_index(out=Ih[hh].ap[:, 8:16], in_max=Vh[hh].ap[:, 8:16], in_values=S2a[hh].ap),
                     r=[S2a[hh].buf, Vb[hh]], w=[Ib[hh]])
            P.op("dve", lambda E: E.tensor_copy(out=If.ap, in_=Iu.ap), r=Ib, w=[If.buf])
            V4 = V.r("p (h f) a -> p h f a", f=2)
            I4 = If.r("p (h f) a -> p h f a", f=2)
            c4 = cand.r("p h (a b) -> p h a b", a=16)
            P.op("dve", lambda E: E.tensor_tensor(out=c4.ap, in0=V4[:, :, 0, :].un(3).bc([128, 8, 16, 16]).ap,
                                                  in1=V4[:, :, 1, :].un(2).bc([128, 8, 16, 16]).ap, op=ALU.add), r=Vb, w=[cand.buf])
            for h in range(8):
                P.op("dve", lambda E, h=h: E.max(out=toph[h].ap[:, 0:8], in_=cand.ap[:, h, :]), r=[cand.buf], w=[tb_[h]])
            for h in range(8):
                P.op("dve", lambda E, h=h: E.max_index(out=posh[h].ap[:, 0:8], in_max=toph[h].ap[:, 0:8], in_values=cand.ap[:, h, :]),
                     r=[cand.buf, tb_[h]], w=[pb2[h]])
            for h in range(8):
                P.op("dve", lambda E, h=h: E.match_replace(out=S2b[h].ap, in_to_replace=toph[h].ap[:, 0:8], in_values=cand.ap[:, h, :],
                                                           imm_value=NEG), r=[cand.buf, tb_[h]], w=[S2b[h].buf])
            for h in range(8):
                P.op("dve", lambda E, h=h: E.max(out=toph[h].ap[:, 8:16], in_=S2b[h].ap), r=[S2b[h].buf], w=[tb_[h]])
            for h in range(8):
                P.op("dve", lambda E, h=h: E.max_index(out=posh[h].ap[:, 8:16], in_max=toph[h].ap[:, 8:16], in_values=S2b[h].ap),
                     r=[S2b[h].buf, tb_[h]], w=[pb2[h]])
            P.op("dve", lambda E: E.tensor_single_scalar(out=pa_.ap, in_=pos.ap, scalar=4, op=ALU.logical_shift_right),
                 r=pb2, w=[pa_.buf])
            P.op("dve", lambda E: E.tensor_single_scalar(out=pb_.ap, in_=pos.ap, scalar=15, op=ALU.bitwise_and),
                 r=pb2, w=[pb_.buf])
            P.cp("dve", af, pa_)
            P.cp("dve", bf, pb_)
            io4 = iota16.un(1).un(1).bc([128, 8, 16, 16])
            P.tt("dve", eq, af.un(3).bc([128, 8, 16, 16]), io4, ALU.is_equal)
            P.tt("dve", eq, eq, I4[:, :, 0, :].un(2).bc([128, 8, 16, 16]), ALU.mult)
            P.reduce(i1s, eq, ALU.add)
            P.tt("dve", eq, bf.un(3).bc([128, 8, 16, 16]), io4, ALU.is_equal)
            P.tt("dve", eq, eq, I4[:, :, 1, :].un(2).bc([128, 8, 16, 16]), ALU.mult)
            P.reduce(i2s, eq, ALU.add)
            P.op("dve", lambda E: E.tensor_tensor(out=gw.ap, in0=top.ap, in1=top[:, :, 0:1].bc([128, 8, 16]).ap, op=ALU.subtract),
                 r=tb_, w=[gw.buf])
            P.act(gw, gw, AF.Exp)
            P.reduce(gsum, gw, ALU.add)
            P.recip(gsum, gsum)
            P.tt("dve", gw, gw, gsum.un(2).bc([128, 8, 16]), ALU.mult)
            for src, dst in ((i1s, i1T), (i2s, i2T), (gw, gT)):
                ps = ps_next()
                P.tr(ps[:, 0:128], src.r("p h k -> p (h k)"), ident_f)
                P.cp("act", dst, ps[:, 0:128])
            io3 = iota128.un(1).bc([128, 64, 128])
            for hf in range(2):
                ts_ = slice(hf * 64, hf * 64 + 64)
                P.tt("dve", Pm, io3, i1T[:, ts_].un(2).bc([128, 64, 128]), ALU.is_equal)
                P.tt("dve", Qm, io3, i2T[:, ts_].un(2).bc([128, 64, 128]), ALU.is_equal)
                P.tt("dve", Qm, Qm, gT[:, ts_].un(2).bc([128, 64, 128]), ALU.mult)
                for t8 in range(8):
                    ps = ps_next()
                    for k in range(8):
                        t_ = t8 * 8 + k
                        P.mm(ps[:, k * 128:(k + 1) * 128], Pm[:, t_, :], Qm[:, t_, :])
                    t0_ = hf * 64 + t8 * 8
                    P.cp("act", Gst[:, :, t0_:t0_ + 8].r("p i t -> p t i"), ps.r("p (t i) -> p t i", t=8))
            P.dma("sp", Gd[:, :, rows], Gst)
        P.barrier()
        if stop == "B3a":
            return
        Z = Stack(nc, Zbase, SB_TOP)
        ACC = Z.alloc([NTT, D], F32, "ACC")
        Z2 = Stack(nc, Z.cur, SB_TOP)
        JG = 4
        ustg = [Z2.alloc([D], F32, "ustg%d" % i) for i in range(3)]
        vstg = [Z2.alloc([D], F32, "vstg%d" % i) for i in range(2)]
        ubf = [Z2.alloc([D], BF16, "ubf%d" % i) for i in range(2)]
        uTt = [Z2.alloc([16, 128], BF16, "uTt%d" % i) for i in range(3)]
        vbf = [Z2.alloc([D], BF16, "vbf%d" % b) for b in range(JG)]
        Wt = [Z2.alloc([T], BF16, "Wt%d" % b) for b in range(JG)]
        Gt = [Z2.alloc([T], BF16, "Gt%d" % i) for i in range(2)]
        ge = [Z2.alloc([T], BF16, "ge%d" % i) for i in range(2)]
        psT = PS[0]
        psA = [PS[1], PS[2]]
        psO = [TT(PS[3].ap[:, 0:512], Buf("psO0", "ps")), TT(PS[3].ap[:, 512:1024], Buf("psO1", "ps"))]
        urows = W["peer_u"][l].r("(i j) d -> j i d", j=128)
        vrows = W["peer_v"][l].r("(i j) d -> j i d", j=128)
        NJ = 128

        def load_cast(j):
            s_u = ustg[j % 3]
            P.dma("sp", s_u, urows[j])
            P.cp("dve", ubf[j % 2], s_u)

        def trans(j):
            psb = psT.bitcast(BF16).r("p (k i) -> p k i", k=16)
            ub = ubf[j % 2]
            for kt in range(16):
                P.tr(psb[:, kt, :], ub[:, kt * 128:(kt + 1) * 128], ident_b)

        def evac(j):
            psb = psT.bitcast(BF16).r("p (k i) -> p k i", k=16)
            P.cp("act", uTt[j % 3], psb)

        load_cast(0)
        trans(0)
        evac(0)
        load_cast(1)
        trans(1)
        evac(1)
        load_cast(2)
        P.dma("sp", Gt[0], Gd[:, 0, :])
        for j in range(NJ):
            jj = j % JG
            if j + 3 < NJ:
                load_cast(j + 3)
            s_v = vstg[j % 2]
            P.dma("sp", s_v, vrows[j])
            P.cp("act", vbf[jj], s_v)
            if j + 1 < NJ:
                P.dma("sp", Gt[(j + 1) % 2], Gd[:, j + 1, :])
            pa2 = psA[j % 2]
            ut = uTt[j % 3]
            for half in range(2):
                hs = slice(half * 512, half * 512 + 512)
                for kt in range(16):
                    P.mm(pa2[:, hs], ut[:, kt, :], h2T[:, kt, hs], start=(kt == 0), stop=(kt == 15))
            if j + 2 < NJ:
                trans(j + 2)
            e_ = ge[j % 2]
            P.act(e_, pa2, AF.Gelu_apprx_tanh)
            if j + 2 < NJ:
                evac(j + 2)
            P.tt("dve", Wt[jj], e_, Gt[j % 2], ALU.mult)
            if jj == JG - 1:
                first = (j == JG - 1)
                for tt_ in range(NTT):
                    for dc in range(4):
                        po = psO[(tt_ * 4 + dc) % 2]
                        for q_ in range(JG):
                            P.mm(po, Wt[q_][:, tt_ * 128:(tt_ + 1) * 128], vbf[q_][:, dc * 512:(dc + 1) * 512],
                                 start=(q_ == 0), stop=(q_ == JG - 1))
                        dst = ACC[:, tt_, dc * 512:(dc + 1) * 512]
                        if first:
                            P.cp("act", dst, po)
                        else:
                            P.tt("dve", dst, dst, po, ALU.add)
        P.barrier()
        Z2.reset()
        x1 = [Z2.alloc([D], F32, "x1_%d" % i) for i in range(2)]
        tq2 = Z2.alloc([D], F32, "tq2")
        bc3 = {k: Z2.alloc([D], F32, k) for k in ("gt2", "g2", "b2")}
        st = Z2.alloc([4, 6], F32, "stC")
        mv = Z2.alloc([2], F32, "mvC")
        rs = Z2.alloc([1], F32, "rsC")
        bload(bc3["gt2"], mod_d[l], u, 5 * D)
        bload(bc3["g2"], W["ln2_g"], l, 0)
        bload(bc3["b2"], W["ln2_b"], l, 0)
        for tt_ in range(NTT):
            rows = slice(tt_ * 128, tt_ * 128 + 128)
            x1_ = x1[tt_ % 2]
            acc = ACC[:, tt_, :]
            P.dma("sp", x1_, x1res[rows, :], g_x[0])
            if tt_ == 0:
                dbg_store("peer", acc)
            P.tt("dve", acc, acc, bc3["gt2"], ALU.mult)
            P.stt(acc, x1_, ALPHA, acc, ALU.mult, ALU.add)
            ln_tm(acc, tq2, st, mv, rs)
            P.tt("dve", tq2, tq2, bc3["g2"], ALU.mult)
            P.tt("dve", x1_, tq2, bc3["b2"], ALU.add)
            if last:
                P.dma("sp", yout[u][rows, :], x1_, g_st)
            else:
                P.dma("sp", xres[u][rows, :], x1_, g_st)
        P.barrier()

    phase_mod()
    ret = None
    for l in range(nlayers):
        phase_setup(l)
        if stop == "setup":
            break
        for u in units:
            ret = phase_A(l, u)
            if stop is not None and stop.startswith("A"):
                break
            phase_B(l, u, last=(l == nlayers - 1))
            if stop is not None:
                break
        if stop is not None:
            break
    if ret is not None and "ret" in dbg_out:
        src = ret
        P.dma("sp", dbg_out["ret"], src, g_st)
    P.final_wait()
    P.emit()
    return nc


def _grid_pos():
    rows = T // 64
    t = np.arange(rows * 64)
    r = (t // 64).astype(np.float32)
    col = (t % 64).astype(np.float32)
    nf = D // 4
    freq = (1.0 / (np.float32(10000.0) ** (np.arange(nf, dtype=np.float32) / np.float32(nf)))).astype(np.float32)
    ar = r[:, None] * freq
    ac = col[:, None] * freq
    return np.concatenate([np.sin(ar), np.cos(ar), np.sin(ac), np.cos(ac)], -1).astype(np.float32)


WEIGHT_NAMES = ["w_mod", "b_mod", "w_in", "s5_lam_re", "s5_lam_im", "s5_log_dt", "s5_b_re", "s5_b_im", "s5_c_re",
                "s5_c_im", "s5_d", "s5_w_glu", "sc_conv_w", "sc_conv_b", "cf_conv_w", "cf_conv_b", "cf_ln_g", "cf_ln_b",
                "w_pa", "w_pb", "w_pc", "w_o", "ln1_g", "ln1_b", "peer_w_q", "peer_k1", "peer_k2", "peer_u", "peer_v",
                "ln2_g", "ln2_b"]


def make_in_maps(inputs, cores=range(NCORE)):
    f = lambda a: np.ascontiguousarray(np.asarray(a, dtype=np.float32))
    xp = f(inputs["x_prompt"])
    xs = f(inputs["x_sample"])
    stt = f(inputs["state_ssm"])
    c = f(inputs["c"])
    cc = f(inputs["c_ctx"])
    pe = _grid_pos()
    ident = np.eye(128, dtype=np.float32)
    iota = np.tile(np.arange(128, dtype=np.float32)[None, :], (128, 1))
    wts = {k: f(inputs[k]) for k in WEIGHT_NAMES}
    maps = []
    for i in cores:
        m = dict(wts)
        m["xp"] = np.ascontiguousarray(xp[4 * i:4 * i + 4].reshape(T, D))
        m["xs"] = np.ascontiguousarray(xs[i].reshape(T, D))
        m["pe"] = pe
        m["st0"] = np.ascontiguousarray(stt[i])
        m["cond"] = np.ascontiguousarray(np.stack([cc, c[i]], 0))
        m["ident"] = ident
        m["iota128"] = iota
        maps.append(m)
    return maps


def kernel(**inputs):
    nc = build()
    maps = make_in_maps(inputs)
    maps = [{k: m[k] for k in build.declared} for m in maps]
    res = run_bass_kernel_spmd(nc, maps, core_ids=list(range(NCORE)))
    yp = np.concatenate([r["yp"].reshape(4, 256, D) for r in res.results], 0).astype(np.float32)
    ys = np.stack([r["ys"].reshape(T, D) for r in res.results], 0).astype(np.float32)
    nst = np.concatenate([r["nst"] for r in res.results], 0).astype(np.float32)
    return (yp, ys, nst)
```

```python
import math
from contextlib import ExitStack

import numpy as np
import concourse.bass as bass
import concourse.mybir as mybir
from concourse.bass_utils import run_bass_kernel_spmd

F32 = mybir.dt.float32
BF16 = mybir.dt.bfloat16
I32 = mybir.dt.int32
U32 = mybir.dt.uint32
ALU = mybir.AluOpType
AF = mybir.ActivationFunctionType
AX = mybir.AxisListType

D = 2048
NCORE = 8
T = 1024
NTT = 8
KT = 16
NL = 2
ALPHA = 4.0 ** 0.25
LN_EPS = 1e-6
IN_COLS = 9728
GATE0 = 3584
NEG = -1.0e30


class Buf:
    __slots__ = ("name", "lw", "rd", "space", "dsem")

    def __init__(self, name, space="sb"):
        self.name = name
        self.lw = {}
        self.rd = {}
        self.space = space
        self.dsem = None


class Grp:
    __slots__ = ("sem", "cnt")

    def __init__(self, sem):
        self.sem = sem
        self.cnt = 0


class TT:
    __slots__ = ("ap", "buf")

    def __init__(self, ap, buf):
        self.ap = ap
        self.buf = buf

    def __getitem__(self, k):
        return TT(self.ap[k], self.buf)

    def r(self, pat, **kw):
        return TT(self.ap.rearrange(pat, **kw), self.buf)

    def bc(self, shape):
        return TT(self.ap.to_broadcast(list(shape)), self.buf)

    def un(self, ax):
        return TT(self.ap.unsqueeze(ax), self.buf)

    def bitcast(self, dt):
        return TT(self.ap.bitcast(dt), self.buf)


def _a(x):
    return x.ap if isinstance(x, TT) else x


class Prog:
    def __init__(self, nc, es):
        self.nc = nc
        self.es = es
        self.E = dict(pe=nc.tensor, dve=nc.vector, act=nc.scalar, pool=nc.gpsimd, sp=nc.sync)
        self.rec = {k: [] for k in self.E}
        self.esem = {k: es.enter_context(nc.semaphore("es_" + k)) for k in self.E}
        self.ecnt = {k: 0 for k in self.E}
        self.known = {k: {} for k in self.E}
        self.grps = []
        self.free = []
        self.active = []
        self.max_dsem = 84

    def grp(self, name):
        g = Grp(self.es.enter_context(self.nc.semaphore(name)))
        self.grps.append(g)
        return g

    def dsem_for(self, buf):
        if buf.dsem is None:
            if self.free:
                buf.dsem = self.free.pop()
            else:
                assert len(self.grps) < self.max_dsem, "out of DMA semaphores"
                buf.dsem = self.grp("dq%d" % len(self.grps))
            self.active.append(buf)
        return buf.dsem

    def op(self, e, fn, r=(), w=(), grp=None):
        need = {}
        own = self.esem[e]

        def add(tok):
            sem, val = tok
            if e == "pe" and sem is own:
                return
            k = id(sem)
            if k not in need or need[k][1] < val:
                need[k] = (sem, val)

        for b in list(r) + list(w):
            for tok in b.lw.values():
                add(tok)
        for b in w:
            for tok in b.rd.values():
                add(tok)
        waits = []
        kn = self.known[e]
        for k, (sem, val) in need.items():
            if kn.get(k, 0) >= val:
                continue
            kn[k] = val
            waits.append((sem, val))
        if grp is not None:
            grp.cnt += 16
            tok = (grp.sem, grp.cnt)
            inc = (grp.sem, 16)
        else:
            self.ecnt[e] += 1
            tok = (own, self.ecnt[e])
            inc = (own, 1)
        for b in w:
            if b.space == "dram":
                b.lw[id(tok[0])] = tok
            else:
                b.lw = {id(tok[0]): tok}
            b.rd = {}
        for b in r:
            if b in w:
                continue
            b.rd[id(tok[0])] = tok
        self.rec[e].append((waits, fn, inc))

    def barrier(self):
        toks = [(self.esem[k], self.ecnt[k]) for k in self.E] + [(g.sem, g.cnt) for g in self.grps]
        for e in self.E:
            waits = []
            kn = self.known[e]
            for sem, val in toks:
                if val == 0 or sem is self.esem[e]:
                    continue
                if kn.get(id(sem), 0) >= val:
                    continue
                kn[id(sem)] = val
                waits.append((sem, val))
            if waits:
                self.rec[e].append((waits, None, None))
        for b in self.active:
            self.free.append(b.dsem)
            b.dsem = None
        self.active = []

    def final_wait(self):
        e = "sp"
        waits = [(self.esem[k], self.ecnt[k]) for k in self.E if k != e and self.ecnt[k]]
        waits += [(g.sem, g.cnt) for g in self.grps if g.cnt]
        self.rec[e].append((waits, None, None))

    def emit(self):
        with self.nc.Block() as blk:
            def mk(e):
                def body(Eng):
                    for waits, fn, inc in self.rec[e]:
                        for sem, val in waits:
                            Eng.wait_ge(sem, val)
                        if fn is not None:
                            fn(Eng).then_inc(inc[0], inc[1])
                return body
            blk.tensor(mk("pe"))
            blk.vector(mk("dve"))
            blk.scalar(mk("act"))
            blk.gpsimd(mk("pool"))
            blk.sync(mk("sp"))

    def dma(self, q, out, in_, grp=None, **kw):
        if out.buf.space == "sb":
            g = self.dsem_for(out.buf)
        elif in_.buf.space == "sb":
            g = self.dsem_for(in_.buf)
        else:
            g = self.dsem_for(out.buf)
        self.op(q, lambda E: E.dma_start(out=out.ap, in_=in_.ap, **kw), r=[in_.buf], w=[out.buf], grp=g)

    def mm(self, out, lhsT, rhs, start=True, stop=True):
        self.op("pe", lambda E: E.matmul(out=out.ap, lhsT=lhsT.ap, rhs=rhs.ap, start=start, stop=stop),
                r=[lhsT.buf, rhs.buf], w=[out.buf])

    def tr(self, out, in_, ident):
        self.op("pe", lambda E: E.transpose(out=out.ap, in_=in_.ap, identity=ident.ap),
                r=[in_.buf, ident.buf], w=[out.buf])

    def act(self, out, in_, func, bias=None, scale=None, e="act"):
        rb = [in_.buf]
        kw = {}
        if bias is not None:
            kw["bias"] = _a(bias)
            if isinstance(bias, TT):
                rb.append(bias.buf)
        if scale is not None:
            kw["scale"] = _a(scale)
            if isinstance(scale, TT):
                rb.append(scale.buf)
        self.op("act", lambda E: E.activation(out=out.ap, in_=in_.ap, func=func, **kw), r=rb, w=[out.buf])

    def cp(self, e, out, in_):
        if e == "act":
            self.op("act", lambda E: E.copy(out=out.ap, in_=in_.ap), r=[in_.buf], w=[out.buf])
        else:
            self.op(e, lambda E: E.tensor_copy(out=out.ap, in_=in_.ap), r=[in_.buf], w=[out.buf])

    def tt(self, e, out, a, b, op):
        self.op(e, lambda E: E.tensor_tensor(out=out.ap, in0=a.ap, in1=b.ap, op=op), r=[a.buf, b.buf], w=[out.buf])

    def ts(self, e, out, a, s1, op0, s2=None, op1=None):
        rb = [a.buf] + [x.buf for x in (s1, s2) if isinstance(x, TT)]
        if op1 is None:
            self.op(e, lambda E: E.tensor_scalar(out=out.ap, in0=a.ap, scalar1=_a(s1), scalar2=None, op0=op0),
                    r=rb, w=[out.buf])
        else:
            self.op(e, lambda E: E.tensor_scalar(out=out.ap, in0=a.ap, scalar1=_a(s1), scalar2=_a(s2), op0=op0, op1=op1),
                    r=rb, w=[out.buf])

    def stt(self, out, a, s, b, op0, op1, e="dve"):
        rb = [a.buf, b.buf] + ([s.buf] if isinstance(s, TT) else [])
        self.op(e, lambda E: E.scalar_tensor_tensor(out=out.ap, in0=a.ap, scalar=_a(s), in1=b.ap, op0=op0, op1=op1),
                r=rb, w=[out.buf])

    def memset(self, e, out, val):
        self.op(e, lambda E: E.memset(out.ap, val), w=[out.buf])

    def recip(self, out, in_):
        self.op("dve", lambda E: E.reciprocal(out=out.ap, in_=in_.ap), r=[in_.buf], w=[out.buf])

    def scan(self, out, d0, d1, init):
        rb = [d0.buf, d1.buf] + ([init.buf] if isinstance(init, TT) else [])
        self.op("dve", lambda E: E.tensor_tensor_scan(out=out.ap, data0=d0.ap, data1=d1.ap, initial=_a(init),
                                                       op0=ALU.mult, op1=ALU.add), r=rb, w=[out.buf])

    def reduce(self, out, in_, op, axis=AX.X):
        self.op("dve", lambda E: E.tensor_reduce(out=out.ap, in_=in_.ap, axis=axis, op=op), r=[in_.buf], w=[out.buf])


class Stack:
    cnt = [0]

    def __init__(self, nc, base, limit):
        self.nc = nc
        self.base = base
        self.cur = base
        self.limit = limit

    def alloc(self, free_shape, dtype, name):
        nb = int(np.prod(free_shape)) * (2 if dtype == BF16 else 4)
        off = (self.cur + 31) // 32 * 32
        assert off + nb <= self.limit, (name, off, nb, self.limit)
        self.cur = off + nb
        Stack.cnt[0] += 1
        h = self.nc.alloc_sbuf_tensor_at("%s_%d" % (name, Stack.cnt[0]), [128] + list(free_shape), dtype, offset=off)
        return TT(h.ap(), Buf(name))

    def sub(self, nbytes):
        off = (self.cur + 31) // 32 * 32
        assert off + nbytes <= self.limit, (off, nbytes, self.limit)
        self.cur = off + nbytes
        return Stack(self.nc, off, off + nbytes)

    def reset(self):
        self.cur = self.base


def build(dbg=None, stop=None, nlayers=NL, units=(0, 1), b3_tiles=NTT):
    dbg = dbg or {}
    Stack.cnt[0] = 0
    nc = bass.Bass("TRN2", target_bir_lowering=False)
    es = ExitStack()
    P = Prog(nc, es)

    declared = []
    build.declared = declared

    def din(name, shape, dt=F32):
        declared.append(name)
        return TT(nc.dram_tensor(name, list(shape), dt, kind="ExternalInput").ap(), Buf(name, "dram"))

    def dout(name, shape, dt=F32):
        return TT(nc.dram_tensor(name, list(shape), dt, kind="ExternalOutput").ap(), Buf(name, "dram"))

    def dscr(name, shape, dt=F32):
        return TT(nc.dram_tensor(name, list(shape), dt, kind="Internal").ap(), Buf(name, "dram"))

    xin = [din("xp", [T, D]), din("xs", [T, D])]
    pe_d = din("pe", [T, D])
    st0 = din("st0", [NL, 2, 64, 64, 2])
    cond = din("cond", [2, D])
    ident_d = din("ident", [128, 128])
    iota_d = din("iota128", [128, 128])
    WSH = dict([("w_mod", [NL, D, 6 * D]), ("b_mod", [NL, 6 * D]), ("w_in", [NL, D, IN_COLS]),
                ("s5_lam_re", [NL, 2, 64, 64]), ("s5_lam_im", [NL, 2, 64, 64]), ("s5_log_dt", [NL, 2, 64]),
                ("s5_b_re", [NL, 2, 64, 64, 16]), ("s5_b_im", [NL, 2, 64, 64, 16]),
                ("s5_c_re", [NL, 2, 64, 16, 64]), ("s5_c_im", [NL, 2, 64, 16, 64]),
                ("s5_d", [NL, 1024]), ("s5_w_glu", [NL, 1024, 1024]),
                ("sc_conv_w", [NL, 3, 512]), ("sc_conv_b", [NL, 512]),
                ("cf_conv_w", [NL, 31, 512]), ("cf_conv_b", [NL, 512]),
                ("cf_ln_g", [NL, 512]), ("cf_ln_b", [NL, 512]),
                ("w_pa", [NL, 1024, D]), ("w_pb", [NL, 512, D]), ("w_pc", [NL, 512, D]), ("w_o", [NL, D, D]),
                ("ln1_g", [NL, D]), ("ln1_b", [NL, D]),
                ("peer_w_q", [NL, D, D]), ("peer_k1", [NL, 128, 128]), ("peer_k2", [NL, 128, 128]),
                ("peer_u", [NL, 16384, D]), ("peer_v", [NL, 16384, D]),
                ("ln2_g", [NL, D]), ("ln2_b", [NL, D])])

    class LazyW(dict):
        def __missing__(self, nm):
            self[nm] = din(nm, WSH[nm])
            return self[nm]
    W = LazyW()
    yout = [dout("yp", [T, D]), dout("ys", [T, D])]
    nst_o = dout("nst", [4, NL, 2, 64, 64, 2])
    dbg_out = {k: dout("dbg_" + k, v[0], v[1]) for k, v in dbg.items()}

    xres = [dscr("xres0", [T, D]), dscr("xres1", [T, D])]
    ymix = dscr("ymix", [T, D])
    x1res = dscr("x1res", [T, D])
    h2res = dscr("h2res", [T, D])
    mod_d = dscr("mod_d", [NL, 2, 6 * D])
    tabs = dscr("tabs", [64, 128, 2, 260])
    wbd = dscr("wbd", [8, 128, 16, 128], BF16)
    cwd = dscr("cwd", [8, 128, 16, 64], BF16)
    Gd = dscr("Gd", [128, 128, T], BF16)

    psh = nc.alloc_psum_tensor("psum_all", [128, 4096], F32)
    PS = [TT(psh.ap()[:, i * 1024:(i + 1) * 1024], Buf("ps%d" % i, "ps")) for i in range(4)]
    ps_rr = [0]

    def ps_next():
        ps_rr[0] = (ps_rr[0] + 1) % 4
        return PS[ps_rr[0]]

    SB_BASE = (int(nc.sbuf_base) + 63) // 64 * 64
    SB_TOP = int(nc.sbuf_top)
    root = Stack(nc, SB_BASE, SB_TOP)
    G = root.sub(4 * 1024)
    LP = root.sub(4608)
    UL = Stack(nc, root.cur, SB_TOP)

    g_st = g_c = None
    g_x = g_w = g_t = [None, None]

    ident_f = G.alloc([128], F32, "ident_f")
    ident_b = G.alloc([128], BF16, "ident_b")
    iota128 = G.alloc([128], F32, "iota128")
    iota16 = iota128[:, 0:16]
    halfpi = G.alloc([1], F32, "halfpi")
    X4 = G.alloc([4, 128], BF16, "X4")
    CWst = G.alloc([4, 64], BF16, "CWst")
    ones_f = G.alloc([128], F32, "ones_f")
    P.dma("sp", ident_f, ident_d, g_c)
    P.dma("sp", iota128, iota_d, g_c)
    P.cp("dve", ident_b, ident_f)
    P.memset("dve", halfpi, math.pi / 2)
    P.memset("dve", X4, 0.0)
    P.memset("dve", CWst, 0.0)
    P.memset("dve", ones_f, 1.0 / 512.0)

    Rt = LP.alloc([64], F32, "Rt")
    DSK = LP.alloc([8], F32, "DSK")
    H0 = LP.alloc([64, 2], F32, "H0")
    SCW = LP.alloc([4, 3], F32, "SCW")
    SCB = LP.alloc([4], F32, "SCB")
    CFW = LP.alloc([4, 31], F32, "CFW")
    CFB = LP.alloc([4], F32, "CFB")
    CFG = LP.alloc([4], F32, "CFG")
    CFBB = LP.alloc([4], F32, "CFBB")
    KTb = LP.alloc([2, 128], BF16, "KTb")
    NST = LP.alloc([64, 4, 2], F32, "NST")

    WSTG_B = 16 * 256 * 4
    WBF_B = 16 * 256 * 2

    def dbg_store(key, src, dst_slice=None):
        if key in dbg_out:
            dst = dbg_out[key] if dst_slice is None else dbg_out[key][dst_slice]
            P.dma("sp", dst, src, g_st)

    def phase_mod():
        UL.reset()
        condT = UL.alloc([16, 2], F32, "condT")
        bmod = UL.alloc([6 * D], F32, "bmod")
        stg = [UL.alloc([16, 256], F32, "mstg%d" % i) for i in range(2)]
        mo = [UL.alloc([256], F32, "mo%d" % i) for i in range(2)]
        for r_ in range(2):
            P.dma("sp", condT[:, :, r_], cond[r_].r("(kt p) -> p kt", p=128), g_c, allow_slow_non_contiguous=True)
        P.act(condT, condT, AF.Silu)
        for l in range(nlayers):
            P.dma("sp", bmod[0:2, :], TT(W["b_mod"].ap[l:l + 1, :].partition_broadcast(2), W["b_mod"].buf), g_c)
            for c in range(48):
                s_ = stg[c % 2]
                P.dma("sp", s_, W["w_mod"][l][:, c * 256:(c + 1) * 256].r("(kt p) c -> p kt c", p=128), g_w[c % 2])
                ps = ps_next()
                for kt in range(16):
                    P.mm(ps[0:2, 0:256], condT[:, kt, :], s_[:, kt, :], start=(kt == 0), stop=(kt == 15))
                m_ = mo[c % 2]
                P.tt("dve", m_[0:2, :], ps[0:2, 0:256], bmod[0:2, c * 256:(c + 1) * 256], ALU.add)
                if (8 <= c < 16) or (32 <= c < 40):
                    P.ts("dve", m_[0:2, :], m_[0:2, :], 1.0, ALU.add)
                P.dma("sp", mod_d[l][:, c * 256:(c + 1) * 256], m_[0:2, :], g_st)
        P.barrier()

    def phase_setup(l):
        UL.reset()
        A = UL
        nat = [A.alloc([128], F32, "nat%d" % i) for i in range(3)]
        dt2 = A.alloc([2], F32, "dt2")
        LR = A.alloc([64], F32, "LR")
        LI = A.alloc([64], F32, "LI")
        DT = A.alloc([64], F32, "DT")
        P.dma("sp", nat[0][0:64, :], W["s5_lam_re"][l].r("d (gp g2) p -> (d gp) (g2 p)", g2=2), g_c)
        P.dma("sp", nat[1][0:64, :], W["s5_lam_im"][l].r("d (gp g2) p -> (d gp) (g2 p)", g2=2), g_c)
        P.dma("sp", dt2[0:64, :], W["s5_log_dt"][l].r("d (gp g2) -> (d gp) g2", g2=2), g_c)
        P.cp("dve", nat[2][0:64, :].r("q (a b) -> q a b", a=2), dt2[0:64, :].un(2).bc([64, 2, 64]))
        for i, dst in enumerate((LR, LI, DT)):
            ps = ps_next()
            P.tr(ps[:, 0:64], nat[i][0:64, :], ident_f[0:64, 0:64])
            P.cp("act", dst, ps[:, 0:64])
        v = {k: A.alloc([64], F32, k) for k in ("dt", "ang", "c", "s", "t1", "t2", "are", "aim", "den", "am1", "zre", "zim")}
        P.act(v["dt"], DT, AF.Exp)
        P.tt("dve", v["t1"], LR, v["dt"], ALU.mult)
        P.act(Rt, v["t1"], AF.Exp)
        P.tt("dve", v["ang"], LI, v["dt"], ALU.mult)
        P.act(v["s"], v["ang"], AF.Sin, scale=1.0 / 32.0)
        P.act(v["c"], v["ang"], AF.Sin, bias=halfpi[:, 0:1], scale=1.0 / 32.0)
        for _ in range(5):
            P.tt("dve", v["t1"], v["c"], v["c"], ALU.mult)
            P.tt("dve", v["t2"], v["s"], v["s"], ALU.mult)
            P.stt(v["s"], v["c"], 2.0, v["s"], ALU.mult, ALU.mult)
            P.tt("dve", v["c"], v["t1"], v["t2"], ALU.subtract)
        C1, S1 = v["c"], v["s"]
        P.tt("dve", v["are"], Rt, C1, ALU.mult)
        P.tt("dve", v["aim"], Rt, S1, ALU.mult)
        P.tt("dve", v["t1"], LR, LR, ALU.mult)
        P.tt("dve", v["t2"], LI, LI, ALU.mult)
        P.tt("dve", v["den"], v["t1"], v["t2"], ALU.add)
        P.recip(v["den"], v["den"])
        P.ts("dve", v["am1"], v["are"], -1.0, ALU.add)
        P.tt("dve", v["t1"], v["am1"], LR, ALU.mult)
        P.tt("dve", v["t2"], v["aim"], LI, ALU.mult)
        P.tt("dve", v["t1"], v["t1"], v["t2"], ALU.add)
        P.tt("dve", v["zre"], v["t1"], v["den"], ALU.mult)
        P.tt("dve", v["t1"], v["aim"], LR, ALU.mult)
        P.tt("dve", v["t2"], v["am1"], LI, ALU.mult)
        P.tt("dve", v["t1"], v["t1"], v["t2"], ALU.subtract)
        P.tt("dve", v["zim"], v["t1"], v["den"], ALU.mult)
        BRE = A.alloc([64, 16], F32, "BRE")
        BIM = A.alloc([64, 16], F32, "BIM")
        tA = A.alloc([64, 16], F32, "tA")
        tB = A.alloc([64, 16], F32, "tB")
        BB = [A.alloc([64, 16], BF16, "BBre"), A.alloc([64, 16], BF16, "BBim")]
        P.dma("sp", BRE, W["s5_b_re"][l].r("d (gp g2) p s -> (g2 p) (d gp) s", g2=2), g_c)
        P.dma("sp", BIM, W["s5_b_im"][l].r("d (gp g2) p s -> (g2 p) (d gp) s", g2=2), g_c)
        zr = v["zre"].un(2).bc([128, 64, 16])
        zi = v["zim"].un(2).bc([128, 64, 16])
        P.tt("dve", tA, BRE, zr, ALU.mult)
        P.tt("dve", tB, BIM, zi, ALU.mult)
        P.tt("dve", BB[0], tA, tB, ALU.subtract)
        P.tt("dve", tA, BIM, zr, ALU.mult)
        P.tt("dve", tB, BRE, zi, ALU.mult)
        P.tt("dve", BB[1], tA, tB, ALU.add)
        wst = [A.alloc([4, 128], BF16, "wst%d" % i) for i in range(2)]
        it = 0
        for d in range(2):
            for ct in range(8):
                rt0 = d * 32 + ct * 4
                for reim in range(2):
                    for j in range(4):
                        P.cp("dve", X4[0:64, j, 32 * j:32 * j + 16], BB[reim][0:64, rt0 + j, :])
                        P.cp("dve", X4[64:128, j, 32 * j + 16:32 * j + 32], BB[reim][64:128, rt0 + j, :])
                    ps = ps_next()
                    psb = ps[:, 0:256].bitcast(BF16).r("p (j c) -> p j c", j=4)
                    for j in range(4):
                        P.tr(psb[:, j, :], X4[:, j, :], ident_b)
                    ws_ = wst[it % 2]
                    it += 1
                    P.cp("act", ws_, psb)
                    P.dma("sp", wbd[ct][:, d * 8 + reim:d * 8 + 8:2, :], ws_, g_st)
        CN = [A.alloc([64], F32, "CN%d" % i) for i in range(2)]
        CN2 = A.alloc([128], BF16, "CN2")
        cst = [A.alloc([4, 64], BF16, "cst%d" % i) for i in range(2)]
        it = 0
        for d in range(2):
            for ct in range(8):
                for reim in range(2):
                    src = W["s5_c_im" if reim else "s5_c_re"][l, d, 8 * ct:8 * ct + 8].r("g s p -> (g s) p")
                    cn = CN[it % 2]
                    P.dma("sp", cn, src, g_t[it % 2])
                    sgn = -1.0 if reim else 1.0
                    P.ts("dve", CN2[:, 0:64], cn, sgn, ALU.mult)
                    P.ts("dve", CN2[:, 64:128], cn, sgn, ALU.mult)
                    ps = ps_next()
                    psb = ps[:, 0:64].bitcast(BF16)
                    P.tr(psb, CN2, ident_b)
                    for j in range(4):
                        jl = j % 2
                        P.cp("dve", CWst[0:64, j, 32 * jl:32 * jl + 16], psb[0:64, 32 * j:32 * j + 16])
                        P.cp("dve", CWst[64:128, j, 32 * jl + 16:32 * jl + 32], psb[64:128, 32 * j + 16:32 * j + 32])
                    cs_ = cst[it % 2]
                    it += 1
                    P.cp("dve", cs_, CWst)
                    P.dma("sp", cwd[ct][:, d * 8 + reim:d * 8 + 8:2, :], cs_, g_st)
        ER = A.alloc([16, 260], F32, "ER")
        EI = A.alloc([16, 260], F32, "EI")
        q = {k: A.alloc([16, 128], F32, k) for k in ("q1", "q2", "q3", "q4")}
        MR = A.alloc([16], F32, "MR")
        MI = A.alloc([16], F32, "MI")
        m1 = A.alloc([16], F32, "m1")
        m2 = A.alloc([16], F32, "m2")
        for gq in range(4):
            sl = slice(gq * 16, gq * 16 + 16)
            P.memset("dve", ER[:, :, 0:1], 1.0)
            P.memset("dve", EI[:, :, 0:1], 0.0)
            P.cp("dve", MR, C1[:, sl])
            P.cp("dve", MI, S1[:, sl])
            k = 1
            while k < 260:
                cnt = min(k, 260 - k)
                o = 0
                while o < cnt:
                    n = min(128, cnt - o)
                    mrb = MR.un(2).bc([128, 16, n])
                    mib = MI.un(2).bc([128, 16, n])
                    sr = ER[:, :, o:o + n]
                    si = EI[:, :, o:o + n]
                    P.tt("dve", q["q1"][:, :, 0:n], sr, mrb, ALU.mult)
                    P.tt("dve", q["q2"][:, :, 0:n], si, mib, ALU.mult)
                    P.tt("dve", q["q3"][:, :, 0:n], sr, mib, ALU.mult)
                    P.tt("dve", q["q4"][:, :, 0:n], si, mrb, ALU.mult)
                    P.tt("dve", ER[:, :, k + o:k + o + n], q["q1"][:, :, 0:n], q["q2"][:, :, 0:n], ALU.subtract)
                    P.tt("dve", EI[:, :, k + o:k + o + n], q["q3"][:, :, 0:n], q["q4"][:, :, 0:n], ALU.add)
                    o += n
                if 2 * k < 260:
                    P.tt("dve", m1, MR, MR, ALU.mult)
                    P.tt("dve", m2, MI, MI, ALU.mult)
                    P.stt(MI, MR, 2.0, MI, ALU.mult, ALU.mult)
                    P.tt("dve", MR, m1, m2, ALU.subtract)
                k *= 2
            P.dma("sp", tabs[gq * 16:gq * 16 + 16, :, 0, :].r("rt p t -> p rt t"), ER, g_st)
            P.dma("sp", tabs[gq * 16:gq * 16 + 16, :, 1, :].r("rt p t -> p rt t"), EI, g_st)
        P.dma("sp", DSK, W["s5_d"][l].r("(ct p) -> p ct", p=128), g_c, allow_slow_non_contiguous=True)
        P.dma("sp", H0, st0[l].r("d (gp g2) p c -> (g2 p) (d gp) c", g2=2), g_c)
        cwn = A.alloc([512], F32, "cwn")
        for nm_, dst_, nk_ in (("sc_conv_w", SCW, 3), ("cf_conv_w", CFW, 31)):
            P.dma("sp", cwn[0:nk_, :], W[nm_][l], g_c)
            for ct_ in range(4):
                ps = ps_next()
                P.tr(ps[:, 0:nk_], cwn[0:nk_, ct_ * 128:(ct_ + 1) * 128], ident_f[0:nk_, 0:nk_])
                P.cp("act", dst_[:, ct_, :], ps[:, 0:nk_])
        P.dma("sp", SCB, W["sc_conv_b"][l].r("(ct p) -> p ct", p=128), g_c, allow_slow_non_contiguous=True)
        P.dma("sp", CFB, W["cf_conv_b"][l].r("(ct p) -> p ct", p=128), g_c, allow_slow_non_contiguous=True)
        P.dma("sp", CFG, W["cf_ln_g"][l].r("(ct p) -> p ct", p=128), g_c, allow_slow_non_contiguous=True)
        P.dma("sp", CFBB, W["cf_ln_b"][l].r("(ct p) -> p ct", p=128), g_c, allow_slow_non_contiguous=True)
        dbg_store("CFW", CFW)
        dbg_store("SCW", SCW)
        kn = [A.alloc([128], F32, "kn%d" % i) for i in range(2)]
        for hf, nm in enumerate(("peer_k1", "peer_k2")):
            P.dma("sp", kn[hf], W[nm][l], g_c)
            ps = ps_next()
            P.tr(ps[:, 0:128], kn[hf], ident_f)
            P.cp("act", KTb[:, hf, :], ps[:, 0:128])
        P.barrier()

    wslots = {}
    w_it = [0]

    def load_w(W2d, r0, nk, c0, ncol):
        i = w_it[0] % 2
        w_it[0] += 1
        stg, wbf = wslots["stg"][i], wslots["wbf"][i]
        P.dma("sp", stg[:, 0:nk, 0:ncol], W2d[r0:r0 + 128 * nk, c0:c0 + ncol].r("(kt p) c -> p kt c", p=128), g_w[i])
        if i == 0:
            P.cp("dve", wbf[:, 0:nk, 0:ncol], stg[:, 0:nk, 0:ncol])
        else:
            P.cp("act", wbf[:, 0:nk, 0:ncol], stg[:, 0:nk, 0:ncol])
        return wbf

    def linear_fm(in_tiles, W2d, r0, c0, ncols, consume, tile0=0):
        nk = len(in_tiles)
        for cc in range(0, ncols, 256):
            wb = load_w(W2d, r0, nk, c0 + cc, 256)
            for ti in range(2):
                ps = ps_next()
                for half in range(2):
                    hs = slice(half * 512, half * 512 + 512)
                    for kt in range(nk):
                        P.mm(ps[:, hs], wb[:, kt, ti * 128:(ti + 1) * 128], in_tiles[kt][:, hs],
                             start=(kt == 0), stop=(kt == nk - 1))
                consume(tile0 + cc // 128 + ti, ps)

    def ln_tm(x, out, st, mv, rs, e="dve"):
        for c in range(4):
            P.op("dve", lambda E, c=c: E.bn_stats(out=st.ap[:, c, :], in_=x.ap[:, c * 512:(c + 1) * 512]),
                 r=[x.buf], w=[st.buf])
        P.op("dve", lambda E: E.bn_aggr(out=mv.ap, in_=st.ap.rearrange("p a b -> p (a b)")), r=[st.buf], w=[mv.buf])
        P.ts("dve", rs, mv[:, 1:2], LN_EPS, ALU.add)
        P.act(rs, rs, AF.Sqrt)
        P.recip(rs, rs)
        P.ts("dve", out, x, mv[:, 0:1], ALU.subtract, rs[:, 0:1], ALU.mult)

    def phase_A(l, u):
        nseq, SL = (4, 256) if u == 0 else (1, 1024)
        UL.reset()
        hT = UL.alloc([16, T], BF16, "hT")
        bufP = UL.alloc([8, T], BF16, "bufP")
        bufQ = UL.alloc([8, T], BF16, "bufQ")
        ysT = UL.alloc([4, T], BF16, "ysT")
        zT = UL.alloc([4, T], BF16, "zT")
        wslots["wbf"] = [UL.alloc([16, 256], BF16, "wbf%d" % i) for i in range(2)]
        wslots["stg"] = [UL.alloc([16, 256], F32, "wstg%d" % i) for i in range(2)]
        X = Stack(nc, UL.cur, SB_TOP)

        X.reset()
        xt = [X.alloc([D], F32, "xt%d" % i) for i in range(2)]
        tmp = X.alloc([D], F32, "tmpA0")
        hb = X.alloc([D], BF16, "hb")
        sc1b = X.alloc([D], F32, "sc1b")
        sh1b = X.alloc([D], F32, "sh1b")
        pet = X.alloc([D], F32, "pet")
        st = X.alloc([4, 6], F32, "st")
        mv = X.alloc([2], F32, "mv")
        rs = X.alloc([1], F32, "rs")
        P.dma("sp", sh1b, TT(mod_d.ap[l, u:u + 1, 0:D].partition_broadcast(128), mod_d.buf), g_c)
        P.dma("sp", sc1b, TT(mod_d.ap[l, u:u + 1, D:2 * D].partition_broadcast(128), mod_d.buf), g_c)
        for tt_ in range(NTT):
            rows = slice(tt_ * 128, tt_ * 128 + 128)
            x_ = xt[tt_ % 2]
            if l == 0:
                P.dma("sp", x_, xin[u][rows, :], g_x[tt_ % 2])
                if u == 1:
                    P.dma("sp", pet, pe_d[rows, :], g_x[tt_ % 2])
                    P.tt("dve", x_, x_, pet, ALU.add)
                P.dma("sp", xres[u][rows, :], x_, g_st)
            else:
                P.dma("sp", x_, xres[u][rows, :], g_x[tt_ % 2])
            ln_tm(x_, tmp, st, mv, rs)
            P.tt("dve", tmp, tmp, sc1b, ALU.mult)
            P.tt("dve", hb, tmp, sh1b, ALU.add)
            ps = ps_next()
            psb = ps.bitcast(BF16)
            for kt in range(KT):
                P.tr(psb[:, kt * 128:(kt + 1) * 128], hb[:, kt * 128:(kt + 1) * 128], ident_b)
            P.cp("act", hT[:, :, rows], psb.r("p (k t) -> p k t", k=KT))
        dbg_store("hT", hT)
        if stop == "A0":
            return None
        hT_tiles = [hT[:, kt, :] for kt in range(KT)]
        P.barrier()

        uT = bufP
        ya = bufQ

        def cons_u(ti, ps):
            P.cp("act", uT[:, ti, :], ps)
        linear_fm(hT_tiles, W["w_in"][l], 0, 0, 1024, cons_u)
        dbg_store("uT", uT)
        X.reset()
        f = {k: X.alloc([T], F32, k) for k in ("bure", "buim", "t1", "t2", "t3", "br", "bi", "gre", "gim")}
        fbh = {k: X.alloc([T], BF16, k) for k in ("greb", "gimb", "q1", "q2", "q3", "q4")}
        tbb = X.alloc([2, 256], BF16, "tbb")
        hre = X.alloc([T], BF16, "hre")
        him = X.alloc([T], BF16, "him")
        tbs = [X.alloc([2, 260], F32, "tb%d" % i) for i in range(2)]
        wbt = [X.alloc([16, 128], BF16, "wbt%d" % i) for i in range(2)]
        cwt = [X.alloc([16, 64], BF16, "cwt%d" % i) for i in range(2)]
        sm = {k: X.alloc([4], F32, k) for k in ("i_re", "i_im", "s1", "s2")}
        psA, psB, psC, psD = PS

        def v4(t_):
            return t_.r("p (c t) -> p c t", c=4)

        rts = []
        for ct in range(8):
            for d in range(2):
                for j in range(4):
                    rts.append((ct, d, j))

        state = {}

        def issue_bu(i):
            ct, d, j = rts[i]
            if d == 0 and j == 0:
                k = ct % 2
                P.dma("sp", wbt[k], wbd[ct], g_t[k])
                P.dma("sp", cwt[k], cwd[ct], g_t[k])
            rt = d * 32 + ct * 4 + j
            tb = tbs[i % 2]
            P.dma("sp", tb, tabs[rt], g_x[i % 2])
            wb_ = wbt[ct % 2]
            for reim, slot in ((0, psA), (1, psB)):
                for half in range(2):
                    hs = slice(half * 512, half * 512 + 512)
                    P.mm(slot[:, hs], wb_[:, (d * 4 + j) * 2 + reim, :], uT[:, ct, hs])

        issue_bu(0)
        for i, (ct, d, j) in enumerate(rts):
            rt = d * 32 + ct * 4 + j
            tb = tbs[i % 2]
            cw_ = cwt[ct % 2]
            Cb = tb[:, 0, 0:256].un(1).bc([128, 4, 256])
            Sb = tb[:, 1, 0:256].un(1).bc([128, 4, 256])
            for src, dst in ((psA, f["bure"]), (psB, f["buim"])):
                s3 = src.r("p (b t) -> p b t", b=nseq)
                if d == 1:
                    s3 = s3[:, :, ::-1]
                P.cp("act", dst.r("p (b t) -> p b t", b=nseq), s3)
            if i + 1 < len(rts):
                issue_bu(i + 1)
            P.tt("dve", v4(f["t1"]), v4(f["bure"]), Cb, ALU.mult)
            P.tt("dve", v4(f["t2"]), v4(f["buim"]), Sb, ALU.mult)
            P.tt("dve", f["br"], f["t1"], f["t2"], ALU.add)
            P.tt("dve", v4(f["t1"]), v4(f["buim"]), Cb, ALU.mult)
            P.tt("dve", v4(f["t2"]), v4(f["bure"]), Sb, ALU.mult)
            P.tt("dve", f["bi"], f["t1"], f["t2"], ALU.subtract)
            rcol = Rt[:, rt:rt + 1]
            for c in range(4):
                cs = slice(c * 256, c * 256 + 256)
                if u == 0:
                    ire, iim = 0.0, 0.0
                else:
                    if c == 0:
                        pre, pim = H0[:, rt, 0:1], H0[:, rt, 1:2]
                        er, ei = tb[:, 0, 1:2], tb[:, 1, 1:2]
                    else:
                        pre, pim = f["gre"][:, c * 256 - 1:c * 256], f["gim"][:, c * 256 - 1:c * 256]
                        er, ei = tb[:, 0, 256:257], tb[:, 1, 256:257]
                    P.ts("dve", sm["s1"][:, 0:1], pim, ei, ALU.mult)
                    P.stt(sm["i_re"][:, c:c + 1], pre, er, sm["s1"][:, 0:1], ALU.mult, ALU.subtract)
                    P.ts("dve", sm["s2"][:, 0:1], pim, er, ALU.mult)
                    P.stt(sm["i_im"][:, c:c + 1], pre, ei, sm["s2"][:, 0:1], ALU.mult, ALU.add)
                    ire, iim = sm["i_re"][:, c:c + 1], sm["i_im"][:, c:c + 1]
                P.scan(f["gre"][:, cs], rcol.bc([128, 256]), f["br"][:, cs], ire)
                P.scan(f["gim"][:, cs], rcol.bc([128, 256]), f["bi"][:, cs], iim)
            P.cp("act", tbb, tb[:, :, 0:256])
            P.cp("act", fbh["greb"], f["gre"])
            P.cp("act", fbh["gimb"], f["gim"])
            Cbb = tbb[:, 0, :].un(1).bc([128, 4, 256])
            Sbb = tbb[:, 1, :].un(1).bc([128, 4, 256])
            P.tt("dve", v4(fbh["q1"]), v4(fbh["greb"]), Cbb, ALU.mult)
            P.tt("dve", v4(fbh["q2"]), v4(fbh["gimb"]), Sbb, ALU.mult)
            P.tt("dve", hre, fbh["q1"], fbh["q2"], ALU.subtract)
            P.tt("dve", v4(fbh["q3"]), v4(fbh["greb"]), Sbb, ALU.mult)
            P.tt("dve", v4(fbh["q4"]), v4(fbh["gimb"]), Cbb, ALU.mult)
            P.tt("dve", him, fbh["q3"], fbh["q4"], ALU.add)
            if u == 0:
                glr = v4(f["gre"])[:, :, 255]
                gli = v4(f["gim"])[:, :, 255]
                c255, s255 = tb[:, 0, 255:256], tb[:, 1, 255:256]
                P.ts("dve", sm["s1"], gli, s255, ALU.mult)
                P.stt(NST[:, rt, :, 0], glr, c255, sm["s1"], ALU.mult, ALU.subtract)
                P.ts("dve", sm["s2"], gli, c255, ALU.mult)
                P.stt(NST[:, rt, :, 1], glr, s255, sm["s2"], ALU.mult, ALU.add)
            ysl = psC if d == 0 else psD
            jj = j // 2
            for reim, hs_ in ((0, hre), (1, him)):
                for half in range(2):
                    hs = slice(half * 512, half * 512 + 512)
                    P.mm(ysl[64 * jj:64 * jj + 64, hs], cw_[:, (d * 4 + j) * 2 + reim, :], hs_[:, hs],
                         start=(j % 2 == 0 and reim == 0), stop=(j % 2 == 1 and reim == 1))
            if d == 1 and j == 3:
                s3 = psD.r("p (b t) -> p b t", b=nseq)[:, :, ::-1]
                P.cp("act", f["t3"].r("p (b t) -> p b t", b=nseq), s3)
                P.tt("dve", f["t1"], psC, f["t3"], ALU.add)
                P.stt(f["t2"], uT[:, ct, :], DSK[:, ct:ct + 1], f["t1"], ALU.mult, ALU.add)
                P.act(ya[:, ct, :], f["t2"], AF.Gelu_apprx_tanh)
        if u == 0:
            for b_ in range(4):
                P.dma("sp", nst_o[b_, l].r("d (gp g2) p c -> (g2 p) (d gp) c", g2=2), NST[:, :, b_, :], g_st)
        dbg_store("ya", ya)
        dbg_store("nst", nst_o) if False else None
        if stop == "A1s":
            return None
        P.barrier()
        X.reset()
        sg = [X.alloc([T], F32, "sg%d" % i) for i in range(2)]
        yag = bufP
        ya_tiles = [ya[:, k, :] for k in range(8)]

        def cons_glu(ti, ps):
            s_ = sg[ti % 2]
            P.act(s_, ps, AF.Sigmoid)
            P.tt("dve", yag[:, ti, :], ya[:, ti, :], s_, ALU.mult)
        linear_fm(ya_tiles, W["s5_w_glu"][l], 0, 0, 1024, cons_glu)
        dbg_store("yag", yag)
        if stop == "A1":
            return None

        X.reset()
        fb = {k: X.alloc([T], F32, k) for k in ("cb", "cc", "prod", "acc")}
        keep = {}

        def seqv(t_):
            return t_.r("p (b t) -> p b t", b=nseq)

        def cons_sc(ti, ps):
            kind, i = divmod(ti, 4)
            if kind == 0:
                if i not in keep:
                    keep[i] = X.alloc([T], F32, "scb%d" % i)
                P.cp("act", keep[i], ps)
            elif kind == 1:
                if ("c", i) not in keep:
                    keep[("c", i)] = X.alloc([T], F32, "scc%d" % i)
                P.cp("act", keep[("c", i)], ps)
            else:
                P.tt("dve", fb["prod"], keep[("c", i)], ps, ALU.mult)
                P.ts("dve", fb["acc"], fb["prod"], SCW[:, i, 1:2], ALU.mult, SCB[:, i:i + 1], ALU.add)
                a3, p3 = seqv(fb["acc"]), seqv(fb["prod"])
                P.stt(a3[:, :, 1:SL], p3[:, :, 0:SL - 1], SCW[:, i, 0:1], a3[:, :, 1:SL], ALU.mult, ALU.add)
                P.stt(a3[:, :, 0:SL - 1], p3[:, :, 1:SL], SCW[:, i, 2:3], a3[:, :, 0:SL - 1], ALU.mult, ALU.add)
                P.tt("dve", ysT[:, i, :], keep[i], fb["acc"], ALU.mult)
        linear_fm(hT_tiles, W["w_in"][l], 0, 1024, 1536, cons_sc)
        dbg_store("ys", ysT)
        if stop == "A2":
            return None
        P.barrier()

        X.reset()
        ga = [X.alloc([T], F32, "ga%d" % i) for i in range(4)]
        cv = [X.alloc([T], F32, "cv%d" % i) for i in range(4)]
        fc = {k: X.alloc([T], F32, k) for k in ("sq", "rstd", "mr")}

        def cons_cf(ti, ps):
            kind, i = divmod(ti, 4)
            if kind == 0:
                P.cp("act", ga[i], ps)
            else:
                P.act(fc["sq"], ps, AF.Sigmoid)
                P.tt("dve", ga[i], ga[i], fc["sq"], ALU.mult)
                P.ts("dve", cv[i], ga[i], CFW[:, i, 15:16], ALU.mult, CFB[:, i:i + 1], ALU.add)
                a3, g3 = seqv(cv[i]), seqv(ga[i])
                for k in range(31):
                    sh = k - 15
                    if sh == 0:
                        continue
                    if sh > 0:
                        P.stt(a3[:, :, 0:SL - sh], g3[:, :, sh:SL], CFW[:, i, k:k + 1], a3[:, :, 0:SL - sh], ALU.mult, ALU.add)
                    else:
                        P.stt(a3[:, :, -sh:SL], g3[:, :, 0:SL + sh], CFW[:, i, k:k + 1], a3[:, :, -sh:SL], ALU.mult, ALU.add)
        linear_fm(hT_tiles, W["w_in"][l], 0, 2560, 1024, cons_cf)
        dbg_store("glu0", ga[0])
        dbg_store("cv0", cv[0])
        psM, psQ = PS[0], PS[1]
        for i in range(4):
            for half in range(2):
                hs = slice(half * 512, half * 512 + 512)
                P.mm(psM[:, hs], ones_f, cv[i][:, hs], start=(i == 0), stop=(i == 3))
        for i in range(4):
            P.act(ga[i], cv[i], AF.Square)
        for i in range(4):
            for half in range(2):
                hs = slice(half * 512, half * 512 + 512)
                P.mm(psQ[:, hs], ones_f, ga[i][:, hs], start=(i == 0), stop=(i == 3))
        P.cp("act", fc["mr"], psM)
        P.tt("dve", fc["sq"], fc["mr"], fc["mr"], ALU.mult)
        P.tt("dve", fc["rstd"], psQ, fc["sq"], ALU.subtract)
        P.ts("dve", fc["rstd"], fc["rstd"], LN_EPS, ALU.add)
        P.act(fc["rstd"], fc["rstd"], AF.Sqrt)
        P.recip(fc["rstd"], fc["rstd"])
        dbg_store("rstd", fc["rstd"])
        dbg_store("mean", psM) if False else None
        P.tt("dve", fc["mr"], fc["mr"], fc["rstd"], ALU.mult)
        for i in range(4):
            P.tt("dve", ga[i], cv[i], fc["rstd"], ALU.mult)
            P.tt("dve", ga[i], ga[i], fc["mr"], ALU.subtract)
            P.ts("dve", ga[i], ga[i], CFG[:, i:i + 1], ALU.mult, CFBB[:, i:i + 1], ALU.add)
            P.act(zT[:, i, :], ga[i], AF.Silu)
        dbg_store("z", zT)
        if stop == "A3":
            return None
        P.barrier()

        X.reset()
        mT = X.alloc([16, T], BF16, "mT")
        sgm = [X.alloc([T], F32, "sgm%d" % i) for i in range(2)]
        macc = [X.alloc([T], F32, "macc%d" % i) for i in range(2)]
        tmpm = X.alloc([T], F32, "tmpm")
        yag_t = [bufP[:, k, :] for k in range(8)]
        ys_t = [ysT[:, k, :] for k in range(4)]
        z_t = [zT[:, k, :] for k in range(4)]
        branches = [(yag_t, W["w_pa"][l], 0), (ys_t, W["w_pb"][l], 1), (z_t, W["w_pc"][l], 2)]
        for pair in range(8):
            cbase = pair * 256
            for bi, (tiles, Wp, gidx) in enumerate(branches):
                nk = len(tiles)
                wb = load_w(Wp, 0, nk, cbase, 256)
                pp = []
                for ti in range(2):
                    ps = ps_next()
                    for half in range(2):
                        hs = slice(half * 512, half * 512 + 512)
                        for kt in range(nk):
                            P.mm(ps[:, hs], wb[:, kt, ti * 128:(ti + 1) * 128], tiles[kt][:, hs], start=(kt == 0), stop=(kt == nk - 1))
                    pp.append(ps)
                wg = load_w(W["w_in"][l], 0, 16, GATE0 + gidx * D + cbase, 256)
                for ti in range(2):
                    ps = ps_next()
                    for half in range(2):
                        hs = slice(half * 512, half * 512 + 512)
                        for kt in range(16):
                            P.mm(ps[:, hs], wg[:, kt, ti * 128:(ti + 1) * 128], hT_tiles[kt][:, hs], start=(kt == 0), stop=(kt == 15))
                    P.act(sgm[ti], ps, AF.Sigmoid)
                    if bi == 0:
                        P.tt("dve", macc[ti], sgm[ti], pp[ti], ALU.mult)
                    elif bi == 1:
                        P.tt("dve", tmpm, sgm[ti], pp[ti], ALU.mult)
                        P.tt("dve", macc[ti], macc[ti], tmpm, ALU.add)
                    else:
                        P.tt("dve", tmpm, sgm[ti], pp[ti], ALU.mult)
                        P.tt("dve", mT[:, pair * 2 + ti, :], macc[ti], tmpm, ALU.add)
        dbg_store("mT", mT)
        if stop == "A4":
            return None
        ost = [X.alloc([256], F32, "ost%d" % i) for i in range(2)]
        it = 0
        for cc in range(8):
            wb = load_w(W["w_o"][l], 0, 16, cc * 256, 256)
            for tt_ in range(NTT):
                ps = ps_next()
                for kt in range(16):
                    P.mm(ps[:, 0:256], mT[:, kt, tt_ * 128:(tt_ + 1) * 128], wb[:, kt, :], start=(kt == 0), stop=(kt == 15))
                o_ = ost[it % 2]
                it += 1
                P.cp("act", o_, ps[:, 0:256])
                P.dma("sp", ymix[tt_ * 128:(tt_ + 1) * 128, cc * 256:(cc + 1) * 256], o_, g_st)
        P.barrier()
        return None

    def phase_B(l, u, last):
        UL.reset()
        h2T = UL.alloc([16, T], BF16, "h2T")
        Zbase = UL.cur
        qT = UL.alloc([16, T], BF16, "qT")
        Y3base = UL.cur
        wslots["wbf"] = [UL.alloc([16, 256], BF16, "wbfB%d" % i) for i in range(2)]
        Y = Stack(nc, UL.cur, SB_TOP)
        Y.reset()
        wslots["stg"] = None
        xt = [Y.alloc([D], F32, "xtB%d" % i) for i in range(2)]
        yt = [Y.alloc([D], F32, "ytB%d" % i) for i in range(2)]
        t1 = Y.alloc([D], F32, "t1B")
        hb = Y.alloc([D], BF16, "hbB")
        bct = {k: Y.alloc([D], F32, k) for k in ("gt1", "g1", "b1", "sc2", "sh2")}
        st = Y.alloc([4, 6], F32, "stB")
        mv = Y.alloc([2], F32, "mvB")
        rs = Y.alloc([1], F32, "rsB")

        def bload(dst, src2d, row, c0):
            P.dma("sp", dst, TT(src2d.ap[row:row + 1, c0:c0 + D].partition_broadcast(128), src2d.buf), g_c)
        bload(bct["gt1"], mod_d[l], u, 2 * D)
        bload(bct["sc2"], mod_d[l], u, 4 * D)
        bload(bct["sh2"], mod_d[l], u, 3 * D)
        bload(bct["g1"], W["ln1_g"], l, 0)
        bload(bct["b1"], W["ln1_b"], l, 0)
        for tt_ in range(NTT):
            rows = slice(tt_ * 128, tt_ * 128 + 128)
            x_, y_ = xt[tt_ % 2], yt[tt_ % 2]
            P.dma("sp", x_, xres[u][rows, :], g_x[tt_ % 2])
            P.dma("sp", y_, ymix[rows, :], g_x[tt_ % 2])
            P.tt("dve", y_, y_, bct["gt1"], ALU.mult)
            P.stt(y_, x_, ALPHA, y_, ALU.mult, ALU.add)
            ln_tm(y_, t1, st, mv, rs)
            P.tt("dve", t1, t1, bct["g1"], ALU.mult)
            P.tt("dve", x_, t1, bct["b1"], ALU.add)
            P.dma("sp", x1res[rows, :], x_, g_st)
            if tt_ == 0:
                dbg_store("x1", x_)
            ln_tm(x_, t1, st, mv, rs)
            P.tt("dve", t1, t1, bct["sc2"], ALU.mult)
            P.tt("dve", y_, t1, bct["sh2"], ALU.add)
            if tt_ == 0:
                dbg_store("h2", y_)
            P.cp("dve", hb, y_)
            ps = ps_next()
            psb = ps.bitcast(BF16)
            for kt in range(KT):
                P.tr(psb[:, kt * 128:(kt + 1) * 128], hb[:, kt * 128:(kt + 1) * 128], ident_b)
            P.cp("act", h2T[:, :, rows], psb.r("p (k t) -> p k t", k=KT))
        if stop == "B1":
            return
        P.barrier()
        Y.reset()
        wslots["stg"] = [Y.alloc([16, 256], F32, "wstgB%d" % i) for i in range(2)]
        h2T_t = [h2T[:, k, :] for k in range(KT)]

        def cons_q(ti, ps):
            P.cp("act", qT[:, ti, :], ps)
        linear_fm(h2T_t, W["peer_w_q"][l], 0, 0, D, cons_q)
        P.barrier()
        if stop == "B2":
            return
        Y = Stack(nc, Y3base, SB_TOP)
        S = Y.alloc([16, 128], F32, "S")
        S2a = [Y.alloc([128], F32, "S2a%d" % i) for i in range(16)]
        S2b = [Y.alloc([256], F32, "S2b%d" % i) for i in range(8)]
        V = Y.alloc([16, 16], F32, "V")
        Iu = Y.alloc([16, 16], U32, "Iu")
        If = Y.alloc([16, 16], F32, "If")
        cand = Y.alloc([8, 256], F32, "cand")
        top = Y.alloc([8, 16], F32, "top")
        pos = Y.alloc([8, 16], U32, "pos")
        pa_ = Y.alloc([8, 16], U32, "pa")
        pb_ = Y.alloc([8, 16], U32, "pb")
        af = Y.alloc([8, 16], F32, "af")
        bf = Y.alloc([8, 16], F32, "bf")
        eq = Y.alloc([8, 16, 16], F32, "eq")
        i1s = Y.alloc([8, 16], F32, "i1s")
        i2s = Y.alloc([8, 16], F32, "i2s")
        gw = Y.alloc([8, 16], F32, "gw")
        gsum = Y.alloc([8], F32, "gsum")
        i1T = Y.alloc([128], F32, "i1T")
        i2T = Y.alloc([128], F32, "i2T")
        gT = Y.alloc([128], F32, "gT")
        Pm2 = [Y.alloc([32, 128], BF16, "Pm%d" % i) for i in range(2)]
        Qm2 = [Y.alloc([32, 128], BF16, "Qm%d" % i) for i in range(2)]
        Vh = [TT(V.ap[:, i, :], Buf("Vh%d" % i)) for i in range(16)]
        Ih = [TT(Iu.ap[:, i, :], Buf("Ih%d" % i)) for i in range(16)]
        toph = [TT(top.ap[:, i, :], Buf("toph%d" % i)) for i in range(8)]
        posh = [TT(pos.ap[:, i, :], Buf("posh%d" % i)) for i in range(8)]
        Vb = [x.buf for x in Vh]
        Ib = [x.buf for x in Ih]
        tb_ = [x.buf for x in toph]
        pb2 = [x.buf for x in posh]
        Gst = Y.alloc([128, 128], BF16, "Gst")
        for tt_ in range(b3_tiles):
            rows = slice(tt_ * 128, tt_ * 128 + 128)
            for q4 in range(4):
                ps = ps_next()
                for m in range(4):
                    hh = q4 * 4 + m
                    P.mm(ps[:, m * 128:(m + 1) * 128], qT[:, hh, rows], KTb[:, hh % 2, :])
                P.cp("act", S[:, q4 * 4:q4 * 4 + 4, :], ps[:, 0:512].r("p (a n) -> p a n", a=4))
            for hh in range(16):
                P.op("dve", lambda E, hh=hh: E.max(out=Vh[hh].ap[:, 0:8], in_=S.ap[:, hh, :]), r=[S.buf], w=[Vb[hh]])
            for hh in range(16):
                P.op("dve", lambda E, hh=hh: E.max_index(out=Ih[hh].ap[:, 0:8], in_max=Vh[hh].ap[:, 0:8], in_values=S.ap[:, hh, :]),
                     r=[S.buf, Vb[hh]], w=[Ib[hh]])
            for hh in range(16):
                P.op("dve", lambda E, hh=hh: E.match_replace(out=S2a[hh].ap, in_to_replace=Vh[hh].ap[:, 0:8],
                                                             in_values=S.ap[:, hh, :], imm_value=NEG), r=[S.buf, Vb[hh]], w=[S2a[hh].buf])
            for hh in range(16):
                P.op("dve", lambda E, hh=hh: E.max(out=Vh[hh].ap[:, 8:16], in_=S2a[hh].ap), r=[S2a[hh].buf], w=[Vb[hh]])
            for hh in range(16):
                P.op("dve", lambda E, hh=hh: E.max_index(out=Ih[hh].ap[:, 8:16], in_max=Vh[hh].ap[:, 8:16], in_values=S2a[hh].ap),
                     r=[S2a[hh].buf, Vb[hh]], w=[Ib[hh]])
            P.op("dve", lambda E: E.tensor_copy(out=If.ap, in_=Iu.ap), r=Ib, w=[If.buf])
            V4 = V.r("p (h f) a -> p h f a", f=2)
            I4 = If.r("p (h f) a -> p h f a", f=2)
            c4 = cand.r("p h (a b) -> p h a b", a=16)
            P.op("dve", lambda E: E.tensor_tensor(out=c4.ap, in0=V4[:, :, 0, :].un(3).bc([128, 8, 16, 16]).ap,
                                                  in1=V4[:, :, 1, :].un(2).bc([128, 8, 16, 16]).ap, op=ALU.add), r=Vb, w=[cand.buf])
            for h in range(8):
                P.op("dve", lambda E, h=h: E.max(out=toph[h].ap[:, 0:8], in_=cand.ap[:, h, :]), r=[cand.buf], w=[tb_[h]])
            for h in range(8):
                P.op("dve", lambda E, h=h: E.max_index(out=posh[h].ap[:, 0:8], in_max=toph[h].ap[:, 0:8], in_values=cand.ap[:, h, :]),
                     r=[cand.buf, tb_[h]], w=[pb2[h]])
            for h in range(8):
                P.op("dve", lambda E, h=h: E.match_replace(out=S2b[h].ap, in_to_replace=toph[h].ap[:, 0:8], in_values=cand.ap[:, h, :],
                                                           imm_value=NEG), r=[cand.buf, tb_[h]], w=[S2b[h].buf])
            for h in range(8):
                P.op("dve", lambda E, h=h: E.max(out=toph[h].ap[:, 8:16], in_=S2b[h].ap), r=[S2b[h].buf], w=[tb_[h]])
            for h in range(8):
                P.op("dve", lambda E, h=h: E.max_index(out=posh[h].ap[:, 8:16], in_max=toph[h].ap[:, 8:16], in_values=S2b[h].ap),
                     r=[S2b[h].buf, tb_[h]], w=[pb2[h]])
            P.op("dve", lambda E: E.tensor_single_scalar(out=pa_.ap, in_=pos.ap, scalar=4, op=ALU.logical_shift_right),
                 r=pb2, w=[pa_.buf])
            P.op("dve", lambda E: E.tensor_single_scalar(out=pb_.ap, in_=pos.ap, scalar=15, op=ALU.bitwise_and),
                 r=pb2, w=[pb_.buf])
            P.cp("dve", af, pa_)
            P.cp("dve", bf, pb_)
            io4 = iota16.un(1).un(1).bc([128, 8, 16, 16])
            P.tt("dve", eq, af.un(3).bc([128, 8, 16, 16]), io4, ALU.is_equal)
            P.tt("dve", eq, eq, I4[:, :, 0, :].un(2).bc([128, 8, 16, 16]), ALU.mult)
            P.reduce(i1s, eq, ALU.add)
            P.tt("dve", eq, bf.un(3).bc([128, 8, 16, 16]), io4, ALU.is_equal)
            P.tt("dve", eq, eq, I4[:, :, 1, :].un(2).bc([128, 8, 16, 16]), ALU.mult)
            P.reduce(i2s, eq, ALU.add)
            P.op("dve", lambda E: E.tensor_tensor(out=gw.ap, in0=top.ap, in1=top[:, :, 0:1].bc([128, 8, 16]).ap, op=ALU.subtract),
                 r=tb_, w=[gw.buf])
            P.act(gw, gw, AF.Exp)
            P.reduce(gsum, gw, ALU.add)
            P.recip(gsum, gsum)
            P.tt("dve", gw, gw, gsum.un(2).bc([128, 8, 16]), ALU.mult)
            for src, dst in ((i1s, i1T), (i2s, i2T), (gw, gT)):
                ps = ps_next()
                P.tr(ps[:, 0:128], src.r("p h k -> p (h k)"), ident_f)
                P.cp("act", dst, ps[:, 0:128])
            io3 = iota128.un(1).bc([128, 32, 128])
            for qt in range(4):
                Pm, Qm = Pm2[qt % 2], Qm2[qt % 2]
                ts_ = slice(qt * 32, qt * 32 + 32)
                P.tt("dve", Pm, io3, i1T[:, ts_].un(2).bc([128, 32, 128]), ALU.is_equal)
                P.tt("dve", Qm, io3, i2T[:, ts_].un(2).bc([128, 32, 128]), ALU.is_equal)
                P.tt("dve", Qm, Qm, gT[:, ts_].un(2).bc([128, 32, 128]), ALU.mult)
                for t8 in range(4):
                    ps = ps_next()
                    for k in range(8):
                        t_ = t8 * 8 + k
                        P.mm(ps[:, k * 128:(k + 1) * 128], Pm[:, t_, :], Qm[:, t_, :])
                    t0_ = qt * 32 + t8 * 8
                    P.cp("act", Gst[:, :, t0_:t0_ + 8].r("p i t -> p t i"), ps.r("p (t i) -> p t i", t=8))
            P.dma("sp", Gd[:, :, rows], Gst)
        P.barrier()
        if stop == "B3a":
            return
        Z = Stack(nc, Zbase, SB_TOP)
        ACC = Z.alloc([NTT, D], F32, "ACC")
        Z2 = Stack(nc, Z.cur, SB_TOP)
        JG = 4
        ustg = [Z2.alloc([D], F32, "ustg%d" % i) for i in range(3)]
        vstg = [Z2.alloc([D], F32, "vstg%d" % i) for i in range(2)]
        ubf = [Z2.alloc([D], BF16, "ubf%d" % i) for i in range(2)]
        uTt = [Z2.alloc([16, 128], BF16, "uTt%d" % i) for i in range(3)]
        vbf = [Z2.alloc([D], BF16, "vbf%d" % b) for b in range(JG)]
        Wt = [Z2.alloc([T], BF16, "Wt%d" % b) for b in range(JG)]
        Gt = [Z2.alloc([T], BF16, "Gt%d" % i) for i in range(2)]
        ge = [Z2.alloc([T], BF16, "ge%d" % i) for i in range(2)]
        psT = PS[0]
        psA = [PS[1], PS[2]]
        psO = [TT(PS[3].ap[:, 0:512], Buf("psO0", "ps")), TT(PS[3].ap[:, 512:1024], Buf("psO1", "ps"))]
        urows = W["peer_u"][l].r("(i j) d -> j i d", j=128)
        vrows = W["peer_v"][l].r("(i j) d -> j i d", j=128)
        NJ = 128

        def load_cast(j):
            s_u = ustg[j % 3]
            P.dma("sp", s_u, urows[j])
            P.cp("dve", ubf[j % 2], s_u)

        def trans(j):
            psb = psT.bitcast(BF16).r("p (k i) -> p k i", k=16)
            ub = ubf[j % 2]
            for kt in range(16):
                P.tr(psb[:, kt, :], ub[:, kt * 128:(kt + 1) * 128], ident_b)

        def evac(j):
            psb = psT.bitcast(BF16).r("p (k i) -> p k i", k=16)
            P.cp("act", uTt[j % 3], psb)

        load_cast(0)
        trans(0)
        evac(0)
        load_cast(1)
        trans(1)
        evac(1)
        load_cast(2)
        P.dma("sp", Gt[0], Gd[:, 0, :])
        for j in range(NJ):
            jj = j % JG
            if j + 3 < NJ:
                load_cast(j + 3)
            s_v = vstg[j % 2]
            P.dma("sp", s_v, vrows[j])
            P.cp("act", vbf[jj], s_v)
            if j + 1 < NJ:
                P.dma("sp", Gt[(j + 1) % 2], Gd[:, j + 1, :])
            pa2 = psA[j % 2]
            ut = uTt[j % 3]
            for half in range(2):
                hs = slice(half * 512, half * 512 + 512)
                for kt in range(16):
                    P.mm(pa2[:, hs], ut[:, kt, :], h2T[:, kt, hs], start=(kt == 0), stop=(kt == 15))
            if j + 2 < NJ:
                trans(j + 2)
            e_ = ge[j % 2]
            P.act(e_, pa2, AF.Gelu_apprx_tanh)
            if j + 2 < NJ:
                evac(j + 2)
            P.tt("dve", Wt[jj], e_, Gt[j % 2], ALU.mult)
            if jj == JG - 1:
                first = (j == JG - 1)
                for tt_ in range(NTT):
                    for dc in range(4):
                        po = psO[(tt_ * 4 + dc) % 2]
                        for q_ in range(JG):
                            P.mm(po, Wt[q_][:, tt_ * 128:(tt_ + 1) * 128], vbf[q_][:, dc * 512:(dc + 1) * 512],
                                 start=(q_ == 0), stop=(q_ == JG - 1))
                        dst = ACC[:, tt_, dc * 512:(dc + 1) * 512]
                        if first:
                            P.cp("act", dst, po)
                        else:
                            P.tt("dve", dst, dst, po, ALU.add)
        P.barrier()
        Z2.reset()
        x1 = [Z2.alloc([D], F32, "x1_%d" % i) for i in range(2)]
        tq2 = Z2.alloc([D], F32, "tq2")
        bc3 = {k: Z2.alloc([D], F32, k) for k in ("gt2", "g2", "b2")}
        st = Z2.alloc([4, 6], F32, "stC")
        mv = Z2.alloc([2], F32, "mvC")
        rs = Z2.alloc([1], F32, "rsC")
        bload(bc3["gt2"], mod_d[l], u, 5 * D)
        bload(bc3["g2"], W["ln2_g"], l, 0)
        bload(bc3["b2"], W["ln2_b"], l, 0)
        for tt_ in range(NTT):
            rows = slice(tt_ * 128, tt_ * 128 + 128)
            x1_ = x1[tt_ % 2]
            acc = ACC[:, tt_, :]
            P.dma("sp", x1_, x1res[rows, :], g_x[0])
            if tt_ == 0:
                dbg_store("peer", acc)
            P.tt("dve", acc, acc, bc3["gt2"], ALU.mult)
            P.stt(acc, x1_, ALPHA, acc, ALU.mult, ALU.add)
            ln_tm(acc, tq2, st, mv, rs)
            P.tt("dve", tq2, tq2, bc3["g2"], ALU.mult)
            P.tt("dve", x1_, tq2, bc3["b2"], ALU.add)
            if last:
                P.dma("sp", yout[u][rows, :], x1_, g_st)
            else:
                P.dma("sp", xres[u][rows, :], x1_, g_st)
        P.barrier()

    phase_mod()
    ret = None
    for l in range(nlayers):
        phase_setup(l)
        if stop == "setup":
            break
        for u in units:
            ret = phase_A(l, u)
            if stop is not None and stop.startswith("A"):
                break
            phase_B(l, u, last=(l == nlayers - 1))
            if stop is not None:
                break
        if stop is not None:
            break
    if ret is not None and "ret" in dbg_out:
        src = ret
        P.dma("sp", dbg_out["ret"], src, g_st)
    P.final_wait()
    P.emit()
    return nc


def _grid_pos():
    rows = T // 64
    t = np.arange(rows * 64)
    r = (t // 64).astype(np.float32)
    col = (t % 64).astype(np.float32)
    nf = D // 4
    freq = (1.0 / (np.float32(10000.0) ** (np.arange(nf, dtype=np.float32) / np.float32(nf)))).astype(np.float32)
    ar = r[:, None] * freq
    ac = col[:, None] * freq
    return np.concatenate([np.sin(ar), np.cos(ar), np.sin(ac), np.cos(ac)], -1).astype(np.float32)


WEIGHT_NAMES = ["w_mod", "b_mod", "w_in", "s5_lam_re", "s5_lam_im", "s5_log_dt", "s5_b_re", "s5_b_im", "s5_c_re",
                "s5_c_im", "s5_d", "s5_w_glu", "sc_conv_w", "sc_conv_b", "cf_conv_w", "cf_conv_b", "cf_ln_g", "cf_ln_b",
                "w_pa", "w_pb", "w_pc", "w_o", "ln1_g", "ln1_b", "peer_w_q", "peer_k1", "peer_k2", "peer_u", "peer_v",
                "ln2_g", "ln2_b"]


def make_in_maps(inputs, cores=range(NCORE)):
    f = lambda a: np.ascontiguousarray(np.asarray(a, dtype=np.float32))
    xp = f(inputs["x_prompt"])
    xs = f(inputs["x_sample"])
    stt = f(inputs["state_ssm"])
    c = f(inputs["c"])
    cc = f(inputs["c_ctx"])
    pe = _grid_pos()
    ident = np.eye(128, dtype=np.float32)
    iota = np.tile(np.arange(128, dtype=np.float32)[None, :], (128, 1))
    wts = {k: f(inputs[k]) for k in WEIGHT_NAMES}
    maps = []
    for i in cores:
        m = dict(wts)
        m["xp"] = np.ascontiguousarray(xp[4 * i:4 * i + 4].reshape(T, D))
        m["xs"] = np.ascontiguousarray(xs[i].reshape(T, D))
        m["pe"] = pe
        m["st0"] = np.ascontiguousarray(stt[i])
        m["cond"] = np.ascontiguousarray(np.stack([cc, c[i]], 0))
        m["ident"] = ident
        m["iota128"] = iota
        maps.append(m)
    return maps


def kernel(**inputs):
    nc = build()
    maps = make_in_maps(inputs)
    maps = [{k: m[k] for k in build.declared} for m in maps]
    res = run_bass_kernel_spmd(nc, maps, core_ids=list(range(NCORE)))
    yp = np.concatenate([r["yp"].reshape(4, 256, D) for r in res.results], 0).astype(np.float32)
    ys = np.stack([r["ys"].reshape(T, D) for r in res.results], 0).astype(np.float32)
    nst = np.concatenate([r["nst"] for r in res.results], 0).astype(np.float32)
    return (yp, ys, nst)
```

```python
import math
from contextlib import ExitStack

import numpy as np
import concourse.bass as bass
import concourse.mybir as mybir
from concourse.bass_utils import run_bass_kernel_spmd

F32 = mybir.dt.float32
BF16 = mybir.dt.bfloat16
I32 = mybir.dt.int32
U32 = mybir.dt.uint32
ALU = mybir.AluOpType
AF = mybir.ActivationFunctionType
AX = mybir.AxisListType

D = 2048
NCORE = 8
T = 1024
NTT = 8
KT = 16
NL = 2
ALPHA = 4.0 ** 0.25
LN_EPS = 1e-6
IN_COLS = 9728
GATE0 = 3584
NEG = -1.0e30


class Buf:
    __slots__ = ("name", "lw", "rd", "space", "dsem")

    def __init__(self, name, space="sb"):
        self.name = name
        self.lw = {}
        self.rd = {}
        self.space = space
        self.dsem = None


class Grp:
    __slots__ = ("sem", "cnt")

    def __init__(self, sem):
        self.sem = sem
        self.cnt = 0


class TT:
    __slots__ = ("ap", "buf")

    def __init__(self, ap, buf):
        self.ap = ap
        self.buf = buf

    def __getitem__(self, k):
        return TT(self.ap[k], self.buf)

    def r(self, pat, **kw):
        return TT(self.ap.rearrange(pat, **kw), self.buf)

    def bc(self, shape):
        return TT(self.ap.to_broadcast(list(shape)), self.buf)

    def un(self, ax):
        return TT(self.ap.unsqueeze(ax), self.buf)

    def bitcast(self, dt):
        return TT(self.ap.bitcast(dt), self.buf)


def _a(x):
    return x.ap if isinstance(x, TT) else x


class Prog:
    def __init__(self, nc, es):
        self.nc = nc
        self.es = es
        self.E = dict(pe=nc.tensor, dve=nc.vector, act=nc.scalar, pool=nc.gpsimd, sp=nc.sync)
        self.rec = {k: [] for k in self.E}
        self.esem = {k: es.enter_context(nc.semaphore("es_" + k)) for k in self.E}
        self.ecnt = {k: 0 for k in self.E}
        self.known = {k: {} for k in self.E}
        self.grps = []
        self.free = []
        self.active = []
        self.max_dsem = 84

    def grp(self, name):
        g = Grp(self.es.enter_context(self.nc.semaphore(name)))
        self.grps.append(g)
        return g

    def dsem_for(self, buf):
        if buf.dsem is None:
            if self.free:
                buf.dsem = self.free.pop()
            else:
                assert len(self.grps) < self.max_dsem, "out of DMA semaphores"
                buf.dsem = self.grp("dq%d" % len(self.grps))
            self.active.append(buf)
        return buf.dsem

    def op(self, e, fn, r=(), w=(), grp=None):
        need = {}
        own = self.esem[e]

        def add(tok):
            sem, val = tok
            if e == "pe" and sem is own:
                return
            k = id(sem)
            if k not in need or need[k][1] < val:
                need[k] = (sem, val)

        for b in list(r) + list(w):
            for tok in b.lw.values():
                add(tok)
        for b in w:
            for tok in b.rd.values():
                add(tok)
        waits = []
        kn = self.known[e]
        for k, (sem, val) in need.items():
            if kn.get(k, 0) >= val:
                continue
            kn[k] = val
            waits.append((sem, val))
        if grp is not None:
            grp.cnt += 16
            tok = (grp.sem, grp.cnt)
            inc = (grp.sem, 16)
        else:
            self.ecnt[e] += 1
            tok = (own, self.ecnt[e])
            inc = (own, 1)
        for b in w:
            if b.space == "dram":
                b.lw[id(tok[0])] = tok
            else:
                b.lw = {id(tok[0]): tok}
            b.rd = {}
        for b in r:
            if b in w:
                continue
            b.rd[id(tok[0])] = tok
        self.rec[e].append((waits, fn, inc))

    def barrier(self):
        toks = [(self.esem[k], self.ecnt[k]) for k in self.E] + [(g.sem, g.cnt) for g in self.grps]
        for e in self.E:
            waits = []
            kn = self.known[e]
            for sem, val in toks:
                if val == 0 or sem is self.esem[e]:
                    continue
                if kn.get(id(sem), 0) >= val:
                    continue
                kn[id(sem)] = val
                waits.append((sem, val))
            if waits:
                self.rec[e].append((waits, None, None))
        for b in self.active:
            self.free.append(b.dsem)
            b.dsem = None
        self.active = []

    def final_wait(self):
        e = "sp"
        waits = [(self.esem[k], self.ecnt[k]) for k in self.E if k != e and self.ecnt[k]]
        waits += [(g.sem, g.cnt) for g in self.grps if g.cnt]
        self.rec[e].append((waits, None, None))

    def emit(self):
        with self.nc.Block() as blk:
            def mk(e):
                def body(Eng):
                    for waits, fn, inc in self.rec[e]:
                        for sem, val in waits:
                            Eng.wait_ge(sem, val)
                        if fn is not None:
                            fn(Eng).then_inc(inc[0], inc[1])
                return body
            blk.tensor(mk("pe"))
            blk.vector(mk("dve"))
            blk.scalar(mk("act"))
            blk.gpsimd(mk("pool"))
            blk.sync(mk("sp"))

    def dma(self, q, out, in_, grp=None, **kw):
        if out.buf.space == "sb":
            g = self.dsem_for(out.buf)
        elif in_.buf.space == "sb":
            g = self.dsem_for(in_.buf)
        else:
            g = self.dsem_for(out.buf)
        self.op(q, lambda E: E.dma_start(out=out.ap, in_=in_.ap, **kw), r=[in_.buf], w=[out.buf], grp=g)

    def mm(self, out, lhsT, rhs, start=True, stop=True):
        self.op("pe", lambda E: E.matmul(out=out.ap, lhsT=lhsT.ap, rhs=rhs.ap, start=start, stop=stop),
                r=[lhsT.buf, rhs.buf], w=[out.buf])

    def tr(self, out, in_, ident):
        self.op("pe", lambda E: E.transpose(out=out.ap, in_=in_.ap, identity=ident.ap),
                r=[in_.buf, ident.buf], w=[out.buf])

    def act(self, out, in_, func, bias=None, scale=None, e="act"):
        rb = [in_.buf]
        kw = {}
        if bias is not None:
            kw["bias"] = _a(bias)
            if isinstance(bias, TT):
                rb.append(bias.buf)
        if scale is not None:
            kw["scale"] = _a(scale)
            if isinstance(scale, TT):
                rb.append(scale.buf)
        self.op("act", lambda E: E.activation(out=out.ap, in_=in_.ap, func=func, **kw), r=rb, w=[out.buf])

    def cp(self, e, out, in_):
        if e == "act":
            self.op("act", lambda E: E.copy(out=out.ap, in_=in_.ap), r=[in_.buf], w=[out.buf])
        else:
            self.op(e, lambda E: E.tensor_copy(out=out.ap, in_=in_.ap), r=[in_.buf], w=[out.buf])

    def tt(self, e, out, a, b, op):
        self.op(e, lambda E: E.tensor_tensor(out=out.ap, in0=a.ap, in1=b.ap, op=op), r=[a.buf, b.buf], w=[out.buf])

    def ts(self, e, out, a, s1, op0, s2=None, op1=None):
        rb = [a.buf] + [x.buf for x in (s1, s2) if isinstance(x, TT)]
        if op1 is None:
            self.op(e, lambda E: E.tensor_scalar(out=out.ap, in0=a.ap, scalar1=_a(s1), scalar2=None, op0=op0),
                    r=rb, w=[out.buf])
        else:
            self.op(e, lambda E: E.tensor_scalar(out=out.ap, in0=a.ap, scalar1=_a(s1), scalar2=_a(s2), op0=op0, op1=op1),
                    r=rb, w=[out.buf])

    def stt(self, out, a, s, b, op0, op1, e="dve"):
        rb = [a.buf, b.buf] + ([s.buf] if isinstance(s, TT) else [])
        self.op(e, lambda E: E.scalar_tensor_tensor(out=out.ap, in0=a.ap, scalar=_a(s), in1=b.ap, op0=op0, op1=op1),
                r=rb, w=[out.buf])

    def memset(self, e, out, val):
        self.op(e, lambda E: E.memset(out.ap, val), w=[out.buf])

    def recip(self, out, in_):
        self.op("dve", lambda E: E.reciprocal(out=out.ap, in_=in_.ap), r=[in_.buf], w=[out.buf])

    def scan(self, out, d0, d1, init):
        rb = [d0.buf, d1.buf] + ([init.buf] if isinstance(init, TT) else [])
        self.op("dve", lambda E: E.tensor_tensor_scan(out=out.ap, data0=d0.ap, data1=d1.ap, initial=_a(init),
                                                       op0=ALU.mult, op1=ALU.add), r=rb, w=[out.buf])

    def reduce(self, out, in_, op, axis=AX.X):
        self.op("dve", lambda E: E.tensor_reduce(out=out.ap, in_=in_.ap, axis=axis, op=op), r=[in_.buf], w=[out.buf])


class Stack:
    cnt = [0]

    def __init__(self, nc, base, limit):
        self.nc = nc
        self.base = base
        self.cur = base
        self.limit = limit

    def alloc(self, free_shape, dtype, name):
        nb = int(np.prod(free_shape)) * (2 if dtype == BF16 else 4)
        off = (self.cur + 31) // 32 * 32
        assert off + nb <= self.limit, (name, off, nb, self.limit)
        self.cur = off + nb
        Stack.cnt[0] += 1
        h = self.nc.alloc_sbuf_tensor_at("%s_%d" % (name, Stack.cnt[0]), [128] + list(free_shape), dtype, offset=off)
        return TT(h.ap(), Buf(name))

    def sub(self, nbytes):
        off = (self.cur + 31) // 32 * 32
        assert off + nbytes <= self.limit, (off, nbytes, self.limit)
        self.cur = off + nbytes
        return Stack(self.nc, off, off + nbytes)

    def reset(self):
        self.cur = self.base


def build(dbg=None, stop=None, nlayers=NL, units=(0, 1), b3_tiles=NTT):
    dbg = dbg or {}
    Stack.cnt[0] = 0
    nc = bass.Bass("TRN2", target_bir_lowering=False)
    es = ExitStack()
    P = Prog(nc, es)

    declared = []
    build.declared = declared

    def din(name, shape, dt=F32):
        declared.append(name)
        return TT(nc.dram_tensor(name, list(shape), dt, kind="ExternalInput").ap(), Buf(name, "dram"))

    def dout(name, shape, dt=F32):
        return TT(nc.dram_tensor(name, list(shape), dt, kind="ExternalOutput").ap(), Buf(name, "dram"))

    def dscr(name, shape, dt=F32):
        return TT(nc.dram_tensor(name, list(shape), dt, kind="Internal").ap(), Buf(name, "dram"))

    xin = [din("xp", [T, D]), din("xs", [T, D])]
    pe_d = din("pe", [T, D])
    st0 = din("st0", [NL, 2, 64, 64, 2])
    cond = din("cond", [2, D])
    ident_d = din("ident", [128, 128])
    iota_d = din("iota128", [128, 128])
    WSH = dict([("w_mod", [NL, D, 6 * D]), ("b_mod", [NL, 6 * D]), ("w_in", [NL, D, IN_COLS]),
                ("s5_lam_re", [NL, 2, 64, 64]), ("s5_lam_im", [NL, 2, 64, 64]), ("s5_log_dt", [NL, 2, 64]),
                ("s5_b_re", [NL, 2, 64, 64, 16]), ("s5_b_im", [NL, 2, 64, 64, 16]),
                ("s5_c_re", [NL, 2, 64, 16, 64]), ("s5_c_im", [NL, 2, 64, 16, 64]),
                ("s5_d", [NL, 1024]), ("s5_w_glu", [NL, 1024, 1024]),
                ("sc_conv_w", [NL, 3, 512]), ("sc_conv_b", [NL, 512]),
                ("cf_conv_w", [NL, 31, 512]), ("cf_conv_b", [NL, 512]),
                ("cf_ln_g", [NL, 512]), ("cf_ln_b", [NL, 512]),
                ("w_pa", [NL, 1024, D]), ("w_pb", [NL, 512, D]), ("w_pc", [NL, 512, D]), ("w_o", [NL, D, D]),
                ("ln1_g", [NL, D]), ("ln1_b", [NL, D]),
                ("peer_w_q", [NL, D, D]), ("peer_k1", [NL, 128, 128]), ("peer_k2", [NL, 128, 128]),
                ("peer_u", [NL, 16384, D]), ("peer_v", [NL, 16384, D]),
                ("ln2_g", [NL, D]), ("ln2_b", [NL, D])])

    class LazyW(dict):
        def __missing__(self, nm):
            self[nm] = din(nm, WSH[nm])
            return self[nm]
    W = LazyW()
    yout = [dout("yp", [T, D]), dout("ys", [T, D])]
    nst_o = dout("nst", [4, NL, 2, 64, 64, 2])
    dbg_out = {k: dout("dbg_" + k, v[0], v[1]) for k, v in dbg.items()}

    xres = [dscr("xres0", [T, D]), dscr("xres1", [T, D])]
    ymix = dscr("ymix", [T, D])
    x1res = dscr("x1res", [T, D])
    h2res = dscr("h2res", [T, D])
    mod_d = dscr("mod_d", [NL, 2, 6 * D])
    tabs = dscr("tabs", [64, 128, 2, 260])
    wbd = dscr("wbd", [8, 128, 16, 128], BF16)
    cwd = dscr("cwd", [8, 128, 16, 64], BF16)
    Gd = dscr("Gd", [128, 128, T], BF16)

    psh = nc.alloc_psum_tensor("psum_all", [128, 4096], F32)
    PS = [TT(psh.ap()[:, i * 1024:(i + 1) * 1024], Buf("ps%d" % i, "ps")) for i in range(4)]
    ps_rr = [0]

    def ps_next():
        ps_rr[0] = (ps_rr[0] + 1) % 4
        return PS[ps_rr[0]]

    SB_BASE = (int(nc.sbuf_base) + 63) // 64 * 64
    SB_TOP = int(nc.sbuf_top)
    root = Stack(nc, SB_BASE, SB_TOP)
    G = root.sub(4 * 1024)
    LP = root.sub(4608)
    UL = Stack(nc, root.cur, SB_TOP)

    g_st = g_c = None
    g_x = g_w = g_t = [None, None]

    ident_f = G.alloc([128], F32, "ident_f")
    ident_b = G.alloc([128], BF16, "ident_b")
    iota128 = G.alloc([128], F32, "iota128")
    iota16 = iota128[:, 0:16]
    halfpi = G.alloc([1], F32, "halfpi")
    X4 = G.alloc([4, 128], BF16, "X4")
    CWst = G.alloc([4, 64], BF16, "CWst")
    ones_f = G.alloc([128], F32, "ones_f")
    P.dma("sp", ident_f, ident_d, g_c)
    P.dma("sp", iota128, iota_d, g_c)
    P.cp("dve", ident_b, ident_f)
    P.memset("dve", halfpi, math.pi / 2)
    P.memset("dve", X4, 0.0)
    P.memset("dve", CWst, 0.0)
    P.memset("dve", ones_f, 1.0 / 512.0)

    Rt = LP.alloc([64], F32, "Rt")
    DSK = LP.alloc([8], F32, "DSK")
    H0 = LP.alloc([64, 2], F32, "H0")
    SCW = LP.alloc([4, 3], F32, "SCW")
    SCB = LP.alloc([4], F32, "SCB")
    CFW = LP.alloc([4, 31], F32, "CFW")
    CFB = LP.alloc([4], F32, "CFB")
    CFG = LP.alloc([4], F32, "CFG")
    CFBB = LP.alloc([4], F32, "CFBB")
    KTb = LP.alloc([2, 128], BF16, "KTb")
    NST = LP.alloc([64, 4, 2], F32, "NST")

    WSTG_B = 16 * 256 * 4
    WBF_B = 16 * 256 * 2

    def dbg_store(key, src, dst_slice=None):
        if key in dbg_out:
            dst = dbg_out[key] if dst_slice is None else dbg_out[key][dst_slice]
            P.dma("sp", dst, src, g_st)

    def phase_mod():
        UL.reset()
        condT = UL.alloc([16, 2], F32, "condT")
        bmod = UL.alloc([6 * D], F32, "bmod")
        stg = [UL.alloc([16, 256], F32, "mstg%d" % i) for i in range(2)]
        mo = [UL.alloc([256], F32, "mo%d" % i) for i in range(2)]
        for r_ in range(2):
            P.dma("sp", condT[:, :, r_], cond[r_].r("(kt p) -> p kt", p=128), g_c, allow_slow_non_contiguous=True)
        P.act(condT, condT, AF.Silu)
        for l in range(nlayers):
            P.dma("sp", bmod[0:2, :], TT(W["b_mod"].ap[l:l + 1, :].partition_broadcast(2), W["b_mod"].buf), g_c)
            for c in range(48):
                s_ = stg[c % 2]
                P.dma("sp", s_, W["w_mod"][l][:, c * 256:(c + 1) * 256].r("(kt p) c -> p kt c", p=128), g_w[c % 2])
                ps = ps_next()
                for kt in range(16):
                    P.mm(ps[0:2, 0:256], condT[:, kt, :], s_[:, kt, :], start=(kt == 0), stop=(kt == 15))
                m_ = mo[c % 2]
                P.tt("dve", m_[0:2, :], ps[0:2, 0:256], bmod[0:2, c * 256:(c + 1) * 256], ALU.add)
                if (8 <= c < 16) or (32 <= c < 40):
                    P.ts("dve", m_[0:2, :], m_[0:2, :], 1.0, ALU.add)
                P.dma("sp", mod_d[l][:, c * 256:(c + 1) * 256], m_[0:2, :], g_st)
        P.barrier()

    def phase_setup(l):
        UL.reset()
        A = UL
        nat = [A.alloc([128], F32, "nat%d" % i) for i in range(3)]
        dt2 = A.alloc([2], F32, "dt2")
        LR = A.alloc([64], F32, "LR")
        LI = A.alloc([64], F32, "LI")
        DT = A.alloc([64], F32, "DT")
        P.dma("sp", nat[0][0:64, :], W["s5_lam_re"][l].r("d (gp g2) p -> (d gp) (g2 p)", g2=2), g_c)
        P.dma("sp", nat[1][0:64, :], W["s5_lam_im"][l].r("d (gp g2) p -> (d gp) (g2 p)", g2=2), g_c)
        P.dma("sp", dt2[0:64, :], W["s5_log_dt"][l].r("d (gp g2) -> (d gp) g2", g2=2), g_c)
        P.cp("dve", nat[2][0:64, :].r("q (a b) -> q a b", a=2), dt2[0:64, :].un(2).bc([64, 2, 64]))
        for i, dst in enumerate((LR, LI, DT)):
            ps = ps_next()
            P.tr(ps[:, 0:64], nat[i][0:64, :], ident_f[0:64, 0:64])
            P.cp("act", dst, ps[:, 0:64])
        v = {k: A.alloc([64], F32, k) for k in ("dt", "ang", "c", "s", "t1", "t2", "are", "aim", "den", "am1", "zre", "zim")}
        P.act(v["dt"], DT, AF.Exp)
        P.tt("dve", v["t1"], LR, v["dt"], ALU.mult)
        P.act(Rt, v["t1"], AF.Exp)
        P.tt("dve", v["ang"], LI, v["dt"], ALU.mult)
        P.act(v["s"], v["ang"], AF.Sin, scale=1.0 / 32.0)
        P.act(v["c"], v["ang"], AF.Sin, bias=halfpi[:, 0:1], scale=1.0 / 32.0)
        for _ in range(5):
            P.tt("dve", v["t1"], v["c"], v["c"], ALU.mult)
            P.tt("dve", v["t2"], v["s"], v["s"], ALU.mult)
            P.stt(v["s"], v["c"], 2.0, v["s"], ALU.mult, ALU.mult)
            P.tt("dve", v["c"], v["t1"], v["t2"], ALU.subtract)
        C1, S1 = v["c"], v["s"]
        P.tt("dve", v["are"], Rt, C1, ALU.mult)
        P.tt("dve", v["aim"], Rt, S1, ALU.mult)
        P.tt("dve", v["t1"], LR, LR, ALU.mult)
        P.tt("dve", v["t2"], LI, LI, ALU.mult)
        P.tt("dve", v["den"], v["t1"], v["t2"], ALU.add)
        P.recip(v["den"], v["den"])
        P.ts("dve", v["am1"], v["are"], -1.0, ALU.add)
        P.tt("dve", v["t1"], v["am1"], LR, ALU.mult)
        P.tt("dve", v["t2"], v["aim"], LI, ALU.mult)
        P.tt("dve", v["t1"], v["t1"], v["t2"], ALU.add)
        P.tt("dve", v["zre"], v["t1"], v["den"], ALU.mult)
        P.tt("dve", v["t1"], v["aim"], LR, ALU.mult)
        P.tt("dve", v["t2"], v["am1"], LI, ALU.mult)
        P.tt("dve", v["t1"], v["t1"], v["t2"], ALU.subtract)
        P.tt("dve", v["zim"], v["t1"], v["den"], ALU.mult)
        BRE = A.alloc([64, 16], F32, "BRE")
        BIM = A.alloc([64, 16], F32, "BIM")
        tA = A.alloc([64, 16], F32, "tA")
        tB = A.alloc([64, 16], F32, "tB")
        BB = [A.alloc([64, 16], BF16, "BBre"), A.alloc([64, 16], BF16, "BBim")]
        P.dma("sp", BRE, W["s5_b_re"][l].r("d (gp g2) p s -> (g2 p) (d gp) s", g2=2), g_c)
        P.dma("sp", BIM, W["s5_b_im"][l].r("d (gp g2) p s -> (g2 p) (d gp) s", g2=2), g_c)
        zr = v["zre"].un(2).bc([128, 64, 16])
        zi = v["zim"].un(2).bc([128, 64, 16])
        P.tt("dve", tA, BRE, zr, ALU.mult)
        P.tt("dve", tB, BIM, zi, ALU.mult)
        P.tt("dve", BB[0], tA, tB, ALU.subtract)
        P.tt("dve", tA, BIM, zr, ALU.mult)
        P.tt("dve", tB, BRE, zi, ALU.mult)
        P.tt("dve", BB[1], tA, tB, ALU.add)
        wst = [A.alloc([4, 128], BF16, "wst%d" % i) for i in range(2)]
        it = 0
        for d in range(2):
            for ct in range(8):
                rt0 = d * 32 + ct * 4
                for reim in range(2):
                    for j in range(4):
                        P.cp("dve", X4[0:64, j, 32 * j:32 * j + 16], BB[reim][0:64, rt0 + j, :])
                        P.cp("dve", X4[64:128, j, 32 * j + 16:32 * j + 32], BB[reim][64:128, rt0 + j, :])
                    ps = ps_next()
                    psb = ps[:, 0:256].bitcast(BF16).r("p (j c) -> p j c", j=4)
                    for j in range(4):
                        P.tr(psb[:, j, :], X4[:, j, :], ident_b)
                    ws_ = wst[it % 2]
                    it += 1
                    P.cp("act", ws_, psb)
                    P.dma("sp", wbd[ct][:, d * 8 + reim:d * 8 + 8:2, :], ws_, g_st)
        CN = [A.alloc([64], F32, "CN%d" % i) for i in range(2)]
        CN2 = A.alloc([128], BF16, "CN2")
        cst = [A.alloc([4, 64], BF16, "cst%d" % i) for i in range(2)]
        it = 0
        for d in range(2):
            for ct in range(8):
                for reim in range(2):
                    src = W["s5_c_im" if reim else "s5_c_re"][l, d, 8 * ct:8 * ct + 8].r("g s p -> (g s) p")
                    cn = CN[it % 2]
                    P.dma("sp", cn, src, g_t[it % 2])
                    sgn = -1.0 if reim else 1.0
                    P.ts("dve", CN2[:, 0:64], cn, sgn, ALU.mult)
                    P.ts("dve", CN2[:, 64:128], cn, sgn, ALU.mult)
                    ps = ps_next()
                    psb = ps[:, 0:64].bitcast(BF16)
                    P.tr(psb, CN2, ident_b)
                    for j in range(4):
                        jl = j % 2
                        P.cp("dve", CWst[0:64, j, 32 * jl:32 * jl + 16], psb[0:64, 32 * j:32 * j + 16])
                        P.cp("dve", CWst[64:128, j, 32 * jl + 16:32 * jl + 32], psb[64:128, 32 * j + 16:32 * j + 32])
                    cs_ = cst[it % 2]
                    it += 1
                    P.cp("dve", cs_, CWst)
                    P.dma("sp", cwd[ct][:, d * 8 + reim:d * 8 + 8:2, :], cs_, g_st)
        ER = A.alloc([16, 260], F32, "ER")
        EI = A.alloc([16, 260], F32, "EI")
        q = {k: A.alloc([16, 128], F32, k) for k in ("q1", "q2", "q3", "q4")}
        MR = A.alloc([16], F32, "MR")
        MI = A.alloc([16], F32, "MI")
        m1 = A.alloc([16], F32, "m1")
        m2 = A.alloc([16], F32, "m2")
        for gq in range(4):
            sl = slice(gq * 16, gq * 16 + 16)
            P.memset("dve", ER[:, :, 0:1], 1.0)
            P.memset("dve", EI[:, :, 0:1], 0.0)
            P.cp("dve", MR, C1[:, sl])
            P.cp("dve", MI, S1[:, sl])
            k = 1
            while k < 260:
                cnt = min(k, 260 - k)
                o = 0
                while o < cnt:
                    n = min(128, cnt - o)
                    mrb = MR.un(2).bc([128, 16, n])
                    mib = MI.un(2).bc([128, 16, n])
                    sr = ER[:, :, o:o + n]
                    si = EI[:, :, o:o + n]
                    P.tt("dve", q["q1"][:, :, 0:n], sr, mrb, ALU.mult)
                    P.tt("dve", q["q2"][:, :, 0:n], si, mib, ALU.mult)
                    P.tt("dve", q["q3"][:, :, 0:n], sr, mib, ALU.mult)
                    P.tt("dve", q["q4"][:, :, 0:n], si, mrb, ALU.mult)
                    P.tt("dve", ER[:, :, k + o:k + o + n], q["q1"][:, :, 0:n], q["q2"][:, :, 0:n], ALU.subtract)
                    P.tt("dve", EI[:, :, k + o:k + o + n], q["q3"][:, :, 0:n], q["q4"][:, :, 0:n], ALU.add)
                    o += n
                if 2 * k < 260:
                    P.tt("dve", m1, MR, MR, ALU.mult)
                    P.tt("dve", m2, MI, MI, ALU.mult)
                    P.stt(MI, MR, 2.0, MI, ALU.mult, ALU.mult)
                    P.tt("dve", MR, m1, m2, ALU.subtract)
                k *= 2
            P.dma("sp", tabs[gq * 16:gq * 16 + 16, :, 0, :].r("rt p t -> p rt t"), ER, g_st)
            P.dma("sp", tabs[gq * 16:gq * 16 + 16, :, 1, :].r("rt p t -> p rt t"), EI, g_st)
        P.dma("sp", DSK, W["s5_d"][l].r("(ct p) -> p ct", p=128), g_c, allow_slow_non_contiguous=True)
        P.dma("sp", H0, st0[l].r("d (gp g2) p c -> (g2 p) (d gp) c", g2=2), g_c)
        cwn = A.alloc([512], F32, "cwn")
        for nm_, dst_, nk_ in (("sc_conv_w", SCW, 3), ("cf_conv_w", CFW, 31)):
            P.dma("sp", cwn[0:nk_, :], W[nm_][l], g_c)
            for ct_ in range(4):
                ps = ps_next()
                P.tr(ps[:, 0:nk_], cwn[0:nk_, ct_ * 128:(ct_ + 1) * 128], ident_f[0:nk_, 0:nk_])
                P.cp("act", dst_[:, ct_, :], ps[:, 0:nk_])
        P.dma("sp", SCB, W["sc_conv_b"][l].r("(ct p) -> p ct", p=128), g_c, allow_slow_non_contiguous=True)
        P.dma("sp", CFB, W["cf_conv_b"][l].r("(ct p) -> p ct", p=128), g_c, allow_slow_non_contiguous=True)
        P.dma("sp", CFG, W["cf_ln_g"][l].r("(ct p) -> p ct", p=128), g_c, allow_slow_non_contiguous=True)
        P.dma("sp", CFBB, W["cf_ln_b"][l].r("(ct p) -> p ct", p=128), g_c, allow_slow_non_contiguous=True)
        dbg_store("CFW", CFW)
        dbg_store("SCW", SCW)
        kn = [A.alloc([128], F32, "kn%d" % i) for i in range(2)]
        for hf, nm in enumerate(("peer_k1", "peer_k2")):
            P.dma("sp", kn[hf], W[nm][l], g_c)
            ps = ps_next()
            P.tr(ps[:, 0:128], kn[hf], ident_f)
            P.cp("act", KTb[:, hf, :], ps[:, 0:128])
        P.barrier()

    wslots = {}
    w_it = [0]

    def load_w(W2d, r0, nk, c0, ncol):
        i = w_it[0] % 2
        w_it[0] += 1
        stg, wbf = wslots["stg"][i], wslots["wbf"][i]
        P.dma("sp", stg[:, 0:nk, 0:ncol], W2d[r0:r0 + 128 * nk, c0:c0 + ncol].r("(kt p) c -> p kt c", p=128), g_w[i])
        if i == 0:
            P.cp("dve", wbf[:, 0:nk, 0:ncol], stg[:, 0:nk, 0:ncol])
        else:
            P.cp("act", wbf[:, 0:nk, 0:ncol], stg[:, 0:nk, 0:ncol])
        return wbf

    def linear_fm(in_tiles, W2d, r0, c0, ncols, consume, tile0=0):
        nk = len(in_tiles)
        for cc in range(0, ncols, 256):
            wb = load_w(W2d, r0, nk, c0 + cc, 256)
            for ti in range(2):
                ps = ps_next()
                for half in range(2):
                    hs = slice(half * 512, half * 512 + 512)
                    for kt in range(nk):
                        P.mm(ps[:, hs], wb[:, kt, ti * 128:(ti + 1) * 128], in_tiles[kt][:, hs],
                             start=(kt == 0), stop=(kt == nk - 1))
                consume(tile0 + cc // 128 + ti, ps)

    def ln_tm(x, out, st, mv, rs, e="dve"):
        for c in range(4):
            P.op("dve", lambda E, c=c: E.bn_stats(out=st.ap[:, c, :], in_=x.ap[:, c * 512:(c + 1) * 512]),
                 r=[x.buf], w=[st.buf])
        P.op("dve", lambda E: E.bn_aggr(out=mv.ap, in_=st.ap.rearrange("p a b -> p (a b)")), r=[st.buf], w=[mv.buf])
        P.ts("dve", rs, mv[:, 1:2], LN_EPS, ALU.add)
        P.act(rs, rs, AF.Sqrt)
        P.recip(rs, rs)
        P.ts("dve", out, x, mv[:, 0:1], ALU.subtract, rs[:, 0:1], ALU.mult)

    def phase_A(l, u):
        nseq, SL = (4, 256) if u == 0 else (1, 1024)
        UL.reset()
        hT = UL.alloc([16, T], BF16, "hT")
        bufP = UL.alloc([8, T], BF16, "bufP")
        bufQ = UL.alloc([8, T], BF16, "bufQ")
        ysT = UL.alloc([4, T], BF16, "ysT")
        zT = UL.alloc([4, T], BF16, "zT")
        wslots["wbf"] = [UL.alloc([16, 256], BF16, "wbf%d" % i) for i in range(2)]
        wslots["stg"] = [UL.alloc([16, 256], F32, "wstg%d" % i) for i in range(2)]
        X = Stack(nc, UL.cur, SB_TOP)

        X.reset()
        xt = [X.alloc([D], F32, "xt%d" % i) for i in range(2)]
        tmp = X.alloc([D], F32, "tmpA0")
        hb = X.alloc([D], BF16, "hb")
        sc1b = X.alloc([D], F32, "sc1b")
        sh1b = X.alloc([D], F32, "sh1b")
        pet = X.alloc([D], F32, "pet")
        st = X.alloc([4, 6], F32, "st")
        mv = X.alloc([2], F32, "mv")
        rs = X.alloc([1], F32, "rs")
        P.dma("sp", sh1b, TT(mod_d.ap[l, u:u + 1, 0:D].partition_broadcast(128), mod_d.buf), g_c)
        P.dma("sp", sc1b, TT(mod_d.ap[l, u:u + 1, D:2 * D].partition_broadcast(128), mod_d.buf), g_c)
        for tt_ in range(NTT):
            rows = slice(tt_ * 128, tt_ * 128 + 128)
            x_ = xt[tt_ % 2]
            if l == 0:
                P.dma("sp", x_, xin[u][rows, :], g_x[tt_ % 2])
                if u == 1:
                    P.dma("sp", pet, pe_d[rows, :], g_x[tt_ % 2])
                    P.tt("dve", x_, x_, pet, ALU.add)
                P.dma("sp", xres[u][rows, :], x_, g_st)
            else:
                P.dma("sp", x_, xres[u][rows, :], g_x[tt_ % 2])
            ln_tm(x_, tmp, st, mv, rs)
            P.tt("dve", tmp, tmp, sc1b, ALU.mult)
            P.tt("dve", hb, tmp, sh1b, ALU.add)
            ps = ps_next()
            psb = ps.bitcast(BF16)
            for kt in range(KT):
                P.tr(psb[:, kt * 128:(kt + 1) * 128], hb[:, kt * 128:(kt + 1) * 128], ident_b)
            P.cp("act", hT[:, :, rows], psb.r("p (k t) -> p k t", k=KT))
        dbg_store("hT", hT)
        if stop == "A0":
            return None
        hT_tiles = [hT[:, kt, :] for kt in range(KT)]
        P.barrier()

        uT = bufP
        ya = bufQ

        def cons_u(ti, ps):
            P.cp("act", uT[:, ti, :], ps)
        linear_fm(hT_tiles, W["w_in"][l], 0, 0, 1024, cons_u)
        dbg_store("uT", uT)
        X.reset()
        f = {k: X.alloc([T], F32, k) for k in ("bure", "buim", "t1", "t2", "t3", "br", "bi", "gre", "gim")}
        fbh = {k: X.alloc([T], BF16, k) for k in ("greb", "gimb", "q1", "q2", "q3", "q4")}
        tbb = X.alloc([2, 256], BF16, "tbb")
        hre = X.alloc([T], BF16, "hre")
        him = X.alloc([T], BF16, "him")
        tbs = [X.alloc([2, 260], F32, "tb%d" % i) for i in range(2)]
        wbt = [X.alloc([16, 128], BF16, "wbt%d" % i) for i in range(2)]
        cwt = [X.alloc([16, 64], BF16, "cwt%d" % i) for i in range(2)]
        sm = {k: X.alloc([4], F32, k) for k in ("i_re", "i_im", "s1", "s2")}
        psA, psB, psC, psD = PS

        def v4(t_):
            return t_.r("p (c t) -> p c t", c=4)

        rts = []
        for ct in range(8):
            for d in range(2):
                for j in range(4):
                    rts.append((ct, d, j))

        state = {}

        def issue_bu(i):
            ct, d, j = rts[i]
            if d == 0 and j == 0:
                k = ct % 2
                P.dma("sp", wbt[k], wbd[ct], g_t[k])
                P.dma("sp", cwt[k], cwd[ct], g_t[k])
            rt = d * 32 + ct * 4 + j
            tb = tbs[i % 2]
            P.dma("sp", tb, tabs[rt], g_x[i % 2])
            wb_ = wbt[ct % 2]
            for reim, slot in ((0, psA), (1, psB)):
                for half in range(2):
                    hs = slice(half * 512, half * 512 + 512)
                    P.mm(slot[:, hs], wb_[:, (d * 4 + j) * 2 + reim, :], uT[:, ct, hs])

        issue_bu(0)
        for i, (ct, d, j) in enumerate(rts):
            rt = d * 32 + ct * 4 + j
            tb = tbs[i % 2]
            cw_ = cwt[ct % 2]
            Cb = tb[:, 0, 0:256].un(1).bc([128, 4, 256])
            Sb = tb[:, 1, 0:256].un(1).bc([128, 4, 256])
            for src, dst in ((psA, f["bure"]), (psB, f["buim"])):
                s3 = src.r("p (b t) -> p b t", b=nseq)
                if d == 1:
                    s3 = s3[:, :, ::-1]
                P.cp("act", dst.r("p (b t) -> p b t", b=nseq), s3)
            if i + 1 < len(rts):
                issue_bu(i + 1)
            P.tt("dve", v4(f["t1"]), v4(f["bure"]), Cb, ALU.mult)
            P.tt("dve", v4(f["t2"]), v4(f["buim"]), Sb, ALU.mult)
            P.tt("dve", f["br"], f["t1"], f["t2"], ALU.add)
            P.tt("dve", v4(f["t1"]), v4(f["buim"]), Cb, ALU.mult)
            P.tt("dve", v4(f["t2"]), v4(f["bure"]), Sb, ALU.mult)
            P.tt("dve", f["bi"], f["t1"], f["t2"], ALU.subtract)
            rcol = Rt[:, rt:rt + 1]
            for c in range(4):
                cs = slice(c * 256, c * 256 + 256)
                if u == 0:
                    ire, iim = 0.0, 0.0
                else:
                    if c == 0:
                        pre, pim = H0[:, rt, 0:1], H0[:, rt, 1:2]
                        er, ei = tb[:, 0, 1:2], tb[:, 1, 1:2]
                    else:
                        pre, pim = f["gre"][:, c * 256 - 1:c * 256], f["gim"][:, c * 256 - 1:c * 256]
                        er, ei = tb[:, 0, 256:257], tb[:, 1, 256:257]
                    P.ts("dve", sm["s1"][:, 0:1], pim, ei, ALU.mult)
                    P.stt(sm["i_re"][:, c:c + 1], pre, er, sm["s1"][:, 0:1], ALU.mult, ALU.subtract)
                    P.ts("dve", sm["s2"][:, 0:1], pim, er, ALU.mult)
                    P.stt(sm["i_im"][:, c:c + 1], pre, ei, sm["s2"][:, 0:1], ALU.mult, ALU.add)
                    ire, iim = sm["i_re"][:, c:c + 1], sm["i_im"][:, c:c + 1]
                P.scan(f["gre"][:, cs], rcol.bc([128, 256]), f["br"][:, cs], ire)
                P.scan(f["gim"][:, cs], rcol.bc([128, 256]), f["bi"][:, cs], iim)
            P.cp("act", tbb, tb[:, :, 0:256])
            P.cp("act", fbh["greb"], f["gre"])
            P.cp("act", fbh["gimb"], f["gim"])
            Cbb = tbb[:, 0, :].un(1).bc([128, 4, 256])
            Sbb = tbb[:, 1, :].un(1).bc([128, 4, 256])
            P.tt("dve", v4(fbh["q1"]), v4(fbh["greb"]), Cbb, ALU.mult)
            P.tt("dve", v4(fbh["q2"]), v4(fbh["gimb"]), Sbb, ALU.mult)
            P.tt("dve", hre, fbh["q1"], fbh["q2"], ALU.subtract)
            P.tt("dve", v4(fbh["q3"]), v4(fbh["greb"]), Sbb, ALU.mult)
            P.tt("dve", v4(fbh["q4"]), v4(fbh["gimb"]), Cbb, ALU.mult)
            P.tt("dve", him, fbh["q3"], fbh["q4"], ALU.add)
            if u == 0:
                glr = v4(f["gre"])[:, :, 255]
                gli = v4(f["gim"])[:, :, 255]
                c255, s255 = tb[:, 0, 255:256], tb[:, 1, 255:256]
                P.ts("dve", sm["s1"], gli, s255, ALU.mult)
                P.stt(NST[:, rt, :, 0], glr, c255, sm["s1"], ALU.mult, ALU.subtract)
                P.ts("dve", sm["s2"], gli, c255, ALU.mult)
                P.stt(NST[:, rt, :, 1], glr, s255, sm["s2"], ALU.mult, ALU.add)
            ysl = psC if d == 0 else psD
            jj = j // 2
            for reim, hs_ in ((0, hre), (1, him)):
                for half in range(2):
                    hs = slice(half * 512, half * 512 + 512)
                    P.mm(ysl[64 * jj:64 * jj + 64, hs], cw_[:, (d * 4 + j) * 2 + reim, :], hs_[:, hs],
                         start=(j % 2 == 0 and reim == 0), stop=(j % 2 == 1 and reim == 1))
            if d == 1 and j == 3:
                s3 = psD.r("p (b t) -> p b t", b=nseq)[:, :, ::-1]
                P.cp("act", f["t3"].r("p (b t) -> p b t", b=nseq), s3)
                P.tt("dve", f["t1"], psC, f["t3"], ALU.add)
                P.stt(f["t2"], uT[:, ct, :], DSK[:, ct:ct + 1], f["t1"], ALU.mult, ALU.add)
                P.act(ya[:, ct, :], f["t2"], AF.Gelu_apprx_tanh)
        if u == 0:
            for b_ in range(4):
                P.dma("sp", nst_o[b_, l].r("d (gp g2) p c -> (g2 p) (d gp) c", g2=2), NST[:, :, b_, :], g_st)
        dbg_store("ya", ya)
        dbg_store("nst", nst_o) if False else None
        if stop == "A1s":
            return None
        P.barrier()
        X.reset()
        sg = [X.alloc([T], F32, "sg%d" % i) for i in range(2)]
        yag = bufP
        ya_tiles = [ya[:, k, :] for k in range(8)]

        def cons_glu(ti, ps):
            s_ = sg[ti % 2]
            P.act(s_, ps, AF.Sigmoid)
            P.tt("dve", yag[:, ti, :], ya[:, ti, :], s_, ALU.mult)
        linear_fm(ya_tiles, W["s5_w_glu"][l], 0, 0, 1024, cons_glu)
        dbg_store("yag", yag)
        if stop == "A1":
            return None

        P.barrier()
        X.reset()
        fb = {k: X.alloc([T], F32, k) for k in ("cb", "cc", "prod", "acc")}
        keep = {}

        def seqv(t_):
            return t_.r("p (b t) -> p b t", b=nseq)

        def cons_sc(ti, ps):
            kind, i = divmod(ti, 4)
            if kind == 0:
                if i not in keep:
                    keep[i] = X.alloc([T], F32, "scb%d" % i)
                P.cp("act", keep[i], ps)
            elif kind == 1:
                if ("c", i) not in keep:
                    keep[("c", i)] = X.alloc([T], F32, "scc%d" % i)
                P.cp("act", keep[("c", i)], ps)
            else:
                P.tt("dve", fb["prod"], keep[("c", i)], ps, ALU.mult)
                P.ts("dve", fb["acc"], fb["prod"], SCW[:, i, 1:2], ALU.mult, SCB[:, i:i + 1], ALU.add)
                a3, p3 = seqv(fb["acc"]), seqv(fb["prod"])
                P.stt(a3[:, :, 1:SL], p3[:, :, 0:SL - 1], SCW[:, i, 0:1], a3[:, :, 1:SL], ALU.mult, ALU.add)
                P.stt(a3[:, :, 0:SL - 1], p3[:, :, 1:SL], SCW[:, i, 2:3], a3[:, :, 0:SL - 1], ALU.mult, ALU.add)
                P.tt("dve", ysT[:, i, :], keep[i], fb["acc"], ALU.mult)
        linear_fm(hT_tiles, W["w_in"][l], 0, 1024, 1536, cons_sc)
        dbg_store("ys", ysT)
        if stop == "A2":
            return None
        P.barrier()

        X.reset()
        ga = [X.alloc([T], F32, "ga%d" % i) for i in range(4)]
        cv = [X.alloc([T], F32, "cv%d" % i) for i in range(4)]
        fc = {k: X.alloc([T], F32, k) for k in ("sq", "rstd", "mr")}

        def cons_cf(ti, ps):
            kind, i = divmod(ti, 4)
            if kind == 0:
                P.cp("act", ga[i], ps)
            else:
                P.act(fc["sq"], ps, AF.Sigmoid)
                P.tt("dve", ga[i], ga[i], fc["sq"], ALU.mult)
                P.ts("dve", cv[i], ga[i], CFW[:, i, 15:16], ALU.mult, CFB[:, i:i + 1], ALU.add)
                a3, g3 = seqv(cv[i]), seqv(ga[i])
                for k in range(31):
                    sh = k - 15
                    if sh == 0:
                        continue
                    if sh > 0:
                        P.stt(a3[:, :, 0:SL - sh], g3[:, :, sh:SL], CFW[:, i, k:k + 1], a3[:, :, 0:SL - sh], ALU.mult, ALU.add)
                    else:
                        P.stt(a3[:, :, -sh:SL], g3[:, :, 0:SL + sh], CFW[:, i, k:k + 1], a3[:, :, -sh:SL], ALU.mult, ALU.add)
        linear_fm(hT_tiles, W["w_in"][l], 0, 2560, 1024, cons_cf)
        dbg_store("glu0", ga[0])
        dbg_store("cv0", cv[0])
        psM, psQ = PS[0], PS[1]
        for i in range(4):
            for half in range(2):
                hs = slice(half * 512, half * 512 + 512)
                P.mm(psM[:, hs], ones_f, cv[i][:, hs], start=(i == 0), stop=(i == 3))
        for i in range(4):
            P.act(ga[i], cv[i], AF.Square)
        for i in range(4):
            for half in range(2):
                hs = slice(half * 512, half * 512 + 512)
                P.mm(psQ[:, hs], ones_f, ga[i][:, hs], start=(i == 0), stop=(i == 3))
        P.cp("act", fc["mr"], psM)
        P.tt("dve", fc["sq"], fc["mr"], fc["mr"], ALU.mult)
        P.tt("dve", fc["rstd"], psQ, fc["sq"], ALU.subtract)
        P.ts("dve", fc["rstd"], fc["rstd"], LN_EPS, ALU.add)
        P.act(fc["rstd"], fc["rstd"], AF.Sqrt)
        P.recip(fc["rstd"], fc["rstd"])
        dbg_store("rstd", fc["rstd"])
        dbg_store("mean", psM) if False else None
        P.tt("dve", fc["mr"], fc["mr"], fc["rstd"], ALU.mult)
        for i in range(4):
            P.tt("dve", ga[i], cv[i], fc["rstd"], ALU.mult)
            P.tt("dve", ga[i], ga[i], fc["mr"], ALU.subtract)
            P.ts("dve", ga[i], ga[i], CFG[:, i:i + 1], ALU.mult, CFBB[:, i:i + 1], ALU.add)
            P.act(zT[:, i, :], ga[i], AF.Silu)
        dbg_store("z", zT)
        if stop == "A3":
            return None
        P.barrier()

        X.reset()
        mT = X.alloc([16, T], BF16, "mT")
        sgm = [X.alloc([T], F32, "sgm%d" % i) for i in range(2)]
        macc = [X.alloc([T], F32, "macc%d" % i) for i in range(2)]
        tmpm = X.alloc([T], F32, "tmpm")
        yag_t = [bufP[:, k, :] for k in range(8)]
        ys_t = [ysT[:, k, :] for k in range(4)]
        z_t = [zT[:, k, :] for k in range(4)]
        branches = [(yag_t, W["w_pa"][l], 0), (ys_t, W["w_pb"][l], 1), (z_t, W["w_pc"][l], 2)]
        for pair in range(8):
            cbase = pair * 256
            for bi, (tiles, Wp, gidx) in enumerate(branches):
                nk = len(tiles)
                wb = load_w(Wp, 0, nk, cbase, 256)
                pp = []
                for ti in range(2):
                    ps = ps_next()
                    for half in range(2):
                        hs = slice(half * 512, half * 512 + 512)
                        for kt in range(nk):
                            P.mm(ps[:, hs], wb[:, kt, ti * 128:(ti + 1) * 128], tiles[kt][:, hs], start=(kt == 0), stop=(kt == nk - 1))
                    pp.append(ps)
                wg = load_w(W["w_in"][l], 0, 16, GATE0 + gidx * D + cbase, 256)
                for ti in range(2):
                    ps = ps_next()
                    for half in range(2):
                        hs = slice(half * 512, half * 512 + 512)
                        for kt in range(16):
                            P.mm(ps[:, hs], wg[:, kt, ti * 128:(ti + 1) * 128], hT_tiles[kt][:, hs], start=(kt == 0), stop=(kt == 15))
                    P.act(sgm[ti], ps, AF.Sigmoid)
                    if bi == 0:
                        P.tt("dve", macc[ti], sgm[ti], pp[ti], ALU.mult)
                    elif bi == 1:
                        P.tt("dve", tmpm, sgm[ti], pp[ti], ALU.mult)
                        P.tt("dve", macc[ti], macc[ti], tmpm, ALU.add)
                    else:
                        P.tt("dve", tmpm, sgm[ti], pp[ti], ALU.mult)
                        P.tt("dve", mT[:, pair * 2 + ti, :], macc[ti], tmpm, ALU.add)
        dbg_store("mT", mT)
        if stop == "A4":
            return None
        ost = [X.alloc([256], F32, "ost%d" % i) for i in range(2)]
        it = 0
        for cc in range(8):
            wb = load_w(W["w_o"][l], 0, 16, cc * 256, 256)
            for tt_ in range(NTT):
                ps = ps_next()
                for kt in range(16):
                    P.mm(ps[:, 0:256], mT[:, kt, tt_ * 128:(tt_ + 1) * 128], wb[:, kt, :], start=(kt == 0), stop=(kt == 15))
                o_ = ost[it % 2]
                it += 1
                P.cp("act", o_, ps[:, 0:256])
                P.dma("sp", ymix[tt_ * 128:(tt_ + 1) * 128, cc * 256:(cc + 1) * 256], o_, g_st)
        P.barrier()
        return None

    def phase_B(l, u, last):
        UL.reset()
        h2T = UL.alloc([16, T], BF16, "h2T")
        Zbase = UL.cur
        qT = UL.alloc([16, T], BF16, "qT")
        Y3base = UL.cur
        wslots["wbf"] = [UL.alloc([16, 256], BF16, "wbfB%d" % i) for i in range(2)]
        Y = Stack(nc, UL.cur, SB_TOP)
        Y.reset()
        wslots["stg"] = None
        xt = [Y.alloc([D], F32, "xtB%d" % i) for i in range(2)]
        yt = [Y.alloc([D], F32, "ytB%d" % i) for i in range(2)]
        t1 = Y.alloc([D], F32, "t1B")
        hb = Y.alloc([D], BF16, "hbB")
        bct = {k: Y.alloc([D], F32, k) for k in ("gt1", "g1", "b1", "sc2", "sh2")}
        st = Y.alloc([4, 6], F32, "stB")
        mv = Y.alloc([2], F32, "mvB")
        rs = Y.alloc([1], F32, "rsB")

        def bload(dst, src2d, row, c0):
            P.dma("sp", dst, TT(src2d.ap[row:row + 1, c0:c0 + D].partition_broadcast(128), src2d.buf), g_c)
        bload(bct["gt1"], mod_d[l], u, 2 * D)
        bload(bct["sc2"], mod_d[l], u, 4 * D)
        bload(bct["sh2"], mod_d[l], u, 3 * D)
        bload(bct["g1"], W["ln1_g"], l, 0)
        bload(bct["b1"], W["ln1_b"], l, 0)
        for tt_ in range(NTT):
            rows = slice(tt_ * 128, tt_ * 128 + 128)
            x_, y_ = xt[tt_ % 2], yt[tt_ % 2]
            P.dma("sp", x_, xres[u][rows, :], g_x[tt_ % 2])
            P.dma("sp", y_, ymix[rows, :], g_x[tt_ % 2])
            P.tt("dve", y_, y_, bct["gt1"], ALU.mult)
            P.stt(y_, x_, ALPHA, y_, ALU.mult, ALU.add)
            ln_tm(y_, t1, st, mv, rs)
            P.tt("dve", t1, t1, bct["g1"], ALU.mult)
            P.tt("dve", x_, t1, bct["b1"], ALU.add)
            P.dma("sp", x1res[rows, :], x_, g_st)
            if tt_ == 0:
                dbg_store("x1", x_)
            ln_tm(x_, t1, st, mv, rs)
            P.tt("dve", t1, t1, bct["sc2"], ALU.mult)
            P.tt("dve", y_, t1, bct["sh2"], ALU.add)
            if tt_ == 0:
                dbg_store("h2", y_)
            P.cp("dve", hb, y_)
            ps = ps_next()
            psb = ps.bitcast(BF16)
            for kt in range(KT):
                P.tr(psb[:, kt * 128:(kt + 1) * 128], hb[:, kt * 128:(kt + 1) * 128], ident_b)
            P.cp("act", h2T[:, :, rows], psb.r("p (k t) -> p k t", k=KT))
        if stop == "B1":
            return
        P.barrier()
        Y.reset()
        wslots["stg"] = [Y.alloc([16, 256], F32, "wstgB%d" % i) for i in range(2)]
        h2T_t = [h2T[:, k, :] for k in range(KT)]

        def cons_q(ti, ps):
            P.cp("act", qT[:, ti, :], ps)
        linear_fm(h2T_t, W["peer_w_q"][l], 0, 0, D, cons_q)
        P.barrier()
        if stop == "B2":
            return
        Y = Stack(nc, Y3base, SB_TOP)
        S = Y.alloc([16, 128], F32, "S")
        S2a = [Y.alloc([128], F32, "S2a%d" % i) for i in range(16)]
        S2b = [Y.alloc([256], F32, "S2b%d" % i) for i in range(8)]
        V = Y.alloc([16, 16], F32, "V")
        Iu = Y.alloc([16, 16], U32, "Iu")
        If = Y.alloc([16, 16], F32, "If")
        cand = Y.alloc([8, 256], F32, "cand")
        top = Y.alloc([8, 16], F32, "top")
        pos = Y.alloc([8, 16], U32, "pos")
        pa_ = Y.alloc([8, 16], U32, "pa")
        pb_ = Y.alloc([8, 16], U32, "pb")
        af = Y.alloc([8, 16], F32, "af")
        bf = Y.alloc([8, 16], F32, "bf")
        eq = Y.alloc([8, 16, 16], F32, "eq")
        i1s = Y.alloc([8, 16], F32, "i1s")
        i2s = Y.alloc([8, 16], F32, "i2s")
        gw = Y.alloc([8, 16], F32, "gw")
        gsum = Y.alloc([8], F32, "gsum")
        i1T = Y.alloc([128], F32, "i1T")
        i2T = Y.alloc([128], F32, "i2T")
        gT = Y.alloc([128], F32, "gT")
        Pm2 = [Y.alloc([32, 128], BF16, "Pm%d" % i) for i in range(2)]
        Qm2 = [Y.alloc([32, 128], BF16, "Qm%d" % i) for i in range(2)]
        Vh = [TT(V.ap[:, i, :], Buf("Vh%d" % i)) for i in range(16)]
        Ih = [TT(Iu.ap[:, i, :], Buf("Ih%d" % i)) for i in range(16)]
        toph = [TT(top.ap[:, i, :], Buf("toph%d" % i)) for i in range(8)]
        posh = [TT(pos.ap[:, i, :], Buf("posh%d" % i)) for i in range(8)]
        Vb = [x.buf for x in Vh]
        Ib = [x.buf for x in Ih]
        tb_ = [x.buf for x in toph]
        pb2 = [x.buf for x in posh]
        Gst = Y.alloc([128, 128], BF16, "Gst")
        for tt_ in range(b3_tiles):
            rows = slice(tt_ * 128, tt_ * 128 + 128)
            for q4 in range(4):
                ps = ps_next()
                for m in range(4):
                    hh = q4 * 4 + m
                    P.mm(ps[:, m * 128:(m + 1) * 128], qT[:, hh, rows], KTb[:, hh % 2, :])
                P.cp("act", S[:, q4 * 4:q4 * 4 + 4, :], ps[:, 0:512].r("p (a n) -> p a n", a=4))
            for hh in range(16):
                P.op("dve", lambda E, hh=hh: E.max(out=Vh[hh].ap[:, 0:8], in_=S.ap[:, hh, :]), r=[S.buf], w=[Vb[hh]])
            for hh in range(16):
                P.op("dve", lambda E, hh=hh: E.max_index(out=Ih[hh].ap[:, 0:8], in_max=Vh[hh].ap[:, 0:8], in_values=S.ap[:, hh, :]),
                     r=[S.buf, Vb[hh]], w=[Ib[hh]])
            for hh in range(16):
                P.op("dve", lambda E, hh=hh: E.match_replace(out=S2a[hh].ap, in_to_replace=Vh[hh].ap[:, 0:8],
                                                             in_values=S.ap[:, hh, :], imm_value=NEG), r=[S.buf, Vb[hh]], w=[S2a[hh].buf])
            for hh in range(16):
                P.op("dve", lambda E, hh=hh: E.max(out=Vh[hh].ap[:, 8:16], in_=S2a[hh].ap), r=[S2a[hh].buf], w=[Vb[hh]])
            for hh in range(16):
                P.op("dve", lambda E, hh=hh: E.max_index(out=Ih[hh].ap[:, 8:16], in_max=Vh[hh].ap[:, 8:16], in_values=S2a[hh].ap),
                     r=[S2a[hh].buf, Vb[hh]], w=[Ib[hh]])
            P.op("dve", lambda E: E.tensor_copy(out=If.ap, in_=Iu.ap), r=Ib, w=[If.buf])
            V4 = V.r("p (h f) a -> p h f a", f=2)
            I4 = If.r("p (h f) a -> p h f a", f=2)
            c4 = cand.r("p h (a b) -> p h a b", a=16)
            P.op("dve", lambda E: E.tensor_tensor(out=c4.ap, in0=V4[:, :, 0, :].un(3).bc([128, 8, 16, 16]).ap,
                                                  in1=V4[:, :, 1, :].un(2).bc([128, 8, 16, 16]).ap, op=ALU.add), r=Vb, w=[cand.buf])
            for h in range(8):
                P.op("dve", lambda E, h=h: E.max(out=toph[h].ap[:, 0:8], in_=cand.ap[:, h, :]), r=[cand.buf], w=[tb_[h]])
            for h in range(8):
                P.op("dve", lambda E, h=h: E.max_index(out=posh[h].ap[:, 0:8], in_max=toph[h].ap[:, 0:8], in_values=cand.ap[:, h, :]),
                     r=[cand.buf, tb_[h]], w=[pb2[h]])
            for h in range(8):
                P.op("dve", lambda E, h=h: E.match_replace(out=S2b[h].ap, in_to_replace=toph[h].ap[:, 0:8], in_values=cand.ap[:, h, :],
                                                           imm_value=NEG), r=[cand.buf, tb_[h]], w=[S2b[h].buf])
            for h in range(8):
                P.op("dve", lambda E, h=h: E.max(out=toph[h].ap[:, 8:16], in_=S2b[h].ap), r=[S2b[h].buf], w=[tb_[h]])
            for h in range(8):
                P.op("dve", lambda E, h=h: E.max_index(out=posh[h].ap[:, 8:16], in_max=toph[h].ap[:, 8:16], in_values=S2b[h].ap),
                     r=[S2b[h].buf, tb_[h]], w=[pb2[h]])
            P.op("dve", lambda E: E.tensor_single_scalar(out=pa_.ap, in_=pos.ap, scalar=4, op=ALU.logical_shift_right),
                 r=pb2, w=[pa_.buf])
            P.op("dve", lambda E: E.tensor_single_scalar(out=pb_.ap, in_=pos.ap, scalar=15, op=ALU.bitwise_and),
                 r=pb2, w=[pb_.buf])
            P.cp("dve", af, pa_)
            P.cp("dve", bf, pb_)
            io4 = iota16.un(1).un(1).bc([128, 8, 16, 16])
            P.tt("dve", eq, af.un(3).bc([128, 8, 16, 16]), io4, ALU.is_equal)
            P.tt("dve", eq, eq, I4[:, :, 0, :].un(2).bc([128, 8, 16, 16]), ALU.mult)
            P.reduce(i1s, eq, ALU.add)
            P.tt("dve", eq, bf.un(3).bc([128, 8, 16, 16]), io4, ALU.is_equal)
            P.tt("dve", eq, eq, I4[:, :, 1, :].un(2).bc([128, 8, 16, 16]), ALU.mult)
            P.reduce(i2s, eq, ALU.add)
            P.op("dve", lambda E: E.tensor_tensor(out=gw.ap, in0=top.ap, in1=top[:, :, 0:1].bc([128, 8, 16]).ap, op=ALU.subtract),
                 r=tb_, w=[gw.buf])
            P.act(gw, gw, AF.Exp)
            P.reduce(gsum, gw, ALU.add)
            P.recip(gsum, gsum)
            P.tt("dve", gw, gw, gsum.un(2).bc([128, 8, 16]), ALU.mult)
            for src, dst in ((i1s, i1T), (i2s, i2T), (gw, gT)):
                ps = ps_next()
                P.tr(ps[:, 0:128], src.r("p h k -> p (h k)"), ident_f)
                P.cp("act", dst, ps[:, 0:128])
            io3 = iota128.un(1).bc([128, 32, 128])
            for qt in range(4):
                Pm, Qm = Pm2[qt % 2], Qm2[qt % 2]
                ts_ = slice(qt * 32, qt * 32 + 32)
                P.tt("dve", Pm, io3, i1T[:, ts_].un(2).bc([128, 32, 128]), ALU.is_equal)
                P.tt("dve", Qm, io3, i2T[:, ts_].un(2).bc([128, 32, 128]), ALU.is_equal)
                P.tt("dve", Qm, Qm, gT[:, ts_].un(2).bc([128, 32, 128]), ALU.mult)
                for t8 in range(4):
                    ps = ps_next()
                    for k in range(8):
                        t_ = t8 * 8 + k
                        P.mm(ps[:, k * 128:(k + 1) * 128], Pm[:, t_, :], Qm[:, t_, :])
                    t0_ = qt * 32 + t8 * 8
                    P.cp("act", Gst[:, :, t0_:t0_ + 8].r("p i t -> p t i"), ps.r("p (t i) -> p t i", t=8))
            P.dma("sp", Gd[:, :, rows], Gst)
        P.barrier()
        if stop == "B3a":
            return
        Z = Stack(nc, Zbase, SB_TOP)
        ACC = Z.alloc([NTT, D], F32, "ACC")
        Z2 = Stack(nc, Z.cur, SB_TOP)
        JG = 4
        ustg = [Z2.alloc([D], F32, "ustg%d" % i) for i in range(3)]
        vstg = [Z2.alloc([D], F32, "vstg%d" % i) for i in range(2)]
        ubf = [Z2.alloc([D], BF16, "ubf%d" % i) for i in range(2)]
        uTt = [Z2.alloc([16, 128], BF16, "uTt%d" % i) for i in range(3)]
        vbf = [Z2.alloc([D], BF16, "vbf%d" % b) for b in range(JG)]
        Wt = [Z2.alloc([T], BF16, "Wt%d" % b) for b in range(JG)]
        Gt = [Z2.alloc([T], BF16, "Gt%d" % i) for i in range(2)]
        ge = [Z2.alloc([T], BF16, "ge%d" % i) for i in range(2)]
        psT = PS[0]
        psA = [PS[1], PS[2]]
        psO = [TT(PS[3].ap[:, 0:512], Buf("psO0", "ps")), TT(PS[3].ap[:, 512:1024], Buf("psO1", "ps"))]
        urows = W["peer_u"][l].r("(i j) d -> j i d", j=128)
        vrows = W["peer_v"][l].r("(i j) d -> j i d", j=128)
        NJ = 128

        def load_cast(j):
            s_u = ustg[j % 3]
            P.dma("sp", s_u, urows[j])
            P.cp("dve", ubf[j % 2], s_u)

        def trans(j):
            psb = psT.bitcast(BF16).r("p (k i) -> p k i", k=16)
            ub = ubf[j % 2]
            for kt in range(16):
                P.tr(psb[:, kt, :], ub[:, kt * 128:(kt + 1) * 128], ident_b)

        def evac(j):
            psb = psT.bitcast(BF16).r("p (k i) -> p k i", k=16)
            P.cp("act", uTt[j % 3], psb)

        load_cast(0)
        trans(0)
        evac(0)
        load_cast(1)
        trans(1)
        evac(1)
        load_cast(2)
        P.dma("sp", Gt[0], Gd[:, 0, :])
        for j in range(NJ):
            jj = j % JG
            if j + 3 < NJ:
                load_cast(j + 3)
            s_v = vstg[j % 2]
            P.dma("sp", s_v, vrows[j])
            P.cp("act", vbf[jj], s_v)
            if j + 1 < NJ:
                P.dma("sp", Gt[(j + 1) % 2], Gd[:, j + 1, :])
            pa2 = psA[j % 2]
            ut = uTt[j % 3]
            for half in range(2):
                hs = slice(half * 512, half * 512 + 512)
                for kt in range(16):
                    P.mm(pa2[:, hs], ut[:, kt, :], h2T[:, kt, hs], start=(kt == 0), stop=(kt == 15))
            if j + 2 < NJ:
                trans(j + 2)
            e_ = ge[j % 2]
            P.act(e_, pa2, AF.Gelu_apprx_tanh)
            if j + 2 < NJ:
                evac(j + 2)
            P.tt("dve", Wt[jj], e_, Gt[j % 2], ALU.mult)
            if jj == JG - 1:
                first = (j == JG - 1)
                for tt_ in range(NTT):
                    for dc in range(4):
                        po = psO[(tt_ * 4 + dc) % 2]
                        for q_ in range(JG):
                            P.mm(po, Wt[q_][:, tt_ * 128:(tt_ + 1) * 128], vbf[q_][:, dc * 512:(dc + 1) * 512],
                                 start=(q_ == 0), stop=(q_ == JG - 1))
                        dst = ACC[:, tt_, dc * 512:(dc + 1) * 512]
                        if first:
                            P.cp("act", dst, po)
                        else:
                            P.tt("dve", dst, dst, po, ALU.add)
        P.barrier()
        Z2.reset()
        x1 = [Z2.alloc([D], F32, "x1_%d" % i) for i in range(2)]
        tq2 = Z2.alloc([D], F32, "tq2")
        bc3 = {k: Z2.alloc([D], F32, k) for k in ("gt2", "g2", "b2")}
        st = Z2.alloc([4, 6], F32, "stC")
        mv = Z2.alloc([2], F32, "mvC")
        rs = Z2.alloc([1], F32, "rsC")
        bload(bc3["gt2"], mod_d[l], u, 5 * D)
        bload(bc3["g2"], W["ln2_g"], l, 0)
        bload(bc3["b2"], W["ln2_b"], l, 0)
        for tt_ in range(NTT):
            rows = slice(tt_ * 128, tt_ * 128 + 128)
            x1_ = x1[tt_ % 2]
            acc = ACC[:, tt_, :]
            P.dma("sp", x1_, x1res[rows, :], g_x[0])
            if tt_ == 0:
                dbg_store("peer", acc)
            P.tt("dve", acc, acc, bc3["gt2"], ALU.mult)
            P.stt(acc, x1_, ALPHA, acc, ALU.mult, ALU.add)
            ln_tm(acc, tq2, st, mv, rs)
            P.tt("dve", tq2, tq2, bc3["g2"], ALU.mult)
            P.tt("dve", x1_, tq2, bc3["b2"], ALU.add)
            if last:
                P.dma("sp", yout[u][rows, :], x1_, g_st)
            else:
                P.dma("sp", xres[u][rows, :], x1_, g_st)
        P.barrier()

    phase_mod()
    ret = None
    for l in range(nlayers):
        phase_setup(l)
        if stop == "setup":
            break
        for u in units:
            ret = phase_A(l, u)
            if stop is not None and stop.startswith("A"):
                break
            phase_B(l, u, last=(l == nlayers - 1))
            if stop is not None:
                break
        if stop is not None:
            break
    if ret is not None and "ret" in dbg_out:
        src = ret
        P.dma("sp", dbg_out["ret"], src, g_st)
    P.final_wait()
    P.emit()
    return nc


def _grid_pos():
    rows = T // 64
    t = np.arange(rows * 64)
    r = (t // 64).astype(np.float32)
    col = (t % 64).astype(np.float32)
    nf = D // 4
    freq = (1.0 / (np.float32(10000.0) ** (np.arange(nf, dtype=np.float32) / np.float32(nf)))).astype(np.float32)
    ar = r[:, None] * freq
    ac = col[:, None] * freq
    return np.concatenate([np.sin(ar), np.cos(ar), np.sin(ac), np.cos(ac)], -1).astype(np.float32)


WEIGHT_NAMES = ["w_mod", "b_mod", "w_in", "s5_lam_re", "s5_lam_im", "s5_log_dt", "s5_b_re", "s5_b_im", "s5_c_re",
                "s5_c_im", "s5_d", "s5_w_glu", "sc_conv_w", "sc_conv_b", "cf_conv_w", "cf_conv_b", "cf_ln_g", "cf_ln_b",
                "w_pa", "w_pb", "w_pc", "w_o", "ln1_g", "ln1_b", "peer_w_q", "peer_k1", "peer_k2", "peer_u", "peer_v",
                "ln2_g", "ln2_b"]


def make_in_maps(inputs, cores=range(NCORE)):
    f = lambda a: np.ascontiguousarray(np.asarray(a, dtype=np.float32))
    xp = f(inputs["x_prompt"])
    xs = f(inputs["x_sample"])
    stt = f(inputs["state_ssm"])
    c = f(inputs["c"])
    cc = f(inputs["c_ctx"])
    pe = _grid_pos()
    ident = np.eye(128, dtype=np.float32)
    iota = np.tile(np.arange(128, dtype=np.float32)[None, :], (128, 1))
    wts = {k: f(inputs[k]) for k in WEIGHT_NAMES}
    maps = []
    for i in cores:
        m = dict(wts)
        m["xp"] = np.ascontiguousarray(xp[4 * i:4 * i + 4].reshape(T, D))
        m["xs"] = np.ascontiguousarray(xs[i].reshape(T, D))
        m["pe"] = pe
        m["st0"] = np.ascontiguousarray(stt[i])
        m["cond"] = np.ascontiguousarray(np.stack([cc, c[i]], 0))
        m["ident"] = ident
        m["iota128"] = iota
        maps.append(m)
    return maps


def kernel(**inputs):
    nc = build()
    maps = make_in_maps(inputs)
    maps = [{k: m[k] for k in build.declared} for m in maps]
    res = run_bass_kernel_spmd(nc, maps, core_ids=list(range(NCORE)))
    yp = np.concatenate([r["yp"].reshape(4, 256, D) for r in res.results], 0).astype(np.float32)
    ys = np.stack([r["ys"].reshape(T, D) for r in res.results], 0).astype(np.float32)
    nst = np.concatenate([r["nst"] for r in res.results], 0).astype(np.float32)
    return (yp, ys, nst)
```
